# Optimizing a Trainium2 kernel written in Bass

```python
import math
import jax
import jax.numpy as jnp
from jax import lax
import numpy as np

D_MODEL = 1024
BATCH = 2
SEQ = 16384
DEPTH = 2

N_MIXERS = 2
N_S5 = (DEPTH + 1) // 2
N_NSA = DEPTH // 2
RMS_EPS = 1e-6
NEG = -1e30

S5_GROUP = 16
S5_GROUPS = D_MODEL // S5_GROUP
S5_STATE = 64
S5_CHUNK = 128
DT_MIN = 1e-3
DT_MAX = 1e-1

NSA_HEADS = 16
NSA_HEAD_DIM = 64
NSA_KV_GROUPS = 4
NSA_REP = NSA_HEADS // NSA_KV_GROUPS
NSA_Q_W = NSA_HEADS * NSA_HEAD_DIM
NSA_KV_W = NSA_KV_GROUPS * NSA_HEAD_DIM
NSA_N_GATES = 3
NSA_PROJ_SIZES = (NSA_Q_W, NSA_KV_W, NSA_KV_W, NSA_KV_W, NSA_KV_W, NSA_KV_W, NSA_KV_W, NSA_HEADS * NSA_N_GATES)
NSA_PROJ_W = sum(NSA_PROJ_SIZES)
CMP_BLOCK = 32
CMP_STRIDE = 16
CMP_RATIO = CMP_BLOCK // CMP_STRIDE
CMP_HIDDEN = 256
SEL_BLOCK = 64
SEL_RATIO = SEL_BLOCK // CMP_STRIDE
SEL_TOPN = 16
WINDOW = 512
Q_BLOCK = 128
ROPE_THETA = 10000.0

PEER_HEADS = 8
PEER_NKEYS = 128
PEER_EXPERTS = PEER_NKEYS * PEER_NKEYS
PEER_KEY_DIM = 256
PEER_HALF = PEER_KEY_DIM // 2
PEER_TOPK = 16
PEER_CHUNK = 128

kernel_name = 'hybrid_s5_nsa_peer_adaln'


def rmsnorm(x, g):
    xf = x.astype(jnp.float32)
    y = xf * lax.rsqrt(jnp.mean(xf * xf, axis=-1, keepdims=True) + RMS_EPS)
    return (y * g.astype(jnp.float32)).astype(x.dtype)


def modulate(h, shift, scale):
    return h * (1.0 + scale[:, None, :]) + shift[:, None, :]


def rope(x, pos):
    half = x.shape[-1] // 2
    freqs = ROPE_THETA ** (-jnp.arange(half, dtype=jnp.float32) / half)
    ang = pos[:, None] * freqs[None, :]
    cos = jnp.cos(ang)[:, None, :]
    sin = jnp.sin(ang)[:, None, :]
    xf = x.astype(jnp.float32)
    x1, x2 = xf[..., :half], xf[..., half:]
    return jnp.concatenate([x1 * cos - x2 * sin, x1 * sin + x2 * cos], axis=-1).astype(x.dtype)


def s5_mixer(h, w_in, a_re, a_im, log_dt, b_re, b_im, c_re, c_im, d_skip, w_glu):
    b, s, _ = h.shape
    G, P, L = S5_GROUPS, S5_STATE, S5_CHUNK
    u = (h @ w_in).astype(jnp.float32)
    a = lax.complex(a_re.astype(jnp.float32), a_im.astype(jnp.float32))
    dt = jnp.exp(log_dt.astype(jnp.float32))[:, None]
    a_dt = a * dt
    a_bar = jnp.exp(a_dt)
    bmat = lax.complex(b_re.astype(jnp.float32), b_im.astype(jnp.float32))
    b_bar = ((a_bar - 1.0) / a)[..., None] * bmat
    cmat = lax.complex(c_re.astype(jnp.float32), c_im.astype(jnp.float32))
    n_chunks = s // L
    u_chunks = u.reshape(b, n_chunks, L, G, S5_GROUP).transpose(1, 0, 2, 3, 4)
    powers = jnp.exp(a_dt[None] * jnp.arange(1, L + 1, dtype=jnp.float32)[:, None, None])
    a_seq = jnp.broadcast_to(a_bar, (b, L, G, P))

    def combine(e1, e2):
        a1, b1 = e1
        a2, b2 = e2
        return a1 * a2, a2 * b1 + b2

    def step(state, u_c):
        bu = jnp.einsum('blgi,gpi->blgp', u_c.astype(jnp.complex64), b_bar)
        _, h_loc = lax.associative_scan(combine, (a_seq, bu), axis=1)
        hs = h_loc + powers[None] * state[:, None]
        y = jnp.real(jnp.einsum('blgp,gip->blgi', hs, cmat))
        return hs[:, -1], y

    state0 = jnp.zeros((b, G, P), jnp.complex64)
    _, y = lax.scan(step, state0, u_chunks)
    y = y.transpose(1, 0, 2, 3, 4).reshape(b, s, D_MODEL)
    y = jax.nn.gelu(y + d_skip.astype(jnp.float32) * u).astype(h.dtype)
    ya, yg = jnp.split(y @ w_glu, 2, axis=-1)
    return ya * jax.nn.sigmoid(yg)


def _compress(kv, pe, w1, w2):
    b, s, g, d = kv.shape
    nc = s // CMP_STRIDE - CMP_RATIO + 1
    pieces = kv.reshape(b, s // CMP_STRIDE, CMP_STRIDE, g, d)
    blocks = jnp.concatenate([pieces[:, r:r + nc] for r in range(CMP_RATIO)], axis=2)
    blocks = blocks + pe[None, None, :, None, :]
    flat = blocks.transpose(0, 1, 3, 2, 4).reshape(b, nc, g, CMP_BLOCK * d)
    return jax.nn.gelu(flat @ w1) @ w2


def nsa_mixer(h, w_proj, pe_k, pe_v, wk1, wk2, wv1, wv2, w_o):
    b, s, _ = h.shape
    G, R, d = NSA_KV_GROUPS, NSA_REP, NSA_HEAD_DIM
    split_at = [int(v) for v in np.cumsum(NSA_PROJ_SIZES)[:-1]]
    q, kc, vc, ks, vs, kw, vw, gl = jnp.split(h @ w_proj, split_at, axis=-1)
    pos = jnp.arange(s, dtype=jnp.float32)
    q = rope(q.reshape(b, s, NSA_HEADS, d), pos)
    ks = rope(ks.reshape(b, s, G, d), pos)
    kw = rope(kw.reshape(b, s, G, d), pos)
    kc = kc.reshape(b, s, G, d)
    vc = vc.reshape(b, s, G, d)
    vs = vs.reshape(b, s, G, d)
    vw = vw.reshape(b, s, G, d)
    gates = jax.nn.sigmoid(gl.astype(jnp.float32)).reshape(b, s, G, R, NSA_N_GATES)

    k_cmp = _compress(kc, pe_k, wk1, wk2)
    v_cmp = _compress(vc, pe_v, wv1, wv2).astype(jnp.float32)
    nc = k_cmp.shape[1]
    cmp_end = jnp.arange(nc) * CMP_STRIDE + CMP_BLOCK - 1
    k_cmp = rope(k_cmp, cmp_end.astype(jnp.float32)).astype(jnp.float32)

    nsb = s // SEL_BLOCK
    n_sel = min(SEL_TOPN, nsb)
    ks_blk = ks.reshape(b, nsb, SEL_BLOCK, G, d).transpose(0, 3, 1, 2, 4)
    vs_blk = vs.reshape(b, nsb, SEL_BLOCK, G, d).transpose(0, 3, 1, 2, 4)
    kw_pad = jnp.pad(kw, ((0, 0), (WINDOW, 0), (0, 0), (0, 0)))
    vw_pad = jnp.pad(vw, ((0, 0), (WINDOW, 0), (0, 0), (0, 0)))

    nqb = s // Q_BLOCK
    q_blocks = q.reshape(b, nqb, Q_BLOCK, G, R, d).transpose(1, 0, 2, 3, 4, 5)
    g_blocks = gates.reshape(b, nqb, Q_BLOCK, G, R, NSA_N_GATES).transpose(1, 0, 2, 3, 4, 5)
    scale = NSA_HEAD_DIM ** -0.5
    blk = jnp.arange(nsb)
    b_idx = jnp.arange(b)[:, None, None, None]
    g_idx = jnp.arange(G)[None, :, None, None]

    def attend(args):
        qi, qb, gb = args
        t = qi * Q_BLOCK + jnp.arange(Q_BLOCK)
        qf = qb.astype(jnp.float32) * scale
        valid_c = cmp_end[None, :] <= t[:, None]
        sc_c = jnp.einsum('bqgrd,bngd->bqgrn', qf, k_cmp)
        sc_c = jnp.where(valid_c[None, :, None, None, :], sc_c, NEG)
        p_c = jax.nn.softmax(sc_c, axis=-1) * valid_c.any(-1)[None, :, None, None, None]
        o_c = jnp.einsum('bqgrn,bngd->bqgrd', p_c, v_cmp)
        p_grp = p_c.sum(axis=3)
        p_pad = jnp.pad(p_grp, ((0, 0), (0, 0), (0, 0), (CMP_RATIO - 1, SEL_RATIO * nsb - nc)))
        imp = sum(p_pad[..., k:k + SEL_RATIO * nsb:SEL_RATIO] for k in range(SEL_RATIO + CMP_RATIO - 1))
        cur = t // SEL_BLOCK
        forced = (blk[None, :] == 0) | (blk[None, :] == cur[:, None]) | (blk[None, :] == cur[:, None] - 1)
        causal = blk[None, :] * SEL_BLOCK <= t[:, None]
        imp = jnp.where(forced[None, :, None, :], 1e9, jnp.where(causal[None, :, None, :], imp, -1e9))
        _, sel = lax.top_k(imp, n_sel)
        sel = sel.transpose(0, 2, 1, 3)
        k_sel = ks_blk[b_idx, g_idx, sel].astype(jnp.float32)
        v_sel = vs_blk[b_idx, g_idx, sel].astype(jnp.float32)
        sc_s = jnp.einsum('bqgrd,bgqnkd->bqgrnk', qf, k_sel)
        key_pos = sel[..., None] * SEL_BLOCK + jnp.arange(SEL_BLOCK)
        valid_s = (key_pos <= t[None, None, :, None, None]).transpose(0, 2, 1, 3, 4)[:, :, :, None]
        sc_s = jnp.where(valid_s, sc_s, NEG)
        p_s = jax.nn.softmax(sc_s.reshape(b, Q_BLOCK, G, R, n_sel * SEL_BLOCK), axis=-1).reshape(sc_s.shape)
        o_s = jnp.einsum('bqgrnk,bgqnkd->bqgrd', p_s, v_sel)
        k_w = lax.dynamic_slice_in_dim(kw_pad, qi * Q_BLOCK, WINDOW + Q_BLOCK, axis=1).astype(jnp.float32)
        v_w = lax.dynamic_slice_in_dim(vw_pad, qi * Q_BLOCK, WINDOW + Q_BLOCK, axis=1).astype(jnp.float32)
        kpos = qi * Q_BLOCK - WINDOW + jnp.arange(WINDOW + Q_BLOCK)
        valid_w = (kpos[None, :] <= t[:, None]) & (kpos[None, :] > t[:, None] - WINDOW) & (kpos[None, :] >= 0)
        sc_w = jnp.einsum('bqgrd,bkgd->bqgrk', qf, k_w)
        sc_w = jnp.where(valid_w[None, :, None, None, :], sc_w, NEG)
        p_w = jax.nn.softmax(sc_w, axis=-1)
        o_w = jnp.einsum('bqgrk,bkgd->bqgrd', p_w, v_w)
        return gb[..., 0:1] * o_c + gb[..., 1:2] * o_s + gb[..., 2:3] * o_w

    o = lax.map(attend, (jnp.arange(nqb), q_blocks, g_blocks))
    o = o.transpose(1, 0, 2, 3, 4, 5).reshape(b, s, NSA_Q_W).astype(h.dtype)
    return o @ w_o


def peer(h, w_q, sub_keys, u_tab, v_tab):
    b, s, _ = h.shape
    H, K = PEER_HEADS, PEER_TOPK
    qk = (h @ w_q).reshape(b, s, H, 2, PEER_HALF).astype(jnp.float32)
    sc = jnp.einsum('bshcd,hckd->bshck', qk, sub_keys.astype(jnp.float32))
    s1, i1 = lax.top_k(sc[..., 0, :], K)
    s2, i2 = lax.top_k(sc[..., 1, :], K)
    cand = (s1[..., :, None] + s2[..., None, :]).reshape(b, s, H, K * K)
    cidx = (i1[..., :, None] * PEER_NKEYS + i2[..., None, :]).reshape(b, s, H, K * K)
    top_s, top_pos = lax.top_k(cand, K)
    experts = jnp.take_along_axis(cidx, top_pos, axis=-1)
    gates = jax.nn.softmax(top_s, axis=-1)
    n_chunks = (b * s) // PEER_CHUNK
    h_c = h.reshape(n_chunks, PEER_CHUNK, D_MODEL)
    e_c = experts.reshape(n_chunks, PEER_CHUNK, H * K)
    g_c = gates.reshape(n_chunks, PEER_CHUNK, H * K)

    def chunk(args):
        hc, ec, gc = args
        u = u_tab[ec]
        v = v_tab[ec]
        act = jax.nn.gelu(jnp.einsum('cd,ced->ce', hc, u).astype(jnp.float32)) * gc
        return jnp.einsum('ce,ced->cd', act.astype(v.dtype), v)

    out = lax.map(chunk, (h_c, e_c, g_c))
    return out.reshape(b, s, D_MODEL)


def setup_inputs(seed: int = 0) -> dict:
    key = jax.random.key(seed)
    ks = jax.random.split(key, 32)
    D = D_MODEL
    G, P = S5_GROUPS, S5_STATE

    def nrm(k, shape, sc):
        return jax.random.normal(k, shape, jnp.float32) * sc

    n_idx = jnp.arange(P, dtype=jnp.float32)
    return {
        'x': nrm(ks[0], (BATCH, SEQ, D), 1.0),
        'c': nrm(ks[1], (BATCH, D), 1.0),
        'norm1_g': 1.0 + nrm(ks[2], (DEPTH, D), 0.02),
        'norm2_g': 1.0 + nrm(ks[3], (DEPTH, D), 0.02),
        'ada_w': nrm(ks[4], (DEPTH, D, 6 * D), 0.5 * D ** -0.5),
        'ada_b': nrm(ks[5], (DEPTH, 6 * D), 0.02),
        's5_w_in': nrm(ks[6], (N_S5, D, D), D ** -0.5),
        's5_a_re': -0.5 + nrm(ks[7], (N_S5, G, P), 0.01),
        's5_a_im': math.pi * n_idx + nrm(ks[8], (N_S5, G, P), 0.01),
        's5_log_dt': jax.random.uniform(ks[9], (N_S5, G), jnp.float32, math.log(DT_MIN), math.log(DT_MAX)),
        's5_b_re': nrm(ks[10], (N_S5, G, P, S5_GROUP), (2 * S5_GROUP) ** -0.5),
        's5_b_im': nrm(ks[11], (N_S5, G, P, S5_GROUP), (2 * S5_GROUP) ** -0.5),
        's5_c_re': nrm(ks[12], (N_S5, G, S5_GROUP, P), P ** -0.5),
        's5_c_im': nrm(ks[13], (N_S5, G, S5_GROUP, P), P ** -0.5),
        's5_d': nrm(ks[14], (N_S5, D), 1.0),
        's5_w_glu': nrm(ks[15], (N_S5, D, 2 * D), D ** -0.5),
        'nsa_w_proj': nrm(ks[16], (N_NSA, D, NSA_PROJ_W), D ** -0.5),
        'nsa_pe_k': nrm(ks[17], (N_NSA, CMP_BLOCK, NSA_HEAD_DIM), 0.1),
        'nsa_pe_v': nrm(ks[18], (N_NSA, CMP_BLOCK, NSA_HEAD_DIM), 0.1),
        'nsa_wk1': nrm(ks[19], (N_NSA, CMP_BLOCK * NSA_HEAD_DIM, CMP_HIDDEN), (CMP_BLOCK * NSA_HEAD_DIM) ** -0.5),
        'nsa_wk2': nrm(ks[20], (N_NSA, CMP_HIDDEN, NSA_HEAD_DIM), CMP_HIDDEN ** -0.5),
        'nsa_wv1': nrm(ks[21], (N_NSA, CMP_BLOCK * NSA_HEAD_DIM, CMP_HIDDEN), (CMP_BLOCK * NSA_HEAD_DIM) ** -0.5),
        'nsa_wv2': nrm(ks[22], (N_NSA, CMP_HIDDEN, NSA_HEAD_DIM), CMP_HIDDEN ** -0.5),
        'nsa_w_o': nrm(ks[23], (N_NSA, NSA_Q_W, D), NSA_Q_W ** -0.5),
        'peer_w_q': nrm(ks[24], (DEPTH, D, PEER_HEADS * PEER_KEY_DIM), D ** -0.5),
        'peer_sub_keys': nrm(ks[25], (DEPTH, PEER_HEADS, 2, PEER_NKEYS, PEER_HALF), PEER_HALF ** -0.5),
        'peer_u': nrm(ks[26], (DEPTH, PEER_EXPERTS, D), D ** -0.5),
        'peer_v': nrm(ks[27], (DEPTH, PEER_EXPERTS, D), 0.5),
        'final_g': 1.0 + nrm(ks[28], (D,), 0.02),
    }


def reference(x, c, norm1_g, norm2_g, ada_w, ada_b, s5_w_in, s5_a_re, s5_a_im, s5_log_dt,
              s5_b_re, s5_b_im, s5_c_re, s5_c_im, s5_d, s5_w_glu, nsa_w_proj, nsa_pe_k, nsa_pe_v,
              nsa_wk1, nsa_wk2, nsa_wv1, nsa_wv2, nsa_w_o, peer_w_q, peer_sub_keys, peer_u, peer_v,
              final_g):
    c_act = jax.nn.silu(c)
    for i in range(DEPTH):
        mod = c_act @ ada_w[i] + ada_b[i]
        sh1, sc1, g1, sh2, sc2, g2 = jnp.split(mod, 6, axis=-1)
        hm = modulate(rmsnorm(x, norm1_g[i]), sh1, sc1)
        j = i // N_MIXERS
        if i % N_MIXERS == 0:
            y = s5_mixer(hm, s5_w_in[j], s5_a_re[j], s5_a_im[j], s5_log_dt[j], s5_b_re[j], s5_b_im[j],
                         s5_c_re[j], s5_c_im[j], s5_d[j], s5_w_glu[j])
        else:
            y = nsa_mixer(hm, nsa_w_proj[j], nsa_pe_k[j], nsa_pe_v[j], nsa_wk1[j], nsa_wk2[j],
                          nsa_wv1[j], nsa_wv2[j], nsa_w_o[j])
        x = x + g1[:, None, :] * y
        hf = modulate(rmsnorm(x, norm2_g[i]), sh2, sc2)
        x = x + g2[:, None, :] * peer(hf, peer_w_q[i], peer_sub_keys[i], peer_u[i], peer_v[i])
    return rmsnorm(x, final_g)
```

```python
from contextlib import ExitStack
import numpy as np
import concourse.bass as bass
import concourse.mybir as mybir

F32 = mybir.dt.float32
BF16 = mybir.dt.bfloat16
I32 = mybir.dt.int32
U32 = mybir.dt.uint32
AF = mybir.ActivationFunctionType
ALU = mybir.AluOpType
AX = mybir.AxisListType


class Sched:
    N_DMA_SEMS = 24
    SEM_MAX = 12000
    DMA_SEM_MAX = 12000

    def __init__(self, nc, es):
        self.nc = nc
        self.es = es
        self.engs = {'pe': nc.tensor, 'dve': nc.vector, 'act': nc.scalar,
                     'pool': nc.gpsimd, 'sp': nc.sync}
        self.dpool = [[es.enter_context(nc.semaphore("ds_%d_0" % i))] for i in range(self.N_DMA_SEMS)]
        self.csem = {}
        self.ccnt = {}
        self.cep = {}
        self.ctot = {}
        self.cpool = {}
        for k in ('pe', 'dve', 'act', 'pool'):
            n_ep = {'pe': 8, 'dve': 8, 'act': 6, 'pool': 3}[k]
            self.cpool[k] = [es.enter_context(nc.semaphore("cs_%s_%d" % (k, j))) for j in range(n_ep)]
            self.csem[k] = self.cpool[k][0]
            self.ccnt[k] = 0
            self.cep[k] = 0
            self.ctot[k] = 0
        self.dsem = [self.dpool[i][0] for i in range(self.N_DMA_SEMS)]
        self.dcnt = [0] * self.N_DMA_SEMS
        self.dep = [0] * self.N_DMA_SEMS
        for i in range(self.N_DMA_SEMS):
            self.dpool[i].append(es.enter_context(nc.semaphore("ds_%d_1" % i)))
        self.drr = 0
        self.seen = {k: {} for k in self.engs}
        self.lastw = {}
        self.readers = {}
        self.n_instr = 0
        self.n_wait = 0

    def _deps(self, reads, writes):
        deps = []
        for k in reads:
            w = self.lastw.get(k)
            if w is not None:
                deps.append(w)
        for k in writes:
            w = self.lastw.get(k)
            if w is not None:
                deps.append(w)
            deps.extend(self.readers.get(k, ()))
        return deps

    def _wait(self, e, deps, skip_self=None):
        eng = self.engs[e]
        best = {}
        for (sid, sem, val) in deps:
            if skip_self is not None and sid.startswith(skip_self):
                continue
            if best.get(sid, (None, 0))[1] < val:
                best[sid] = (sem, val)
        for sid, (sem, val) in best.items():
            if self.seen[e].get(sid, 0) < val:
                eng.wait_ge(sem, val)
                self.seen[e][sid] = val
                self.n_wait += 1

    def _record(self, ev, reads, writes):
        for k in reads:
            self.readers.setdefault(k, []).append(ev)
        for k in writes:
            self.lastw[k] = ev
            self.readers[k] = []

    def op(self, e, fn, reads=(), writes=(), pe_chain=False):
        deps = self._deps(reads, writes)
        self._wait(e, deps, skip_self=('c_pe_' if (pe_chain and e == 'pe') else None))
        ins = fn()
        if self.ccnt[e] >= self.SEM_MAX:
            self.cep[e] += 1
            self.csem[e] = self.cpool[e][self.cep[e]]
            self.ccnt[e] = 0
        self.ccnt[e] += 1
        self.ctot[e] += 1
        ins.then_inc(self.csem[e], 1)
        ev = ('c_%s_%d' % (e, self.cep[e]), self.csem[e], self.ccnt[e])
        self._record(ev, reads, writes)
        self.n_instr += 1
        return ev

    def dma(self, q, fn, reads=(), writes=()):
        deps = self._deps(reads, writes)
        self._wait(q, deps)
        i = self.drr
        self.drr = (self.drr + 1) % self.N_DMA_SEMS
        sid = 'd_%d_%d' % (i, self.dep[i])
        if self.seen[q].get(sid, 0) < self.dcnt[i]:
            self.engs[q].wait_ge(self.dsem[i], self.dcnt[i])
            self.seen[q][sid] = self.dcnt[i]
        if self.dcnt[i] >= self.DMA_SEM_MAX:
            self.dep[i] += 1
            self.dsem[i] = self.dpool[i][self.dep[i]]
            self.dcnt[i] = 0
            sid = 'd_%d_%d' % (i, self.dep[i])
        ins = fn()
        self.dcnt[i] += 16
        ins.then_inc(self.dsem[i], 16)
        ev = (sid, self.dsem[i], self.dcnt[i])
        self._record(ev, reads, writes)
        self.n_instr += 1
        return ev

    def finish(self, keys):
        deps = []
        for k in keys:
            w = self.lastw.get(k)
            if w is not None:
                deps.append(w)
        self._wait('sp', deps)
        alld = []
        for k in ('pe', 'dve', 'act', 'pool'):
            if self.ccnt[k]:
                alld.append(('c_%s_%d' % (k, self.cep[k]), self.csem[k], self.ccnt[k]))
        for i in range(self.N_DMA_SEMS):
            if self.dcnt[i]:
                alld.append(('d_%d_%d' % (i, self.dep[i]), self.dsem[i], self.dcnt[i]))
        self._wait('sp', alld)


def sched_barrier(S):
    alld = []
    for k in ('pe', 'dve', 'act', 'pool'):
        if S.ccnt[k]:
            alld.append(('c_%s_%d' % (k, S.cep[k]), S.csem[k], S.ccnt[k]))
    for i in range(S.N_DMA_SEMS):
        if S.dcnt[i]:
            alld.append(('d_%d_%d' % (i, S.dep[i]), S.dsem[i], S.dcnt[i]))
    for e in ('pe', 'dve', 'act', 'pool', 'sp'):
        S._wait(e, alld)


Sched.barrier = sched_barrier


RMS_EPS = 1e-6
IOA = bass.IndirectOffsetOnAxis


def build_tok(NT, mode, final, gelu_func=None, dbg=None):
    nc = bass.Bass("TRN2", target_bir_lowering=False)
    T = NT * 128
    MIXN = 2048 if mode == 'glu' else 1024
    D = 1024
    din = lambda n, s, d=F32: nc.dram_tensor(n, s, d, kind="ExternalInput").ap()
    x = din("x", [T, D])
    mixT = din("mixT", [D, T])
    c_l = din("c_l", [128, 8])
    adaw = din("adaw", [D, 4096])
    adab = din("adab", [1, 4096])
    n2g = din("n2g", [1, D])
    fing = din("fing", [1, D])
    wmix_d = din("wmix", [D, MIXN])
    wq_d = din("wq", [D, 2048])
    sk_d = din("sk", [16, 128, 128])
    u_tab = din("u_tab", [16384, D])
    v_tab = din("v_tab", [16384, D])
    identf_d = din("identf", [128, 128])
    iota16_d = din("iota16", [128, 16])
    out = nc.dram_tensor("out", [T, D], F32, kind="ExternalOutput").ap()

    es = ExitStack()
    with es:
        es.enter_context(nc.allow_low_precision("bf16 matmul operands"))
        es.enter_context(nc.allow_non_contiguous_dma("small layout loads"))
        S = Sched(nc, es)
        sb = lambda n, s, d=F32: es.enter_context(nc.sbuf_tensor("s_" + n, s, d))
        ps = lambda n, s, d=F32: es.enter_context(nc.psum_tensor("p_" + n, s, d))
        V, A, P, PE = nc.vector, nc.scalar, nc.gpsimd, nc.tensor

        identf = sb("identf", [128, 128])
        identb = sb("identb", [128, 128], BF16)
        iota16 = sb("iota16", [128, 16])
        epsT = sb("epsT", [128, 1])
        csil = sb("csil", [128, 8])
        csil_rep = sb("csil_rep", [128, 8, 128])
        modrep = sb("modrep", [128, 4096])
        gk2 = sb("gk2", [128, D])
        fing_rep = sb("fing_rep", [128, D])
        ot = sb("ot", [128, D])
        n2g_rep = ot
        wq = sb("wq", [128, 8, 2048], BF16)
        wmix = sb("wmix", [128, 8, MIXN], BF16)
        skT = sb("skT", [128, 16, 128], BF16)
        stg = [sb("stg%d" % i, [128, 8, 256]) for i in range(2)]
        adab_rep = sb("adab_rep", [128, 256])

        psA = ps("psA", [128, 2048])
        psB = ps("psB", [128, 1024])
        psT = ps("psT", [128, 1024], BF16)
        psM = ps("psM", [128, 512])

        S.dma('sp', lambda: nc.sync.dma_start(out=identf[:], in_=identf_d[:, :]), writes=['identf'])
        S.dma('sp', lambda: nc.sync.dma_start(out=iota16[:], in_=iota16_d[:, :]), writes=['iota4'])
        S.dma('sp', lambda: nc.sync.dma_start(out=csil[:], in_=c_l[:, :]), writes=['csil'])
        S.dma('sp', lambda: nc.sync.dma_start(out=n2g_rep[:], in_=n2g[0:1, :].partition_broadcast(128)), writes=['ot'])
        if final:
            S.dma('sp', lambda: nc.sync.dma_start(out=fing_rep[:], in_=fing[0:1, :].partition_broadcast(128)), writes=['fing_rep'])
        S.op('dve', lambda: V.tensor_copy(out=identb[:], in_=identf[:]), reads=['identf'], writes=['identb'])
        S.op('dve', lambda: V.memset(epsT[:], RMS_EPS), writes=['epsT'])
        S.op('act', lambda: A.activation(out=csil[:], in_=csil[:], func=AF.Silu), reads=['csil'], writes=['csil'])
        S.op('dve', lambda: V.tensor_copy(out=csil_rep[:], in_=csil[:].unsqueeze(2).to_broadcast([128, 8, 128])),
             reads=['csil'], writes=['csil_rep'])
        for j in range(16):
            st = stg[j % 2]
            k_st = 'stg%d' % (j % 2)
            S.dma('sp', lambda: nc.sync.dma_start(
                out=st[:], in_=adaw[:, j * 256:(j + 1) * 256].rearrange("(kc k) n -> k kc n", k=128)), writes=[k_st])
            S.dma('sp', lambda: nc.sync.dma_start(
                out=adab_rep[:], in_=adab[0:1, j * 256:(j + 1) * 256].partition_broadcast(128)), writes=['adab_rep'])
            for kc in range(8):
                S.op('pe', lambda: PE.matmul(psM[:, 0:256], lhsT=csil_rep[:, kc, :], rhs=st[:, kc, :], start=(kc == 0), stop=(kc == 7)),
                     reads=['csil_rep', k_st], writes=['psM'], pe_chain=(kc > 0))
            S.op('dve', lambda: V.tensor_tensor(out=modrep[:, j * 256:(j + 1) * 256], in0=psM[:, 0:256], in1=adab_rep[:], op=ALU.add),
                 reads=['psM', 'adab_rep'], writes=['modrep'])
        g1 = modrep[:, 0:1024]
        sh2 = modrep[:, 1024:2048]
        sc2 = modrep[:, 2048:3072]
        g2 = modrep[:, 3072:4096]
        S.op('dve', lambda: V.scalar_tensor_tensor(out=gk2[:], in0=sc2, scalar=1.0, in1=n2g_rep[:], op0=ALU.add, op1=ALU.mult),
             reads=['modrep', 'ot'], writes=['gk2'])
        cast_i = [0]

        def load_cast(dst3, src2d, ncols, key):
            for c0 in range(0, ncols, 256):
                i = cast_i[0] % 2
                cast_i[0] += 1
                st = stg[i]
                S.dma('sp', lambda: nc.sync.dma_start(
                    out=st[:], in_=src2d[:, c0:c0 + 256].rearrange("(kc k) n -> k kc n", k=128)), writes=['stg%d' % i])
                if i == 0:
                    S.op('dve', lambda: V.tensor_copy(out=dst3[:, :, c0:c0 + 256], in_=st[:]), reads=['stg%d' % i], writes=[key])
                else:
                    S.op('act', lambda: A.copy(out=dst3[:, :, c0:c0 + 256], in_=st[:]), reads=['stg%d' % i], writes=[key])

        load_cast(wmix, wmix_d, MIXN, 'wmix')
        load_cast(wq, wq_d, 2048, 'wq')
        for j in range(16):
            st = stg[j % 2]
            k_st = 'stg%d' % (j % 2)
            S.dma('sp', lambda: nc.sync.dma_start(out=st[:, 0, 0:128], in_=sk_d[j, :, :]), writes=[k_st])
            S.op('pe', lambda: PE.transpose(out=psM[:, 0:128], in_=st[:, 0, 0:128], identity=identf[:]),
                 reads=[k_st, 'identf'], writes=['psM'])
            S.op('act', lambda: A.copy(out=skT[:, j, :], in_=psM[:, 0:128]), reads=['psM'], writes=['skT'])

        xt = [sb("xt%d" % i, [128, D]) for i in range(2)]
        mT = sb("mT", [128, 8, 128])
        mTb = sb("mTb", [128, 8, 128], BF16)
        x1 = sb("x1", [128, D])
        junk = sb("junk", [128, D], BF16)
        ss = sb("ss", [128, 1])
        rs = sb("rs", [128, 1])
        hf = sb("hf", [128, D])
        hfb = sb("hfb", [128, D], BF16)
        hfT = sb("hfT", [128, 8, 128], BF16)
        qkT = sb("qkT", [128, 16, 128], BF16)
        scw = sb("scw", [128, 16, 128])
        vals = sb("vals", [128, 16, 16])
        idx = sb("idx", [128, 16, 16], U32)
        idxf = sb("idxf", [128, 16, 16])
        cand = sb("cand", [128, 8, 256])
        wk2 = sb("wk2", [128, 2048])
        scw2 = wk2[:].rearrange("p (a b) -> p a b", b=128)
        cand2 = wk2[:].rearrange("p (a b) -> p a b", b=256)
        tops = sb("tops", [128, 8, 16])
        pos = sb("pos", [128, 8, 16], U32)
        au = sb("au", [128, 8, 16], U32)
        bu = sb("bu", [128, 8, 16], U32)
        af = sb("af", [128, 8, 16])
        bf = sb("bf", [128, 8, 16])
        eq = scw[:].rearrange("p a b -> p (a b)").rearrange("p (h k c) -> p h k c", h=8, k=16)
        sel1 = sb("sel1", [128, 8, 16])
        sel2 = sb("sel2", [128, 8, 16])
        ef = sb("ef", [128, 128])
        eidx = sb("eidx", [128, 128], U32)
        gat = sb("gat", [128, 8, 16])
        gsum = sb("gsum", [128, 8])
        sdot = sb("sdot", [128, 128])
        actv = sb("actv", [128, 128])
        NB = 4
        ug = [sb("ug%d" % i, [128, D]) for i in range(NB)]
        vg = ug
        acc = sb("acc", [128, D])
        sg = acc

        gfunc = gelu_func if gelu_func is not None else AF.Gelu_apprx_tanh

        for t in range(NT):
            xb = xt[t % 2]
            kx = 'xt%d' % (t % 2)
            r0 = t * 128
            S.dma('sp', lambda: nc.sync.dma_start(out=xb[:], in_=x[r0:r0 + 128, :]), writes=[kx])
            S.dma('sp', lambda: nc.sync.dma_start(
                out=mT[:], in_=mixT[:, r0:r0 + 128].rearrange("(kc k) n -> k kc n", k=128)), writes=['mT'])
            S.op('act', lambda: A.copy(out=mTb[:], in_=mT[:]), reads=['mT'], writes=['mTb'])
            for nb in range(MIXN // 512):
                for kc in range(8):
                    S.op('pe', lambda: PE.matmul(psA[:, nb * 512:(nb + 1) * 512], lhsT=mTb[:, kc, :],
                                                 rhs=wmix[:, kc, nb * 512:(nb + 1) * 512], start=(kc == 0), stop=(kc == 7)),
                         reads=['mTb', 'wmix'], writes=['psA'], pe_chain=not (nb == 0 and kc == 0))
            if mode == 'glu':
                S.op('act', lambda: A.activation(out=sg[:], in_=psA[:, 1024:2048], func=AF.Sigmoid), reads=['psA'], writes=['acc'])
                S.op('dve', lambda: V.tensor_tensor(out=sg[:], in0=sg[:], in1=g1, op=ALU.mult), reads=['acc', 'modrep'], writes=['acc'])
                S.op('dve', lambda: V.tensor_tensor(out=x1[:], in0=psA[:, 0:1024], in1=sg[:], op=ALU.mult),
                     reads=['psA', 'acc'], writes=['x1'])
            else:
                S.op('dve', lambda: V.tensor_tensor(out=x1[:], in0=psA[:, 0:1024], in1=g1, op=ALU.mult),
                     reads=['psA', 'modrep'], writes=['x1'])
            S.op('dve', lambda: V.tensor_tensor(out=x1[:], in0=x1[:], in1=xb[:], op=ALU.add), reads=['x1', kx], writes=['x1'])
            S.op('act', lambda: A.activation(out=junk[:], in_=x1[:], func=AF.Square, accum_out=ss[:]), reads=['x1'], writes=['junk', 'ss'])
            S.op('act', lambda: A.activation(out=rs[:], in_=ss[:], func=AF.Sqrt, bias=epsT[:], scale=1.0 / D),
                 reads=['ss', 'epsT'], writes=['rs'])
            S.op('dve', lambda: V.reciprocal(out=rs[:], in_=rs[:]), reads=['rs'], writes=['rs'])
            S.op('dve', lambda: V.scalar_tensor_tensor(out=hf[:], in0=x1[:], scalar=rs[:], in1=gk2[:], op0=ALU.mult, op1=ALU.mult),
                 reads=['x1', 'rs', 'gk2'], writes=['hf'])
            S.op('dve', lambda: V.tensor_tensor(out=hf[:], in0=hf[:], in1=sh2, op=ALU.add), reads=['hf', 'modrep'], writes=['hf'])
            S.op('act', lambda: A.copy(out=hfb[:], in_=hf[:]), reads=['hf'], writes=['hfb'])
            for kc in range(8):
                S.op('pe', lambda: PE.transpose(out=psT[:, kc * 128:(kc + 1) * 128], in_=hfb[:, kc * 128:(kc + 1) * 128], identity=identb[:]),
                     reads=['hfb', 'identb'], writes=['psT'], pe_chain=(kc > 0))
            S.op('dve', lambda: V.tensor_copy(out=hfT[:].rearrange("p a b -> p (a b)"), in_=psT[:]), reads=['psT'], writes=['hfT'])
            for half in range(2):
                for jj in range(8):
                    j = half * 8 + jj
                    for kc in range(8):
                        S.op('pe', lambda: PE.matmul(psB[:, jj * 128:(jj + 1) * 128], lhsT=wq[:, kc, j * 128:(j + 1) * 128],
                                                     rhs=hfT[:, kc, :], start=(kc == 0), stop=(kc == 7)),
                             reads=['wq', 'hfT'], writes=['psB'], pe_chain=not (jj == 0 and kc == 0))
                S.op('act', lambda: A.copy(out=qkT[:, half * 8:(half + 1) * 8, :].rearrange("p a b -> p (a b)"), in_=psB[:]),
                     reads=['psB'], writes=['qkT'])
            for j in range(16):
                S.op('pe', lambda: PE.matmul(psA[:, j * 128:(j + 1) * 128], lhsT=qkT[:, j, :], rhs=skT[:, j, :], start=True, stop=True),
                     reads=['qkT', 'skT'], writes=['psA'], pe_chain=(j > 0))
            S.op('act', lambda: A.copy(out=scw[:].rearrange("p a b -> p (a b)"), in_=psA[:]), reads=['psA'], writes=['scw'])
            for j in range(16):
                S.op('dve', lambda: V.max(out=vals[:, j, 0:8], in_=scw[:, j, :]), reads=['scw'], writes=['vals'])
                S.op('dve', lambda: V.max_index(out=idx[:, j, 0:8], in_max=vals[:, j, 0:8], in_values=scw[:, j, :]),
                     reads=['scw', 'vals'], writes=['idx'])
                S.op('dve', lambda: V.match_replace(out=scw2[:, j, :], in_to_replace=vals[:, j, 0:8], in_values=scw[:, j, :], imm_value=-1e30),
                     reads=['scw', 'vals'], writes=['wk2'])
                S.op('dve', lambda: V.max(out=vals[:, j, 8:16], in_=scw2[:, j, :]), reads=['wk2'], writes=['vals'])
                S.op('dve', lambda: V.max_index(out=idx[:, j, 8:16], in_max=vals[:, j, 8:16], in_values=scw2[:, j, :]),
                     reads=['wk2', 'vals'], writes=['idx'])
            vals4 = vals[:].rearrange("p (h c) k -> p h c k", c=2)
            S.op('dve', lambda: V.tensor_tensor(out=cand[:].rearrange("p h (a b) -> p h a b", b=16),
                                                in0=vals4[:, :, 0, :].unsqueeze(3).to_broadcast([128, 8, 16, 16]),
                                                in1=vals4[:, :, 1, :].unsqueeze(2).to_broadcast([128, 8, 16, 16]), op=ALU.add),
                 reads=['vals'], writes=['cand'])
            for h in range(8):
                S.op('dve', lambda: V.max(out=tops[:, h, 0:8], in_=cand[:, h, :]), reads=['cand'], writes=['tops'])
                S.op('dve', lambda: V.max_index(out=pos[:, h, 0:8], in_max=tops[:, h, 0:8], in_values=cand[:, h, :]),
                     reads=['cand', 'tops'], writes=['pos'])
                S.op('dve', lambda: V.match_replace(out=cand2[:, h, :], in_to_replace=tops[:, h, 0:8], in_values=cand[:, h, :], imm_value=-1e30),
                     reads=['cand', 'tops'], writes=['wk2'])
                S.op('dve', lambda: V.max(out=tops[:, h, 8:16], in_=cand2[:, h, :]), reads=['wk2'], writes=['tops'])
                S.op('dve', lambda: V.max_index(out=pos[:, h, 8:16], in_max=tops[:, h, 8:16], in_values=cand2[:, h, :]),
                     reads=['wk2', 'tops'], writes=['pos'])
            S.op('dve', lambda: V.tensor_single_scalar(out=au[:], in_=pos[:], scalar=4, op=ALU.logical_shift_right), reads=['pos'], writes=['au'])
            S.op('dve', lambda: V.tensor_single_scalar(out=bu[:], in_=pos[:], scalar=15, op=ALU.bitwise_and), reads=['pos'], writes=['bu'])
            S.op('dve', lambda: V.tensor_copy(out=af[:], in_=au[:]), reads=['au'], writes=['af'])
            S.op('dve', lambda: V.tensor_copy(out=bf[:], in_=bu[:]), reads=['bu'], writes=['bf'])
            S.op('dve', lambda: V.tensor_copy(out=idxf[:], in_=idx[:]), reads=['idx'], writes=['idxf'])
            idxf4 = idxf[:].rearrange("p (h c) k -> p h c k", c=2)
            for (sf, cc, sel) in ((af, 0, sel1), (bf, 1, sel2)):
                ksel = 'sel1' if cc == 0 else 'sel2'
                S.op('dve', lambda: V.tensor_tensor(out=eq[:], in0=sf[:].unsqueeze(3).to_broadcast([128, 8, 16, 16]),
                                                    in1=iota16[:].unsqueeze(1).unsqueeze(1).to_broadcast([128, 8, 16, 16]), op=ALU.is_equal), reads=['af', 'bf', 'iota4'], writes=['scw'])
                S.op('dve', lambda: V.tensor_tensor(out=eq[:], in0=eq[:], in1=idxf4[:, :, cc, :].unsqueeze(2).to_broadcast([128, 8, 16, 16]),
                                                    op=ALU.mult), reads=['scw', 'idxf'], writes=['scw'])
                S.op('dve', lambda: V.tensor_reduce(out=sel[:], in_=eq[:], axis=AX.X, op=ALU.add), reads=['scw'], writes=[ksel])
            S.op('dve', lambda: V.scalar_tensor_tensor(out=ef[:], in0=sel1[:].rearrange("p h k -> p (h k)"), scalar=128.0,
                                                       in1=sel2[:].rearrange("p h k -> p (h k)"), op0=ALU.mult, op1=ALU.add),
                 reads=['sel1', 'sel2'], writes=['ef'])
            S.op('dve', lambda: V.tensor_copy(out=eidx[:], in_=ef[:]), reads=['ef'], writes=['eidx'])
            S.op('dve', lambda: V.tensor_tensor(out=gat[:], in0=tops[:], in1=tops[:, :, 0:1].to_broadcast([128, 8, 16]), op=ALU.subtract),
                 reads=['tops'], writes=['gat'])
            S.op('act', lambda: A.activation(out=gat[:], in_=gat[:], func=AF.Exp), reads=['gat'], writes=['gat'])
            S.op('dve', lambda: V.tensor_reduce(out=gsum[:], in_=gat[:], axis=AX.X, op=ALU.add), reads=['gat'], writes=['gsum'])
            S.op('dve', lambda: V.reciprocal(out=gsum[:], in_=gsum[:]), reads=['gsum'], writes=['gsum'])
            S.op('dve', lambda: V.tensor_tensor(out=gat[:], in0=gat[:], in1=gsum[:].unsqueeze(2).to_broadcast([128, 8, 16]), op=ALU.mult),
                 reads=['gat', 'gsum'], writes=['gat'])
            if dbg == 'idx':
                S.op('dve', lambda: V.tensor_copy(out=ot[:], in_=hf[:]), reads=['hf'], writes=['ot'])
                S.op('dve', lambda: V.tensor_copy(out=ot[:, 0:128], in_=ef[:]), reads=['ef'], writes=['ot'])
                S.op('dve', lambda: V.tensor_copy(out=ot[:, 128:256], in_=gat[:].rearrange("p h k -> p (h k)")), reads=['gat'], writes=['ot'])
                S.op('dve', lambda: V.tensor_copy(out=ot[:, 256:384], in_=tops[:].rearrange("p h k -> p (h k)")), reads=['tops'], writes=['ot'])
                S.op('dve', lambda: V.tensor_copy(out=ot[:, 384:640], in_=idxf[:].rearrange("p h k -> p (h k)")), reads=['idxf'], writes=['ot'])
                S.op('dve', lambda: V.tensor_copy(out=ot[:, 640:896], in_=vals[:].rearrange("p h k -> p (h k)")), reads=['vals'], writes=['ot'])
                S.dma('sp', lambda: nc.sync.dma_start(out=out[r0:r0 + 128, :], in_=ot[:]), reads=['ot'], writes=['out'])
                continue
            for s in range(128):
                b = s % NB
                S.dma('pool', lambda: P.indirect_dma_start(out=ug[b][:], out_offset=None, in_=u_tab[:, :],
                                                            in_offset=IOA(ap=eidx[:, s:s + 1], axis=0)),
                      reads=['eidx'], writes=['ug%d' % b])
                S.op('dve', lambda: V.scalar_tensor_tensor(out=junk[:], in0=ug[b][:], scalar=1.0, in1=hf[:],
                                                           op0=ALU.mult, op1=ALU.mult, accum_out=sdot[:, s:s + 1]),
                     reads=['ug%d' % b, 'hf'], writes=['junk', 'sdot'])
            if dbg == 'u':
                S.op('dve', lambda: V.tensor_copy(out=ot[:], in_=hf[:]), reads=['hf'], writes=['ot'])
                S.op('dve', lambda: V.tensor_copy(out=ot[:, 0:128], in_=sdot[:]), reads=['sdot'], writes=['ot'])
                S.op('dve', lambda: V.tensor_copy(out=ot[:, 128:256], in_=ef[:]), reads=['ef'], writes=['ot'])
                S.dma('sp', lambda: nc.sync.dma_start(out=out[r0:r0 + 128, :], in_=ot[:]), reads=['ot'], writes=['out'])
                continue
            S.op('dve', lambda: V.tensor_tensor(out=actv[:], in0=sdot[:], in1=sdot[:], op=ALU.mult), reads=['sdot'], writes=['actv'])
            S.op('dve', lambda: V.tensor_scalar(out=actv[:], in0=actv[:], scalar1=0.044715, scalar2=1.0, op0=ALU.mult, op1=ALU.add),
                 reads=['actv'], writes=['actv'])
            S.op('dve', lambda: V.tensor_tensor(out=actv[:], in0=actv[:], in1=sdot[:], op=ALU.mult), reads=['actv', 'sdot'], writes=['actv'])
            S.op('act', lambda: A.activation(out=actv[:], in_=actv[:], func=AF.Sigmoid, scale=1.5957691216), reads=['actv'], writes=['actv'])
            S.op('dve', lambda: V.tensor_tensor(out=actv[:], in0=actv[:], in1=sdot[:], op=ALU.mult), reads=['actv', 'sdot'], writes=['actv'])
            S.op('dve', lambda: V.tensor_tensor(out=actv[:], in0=actv[:], in1=gat[:].rearrange("p h k -> p (h k)"), op=ALU.mult),
                 reads=['actv', 'gat'], writes=['actv'])
            for s in range(128):
                b = s % NB
                S.dma('pool', lambda: P.indirect_dma_start(out=vg[b][:], out_offset=None, in_=v_tab[:, :],
                                                            in_offset=IOA(ap=eidx[:, s:s + 1], axis=0)),
                      reads=['eidx'], writes=['ug%d' % b])
                if s == 0:
                    S.op('dve', lambda: V.tensor_scalar(out=acc[:], in0=vg[b][:], scalar1=actv[:, 0:1], scalar2=None, op0=ALU.mult),
                         reads=['ug%d' % b, 'actv'], writes=['acc'])
                else:
                    S.op('dve', lambda: V.scalar_tensor_tensor(out=acc[:], in0=vg[b][:], scalar=actv[:, s:s + 1], in1=acc[:],
                                                               op0=ALU.mult, op1=ALU.add),
                         reads=['ug%d' % b, 'actv', 'acc'], writes=['acc'])
            S.op('dve', lambda: V.tensor_tensor(out=acc[:], in0=acc[:], in1=g2, op=ALU.mult), reads=['acc', 'modrep'], writes=['acc'])
            S.op('dve', lambda: V.tensor_tensor(out=ot[:], in0=acc[:], in1=x1[:], op=ALU.add), reads=['acc', 'x1'], writes=['ot'])
            if final:
                S.op('act', lambda: A.activation(out=junk[:], in_=ot[:], func=AF.Square, accum_out=ss[:]), reads=['ot'], writes=['junk', 'ss'])
                S.op('act', lambda: A.activation(out=rs[:], in_=ss[:], func=AF.Sqrt, bias=epsT[:], scale=1.0 / D),
                     reads=['ss', 'epsT'], writes=['rs'])
                S.op('dve', lambda: V.reciprocal(out=rs[:], in_=rs[:]), reads=['rs'], writes=['rs'])
                S.op('dve', lambda: V.scalar_tensor_tensor(out=ot[:], in0=ot[:], scalar=rs[:], in1=fing_rep[:], op0=ALU.mult, op1=ALU.mult),
                     reads=['ot', 'rs', 'fing_rep'], writes=['ot'])
            S.dma('sp', lambda: nc.sync.dma_start(out=out[r0:r0 + 128, :], in_=ot[:]), reads=['ot'], writes=['out'])
        S.finish(['out'])
        print("tok program: instr", S.n_instr, "waits", S.n_wait)
    return nc


def tok_consts():
    iota16 = np.broadcast_to(np.arange(16, dtype=np.float32)[None, :], (128, 16)).copy()
    return {"identf": np.eye(128, dtype=np.float32), "iota16": iota16}

import math

RMS_EPS = 1e-6
PI = math.pi


def build_s5(NCH):
    nc = bass.Bass("TRN2", target_bir_lowering=False)
    T = NCH * 128
    D = 1024
    din = lambda n, s, d=F32: nc.dram_tensor(n, s, d, kind="ExternalInput").ap()
    x = din("x", [T, D])
    c_l = din("c_l", [128, 8])
    adaw = din("adaw", [D, 2048])
    adab = din("adab", [1, 2048])
    n1g = din("n1g", [1, D])
    win_d = din("win", [D, 256])
    are_c_d = din("are_c", [128, 16]); aim_c_d = din("aim_c", [128, 16]); ldt_c_d = din("ldt_c", [128, 16])
    are_r_d = din("are_r", [128, 2048]); aim_r_d = din("aim_r", [128, 2048]); ldt_r_d = din("ldt_r", [128, 2048])
    X1p_d = din("X1p", [128, 2048]); X2p_d = din("X2p", [128, 2048])
    CcP_d = din("CcP", [128, 2048]); CcSP_d = din("CcSP", [128, 2048])
    dcol_d = din("dcol", [128, 2])
    identf_d = din("identf", [128, 128]); swapm_d = din("swapm", [128, 128])
    sgnc_d = din("sgn_c", [128, 1]); sgnr_d = din("sgn_r", [128, 128])
    mrow_d = din("mrow", [128, 128]); mask01_d = din("mask01", [128, 512])
    out = nc.dram_tensor("out", [256, T], F32, kind="ExternalOutput").ap()

    es = ExitStack()
    with es:
        es.enter_context(nc.allow_low_precision("bf16 matmul operands"))
        es.enter_context(nc.allow_non_contiguous_dma("small layout loads"))
        S = Sched(nc, es)
        sb = lambda n, s, d=F32: es.enter_context(nc.sbuf_tensor("s_" + n, s, d))
        ps = lambda n, s, d=F32: es.enter_context(nc.psum_tensor("p_" + n, s, d))
        V, A, P, PE = nc.vector, nc.scalar, nc.gpsimd, nc.tensor

        def ld(dst, src, key):
            S.dma('sp', lambda: nc.sync.dma_start(out=dst, in_=src), writes=[key])

        identf = sb("identf", [128, 128]); identb = sb("identb", [128, 128], BF16)
        epsT = sb("epsT", [128, 1])
        modrep = sb("modrep", [128, 2048])
        gk1 = sb("gk1", [128, D])
        win = sb("win", [128, 8, 256], BF16)
        Ainv_r = sb("Ainv_r", [128, 16, 128]); Ainv_i = sb("Ainv_i", [128, 16, 128])
        Apow_r = sb("Apow_r", [128, 16, 128]); Apow_i = sb("Apow_i", [128, 16, 128])
        Rot = sb("Rot", [128, 16, 128])
        Bpad = sb("Bpad", [128, 16, 128], BF16); BpadS = sb("BpadS", [128, 16, 128], BF16)
        Cc = sb("Cc", [128, 16, 128], BF16); CcS = sb("CcS", [128, 16, 128], BF16)
        dcol = sb("dcol", [128, 2])
        mask01 = sb("mask01", [128, 512])

        psT = ps("psT", [128, 1024], BF16)
        psU = ps("psU", [128, 512])
        psBU = [ps("psBU%d" % i, [128, 512]) for i in range(2)]
        psBS = [ps("psBS%d" % i, [128, 512]) for i in range(2)]
        psI = ps("psI", [128, 512])
        psY = ps("psY", [128, 512])

        ld(identf[:], identf_d[:, :], 'identf')
        ld(dcol[:], dcol_d[:, :], 'dcol')
        ld(mask01[:], mask01_d[:, :], 'mask01')
        S.op('dve', lambda: V.tensor_copy(out=identb[:], in_=identf[:]), reads=['identf'], writes=['identb'])
        S.op('dve', lambda: V.memset(epsT[:], RMS_EPS), writes=['epsT'])

        es2 = ExitStack()
        with es2:
            tb = lambda n, s, d=F32: es2.enter_context(nc.sbuf_tensor("t_" + n, s, d))
            csil = tb("csil", [128, 8]); csil_rep = tb("csil_rep", [128, 8, 128])
            stg = [tb("stg%d" % i, [128, 8, 256]) for i in range(2)]
            adab_rep = tb("adab_rep", [128, 256])
            n1g_rep = tb("n1g_rep", [128, D])
            ld(csil[:], c_l[:, :], 'csil')
            ld(n1g_rep[:], n1g[0:1, :].partition_broadcast(128), 'n1g_rep')
            S.op('act', lambda: A.activation(out=csil[:], in_=csil[:], func=AF.Silu), reads=['csil'], writes=['csil'])
            S.op('dve', lambda: V.tensor_copy(out=csil_rep[:], in_=csil[:].unsqueeze(2).to_broadcast([128, 8, 128])),
                 reads=['csil'], writes=['csil_rep'])
            for j in range(8):
                st = stg[j % 2]; k_st = 'stg%d' % (j % 2)
                ld(st[:], adaw[:, j * 256:(j + 1) * 256].rearrange("(kc k) n -> k kc n", k=128), k_st)
                ld(adab_rep[:], adab[0:1, j * 256:(j + 1) * 256].partition_broadcast(128), 'adab_rep')
                for kc in range(8):
                    S.op('pe', lambda: PE.matmul(psU[:, 0:256], lhsT=csil_rep[:, kc, :], rhs=st[:, kc, :], start=(kc == 0), stop=(kc == 7)),
                         reads=['csil_rep', k_st], writes=['psU'], pe_chain=(kc > 0))
                S.op('dve', lambda: V.tensor_tensor(out=modrep[:, j * 256:(j + 1) * 256], in0=psU[:, 0:256], in1=adab_rep[:], op=ALU.add),
                     reads=['psU', 'adab_rep'], writes=['modrep'])
            sh1 = modrep[:, 0:1024]; sc1 = modrep[:, 1024:2048]
            S.op('dve', lambda: V.scalar_tensor_tensor(out=gk1[:], in0=sc1, scalar=1.0, in1=n1g_rep[:], op0=ALU.add, op1=ALU.mult),
                 reads=['modrep', 'n1g_rep'], writes=['gk1'])
            ld(stg[0][:], win_d[:, :].rearrange("(kc k) n -> k kc n", k=128), 'stg0')
            S.op('dve', lambda: V.tensor_copy(out=win[:], in_=stg[0][:]), reads=['stg0'], writes=['win'])

            def emit_sin(outap, th, ki, kf, tmp, keys, shift=0.0):
                kth, kout, kki, kkf, ktmp = keys
                if shift != 0.0:
                    S.op('dve', lambda: V.tensor_scalar(out=th, in0=th, scalar1=shift, scalar2=None, op0=ALU.add), reads=[kth], writes=[kth])
                S.op('dve', lambda: V.tensor_scalar(out=tmp, in0=th, scalar1=1.0 / (2 * PI), scalar2=None, op0=ALU.mult), reads=[kth], writes=[ktmp])
                S.op('dve', lambda: V.tensor_copy(out=ki, in_=tmp), reads=[ktmp], writes=[kki])
                S.op('dve', lambda: V.tensor_copy(out=kf, in_=ki), reads=[kki], writes=[kkf])
                S.op('dve', lambda: V.scalar_tensor_tensor(out=th, in0=kf, scalar=-2 * PI, in1=th, op0=ALU.mult, op1=ALU.add),
                     reads=[kkf, kth], writes=[kth])
                S.op('dve', lambda: V.tensor_scalar(out=tmp, in0=th, scalar1=PI, scalar2=-2 * PI, op0=ALU.is_gt, op1=ALU.mult), reads=[kth], writes=[ktmp])
                S.op('dve', lambda: V.tensor_tensor(out=th, in0=th, in1=tmp, op=ALU.add), reads=[kth, ktmp], writes=[kth])
                S.op('dve', lambda: V.tensor_scalar(out=tmp, in0=th, scalar1=-PI, scalar2=2 * PI, op0=ALU.is_lt, op1=ALU.mult), reads=[kth], writes=[ktmp])
                S.op('dve', lambda: V.tensor_tensor(out=th, in0=th, in1=tmp, op=ALU.add), reads=[kth, ktmp], writes=[kth])
                S.op('dve', lambda: V.tensor_scalar(out=th, in0=th, scalar1=3.1415925, scalar2=-3.1415925, op0=ALU.min, op1=ALU.max), reads=[kth], writes=[kth])
                S.op('act', lambda: A.activation(out=outap, in_=th, func=AF.Sin), reads=[kth], writes=[kout])

            are_c = tb("are_c", [128, 16]); aim_c = tb("aim_c", [128, 16]); dtc = tb("dtc", [128, 16])
            adr_c = tb("adr_c", [128, 16]); nadr_c = tb("nadr_c", [128, 16]); adi_c = tb("adi_c", [128, 16])
            mrow = tb("mrow", [128, 128]); swapm = tb("swapm", [128, 128]); sgn_c = tb("sgn_c", [128, 1]); sgn_r = tb("sgn_r", [128, 128])
            ld(are_c[:], are_c_d[:, :], 'are_c'); ld(aim_c[:], aim_c_d[:, :], 'aim_c'); ld(dtc[:], ldt_c_d[:, :], 'dtc')
            ld(mrow[:], mrow_d[:, :], 'mrow'); ld(swapm[:], swapm_d[:, :], 'swapm'); ld(sgn_c[:], sgnc_d[:, :], 'sgn_c'); ld(sgn_r[:], sgnr_d[:, :], 'sgn_r')
            S.op('act', lambda: A.activation(out=dtc[:], in_=dtc[:], func=AF.Exp), reads=['dtc'], writes=['dtc'])
            S.op('dve', lambda: V.tensor_tensor(out=adr_c[:], in0=are_c[:], in1=dtc[:], op=ALU.mult), reads=['are_c', 'dtc'], writes=['adr_c'])
            S.op('dve', lambda: V.tensor_scalar(out=nadr_c[:], in0=adr_c[:], scalar1=-1.0, scalar2=None, op0=ALU.mult), reads=['adr_c'], writes=['nadr_c'])
            S.op('dve', lambda: V.tensor_tensor(out=adi_c[:], in0=aim_c[:], in1=dtc[:], op=ALU.mult), reads=['aim_c', 'dtc'], writes=['adi_c'])
            T1 = tb("T1", [128, 2048]); T2 = tb("T2", [128, 2048]); T3 = tb("T3", [128, 2048]); T4 = tb("T4", [128, 2048])
            T5 = tb("T5", [128, 2048]); T6 = tb("T6", [128, 2048]); TI = tb("TI", [128, 2048], I32)
            T1v = T1[:].rearrange("p (g m) -> p g m", m=128); T2v = T2[:].rearrange("p (g m) -> p g m", m=128)
            T3v = T3[:].rearrange("p (g m) -> p g m", m=128); T4v = T4[:].rearrange("p (g m) -> p g m", m=128)
            for g in range(16):
                S.op('act', lambda: A.activation(out=T1v[:, g, :], in_=mrow[:], func=AF.Exp, scale=adr_c[:, g:g + 1]), reads=['mrow', 'adr_c'], writes=['T1'])
                S.op('act', lambda: A.activation(out=T2v[:, g, :], in_=mrow[:], func=AF.Exp, scale=nadr_c[:, g:g + 1]), reads=['mrow', 'nadr_c'], writes=['T2'])
                S.op('dve', lambda: V.tensor_scalar(out=T3v[:, g, :], in0=mrow[:], scalar1=adi_c[:, g:g + 1], scalar2=None, op0=ALU.mult),
                     reads=['mrow', 'adi_c'], writes=['T3'])
            S.op('dve', lambda: V.tensor_copy(out=T4[:], in_=T3[:]), reads=['T3'], writes=['T4'])
            emit_sin(T3[:], T3[:], TI[:], T5[:], T6[:], ('T3', 'T3', 'TI', 'T5', 'T6'))
            emit_sin(T4[:], T4[:], TI[:], T5[:], T6[:], ('T4', 'T4', 'TI', 'T5', 'T6'), shift=PI / 2)
            fl = lambda t: t[:].rearrange("p g m -> p (g m)")
            S.op('dve', lambda: V.tensor_tensor(out=fl(Apow_r), in0=T1[:], in1=T4[:], op=ALU.mult), reads=['T1', 'T4'], writes=['Apow_r'])
            S.op('dve', lambda: V.tensor_tensor(out=fl(Apow_i), in0=T1[:], in1=T3[:], op=ALU.mult), reads=['T1', 'T3'], writes=['Apow_i'])
            S.op('dve', lambda: V.tensor_tensor(out=fl(Ainv_r), in0=T2[:], in1=T4[:], op=ALU.mult), reads=['T2', 'T4'], writes=['Ainv_r'])
            S.op('dve', lambda: V.scalar_tensor_tensor(out=fl(Ainv_i), in0=T2[:], scalar=-1.0, in1=T3[:], op0=ALU.mult, op1=ALU.mult),
                 reads=['T2', 'T3'], writes=['Ainv_i'])
            e128 = tb("e128", [128, 16]); th_s = tb("th_s", [128, 16]); th_c = tb("th_c", [128, 16])
            ki16 = tb("ki16", [128, 16], I32); kf16 = tb("kf16", [128, 16]); tm16 = tb("tm16", [128, 16])
            r128r = tb("r128r", [128, 16]); r128i = tb("r128i", [128, 16])
            S.op('act', lambda: A.activation(out=e128[:], in_=adr_c[:], func=AF.Exp, scale=128.0), reads=['adr_c'], writes=['e128'])
            S.op('dve', lambda: V.tensor_scalar(out=th_s[:], in0=adi_c[:], scalar1=128.0, scalar2=None, op0=ALU.mult), reads=['adi_c'], writes=['th_s'])
            S.op('dve', lambda: V.tensor_copy(out=th_c[:], in_=th_s[:]), reads=['th_s'], writes=['th_c'])
            emit_sin(th_s[:], th_s[:], ki16[:], kf16[:], tm16[:], ('th_s', 'th_s', 'ki16', 'kf16', 'tm16'))
            emit_sin(th_c[:], th_c[:], ki16[:], kf16[:], tm16[:], ('th_c', 'th_c', 'ki16', 'kf16', 'tm16'), shift=PI / 2)
            S.op('dve', lambda: V.tensor_tensor(out=r128r[:], in0=e128[:], in1=th_c[:], op=ALU.mult), reads=['e128', 'th_c'], writes=['r128r'])
            S.op('dve', lambda: V.tensor_tensor(out=r128i[:], in0=e128[:], in1=th_s[:], op=ALU.mult), reads=['e128', 'th_s'], writes=['r128i'])
            S.op('dve', lambda: V.tensor_scalar(out=r128i[:], in0=r128i[:], scalar1=sgn_c[:, 0:1], scalar2=None, op0=ALU.mult),
                 reads=['r128i', 'sgn_c'], writes=['r128i'])
            for g in range(16):
                S.op('dve', lambda: V.tensor_scalar(out=Rot[:, g, :], in0=identf[:], scalar1=r128r[:, g:g + 1], scalar2=None, op0=ALU.mult),
                     reads=['identf', 'r128r'], writes=['Rot'])
                S.op('dve', lambda: V.scalar_tensor_tensor(out=Rot[:, g, :], in0=swapm[:], scalar=r128i[:, g:g + 1], in1=Rot[:, g, :],
                                                           op0=ALU.mult, op1=ALU.add), reads=['swapm', 'r128i', 'Rot'], writes=['Rot'])
            T7 = tb("T7", [128, 2048]); T8 = tb("T8", [128, 2048])
            ld(T1[:], are_r_d[:, :], 'T1'); ld(T2[:], aim_r_d[:, :], 'T2'); ld(T3[:], ldt_r_d[:, :], 'T3')
            S.op('act', lambda: A.activation(out=T3[:], in_=T3[:], func=AF.Exp), reads=['T3'], writes=['T3'])
            S.op('dve', lambda: V.tensor_tensor(out=T4[:], in0=T2[:], in1=T3[:], op=ALU.mult), reads=['T2', 'T3'], writes=['T4'])
            S.op('dve', lambda: V.tensor_tensor(out=T3[:], in0=T1[:], in1=T3[:], op=ALU.mult), reads=['T1', 'T3'], writes=['T3'])
            S.op('act', lambda: A.activation(out=T3[:], in_=T3[:], func=AF.Exp), reads=['T3'], writes=['T3'])
            S.op('dve', lambda: V.tensor_copy(out=T7[:], in_=T4[:]), reads=['T4'], writes=['T7'])
            emit_sin(T4[:], T4[:], TI[:], T5[:], T6[:], ('T4', 'T4', 'TI', 'T5', 'T6'))
            emit_sin(T7[:], T7[:], TI[:], T5[:], T6[:], ('T7', 'T7', 'TI', 'T5', 'T6'), shift=PI / 2)
            S.op('dve', lambda: V.tensor_tensor(out=T4[:], in0=T4[:], in1=T3[:], op=ALU.mult), reads=['T4', 'T3'], writes=['T4'])
            S.op('dve', lambda: V.tensor_tensor(out=T7[:], in0=T7[:], in1=T3[:], op=ALU.mult), reads=['T7', 'T3'], writes=['T7'])
            S.op('dve', lambda: V.tensor_scalar(out=T7[:], in0=T7[:], scalar1=-1.0, scalar2=None, op0=ALU.add), reads=['T7'], writes=['T7'])
            S.op('dve', lambda: V.tensor_tensor(out=T5[:], in0=T1[:], in1=T1[:], op=ALU.mult), reads=['T1'], writes=['T5'])
            S.op('dve', lambda: V.tensor_tensor(out=T6[:], in0=T2[:], in1=T2[:], op=ALU.mult), reads=['T2'], writes=['T6'])
            S.op('dve', lambda: V.tensor_tensor(out=T5[:], in0=T5[:], in1=T6[:], op=ALU.add), reads=['T5', 'T6'], writes=['T5'])
            S.op('dve', lambda: V.reciprocal(out=T5[:], in_=T5[:]), reads=['T5'], writes=['T5'])
            S.op('dve', lambda: V.tensor_tensor(out=T3[:], in0=T7[:], in1=T1[:], op=ALU.mult), reads=['T7', 'T1'], writes=['T3'])
            S.op('dve', lambda: V.tensor_tensor(out=T6[:], in0=T4[:], in1=T2[:], op=ALU.mult), reads=['T4', 'T2'], writes=['T6'])
            S.op('dve', lambda: V.tensor_tensor(out=T3[:], in0=T3[:], in1=T6[:], op=ALU.add), reads=['T3', 'T6'], writes=['T3'])
            S.op('dve', lambda: V.tensor_tensor(out=T3[:], in0=T3[:], in1=T5[:], op=ALU.mult), reads=['T3', 'T5'], writes=['T3'])
            S.op('dve', lambda: V.tensor_tensor(out=T8[:], in0=T4[:], in1=T1[:], op=ALU.mult), reads=['T4', 'T1'], writes=['T8'])
            S.op('dve', lambda: V.tensor_tensor(out=T6[:], in0=T7[:], in1=T2[:], op=ALU.mult), reads=['T7', 'T2'], writes=['T6'])
            S.op('dve', lambda: V.tensor_tensor(out=T8[:], in0=T8[:], in1=T6[:], op=ALU.subtract), reads=['T8', 'T6'], writes=['T8'])
            S.op('dve', lambda: V.tensor_tensor(out=T8[:], in0=T8[:], in1=T5[:], op=ALU.mult), reads=['T8', 'T5'], writes=['T8'])
            ld(T1[:], X1p_d[:, :], 'T1'); ld(T2[:], X2p_d[:, :], 'T2')
            S.op('dve', lambda: V.tensor_tensor(out=T2[:].rearrange("p (g f) -> p g f", f=128), in0=T2[:].rearrange("p (g f) -> p g f", f=128),
                                                in1=sgn_r[:].unsqueeze(1).to_broadcast([128, 16, 128]), op=ALU.mult), reads=['T2', 'sgn_r'], writes=['T2'])
            S.op('dve', lambda: V.tensor_tensor(out=T5[:], in0=T3[:], in1=T1[:], op=ALU.mult), reads=['T3', 'T1'], writes=['T5'])
            S.op('dve', lambda: V.tensor_tensor(out=T6[:], in0=T8[:], in1=T2[:], op=ALU.mult), reads=['T8', 'T2'], writes=['T6'])
            S.op('dve', lambda: V.tensor_tensor(out=fl(Bpad), in0=T5[:], in1=T6[:], op=ALU.add), reads=['T5', 'T6'], writes=['Bpad'])
            S.op('dve', lambda: V.tensor_tensor(out=T5[:], in0=T3[:], in1=T2[:], op=ALU.mult), reads=['T3', 'T2'], writes=['T5'])
            S.op('dve', lambda: V.tensor_tensor(out=T6[:], in0=T8[:], in1=T1[:], op=ALU.mult), reads=['T8', 'T1'], writes=['T6'])
            S.op('dve', lambda: V.tensor_tensor(out=fl(BpadS), in0=T5[:], in1=T6[:], op=ALU.subtract), reads=['T5', 'T6'], writes=['BpadS'])
            ld(T1[:], CcP_d[:, :], 'T1'); ld(T2[:], CcSP_d[:, :], 'T2')
            S.op('dve', lambda: V.tensor_scalar(out=fl(Cc), in0=T1[:], scalar1=sgn_c[:, 0:1], scalar2=None, op0=ALU.mult), reads=['T1', 'sgn_c'], writes=['Cc'])
            S.op('dve', lambda: V.tensor_scalar(out=fl(CcS), in0=T2[:], scalar1=-1.0, scalar2=None, op0=ALU.mult), reads=['T2'], writes=['CcS'])
            S.barrier()
        xt = [sb("xt%d" % i, [128, D]) for i in range(2)]
        junk = sb("junk", [128, D], BF16)
        ss = sb("ss", [128, 1]); rs = sb("rs", [128, 1])
        hm = sb("hm", [128, D]); hmb = sb("hmb", [128, D], BF16)
        hmT = sb("hmT", [128, 8, 128], BF16)
        uT = sb("uT", [128, 2, 128]); uTb = sb("uTb", [128, 2, 128], BF16)
        Vt = [sb("Vt%d" % i, [128, 4, 128]) for i in range(2)]
        Wt = [sb("Wt%d" % i, [128, 4, 128]) for i in range(2)]
        Zt = [sb("Zt%d" % i, [128, 4, 128]) for i in range(4)]
        Hr = [sb("Hr%d" % i, [128, 4, 128], BF16) for i in range(2)]
        Hi = [sb("Hi%d" % i, [128, 4, 128], BF16) for i in range(2)]
        yt = sb("yt", [128, 2, 128]); ga = sb("ga", [128, 2, 128]); yo = sb("yo", [128, 2, 128])
        sh1 = modrep[:, 0:1024]

        for c in range(NCH):
            xb = xt[c % 2]; kx = 'xt%d' % (c % 2)
            r0 = c * 128
            ld(xb[:], x[r0:r0 + 128, :], kx)
            S.op('act', lambda: A.activation(out=junk[:], in_=xb[:], func=AF.Square, accum_out=ss[:]), reads=[kx], writes=['junk', 'ss'])
            S.op('act', lambda: A.activation(out=rs[:], in_=ss[:], func=AF.Sqrt, bias=epsT[:], scale=1.0 / D), reads=['ss', 'epsT'], writes=['rs'])
            S.op('dve', lambda: V.reciprocal(out=rs[:], in_=rs[:]), reads=['rs'], writes=['rs'])
            S.op('dve', lambda: V.scalar_tensor_tensor(out=hm[:], in0=xb[:], scalar=rs[:], in1=gk1[:], op0=ALU.mult, op1=ALU.mult),
                 reads=[kx, 'rs', 'gk1'], writes=['hm'])
            S.op('dve', lambda: V.tensor_tensor(out=hmb[:], in0=hm[:], in1=sh1, op=ALU.add), reads=['hm', 'modrep'], writes=['hmb'])
            for kc in range(8):
                S.op('pe', lambda: PE.transpose(out=psT[:, kc * 128:(kc + 1) * 128], in_=hmb[:, kc * 128:(kc + 1) * 128], identity=identb[:]),
                     reads=['hmb', 'identb'], writes=['psT'], pe_chain=(kc > 0))
            S.op('act', lambda: A.copy(out=hmT[:].rearrange("p a b -> p (a b)"), in_=psT[:]), reads=['psT'], writes=['hmT'])
            for mc in range(2):
                for kc in range(8):
                    S.op('pe', lambda: PE.matmul(psU[:, mc * 128:(mc + 1) * 128], lhsT=win[:, kc, mc * 128:(mc + 1) * 128], rhs=hmT[:, kc, :],
                                                 start=(kc == 0), stop=(kc == 7)), reads=['win', 'hmT'], writes=['psU'],
                         pe_chain=not (mc == 0 and kc == 0))
            S.op('act', lambda: A.copy(out=uT[:].rearrange("p a b -> p (a b)"), in_=psU[:, 0:256]), reads=['psU'], writes=['uT'])
            S.op('act', lambda: A.copy(out=uTb[:].rearrange("p a b -> p (a b)"), in_=psU[:, 0:256]), reads=['psU'], writes=['uTb'])
            for bt in range(4):
                i2 = bt % 2
                kbu = 'psBU%d' % i2; kbs = 'psBS%d' % i2
                kV = 'Vt%d' % i2; kW = 'Wt%d' % i2; kZ = 'Zt%d' % bt; kHr = 'Hr%d' % i2; kHi = 'Hi%d' % i2
                gc = bt // 2
                for gg in range(4):
                    g = bt * 4 + gg
                    S.op('pe', lambda: PE.matmul(psBU[i2][:, gg * 128:(gg + 1) * 128], lhsT=Bpad[:, g, :], rhs=uTb[:, gc, :], start=True, stop=True),
                         reads=['Bpad', 'uTb'], writes=[kbu], pe_chain=(gg > 0))
                for gg in range(4):
                    g = bt * 4 + gg
                    S.op('pe', lambda: PE.matmul(psBS[i2][:, gg * 128:(gg + 1) * 128], lhsT=BpadS[:, g, :], rhs=uTb[:, gc, :], start=True, stop=True),
                         reads=['BpadS', 'uTb'], writes=[kbs], pe_chain=(gg > 0))
                Vf = Vt[i2][:].rearrange("p a b -> p (a b)"); Wf = Wt[i2][:].rearrange("p a b -> p (a b)")
                Zf = Zt[bt][:].rearrange("p a b -> p (a b)")
                gs = slice(bt * 4, bt * 4 + 4)
                S.op('dve', lambda: V.tensor_tensor(out=Vf, in0=psBU[i2][:], in1=Ainv_r[:, gs, :].rearrange("p a b -> p (a b)"), op=ALU.mult),
                     reads=[kbu, 'Ainv_r'], writes=[kV])
                S.op('dve', lambda: V.tensor_tensor(out=Wf, in0=psBS[i2][:], in1=Ainv_i[:, gs, :].rearrange("p a b -> p (a b)"), op=ALU.mult),
                     reads=[kbs, 'Ainv_i'], writes=[kW])
                S.op('dve', lambda: V.tensor_tensor(out=Vf, in0=Vf, in1=Wf, op=ALU.add), reads=[kV, kW], writes=[kV])
                if c > 0:
                    S.op('dve', lambda: V.tensor_tensor(out=Vt[i2][:, :, 0], in0=Vt[i2][:, :, 0], in1=psI[:, bt * 4:bt * 4 + 4], op=ALU.add),
                         reads=[kV, 'psI'], writes=[kV])
                S.op('dve', lambda: V.tensor_tensor_scan(out=Zf, data0=mask01[:], data1=Vf, initial=0.0, op0=ALU.mult, op1=ALU.add),
                     reads=['mask01', kV], writes=[kZ])
                if c < NCH - 1:
                    for gg in range(4):
                        g = bt * 4 + gg
                        S.op('pe', lambda: PE.matmul(psI[:, g:g + 1], lhsT=Rot[:, g, :], rhs=Zt[bt][:, gg, 127:128], start=True, stop=True),
                             reads=['Rot', kZ], writes=['psI'], pe_chain=(gg > 0))
                S.op('dve', lambda: V.tensor_tensor(out=Hr[i2][:].rearrange("p a b -> p (a b)"), in0=Zf,
                                                    in1=Apow_r[:, gs, :].rearrange("p a b -> p (a b)"), op=ALU.mult), reads=[kZ, 'Apow_r'], writes=[kHr])
                S.op('dve', lambda: V.tensor_tensor(out=Hi[i2][:].rearrange("p a b -> p (a b)"), in0=Zf,
                                                    in1=Apow_i[:, gs, :].rearrange("p a b -> p (a b)"), op=ALU.mult), reads=[kZ, 'Apow_i'], writes=[kHi])
                for gg in range(4):
                    g = bt * 4 + gg
                    first = (g % 8 == 0)
                    S.op('pe', lambda: PE.matmul(psY[:, gc * 128:(gc + 1) * 128], lhsT=Cc[:, g, :], rhs=Hr[i2][:, gg, :], start=first, stop=False),
                         reads=['Cc', kHr], writes=['psY'], pe_chain=not first)
                    S.op('pe', lambda: PE.matmul(psY[:, gc * 128:(gc + 1) * 128], lhsT=CcS[:, g, :], rhs=Hi[i2][:, gg, :], start=False, stop=(g % 8 == 7)),
                         reads=['CcS', kHi], writes=['psY'], pe_chain=True)
            ytf = yt[:].rearrange("p a b -> p (a b)"); gaf = ga[:].rearrange("p a b -> p (a b)"); yof = yo[:].rearrange("p a b -> p (a b)")
            for gc in range(2):
                S.op('dve', lambda: V.scalar_tensor_tensor(out=yt[:, gc, :], in0=uT[:, gc, :], scalar=dcol[:, gc:gc + 1],
                                                           in1=psY[:, gc * 128:(gc + 1) * 128], op0=ALU.mult, op1=ALU.add),
                     reads=['uT', 'dcol', 'psY'], writes=['yt'])
            S.op('dve', lambda: V.tensor_tensor(out=gaf, in0=ytf, in1=ytf, op=ALU.mult), reads=['yt'], writes=['ga'])
            S.op('dve', lambda: V.tensor_scalar(out=gaf, in0=gaf, scalar1=0.044715, scalar2=1.0, op0=ALU.mult, op1=ALU.add), reads=['ga'], writes=['ga'])
            S.op('dve', lambda: V.tensor_tensor(out=gaf, in0=gaf, in1=ytf, op=ALU.mult), reads=['ga', 'yt'], writes=['ga'])
            S.op('act', lambda: A.activation(out=gaf, in_=gaf, func=AF.Sigmoid, scale=1.5957691216), reads=['ga'], writes=['ga'])
            S.op('dve', lambda: V.tensor_tensor(out=yof, in0=gaf, in1=ytf, op=ALU.mult), reads=['ga', 'yt'], writes=['yo'])
            S.dma('sp', lambda: nc.sync.dma_start(out=out[:, r0:r0 + 128].rearrange("(gc k) n -> k gc n", k=128), in_=yo[:]), reads=['yo'], writes=['out'])
        S.finish(['out'])
        print("s5 program: instr", S.n_instr, "waits", S.n_wait)
    return nc


def s5_consts():
    f = np.arange(128)
    swap = np.zeros((128, 128), np.float32); swap[f, (f + 64) % 128] = 1.0
    sgn_c = np.where(f < 64, 1.0, -1.0).astype(np.float32)[:, None]
    sgn_r = np.broadcast_to(np.where(f < 64, -1.0, 1.0).astype(np.float32)[None, :], (128, 128)).copy()
    mrow = np.broadcast_to(np.arange(128, dtype=np.float32)[None, :], (128, 128)).copy()
    m01 = np.ones((128, 4, 128), np.float32); m01[:, :, 0] = 0.0
    return {"identf": np.eye(128, dtype=np.float32), "swapm": swap, "sgn_c": sgn_c, "sgn_r": sgn_r, "mrow": mrow,
            "mask01": m01.reshape(128, 512)}


def s5_layouts(a_re, a_im, log_dt, b_re, b_im, c_re, c_im, d, cq):
    G0 = 16 * cq
    f = np.arange(128)
    p = f % 64
    are_c = np.ascontiguousarray(a_re[G0:G0 + 16][:, p].T)
    aim_c = np.ascontiguousarray(a_im[G0:G0 + 16][:, p].T)
    ldt_c = np.ascontiguousarray(np.broadcast_to(log_dt[G0:G0 + 16][None, :], (128, 16)))
    are_r = np.ascontiguousarray(np.broadcast_to(a_re[G0:G0 + 16][:, p].reshape(1, 16 * 128), (128, 2048)))
    aim_r = np.ascontiguousarray(np.broadcast_to(a_im[G0:G0 + 16][:, p].reshape(1, 16 * 128), (128, 2048)))
    ldt_r = np.ascontiguousarray(np.broadcast_to(np.repeat(log_dt[G0:G0 + 16], 128).reshape(1, 2048), (128, 2048)))
    X1p = np.zeros((128, 16, 128), np.float32); X2p = np.zeros((128, 16, 128), np.float32)
    CcP = np.zeros((128, 16, 128), np.float32); CcSP = np.zeros((128, 16, 128), np.float32)
    for g in range(16):
        r0 = (g % 8) * 16
        br = b_re[G0 + g]; bi = b_im[G0 + g]
        X1p[r0:r0 + 16, g, 0:64] = br.T; X1p[r0:r0 + 16, g, 64:128] = bi.T
        X2p[r0:r0 + 16, g, 0:64] = bi.T; X2p[r0:r0 + 16, g, 64:128] = br.T
        cr = c_re[G0 + g]; ci = c_im[G0 + g]
        CcP[0:64, g, r0:r0 + 16] = cr.T; CcP[64:128, g, r0:r0 + 16] = ci.T
        CcSP[0:64, g, r0:r0 + 16] = ci.T; CcSP[64:128, g, r0:r0 + 16] = cr.T
    dcol = np.ascontiguousarray(d[cq * 256:(cq + 1) * 256].reshape(2, 128).T)
    return {"are_c": are_c, "aim_c": aim_c, "ldt_c": ldt_c, "are_r": are_r, "aim_r": aim_r, "ldt_r": ldt_r,
            "X1p": X1p.reshape(128, 2048), "X2p": X2p.reshape(128, 2048), "CcP": CcP.reshape(128, 2048),
            "CcSP": CcSP.reshape(128, 2048), "dcol": dcol}

import math

RMS_EPS = 1e-6


def build_nsa(NS):
    nc = bass.Bass("TRN2", target_bir_lowering=False)
    T = NS * 512
    NT = NS * 4
    D = 1024
    NM = 1024 if T >= 16384 else (T // 16 + 32)
    NMT = (NM + 127) // 128
    din = lambda n, s, d=F32: nc.dram_tensor(n, s, d, kind="ExternalInput").ap()
    x = din("x", [T, D])
    c_l = din("c_l", [128, 8])
    adaw = din("adaw", [D, 2048]); adab = din("adab", [1, 2048]); n1g = din("n1g", [1, D])
    wpf_d = din("wpf", [D, 512]); wpt_d = din("wpt", [D, 524])
    wk1_d = din("wk1", [2048, 256]); wv1_d = din("wv1", [2048, 256])
    wk2_d = din("wk2", [256, 64]); wv2_d = din("wv2", [256, 64])
    pekT_d = din("pekT", [64, 32]); pevT_d = din("pevT", [64, 32])
    cos_d = din("cos2", [64, T]); sin_d = din("sin2", [64, T])
    cosc_d = din("cosc", [64, NM]); sinc_d = din("sinc", [64, NM])
    prot_d = din("prot", [64, 64])
    identf_d = din("identf", [128, 128])
    triT_d = din("triT", [128, 128]); triTs_d = din("triTs", [128, 128])
    cmg_d = din("cmaskG", [128, 16]); cm0_d = din("cmask0", [128, 8])
    hilo_d = din("hilo", [128, 3])
    out = nc.dram_tensor("out", [T, 256], F32, kind="ExternalOutput").ap()

    es = ExitStack()
    with es:
        es.enter_context(nc.allow_low_precision("bf16 matmul operands"))
        es.enter_context(nc.allow_non_contiguous_dma("small layout loads"))
        S = Sched(nc, es)
        sb = lambda n, s, d=F32: es.enter_context(nc.sbuf_tensor("s_" + n, s, d))
        ps = lambda n, s, d=F32: es.enter_context(nc.psum_tensor("p_" + n, s, d))
        V, A, P, PE = nc.vector, nc.scalar, nc.gpsimd, nc.tensor

        def ld(dst, src, key):
            S.dma('sp', lambda: nc.sync.dma_start(out=dst, in_=src), writes=[key])

        identf = sb("identf", [128, 128]); identb = sb("identb", [128, 128], BF16)
        triT = sb("triT", [128, 128], BF16); triTs = sb("triTs", [128, 128], BF16)
        cmaskG = sb("cmaskG", [128, 16]); cmask0 = sb("cmask0", [128, 8]); hilo = sb("hilo", [128, 3])
        epsT = sb("epsT", [128, 1]); ones1 = sb("ones1", [1, 128]); kmx = sb("kmx", [1, 1]); kmax2 = sb("kmax2", [128, 1])
        prot = sb("prot", [64, 64])
        modrep = sb("modrep", [128, 2048]); gk1 = sb("gk1", [128, D])
        wpf = sb("wpf", [128, 8, 512], BF16); wpt = sb("wpt", [128, 8, 524], BF16)
        wk1 = sb("wk1", [64, 32, 256], BF16); wv1 = sb("wv1", [64, 32, 256], BF16)
        wk2 = sb("wk2", [128, 2, 64], BF16); wv2 = sb("wv2", [128, 2, 64], BF16)
        biask = sb("biask", [128, 2]); biasv = sb("biasv", [128, 2])
        ksT = sb("ksT", [65, T], BF16); kwT = sb("kwT", [65, 1024], BF16)
        vsA = sb("vsA", [128, NT, 65], BF16); vwA = sb("vwA", [128, 8, 65], BF16)
        kcmpT = sb("kcmpT", [64, NMT * 128], BF16); vcmp = sb("vcmp", [128, NMT, 64], BF16)
        kcbuf = sb("kcbuf", [64, 528], BF16); vcbuf = sb("vcbuf", [64, 528], BF16)

        psS = [ps("psS%d" % i, [128, 512]) for i in range(2)]
        psOs = ps("psOs", [128, 512]); psOw = ps("psOw", [128, 512])
        psC = ps("psC", [128, 1024])
        psT = ps("psT", [128, 1024], BF16)
        psX = ps("psX", [128, 512])

        ld(identf[:], identf_d[:, :], 'identf')
        ld(cmaskG[:], cmg_d[:, :], 'cmaskG'); ld(cmask0[:], cm0_d[:, :], 'cmask0'); ld(hilo[:], hilo_d[:, :], 'hilo')
        ld(prot[:], prot_d[:, :], 'prot')
        S.op('dve', lambda: V.tensor_copy(out=identb[:], in_=identf[:]), reads=['identf'], writes=['identb'])
        S.op('dve', lambda: V.memset(epsT[:], RMS_EPS), writes=['epsT'])
        S.op('dve', lambda: V.memset(ones1[:], 1.0), writes=['ones1'])
        S.op('dve', lambda: V.memset(kmx[:], 0.0), writes=['kmx'])
        for c0 in range(0, T, 2048):
            S.op('dve', lambda: V.memset(ksT[:, c0:min(T, c0 + 2048)], 1.0), writes=['ksT'])
        S.op('pool', lambda: P.memset(kwT[:], 1.0), writes=['kwT'])
        S.op('dve', lambda: V.memset(vsA[:], 1.0), writes=['vsA'])
        S.op('pool', lambda: P.memset(vwA[:], 1.0), writes=['vwA'])
        S.op('dve', lambda: V.memset(kcmpT[:], 0.0), writes=['kcmpT'])
        S.op('dve', lambda: V.memset(vcmp[:], 0.0), writes=['vcmp'])
        S.op('dve', lambda: V.memset(kcbuf[:], 0.0), writes=['kcbuf'])
        S.op('dve', lambda: V.memset(vcbuf[:], 0.0), writes=['vcbuf'])

        es2 = ExitStack()
        with es2:
            tb = lambda n, s, d=F32: es2.enter_context(nc.sbuf_tensor("t_" + n, s, d))
            csil = tb("csil", [128, 8]); csil_rep = tb("csil_rep", [128, 8, 128])
            stg = [tb("stg%d" % i, [128, 8, 256]) for i in range(2)]
            adab_rep = tb("adab_rep", [128, 256]); n1g_rep = tb("n1g_rep", [128, D])
            tmpf = tb("tmpf", [128, 128])
            ld(tmpf[:], triT_d[:, :], 'tmpf')
            S.op('dve', lambda: V.tensor_copy(out=triT[:], in_=tmpf[:]), reads=['tmpf'], writes=['triT'])
            ld(tmpf[:], triTs_d[:, :], 'tmpf')
            S.op('dve', lambda: V.tensor_copy(out=triTs[:], in_=tmpf[:]), reads=['tmpf'], writes=['triTs'])
            ld(csil[:], c_l[:, :], 'csil')
            ld(n1g_rep[:], n1g[0:1, :].partition_broadcast(128), 'n1g_rep')
            S.op('act', lambda: A.activation(out=csil[:], in_=csil[:], func=AF.Silu), reads=['csil'], writes=['csil'])
            S.op('dve', lambda: V.tensor_copy(out=csil_rep[:], in_=csil[:].unsqueeze(2).to_broadcast([128, 8, 128])),
                 reads=['csil'], writes=['csil_rep'])
            for j in range(8):
                st = stg[j % 2]; k_st = 'stg%d' % (j % 2)
                ld(st[:], adaw[:, j * 256:(j + 1) * 256].rearrange("(kc k) n -> k kc n", k=128), k_st)
                ld(adab_rep[:], adab[0:1, j * 256:(j + 1) * 256].partition_broadcast(128), 'adab_rep')
                for kc in range(8):
                    S.op('pe', lambda: PE.matmul(psX[:, 0:256], lhsT=csil_rep[:, kc, :], rhs=st[:, kc, :], start=(kc == 0), stop=(kc == 7)),
                         reads=['csil_rep', k_st], writes=['psX'], pe_chain=(kc > 0))
                S.op('dve', lambda: V.tensor_tensor(out=modrep[:, j * 256:(j + 1) * 256], in0=psX[:, 0:256], in1=adab_rep[:], op=ALU.add),
                     reads=['psX', 'adab_rep'], writes=['modrep'])
            sc1 = modrep[:, 1024:2048]
            S.op('dve', lambda: V.scalar_tensor_tensor(out=gk1[:], in0=sc1, scalar=1.0, in1=n1g_rep[:], op0=ALU.add, op1=ALU.mult),
                 reads=['modrep', 'n1g_rep'], writes=['gk1'])
            ci = [0]

            def load_cast(dst3, src2d, ncols, key):
                c0 = 0
                while c0 < ncols:
                    w = min(256, ncols - c0)
                    i = ci[0] % 2; ci[0] += 1
                    st = stg[i]
                    ld(st[:, :, 0:w], src2d[:, c0:c0 + w].rearrange("(kc k) n -> k kc n", k=128), 'stg%d' % i)
                    eng = 'dve' if i == 0 else 'act'
                    if i == 0:
                        S.op('dve', lambda: V.tensor_copy(out=dst3[:, :, c0:c0 + w], in_=st[:, :, 0:w]), reads=['stg%d' % i], writes=[key])
                    else:
                        S.op('act', lambda: A.copy(out=dst3[:, :, c0:c0 + w], in_=st[:, :, 0:w]), reads=['stg%d' % i], writes=[key])
                    c0 += w
            load_cast(wpf, wpf_d, 512, 'wpf')
            load_cast(wpt, wpt_d, 524, 'wpt')
            for (wdst, wsrc, key) in ((wk1, wk1_d, 'wk1'), (wv1, wv1_d, 'wv1')):
                for p0 in range(0, 32, 8):
                    i = ci[0] % 2; ci[0] += 1
                    st = stg[i]
                    ld(st[0:64, :, :], wsrc[p0 * 64:(p0 + 8) * 64, :].rearrange("(pos d) h -> d pos h", d=64), 'stg%d' % i)
                    S.op('dve', lambda: V.tensor_copy(out=wdst[:, p0:p0 + 8, :], in_=st[0:64, :, :]), reads=['stg%d' % i], writes=[key])
            for (wdst, wsrc, key) in ((wk2, wk2_d, 'wk2'), (wv2, wv2_d, 'wv2')):
                i = ci[0] % 2; ci[0] += 1
                st = stg[i]
                ld(st[:, 0:2, 0:64], wsrc[:, :].rearrange("(hc k) d -> k hc d", k=128), 'stg%d' % i)
                S.op('dve', lambda: V.tensor_copy(out=wdst[:], in_=st[:, 0:2, 0:64]), reads=['stg%d' % i], writes=[key])
            peb = tb("peb", [64, 32], BF16)
            for (pe_d, w1, bias, key) in ((pekT_d, wk1, biask, 'biask'), (pevT_d, wv1, biasv, 'biasv')):
                ld(tmpf[0:64, 0:32], pe_d[:, :], 'tmpf')
                S.op('dve', lambda: V.tensor_copy(out=peb[:], in_=tmpf[0:64, 0:32]), reads=['tmpf'], writes=['peb'])
                for hc in range(2):
                    for pos in range(32):
                        S.op('pe', lambda: PE.matmul(psX[:, hc:hc + 1], lhsT=w1[:, pos, hc * 128:(hc + 1) * 128], rhs=peb[:, pos:pos + 1],
                                                     start=(pos == 0), stop=(pos == 31)), reads=['wk1', 'wv1', 'peb'], writes=['psX'],
                             pe_chain=(pos > 0))
                S.op('dve', lambda: V.tensor_copy(out=bias[:], in_=psX[:, 0:2]), reads=['psX'], writes=[key])
            S.barrier()

        xt = [sb("xt%d" % i, [128, D]) for i in range(2)]
        junk = sb("junk", [128, D], BF16)
        ss = sb("ss", [128, 1]); rs = sb("rs", [128, 1])
        hm = sb("hm", [128, D]); hmb = sb("hmb", [128, D], BF16)
        hmT4 = sb("hmT4", [128, 8, 512], BF16)
        qtm = sb("qtm", [128, 256]); qsq = sb("qsq", [128, 4, 4])
        ksq = sb("ksq", [128, 2]); km1 = sb("km1", [128, 1]); red = sb("red", [1, 1])
        gates = sb("gates", [128, 4, 12])
        cs2 = [sb("cos%d" % i, [64, 512]) for i in range(2)]
        sn2 = [sb("sin%d" % i, [64, 512]) for i in range(2)]
        cc2 = [sb("cc%d" % i, [64, 32]) for i in range(2)]
        sc2_ = [sb("sc%d" % i, [64, 32]) for i in range(2)]
        xq = sb("xq", [64, 512]); t1 = sb("t1", [64, 512]); t2 = sb("t2", [64, 512])
        qTa = sb("qTa", [65, 4, 512], BF16)
        hid = sb("hid", [128, 2, 32]); hga = sb("hga", [128, 2, 32]); ghk = sb("ghk", [128, 2, 32], BF16); ghv = sb("ghv", [128, 2, 32], BF16); ghv2 = sb("ghv2", [128, 2, 64], BF16)
        kcx = sb("kcx", [64, 32]); kt1 = sb("kt1", [64, 32]); kt2 = sb("kt2", [64, 32])
        cq = sb("cq", [128, 4]); negc = sb("negc", [128, 4], BF16)
        eT = [sb("eT%d" % i, [128, 512], BF16) for i in range(2)]
        pT = [sb("pT%d" % i, [128, 512], BF16) for i in range(2)]
        mfull = sb("mfull", [128, 512], BF16); maskT4 = sb("maskT4", [128, 4, 128], BF16)
        pc = sb("pc", [128, 1024]); pcb = sb("pcb", [128, 1024], BF16); pcT = sb("pcT", [128, 8, 128], BF16)
        pgrp = sb("pgrp", [128, 1032])
        mx = sb("mx", [128, 1]); mx2 = sb("mx2", [128, 2]); sm = sb("sm", [128, 4]); rinv = sb("rinv", [128, 4])
        imp = sb("imp", [128, 256]); impw = sb("impw", [128, 256]); impk = sb("impk", [128, 256]); sel = sb("sel", [128, 256])
        m8a = sb("m8a", [128, 8]); m8b = sb("m8b", [128, 8]); tau = sb("tau", [128, 1])
        oTs = sb("oTs", [65, 512]); oTw = sb("oTw", [65, 512])
        fac = sb("fac", [128, 3, 4]); ot = sb("ot", [128, 4, 64])
        sh1 = modrep[:, 0:1024]
        S.op('dve', lambda: V.memset(impw[:], -1.0), writes=['impw'])
        S.op('dve', lambda: V.memset(pgrp[:], 0.0), writes=['pgrp'])
        S.op('dve', lambda: V.memset(sel[:], 0.0), writes=['sel'])
        S.op('dve', lambda: V.memset(ghv2[:], 0.0), writes=['ghv2'])

        def gelu_tanh(dst, src, tmp, kd, ks_, kt):
            S.op('dve', lambda: V.tensor_tensor(out=tmp, in0=src, in1=src, op=ALU.mult), reads=[ks_], writes=[kt])
            S.op('dve', lambda: V.tensor_scalar(out=tmp, in0=tmp, scalar1=0.044715, scalar2=1.0, op0=ALU.mult, op1=ALU.add), reads=[kt], writes=[kt])
            S.op('dve', lambda: V.tensor_tensor(out=tmp, in0=tmp, in1=src, op=ALU.mult), reads=[kt, ks_], writes=[kt])
            S.op('act', lambda: A.activation(out=tmp, in_=tmp, func=AF.Sigmoid, scale=1.5957691216), reads=[kt], writes=[kt])
            S.op('dve', lambda: V.tensor_tensor(out=dst, in0=tmp, in1=src, op=ALU.mult), reads=[kt, ks_], writes=[kd])

        def rope(dst, src_ps, kps, cosap, sinap, kcos, scale, kdst):
            n = src_ps.shape[-1]
            S.op('act', lambda: A.copy(out=xq[:, 0:n], in_=src_ps), reads=[kps], writes=['xq'])
            S.op('pe', lambda: PE.matmul(psS[1][0:64, 0:n], lhsT=prot[:], rhs=xq[:, 0:n], start=True, stop=True),
                 reads=['prot', 'xq'], writes=['psS1'])
            S.op('dve', lambda: V.scalar_tensor_tensor(out=t1[:, 0:n], in0=xq[:, 0:n], scalar=scale, in1=cosap, op0=ALU.mult, op1=ALU.mult),
                 reads=['xq', kcos], writes=['t1'])
            S.op('dve', lambda: V.scalar_tensor_tensor(out=t2[:, 0:n], in0=psS[1][0:64, 0:n], scalar=scale, in1=sinap, op0=ALU.mult, op1=ALU.mult),
                 reads=['psS1', kcos], writes=['t2'])
            a1 = t1[:, 0:n]; a2 = t2[:, 0:n]
            if len(dst.shape) == 3:
                a1 = a1.rearrange("p (a b) -> p a b", b=dst.shape[2]); a2 = a2.rearrange("p (a b) -> p a b", b=dst.shape[2])
            S.op('dve', lambda: V.tensor_tensor(out=dst, in0=a1, in1=a2, op=ALU.add), reads=['t1', 't2'], writes=[kdst])

        for s in range(NS):
            cb = cs2[s % 2]; snb = sn2[s % 2]; kcos = 'cos%d' % (s % 2)
            ld(cb[:], cos_d[:, s * 512:(s + 1) * 512], kcos)
            ld(snb[:], sin_d[:, s * 512:(s + 1) * 512], kcos)
            ccb = cc2[s % 2]; scb = sc2_[s % 2]; kcc = 'cc%d' % (s % 2)
            if 32 * s + 32 <= NM:
                ld(ccb[:], cosc_d[:, 32 * s:32 * s + 32], kcc)
                ld(scb[:], sinc_d[:, 32 * s:32 * s + 32], kcc)
            for i in range(4):
                ti = s * 4 + i
                xb = xt[ti % 2]; kx = 'xt%d' % (ti % 2)
                r0 = ti * 128
                ld(xb[:], x[r0:r0 + 128, :], kx)
                S.op('act', lambda: A.activation(out=junk[:], in_=xb[:], func=AF.Square, accum_out=ss[:]), reads=[kx], writes=['junk', 'ss'])
                S.op('act', lambda: A.activation(out=rs[:], in_=ss[:], func=AF.Sqrt, bias=epsT[:], scale=1.0 / D), reads=['ss', 'epsT'], writes=['rs'])
                S.op('dve', lambda: V.reciprocal(out=rs[:], in_=rs[:]), reads=['rs'], writes=['rs'])
                S.op('dve', lambda: V.scalar_tensor_tensor(out=hm[:], in0=xb[:], scalar=rs[:], in1=gk1[:], op0=ALU.mult, op1=ALU.mult),
                     reads=[kx, 'rs', 'gk1'], writes=['hm'])
                S.op('dve', lambda: V.tensor_tensor(out=hmb[:], in0=hm[:], in1=sh1, op=ALU.add), reads=['hm', 'modrep'], writes=['hmb'])
                for kc in range(8):
                    S.op('pe', lambda: PE.transpose(out=psT[:, kc * 128:(kc + 1) * 128], in_=hmb[:, kc * 128:(kc + 1) * 128], identity=identb[:]),
                         reads=['hmb', 'identb'], writes=['psT'], pe_chain=(kc > 0))
                S.op('act', lambda: A.copy(out=hmT4[:, :, i * 128:(i + 1) * 128], in_=psT[:].rearrange("p (a b) -> p a b", b=128)),
                     reads=['psT'], writes=['hmT4'])
                for kc in range(8):
                    S.op('pe', lambda: PE.matmul(psS[0][:, 0:384], lhsT=hmT4[:, kc, i * 128:(i + 1) * 128], rhs=wpt[:, kc, 0:384],
                                                 start=(kc == 0), stop=(kc == 7)), reads=['hmT4', 'wpt'], writes=['psS0'], pe_chain=(kc > 0))
                for kc in range(8):
                    S.op('pe', lambda: PE.matmul(psX[:, 0:140], lhsT=hmT4[:, kc, i * 128:(i + 1) * 128], rhs=wpt[:, kc, 384:524],
                                                 start=(kc == 0), stop=(kc == 7)), reads=['hmT4', 'wpt'], writes=['psX'], pe_chain=(kc > 0))
                S.op('act', lambda: A.copy(out=qtm[:], in_=psS[0][:, 0:256]), reads=['psS0'], writes=['qtm'])
                S.op('act', lambda: A.activation(out=junk[:, 0:64], in_=psS[0][:, 256:320], func=AF.Square, accum_out=ksq[:, 0:1]),
                     reads=['psS0'], writes=['junk', 'ksq'])
                S.op('act', lambda: A.activation(out=junk[:, 0:64], in_=psS[0][:, 320:384], func=AF.Square, accum_out=ksq[:, 1:2]),
                     reads=['psS0'], writes=['junk', 'ksq'])
                S.op('dve', lambda: V.tensor_tensor(out=qtm[:], in0=qtm[:], in1=qtm[:], op=ALU.mult), reads=['qtm'], writes=['qtm'])
                S.op('dve', lambda: V.tensor_reduce(out=qsq[:, i, :], in_=qtm[:].rearrange("p (r d) -> p r d", d=64), axis=AX.X, op=ALU.add),
                     reads=['qtm'], writes=['qsq'])
                S.op('dve', lambda: V.tensor_tensor(out=km1[:], in0=ksq[:, 0:1], in1=ksq[:, 1:2], op=ALU.max), reads=['ksq'], writes=['km1'])
                S.op('pe', lambda: PE.transpose(out=psX[0:1, 256:384], in_=km1[:], identity=identf[:]), reads=['km1', 'identf'], writes=['psXk'])
                S.op('dve', lambda: V.tensor_reduce(out=red[:], in_=psX[0:1, 256:384], axis=AX.X, op=ALU.max), reads=['psXk'], writes=['red'])
                S.op('dve', lambda: V.tensor_tensor(out=kmx[:], in0=kmx[:], in1=red[:], op=ALU.max), reads=['kmx', 'red'], writes=['kmx'])
                S.op('pe', lambda: PE.matmul(psX[:, 384:385], lhsT=ones1[:], rhs=kmx[:], start=True, stop=True), reads=['ones1', 'kmx'], writes=['psXk'])
                S.op('dve', lambda: V.tensor_copy(out=kmax2[:], in_=psX[:, 384:385]), reads=['psXk'], writes=['kmax2'])
                S.op('act', lambda: A.copy(out=vsA[:, ti, 0:64], in_=psX[:, 0:64]), reads=['psX'], writes=['vsA'])
                S.op('act', lambda: A.copy(out=vwA[:, ti % 8, 0:64], in_=psX[:, 64:128]), reads=['psX'], writes=['vwA'])
                S.op('act', lambda: A.activation(out=gates[:, i, :], in_=psX[:, 128:140], func=AF.Sigmoid), reads=['psX'], writes=['gates'])
            for blk in range(8):
                pso = psC[0:64, (blk % 2) * 512:(blk % 2) * 512 + 512]
                kps = 'psC'
                for kc in range(8):
                    S.op('pe', lambda: PE.matmul(pso, lhsT=wpf[:, kc, blk * 64:(blk + 1) * 64], rhs=hmT4[:, kc, :], start=(kc == 0), stop=(kc == 7)),
                         reads=['wpf', 'hmT4'], writes=[kps], pe_chain=(kc > 0))
                if blk < 4:
                    dst = qTa[0:64, :, blk * 128:(blk + 1) * 128]
                    rope(dst, pso, kps, cb[:], snb[:], kcos, 0.125, 'qTa')
                elif blk == 4:
                    S.op('act', lambda: A.copy(out=kcbuf[:, 16:528], in_=pso), reads=[kps], writes=['kcbuf'])
                elif blk == 5:
                    S.op('act', lambda: A.copy(out=vcbuf[:, 16:528], in_=pso), reads=[kps], writes=['vcbuf'])
                elif blk == 6:
                    rope(ksT[0:64, s * 512:(s + 1) * 512], pso, kps, cb[:], snb[:], kcos, 1.0, 'ksT')
                else:
                    rope(kwT[0:64, (s % 2) * 512:(s % 2) * 512 + 512], pso, kps, cb[:], snb[:], kcos, 1.0, 'kwT')
            m0 = 32 * s
            for (buf, kbuf, w1, kw1, bias, gh, kgh) in ((kcbuf, 'kcbuf', wk1, 'wk1', biask, ghk, 'ghk'), (vcbuf, 'vcbuf', wv1, 'wv1', biasv, ghv, 'ghv')):
                bview = buf[:, 0:512].rearrange("d (n f) -> d n f", f=16)
                for hc in range(2):
                    for pos in range(32):
                        if pos < 16:
                            rhs = bview[:, :, pos]
                        else:
                            rhs = buf[:, 16:528].rearrange("d (n f) -> d n f", f=16)[:, :, pos - 16]
                        S.op('pe', lambda: PE.matmul(psX[:, hc * 32:(hc + 1) * 32], lhsT=w1[:, pos, hc * 128:(hc + 1) * 128], rhs=rhs,
                                                     start=(pos == 0), stop=(pos == 31)), reads=[kw1, kbuf], writes=['psX'], pe_chain=(pos > 0))
                for hc in range(2):
                    S.op('dve', lambda: V.tensor_scalar(out=hid[:, hc, :], in0=psX[:, hc * 32:(hc + 1) * 32], scalar1=bias[:, hc:hc + 1], scalar2=None,
                                                        op0=ALU.add), reads=['psX', 'biask', 'biasv'], writes=['hid'])
                gelu_tanh(gh[:].rearrange("p a b -> p (a b)"), hid[:].rearrange("p a b -> p (a b)"), hga[:].rearrange("p a b -> p (a b)"),
                          kgh, 'hid', 'hga')
                S.op('dve', lambda: V.tensor_copy(out=buf[:, 0:16], in_=buf[:, 512:528]), reads=[kbuf], writes=[kbuf])
            for hc in range(2):
                S.op('pe', lambda: PE.matmul(psC[0:64, 0:32], lhsT=wk2[:, hc, :], rhs=ghk[:, hc, :], start=(hc == 0), stop=(hc == 1)),
                     reads=['wk2', 'ghk'], writes=['psC'], pe_chain=(hc > 0))
            if m0 + 32 <= NM:
                rope(kcmpT[:, m0:m0 + 32], psC[0:64, 0:32], 'psC', ccb[:], scb[:], kcc, 1.0, 'kcmpT')
                mt, mo = m0 // 128, m0 % 128
                S.op('dve', lambda: V.tensor_copy(out=ghv2[:, :, 32:64], in_=ghv[:]), reads=['ghv'], writes=['ghv2'])
                for hc in range(2):
                    if mo < 96:
                        S.op('pe', lambda: PE.matmul(psC[mo:mo + 32, 512:576], lhsT=ghv2[:, hc, 32:64], rhs=wv2[:, hc, :], start=(hc == 0), stop=(hc == 1)),
                             reads=['wv2', 'ghv2'], writes=['psC'], pe_chain=(hc > 0))
                    else:
                        S.op('pe', lambda: PE.matmul(psC[64:128, 512:576], lhsT=ghv2[:, hc, 0:64], rhs=wv2[:, hc, :], start=(hc == 0), stop=(hc == 1)),
                             reads=['wv2', 'ghv2'], writes=['psC'], pe_chain=(hc > 0))
                S.op('act', lambda: A.copy(out=vcmp[mo:mo + 32, mt, :], in_=psC[mo:mo + 32, 512:576]), reads=['psC'], writes=['vcmp'])

            for i in range(4):
                qi = s * 4 + i
                qblk = qTa[:, i, :]
                import os
                if qi < int(os.environ.get('NSA_QIMIN', '0')):
                    continue
                S.op('dve', lambda: V.tensor_scalar(out=cq[:], in0=qsq[:, i, :], scalar1=kmax2[:, 0:1], scalar2=1.0 / 64, op0=ALU.mult, op1=ALU.mult),
                     reads=['qsq', 'kmax2'], writes=['cq'])
                S.op('act', lambda: A.activation(out=cq[:], in_=cq[:], func=AF.Sqrt), reads=['cq'], writes=['cq'])
                S.op('dve', lambda: V.tensor_scalar(out=negc[:], in0=cq[:], scalar1=-1.0, scalar2=None, op0=ALU.mult), reads=['cq'], writes=['negc'])
                for r in range(4):
                    S.op('pe', lambda: PE.transpose(out=psT[64:65, r * 128:(r + 1) * 128], in_=negc[:, r:r + 1], identity=identb[:]),
                         reads=['negc', 'identb'], writes=['psT'], pe_chain=(r > 0))
                S.op('act', lambda: A.copy(out=qTa[64:65, i, :], in_=psT[64:65, 0:512]), reads=['psT'], writes=['qTa'])

                W = 8 * qi + 8
                import os
                if os.environ.get('NSA_CAPW'):
                    W = min(W, int(os.environ['NSA_CAPW']))
                nt = (W + 127) // 128
                for r in range(4):
                    for c0 in range(0, W, 512):
                        w = min(512, W - c0)
                        S.op('pe', lambda: PE.matmul(psC[:, c0:c0 + w], lhsT=qTa[0:64, i, r * 128:(r + 1) * 128], rhs=kcmpT[:, c0:c0 + w],
                                                     start=True, stop=True), reads=['qTa', 'kcmpT'], writes=['psC'])
                    chunks = [(c0, min(512, W - c0)) for c0 in range(0, W, 512)]
                    for ci_, (c0, w) in enumerate(chunks):
                        S.op('dve', lambda: V.tensor_reduce(out=mx2[:, ci_:ci_ + 1], in_=psC[:, c0:c0 + w], axis=AX.X, op=ALU.max), reads=['psC'], writes=['mx2'])
                    if len(chunks) == 2:
                        S.op('dve', lambda: V.tensor_tensor(out=mx2[:, 0:1], in0=mx2[:, 0:1], in1=mx2[:, 1:2], op=ALU.max), reads=['mx2'], writes=['mx2'])
                    S.op('dve', lambda: V.tensor_scalar(out=mx[:], in0=mx2[:, 0:1], scalar1=-1.0, scalar2=None, op0=ALU.mult), reads=['mx2'], writes=['mx'])
                    for (c0, w) in chunks:
                        S.op('act', lambda: A.activation(out=pc[:, c0:c0 + w], in_=psC[:, c0:c0 + w], func=AF.Exp, bias=mx[:], scale=1.0),
                             reads=['psC', 'mx'], writes=['pc'])
                    if qi == 0:
                        S.op('dve', lambda: V.tensor_tensor(out=pc[:, 0:8], in0=pc[:, 0:8], in1=cmask0[:], op=ALU.mult), reads=['pc', 'cmask0'], writes=['pc'])
                    else:
                        S.op('dve', lambda: V.tensor_tensor(out=pc[:, W - 16:W], in0=pc[:, W - 16:W], in1=cmaskG[:], op=ALU.mult),
                             reads=['pc', 'cmaskG'], writes=['pc'])
                        S.op('dve', lambda: V.memset(pc[:, 0:1], 0.0), reads=[], writes=['pc'])
                    S.op('dve', lambda: V.tensor_reduce(out=sm[:, r:r + 1], in_=pc[:, 0:W], axis=AX.X, op=ALU.add), reads=['pc'], writes=['sm'])
                    S.op('dve', lambda: V.tensor_scalar(out=rinv[:, r:r + 1], in0=sm[:, r:r + 1], scalar1=1e-30, scalar2=None, op0=ALU.add),
                         reads=['sm'], writes=['rinv'])
                    S.op('dve', lambda: V.reciprocal(out=rinv[:, r:r + 1], in_=rinv[:, r:r + 1]), reads=['rinv'], writes=['rinv'])
                    if r == 0:
                        S.op('dve', lambda: V.tensor_scalar(out=pgrp[:, 0:W], in0=pc[:, 0:W], scalar1=rinv[:, 0:1], scalar2=None, op0=ALU.mult),
                             reads=['pc', 'rinv'], writes=['pgrp'])
                    else:
                        S.op('dve', lambda: V.scalar_tensor_tensor(out=pgrp[:, 0:W], in0=pc[:, 0:W], scalar=rinv[:, r:r + 1], in1=pgrp[:, 0:W],
                                                                   op0=ALU.mult, op1=ALU.add), reads=['pc', 'rinv', 'pgrp'], writes=['pgrp'])
                    S.op('act', lambda: A.copy(out=pcb[:, 0:W], in_=pc[:, 0:W]), reads=['pc'], writes=['pcb'])
                    for j in range(nt):
                        wj = min(128, W - j * 128)
                        S.op('pe', lambda: PE.transpose(out=psT[0:wj, j * 128:(j + 1) * 128], in_=pcb[:, j * 128:j * 128 + wj], identity=identb[:]),
                             reads=['pcb', 'identb'], writes=['psT'], pe_chain=(j > 0))
                    for j in range(nt):
                        wj = min(128, W - j * 128)
                        S.op('act', lambda: A.copy(out=pcT[0:wj, j, :], in_=psT[0:wj, j * 128:(j + 1) * 128]), reads=['psT'], writes=['pcT'])
                    for j in range(nt):
                        wj = min(128, W - j * 128)
                        S.op('pe', lambda: PE.matmul(psX[:, r * 64:(r + 1) * 64], lhsT=pcT[0:wj, j, :], rhs=vcmp[0:wj, j, :],
                                                     start=(j == 0), stop=(j == nt - 1)), reads=['pcT', 'vcmp'], writes=['psX'], pe_chain=(j > 0))
                if qi >= 1:
                    Jn = 2 * qi
                    S.op('dve', lambda: V.tensor_reduce(out=imp[:, 0:Jn], in_=pgrp[:, 0:4 * Jn].rearrange("p (j f) -> p j f", f=4), axis=AX.X, op=ALU.add),
                         reads=['pgrp'], writes=['imp'])
                    S.op('dve', lambda: V.tensor_tensor(out=imp[:, 0:Jn], in0=imp[:, 0:Jn],
                                                        in1=pgrp[:, 4:4 + 4 * Jn].rearrange("p (j f) -> p j f", f=4)[:, :, 0], op=ALU.add),
                         reads=['imp', 'pgrp'], writes=['imp'])
                    if Jn - 1 > 1:
                        S.op('dve', lambda: V.tensor_copy(out=impw[:, 1:Jn - 1], in_=imp[:, 1:Jn - 1]), reads=['imp'], writes=['impw'])
                    S.op('dve', lambda: V.tensor_scalar(out=impw[:, Jn - 1:Jn], in0=imp[:, Jn - 1:Jn], scalar1=hilo[:, 0:1], scalar2=hilo[:, 1:2],
                                                        op0=ALU.mult, op1=ALU.add), reads=['imp', 'hilo'], writes=['impw'])
                    Wj = max(Jn, 16)
                    S.op('dve', lambda: V.max(out=m8a[:], in_=impw[:, 0:Wj]), reads=['impw'], writes=['m8a'])
                    S.op('dve', lambda: V.match_replace(out=impk[:, 0:Wj], in_to_replace=m8a[:], in_values=impw[:, 0:Wj], imm_value=-1e30),
                         reads=['impw', 'm8a'], writes=['impk'])
                    S.op('dve', lambda: V.max(out=m8b[:], in_=impk[:, 0:Wj]), reads=['impk'], writes=['m8b'])
                    S.op('dve', lambda: V.tensor_scalar(out=tau[:], in0=m8b[:, 4:5], scalar1=-0.5, scalar2=None, op0=ALU.max), reads=['m8b'], writes=['tau'])
                    S.op('dve', lambda: V.tensor_scalar(out=sel[:, 0:Jn], in0=impw[:, 0:Jn], scalar1=tau[:, 0:1], scalar2=None, op0=ALU.is_ge),
                         reads=['impw', 'tau'], writes=['sel'])
                    S.op('dve', lambda: V.memset(sel[:, 0:1], 1.0), reads=[], writes=['sel'])
                    S.op('dve', lambda: V.tensor_tensor(out=sel[:, Jn - 1:Jn], in0=sel[:, Jn - 1:Jn], in1=hilo[:, 2:3], op=ALU.max),
                         reads=['sel', 'hilo'], writes=['sel'])
                cnt = [0]
                for kt0 in range(0, qi + 1, 4):
                    kts = list(range(kt0, min(kt0 + 4, qi + 1)))
                    nsel = [k for k in kts if k < qi]
                    if nsel:
                        nb = len(nsel)
                        S.op('dve', lambda: V.tensor_copy(out=mfull[:, 0:nb * 128].rearrange("p (j f) -> p j f", f=64),
                                                          in_=sel[:, 2 * kt0:2 * kt0 + 2 * nb].unsqueeze(2).to_broadcast([128, 2 * nb, 64])),
                             reads=['sel'], writes=['mfull'])
                        for k in range(nb):
                            S.op('pe', lambda: PE.transpose(out=psT[:, k * 128:(k + 1) * 128], in_=mfull[:, k * 128:(k + 1) * 128], identity=identb[:]),
                                 reads=['mfull', 'identb'], writes=['psT'], pe_chain=(k > 0))
                        S.op('act', lambda: A.copy(out=maskT4[:, 0:nb, :].rearrange("p a b -> p (a b)"), in_=psT[:, 0:nb * 128]),
                             reads=['psT'], writes=['maskT4'])
                    for k, kt in enumerate(kts):
                        b2 = cnt[0] % 2; cnt[0] += 1
                        S.op('pe', lambda: PE.matmul(psS[b2][:], lhsT=ksT[:, kt * 128:(kt + 1) * 128], rhs=qblk, start=True, stop=True),
                             reads=['ksT', 'qTa'], writes=['psS%d' % b2])
                        S.op('act', lambda: A.activation(out=eT[b2][:], in_=psS[b2][:], func=AF.Exp), reads=['psS%d' % b2], writes=['eT%d' % b2])
                        msk = triT[:] if kt == qi else maskT4[:, k, :]
                        S.op('dve', lambda: V.tensor_tensor(out=pT[b2][:].rearrange("p (r q) -> p r q", q=128),
                                                            in0=eT[b2][:].rearrange("p (r q) -> p r q", q=128),
                                                            in1=msk.unsqueeze(1).to_broadcast([128, 4, 128]), op=ALU.mult),
                             reads=['eT%d' % b2, 'maskT4', 'triT'], writes=['pT%d' % b2])
                        S.op('pe', lambda: PE.matmul(psOs[0:65, :], lhsT=vsA[:, kt, :], rhs=pT[b2][:], start=(kt == 0), stop=(kt == qi)),
                             reads=['vsA', 'pT%d' % b2], writes=['psOs'], pe_chain=(kt > 0))
                wts = [k for k in range(qi - 4, qi + 1) if k >= 0]
                for kt in wts:
                    b2 = cnt[0] % 2; cnt[0] += 1
                    S.op('pe', lambda: PE.matmul(psS[b2][:], lhsT=kwT[:, (kt % 8) * 128:(kt % 8) * 128 + 128], rhs=qblk, start=True, stop=True),
                         reads=['kwT', 'qTa'], writes=['psS%d' % b2])
                    dlt = qi - kt
                    if dlt in (0, 4):
                        S.op('act', lambda: A.activation(out=eT[b2][:], in_=psS[b2][:], func=AF.Exp), reads=['psS%d' % b2], writes=['eT%d' % b2])
                        msk = triT[:] if dlt == 0 else triTs[:]
                        S.op('dve', lambda: V.tensor_tensor(out=pT[b2][:].rearrange("p (r q) -> p r q", q=128),
                                                            in0=eT[b2][:].rearrange("p (r q) -> p r q", q=128),
                                                            in1=msk.unsqueeze(1).to_broadcast([128, 4, 128]), op=ALU.mult),
                             reads=['eT%d' % b2, 'triT', 'triTs'], writes=['pT%d' % b2])
                    else:
                        S.op('act', lambda: A.activation(out=pT[b2][:], in_=psS[b2][:], func=AF.Exp), reads=['psS%d' % b2], writes=['pT%d' % b2])
                    S.op('pe', lambda: PE.matmul(psOw[0:65, :], lhsT=vwA[:, kt % 8, :], rhs=pT[b2][:], start=(kt == wts[0]), stop=(kt == qi)),
                         reads=['vwA', 'pT%d' % b2], writes=['psOw'], pe_chain=(kt > wts[0]))
                S.op('act', lambda: A.copy(out=oTs[:], in_=psOs[0:65, :]), reads=['psOs'], writes=['oTs'])
                S.op('act', lambda: A.copy(out=oTw[:], in_=psOw[0:65, :]), reads=['psOw'], writes=['oTw'])
                for r in range(4):
                    S.op('pe', lambda: PE.transpose(out=psC[:, r * 65:(r + 1) * 65], in_=oTs[:, r * 128:(r + 1) * 128], identity=identf[0:65, 0:65]),
                         reads=['oTs', 'identf'], writes=['psC'], pe_chain=(r > 0))
                for r in range(4):
                    S.op('pe', lambda: PE.transpose(out=psC[:, 512 + r * 65:512 + (r + 1) * 65], in_=oTw[:, r * 128:(r + 1) * 128], identity=identf[0:65, 0:65]),
                         reads=['oTw', 'identf'], writes=['psC'], pe_chain=True)
                g3 = gates[:, i, :].rearrange("p (r k) -> p r k", k=3)
                osum = psC[:, 0:260].rearrange("p (r e) -> p r e", e=65)
                wsum = psC[:, 512:772].rearrange("p (r e) -> p r e", e=65)
                S.op('dve', lambda: V.tensor_tensor(out=fac[:, 0, :], in0=g3[:, :, 0], in1=rinv[:], op=ALU.mult), reads=['gates', 'rinv'], writes=['fac'])
                S.op('dve', lambda: V.reciprocal(out=fac[:, 1, :], in_=osum[:, :, 64]), reads=['psC'], writes=['fac'])
                S.op('dve', lambda: V.reciprocal(out=fac[:, 2, :], in_=wsum[:, :, 64]), reads=['psC'], writes=['fac'])
                S.op('dve', lambda: V.tensor_tensor(out=fac[:, 1, :], in0=fac[:, 1, :], in1=g3[:, :, 1], op=ALU.mult), reads=['fac', 'gates'], writes=['fac'])
                S.op('dve', lambda: V.tensor_tensor(out=fac[:, 2, :], in0=fac[:, 2, :], in1=g3[:, :, 2], op=ALU.mult), reads=['fac', 'gates'], writes=['fac'])
                for r in range(4):
                    S.op('dve', lambda: V.tensor_scalar(out=ot[:, r, :], in0=psX[:, r * 64:(r + 1) * 64], scalar1=fac[:, 0, r:r + 1], scalar2=None, op0=ALU.mult),
                         reads=['psX', 'fac'], writes=['ot'])
                    S.op('dve', lambda: V.scalar_tensor_tensor(out=ot[:, r, :], in0=osum[:, r, 0:64], scalar=fac[:, 1, r:r + 1], in1=ot[:, r, :],
                                                               op0=ALU.mult, op1=ALU.add), reads=['psC', 'fac', 'ot'], writes=['ot'])
                    S.op('dve', lambda: V.scalar_tensor_tensor(out=ot[:, r, :], in0=wsum[:, r, 0:64], scalar=fac[:, 2, r:r + 1], in1=ot[:, r, :],
                                                               op0=ALU.mult, op1=ALU.add), reads=['psC', 'fac', 'ot'], writes=['ot'])
                S.dma('sp', lambda: nc.sync.dma_start(out=out[qi * 128:(qi + 1) * 128, :], in_=ot[:].rearrange("p r d -> p (r d)")),
                      reads=['ot'], writes=['out'])
        S.finish(['out'])
        print("nsa program: instr", S.n_instr, "waits", S.n_wait)
    return nc


def nsa_consts(T):
    NM = 1024 if T >= 16384 else (T // 16 + 32)
    half = 32
    freqs = (10000.0 ** (-np.arange(half, dtype=np.float32) / half)).astype(np.float32)
    pos = np.arange(T, dtype=np.float32)
    ang = pos[None, :] * freqs[:, None]
    cos2 = np.concatenate([np.cos(ang), np.cos(ang)], 0).astype(np.float32)
    sin2 = np.concatenate([np.sin(ang), np.sin(ang)], 0).astype(np.float32)
    m = np.arange(NM, dtype=np.float32)
    cend = (m - 1) * 16 + 31
    angc = cend[None, :] * freqs[:, None]
    cosc = np.concatenate([np.cos(angc), np.cos(angc)], 0).astype(np.float32)
    sinc = np.concatenate([np.sin(angc), np.sin(angc)], 0).astype(np.float32)
    prot = np.zeros((64, 64), np.float32)
    for d in range(32):
        prot[d + 32, d] = -1.0
        prot[d, d + 32] = 1.0
    l = np.arange(128)
    triT = (l[:, None] <= l[None, :]).astype(np.float32)
    triTs = (l[:, None] > l[None, :]).astype(np.float32)
    fl = np.floor((l - 15) / 16.0)
    j = np.arange(16)
    cmaskG = ((j[None, :] - 8) <= fl[:, None]).astype(np.float32)
    j8 = np.arange(8)
    cmask0 = ((j8[None, :] >= 1) & (j8[None, :] <= fl[:, None])).astype(np.float32)
    hi = (l >= 64).astype(np.float32)
    hilo = np.stack([hi, hi - 1.0, 1.0 - hi], 1).astype(np.float32)
    return {"cos2": cos2, "sin2": sin2, "cosc": cosc, "sinc": sinc, "prot": prot, "identf": np.eye(128, dtype=np.float32),
            "triT": triT, "triTs": triTs, "cmaskG": cmaskG, "cmask0": cmask0, "hilo": hilo}


def nsa_weights(w_proj, g):
    q = w_proj[:, 256 * g:256 * g + 256]
    def blk(i):
        return w_proj[:, 1024 + 256 * i + 64 * g:1024 + 256 * i + 64 * g + 64]
    kc, vc, ks, vs, kw, vw = [blk(i) for i in range(6)]
    gl = w_proj[:, 2560 + 12 * g:2560 + 12 * g + 12]
    wpf = np.ascontiguousarray(np.concatenate([q, kc, vc, ks, kw], 1))
    wpt = np.ascontiguousarray(np.concatenate([q, ks, kw, vs, vw, gl], 1))
    return wpf, wpt

from concourse.bass_utils import run_bass_kernel_spmd

N_CORES = 8
SEQ = 16384


def _c_l(c, b):
    return np.ascontiguousarray(np.asarray(c[b], np.float32).reshape(8, 128).T)


def kernel(x, c, norm1_g, norm2_g, ada_w, ada_b, s5_w_in, s5_a_re, s5_a_im, s5_log_dt,
           s5_b_re, s5_b_im, s5_c_re, s5_c_im, s5_d, s5_w_glu, nsa_w_proj, nsa_pe_k, nsa_pe_v,
           nsa_wk1, nsa_wk2, nsa_wv1, nsa_wv2, nsa_w_o, peer_w_q, peer_sub_keys, peer_u, peer_v,
           final_g):
    f = lambda a: np.ascontiguousarray(np.asarray(a, np.float32))
    x = f(x); c = f(c); ada_w = f(ada_w); ada_b = f(ada_b)
    norm1_g = f(norm1_g); norm2_g = f(norm2_g); final_g = f(final_g)
    peer_w_q = f(peer_w_q); peer_sub_keys = f(peer_sub_keys); peer_u = f(peer_u); peer_v = f(peer_v)
    cores = list(range(N_CORES))
    nc1 = build_s5(SEQ // 128)
    s5c = s5_consts()
    maps = []
    for k in cores:
        b, cq = k // 4, k % 4
        m = {"x": x[b], "c_l": _c_l(c, b), "adaw": f(ada_w[0][:, :2048]), "adab": f(ada_b[0][None, :2048]),
             "n1g": f(norm1_g[0][None]), "win": f(np.asarray(s5_w_in[0])[:, cq * 256:(cq + 1) * 256])}
        m.update(s5c)
        m.update(s5_layouts(f(s5_a_re[0]), f(s5_a_im[0]), f(s5_log_dt[0]), f(s5_b_re[0]), f(s5_b_im[0]),
                            f(s5_c_re[0]), f(s5_c_im[0]), f(s5_d[0]), cq))
        maps.append(m)
    r1 = run_bass_kernel_spmd(nc1, maps, core_ids=cores)
    yactT = [r1.results[k]["out"] for k in cores]
    del maps

    def tok_launch(li, mode, final, xin, mixT_of, wmix):
        nct = build_tok(SEQ // 4 // 128, mode, final)
        tc = tok_consts()
        maps = []
        for k in cores:
            b, q4 = k // 4, k % 4
            sl = slice(q4 * 4096, (q4 + 1) * 4096)
            m = {"x": f(xin[b, sl]), "mixT": mixT_of(b, sl), "c_l": _c_l(c, b),
                 "adaw": f(ada_w[li][:, 2048:]), "adab": f(ada_b[li][None, 2048:]),
                 "n2g": f(norm2_g[li][None]), "fing": f(final_g[None]), "wmix": wmix, "wq": peer_w_q[li],
                 "sk": f(peer_sub_keys[li].reshape(16, 128, 128)), "u_tab": peer_u[li], "v_tab": peer_v[li]}
            m.update(tc)
            maps.append(m)
        r = run_bass_kernel_spmd(nct, maps, core_ids=cores)
        xo = np.empty((2, SEQ, 1024), np.float32)
        for k in cores:
            b, q4 = k // 4, k % 4
            xo[b, q4 * 4096:(q4 + 1) * 4096] = r.results[k]["out"]
        return xo

    def mix0(b, sl):
        return f(np.concatenate([yactT[b * 4 + cq][:, sl] for cq in range(4)], axis=0))
    x2 = tok_launch(0, 'glu', False, x, mix0, f(s5_w_glu[0]))
    del yactT
    nc3 = build_nsa(SEQ // 512)
    nsc = nsa_consts(SEQ)
    maps = []
    for k in cores:
        b, g = k // 4, k % 4
        wpf, wpt = nsa_weights(f(nsa_w_proj[0]), g)
        m = {"x": x2[b], "c_l": _c_l(c, b), "adaw": f(ada_w[1][:, :2048]), "adab": f(ada_b[1][None, :2048]),
             "n1g": f(norm1_g[1][None]), "wpf": wpf, "wpt": wpt,
             "wk1": f(nsa_wk1[0]), "wv1": f(nsa_wv1[0]), "wk2": f(nsa_wk2[0]), "wv2": f(nsa_wv2[0]),
             "pekT": f(np.asarray(nsa_pe_k[0]).T), "pevT": f(np.asarray(nsa_pe_v[0]).T)}
        m.update(nsc)
        maps.append(m)
    r3 = run_bass_kernel_spmd(nc3, maps, core_ids=cores)
    o = [r3.results[k]["out"] for k in cores]
    del maps

    def mix1(b, sl):
        return f(np.concatenate([o[b * 4 + g][sl].T for g in range(4)], axis=0))
    out = tok_launch(1, 'wo', True, x2, mix1, f(nsa_w_o[0]))
    return out
```

```python
from contextlib import ExitStack
import numpy as np
import concourse.bass as bass
import concourse.mybir as mybir

F32 = mybir.dt.float32
BF16 = mybir.dt.bfloat16
I32 = mybir.dt.int32
U32 = mybir.dt.uint32
AF = mybir.ActivationFunctionType
ALU = mybir.AluOpType
AX = mybir.AxisListType


class Sched:
    N_DMA_SEMS = 24
    SEM_MAX = 12000
    DMA_SEM_MAX = 12000

    def __init__(self, nc, es):
        self.nc = nc
        self.es = es
        self.engs = {'pe': nc.tensor, 'dve': nc.vector, 'act': nc.scalar,
                     'pool': nc.gpsimd, 'sp': nc.sync}
        self.dpool = [[es.enter_context(nc.semaphore("ds_%d_0" % i))] for i in range(self.N_DMA_SEMS)]
        self.csem = {}
        self.ccnt = {}
        self.cep = {}
        self.ctot = {}
        self.cpool = {}
        for k in ('pe', 'dve', 'act', 'pool'):
            n_ep = {'pe': 8, 'dve': 8, 'act': 6, 'pool': 3}[k]
            self.cpool[k] = [es.enter_context(nc.semaphore("cs_%s_%d" % (k, j))) for j in range(n_ep)]
            self.csem[k] = self.cpool[k][0]
            self.ccnt[k] = 0
            self.cep[k] = 0
            self.ctot[k] = 0
        self.dsem = [self.dpool[i][0] for i in range(self.N_DMA_SEMS)]
        self.dcnt = [0] * self.N_DMA_SEMS
        self.dep = [0] * self.N_DMA_SEMS
        for i in range(self.N_DMA_SEMS):
            self.dpool[i].append(es.enter_context(nc.semaphore("ds_%d_1" % i)))
        self.drr = 0
        self.seen = {k: {} for k in self.engs}
        self.lastw = {}
        self.readers = {}
        self.n_instr = 0
        self.n_wait = 0

    def _deps(self, reads, writes):
        deps = []
        for k in reads:
            w = self.lastw.get(k)
            if w is not None:
                deps.append(w)
        for k in writes:
            w = self.lastw.get(k)
            if w is not None:
                deps.append(w)
            deps.extend(self.readers.get(k, ()))
        return deps

    def _wait(self, e, deps, skip_self=None):
        eng = self.engs[e]
        best = {}
        for (sid, sem, val) in deps:
            if skip_self is not None and sid.startswith(skip_self):
                continue
            if best.get(sid, (None, 0))[1] < val:
                best[sid] = (sem, val)
        for sid, (sem, val) in best.items():
            if self.seen[e].get(sid, 0) < val:
                eng.wait_ge(sem, val)
                self.seen[e][sid] = val
                self.n_wait += 1

    def _record(self, ev, reads, writes):
        for k in reads:
            self.readers.setdefault(k, []).append(ev)
        for k in writes:
            self.lastw[k] = ev
            self.readers[k] = []

    def op(self, e, fn, reads=(), writes=(), pe_chain=False):
        deps = self._deps(reads, writes)
        self._wait(e, deps, skip_self=('c_pe_' if (pe_chain and e == 'pe') else None))
        ins = fn()
        if self.ccnt[e] >= self.SEM_MAX:
            self.cep[e] += 1
            self.csem[e] = self.cpool[e][self.cep[e]]
            self.ccnt[e] = 0
        self.ccnt[e] += 1
        self.ctot[e] += 1
        ins.then_inc(self.csem[e], 1)
        ev = ('c_%s_%d' % (e, self.cep[e]), self.csem[e], self.ccnt[e])
        self._record(ev, reads, writes)
        self.n_instr += 1
        return ev

    def dma(self, q, fn, reads=(), writes=()):
        deps = self._deps(reads, writes)
        self._wait(q, deps)
        i = self.drr
        self.drr = (self.drr + 1) % self.N_DMA_SEMS
        sid = 'd_%d_%d' % (i, self.dep[i])
        if self.seen[q].get(sid, 0) < self.dcnt[i]:
            self.engs[q].wait_ge(self.dsem[i], self.dcnt[i])
            self.seen[q][sid] = self.dcnt[i]
        if self.dcnt[i] >= self.DMA_SEM_MAX:
            self.dep[i] += 1
            self.dsem[i] = self.dpool[i][self.dep[i]]
            self.dcnt[i] = 0
            sid = 'd_%d_%d' % (i, self.dep[i])
        ins = fn()
        self.dcnt[i] += 16
        ins.then_inc(self.dsem[i], 16)
        ev = (sid, self.dsem[i], self.dcnt[i])
        self._record(ev, reads, writes)
        self.n_instr += 1
        return ev

    def finish(self, keys):
        deps = []
        for k in keys:
            w = self.lastw.get(k)
            if w is not None:
                deps.append(w)
        self._wait('sp', deps)
        alld = []
        for k in ('pe', 'dve', 'act', 'pool'):
            if self.ccnt[k]:
                alld.append(('c_%s_%d' % (k, self.cep[k]), self.csem[k], self.ccnt[k]))
        for i in range(self.N_DMA_SEMS):
            if self.dcnt[i]:
                alld.append(('d_%d_%d' % (i, self.dep[i]), self.dsem[i], self.dcnt[i]))
        self._wait('sp', alld)


def sched_barrier(S):
    alld = []
    for k in ('pe', 'dve', 'act', 'pool'):
        if S.ccnt[k]:
            alld.append(('c_%s_%d' % (k, S.cep[k]), S.csem[k], S.ccnt[k]))
    for i in range(S.N_DMA_SEMS):
        if S.dcnt[i]:
            alld.append(('d_%d_%d' % (i, S.dep[i]), S.dsem[i], S.dcnt[i]))
    for e in ('pe', 'dve', 'act', 'pool', 'sp'):
        S._wait(e, alld)


Sched.barrier = sched_barrier


RMS_EPS = 1e-6
IOA = bass.IndirectOffsetOnAxis


def build_tok(NT, mode, final, gelu_func=None, dbg=None):
    nc = bass.Bass("TRN2", target_bir_lowering=False)
    T = NT * 128
    MIXN = 2048 if mode == 'glu' else 1024
    D = 1024
    din = lambda n, s, d=F32: nc.dram_tensor(n, s, d, kind="ExternalInput").ap()
    x = din("x", [T, D])
    mixT = din("mixT", [D, T])
    c_l = din("c_l", [128, 8])
    adaw = din("adaw", [D, 4096])
    adab = din("adab", [1, 4096])
    n2g = din("n2g", [1, D])
    fing = din("fing", [1, D])
    wmix_d = din("wmix", [D, MIXN])
    wq_d = din("wq", [D, 2048])
    sk_d = din("sk", [16, 128, 128])
    u_tab = din("u_tab", [16384, D])
    v_tab = din("v_tab", [16384, D])
    identf_d = din("identf", [128, 128])
    iota16_d = din("iota16", [128, 16])
    out = nc.dram_tensor("out", [T, D], F32, kind="ExternalOutput").ap()
    u_bf = nc.dram_tensor("u_bf", [16384, D], BF16).ap()
    v_bf = nc.dram_tensor("v_bf", [16384, D], BF16).ap()

    es = ExitStack()
    with es:
        es.enter_context(nc.allow_low_precision("bf16 matmul operands"))
        es.enter_context(nc.allow_non_contiguous_dma("small layout loads"))
        S = Sched(nc, es)
        sb = lambda n, s, d=F32: es.enter_context(nc.sbuf_tensor("s_" + n, s, d))
        ps = lambda n, s, d=F32: es.enter_context(nc.psum_tensor("p_" + n, s, d))
        V, A, P, PE = nc.vector, nc.scalar, nc.gpsimd, nc.tensor

        identf = sb("identf", [128, 128])
        identb = sb("identb", [128, 128], BF16)
        iota16 = sb("iota16", [128, 16])
        epsT = sb("epsT", [128, 1])
        csil = sb("csil", [128, 8])
        csil_rep = sb("csil_rep", [128, 8, 128])
        modrep = sb("modrep", [128, 4096])
        gk2 = sb("gk2", [128, D])
        fing_rep = sb("fing_rep", [128, D])
        ot = sb("ot", [128, D])
        n2g_rep = ot
        wq = sb("wq", [128, 8, 2048], BF16)
        wmix = sb("wmix", [128, 8, MIXN], BF16)
        skT = sb("skT", [128, 16, 128], BF16)
        stg = [sb("stg%d" % i, [128, 8, 256]) for i in range(2)]
        adab_rep = sb("adab_rep", [128, 256])

        psA = ps("psA", [128, 2048])
        psB = ps("psB", [128, 1024])
        psT = ps("psT", [128, 1024], BF16)
        psM = ps("psM", [128, 512])

        S.dma('sp', lambda: nc.sync.dma_start(out=identf[:], in_=identf_d[:, :]), writes=['identf'])
        S.dma('sp', lambda: nc.sync.dma_start(out=iota16[:], in_=iota16_d[:, :]), writes=['iota4'])
        S.dma('sp', lambda: nc.sync.dma_start(out=csil[:], in_=c_l[:, :]), writes=['csil'])
        S.dma('sp', lambda: nc.sync.dma_start(out=n2g_rep[:], in_=n2g[0:1, :].partition_broadcast(128)), writes=['ot'])
        if final:
            S.dma('sp', lambda: nc.sync.dma_start(out=fing_rep[:], in_=fing[0:1, :].partition_broadcast(128)), writes=['fing_rep'])
        S.op('dve', lambda: V.tensor_copy(out=identb[:], in_=identf[:]), reads=['identf'], writes=['identb'])
        S.op('dve', lambda: V.memset(epsT[:], RMS_EPS), writes=['epsT'])
        S.op('act', lambda: A.activation(out=csil[:], in_=csil[:], func=AF.Silu), reads=['csil'], writes=['csil'])
        S.op('dve', lambda: V.tensor_copy(out=csil_rep[:], in_=csil[:].unsqueeze(2).to_broadcast([128, 8, 128])),
             reads=['csil'], writes=['csil_rep'])
        for j in range(16):
            st = stg[j % 2]
            k_st = 'stg%d' % (j % 2)
            S.dma('sp', lambda: nc.sync.dma_start(
                out=st[:], in_=adaw[:, j * 256:(j + 1) * 256].rearrange("(kc k) n -> k kc n", k=128)), writes=[k_st])
            S.dma('sp', lambda: nc.sync.dma_start(
                out=adab_rep[:], in_=adab[0:1, j * 256:(j + 1) * 256].partition_broadcast(128)), writes=['adab_rep'])
            for kc in range(8):
                S.op('pe', lambda: PE.matmul(psM[:, 0:256], lhsT=csil_rep[:, kc, :], rhs=st[:, kc, :], start=(kc == 0), stop=(kc == 7)),
                     reads=['csil_rep', k_st], writes=['psM'], pe_chain=(kc > 0))
            S.op('dve', lambda: V.tensor_tensor(out=modrep[:, j * 256:(j + 1) * 256], in0=psM[:, 0:256], in1=adab_rep[:], op=ALU.add),
                 reads=['psM', 'adab_rep'], writes=['modrep'])
        g1 = modrep[:, 0:1024]
        sh2 = modrep[:, 1024:2048]
        sc2 = modrep[:, 2048:3072]
        g2 = modrep[:, 3072:4096]
        S.op('dve', lambda: V.scalar_tensor_tensor(out=gk2[:], in0=sc2, scalar=1.0, in1=n2g_rep[:], op0=ALU.add, op1=ALU.mult),
             reads=['modrep', 'ot'], writes=['gk2'])
        cast_i = [0]

        def load_cast(dst3, src2d, ncols, key):
            for c0 in range(0, ncols, 256):
                i = cast_i[0] % 2
                cast_i[0] += 1
                st = stg[i]
                S.dma('sp', lambda: nc.sync.dma_start(
                    out=st[:], in_=src2d[:, c0:c0 + 256].rearrange("(kc k) n -> k kc n", k=128)), writes=['stg%d' % i])
                if i == 0:
                    S.op('dve', lambda: V.tensor_copy(out=dst3[:, :, c0:c0 + 256], in_=st[:]), reads=['stg%d' % i], writes=[key])
                else:
                    S.op('act', lambda: A.copy(out=dst3[:, :, c0:c0 + 256], in_=st[:]), reads=['stg%d' % i], writes=[key])

        load_cast(wmix, wmix_d, MIXN, 'wmix')
        load_cast(wq, wq_d, 2048, 'wq')
        for j in range(16):
            st = stg[j % 2]
            k_st = 'stg%d' % (j % 2)
            S.dma('sp', lambda: nc.sync.dma_start(out=st[:, 0, 0:128], in_=sk_d[j, :, :]), writes=[k_st])
            S.op('pe', lambda: PE.transpose(out=psM[:, 0:128], in_=st[:, 0, 0:128], identity=identf[:]),
                 reads=[k_st, 'identf'], writes=['psM'])
            S.op('act', lambda: A.copy(out=skT[:, j, :], in_=psM[:, 0:128]), reads=['psM'], writes=['skT'])

        cvb = [sb("cvb%d" % i, [128, 2048], BF16) for i in range(2)]
        cvi = 0
        for (tab, tbf) in ((u_tab, u_bf), (v_tab, v_bf)):
            for c0 in range(0, 16384, 256):
                i = cvi % 2
                st = stg[i]
                k_st = 'stg%d' % i
                S.dma('sp', lambda: nc.sync.dma_start(out=st[:].rearrange("p a b -> p (a b)"),
                                                      in_=tab[c0:c0 + 256, :].rearrange("(p r) d -> p (r d)", r=2)), writes=[k_st])
                e = ('dve', 'act', 'pool')[cvi % 3]
                if e == 'dve':
                    S.op('dve', lambda: V.tensor_copy(out=cvb[i][:], in_=st[:].rearrange("p a b -> p (a b)")), reads=[k_st], writes=['cvb%d' % i])
                elif e == 'act':
                    S.op('act', lambda: A.copy(out=cvb[i][:], in_=st[:].rearrange("p a b -> p (a b)")), reads=[k_st], writes=['cvb%d' % i])
                else:
                    S.op('pool', lambda: P.tensor_copy(out=cvb[i][:], in_=st[:].rearrange("p a b -> p (a b)")), reads=[k_st], writes=['cvb%d' % i])
                S.dma('sp', lambda: nc.sync.dma_start(out=tbf[c0:c0 + 256, :].rearrange("(p r) d -> p (r d)", r=2), in_=cvb[i][:]),
                      reads=['cvb%d' % i], writes=['tbf'])
                cvi += 1

        xt = [sb("xt%d" % i, [128, D]) for i in range(2)]
        mT = sb("mT", [128, 8, 128])
        mTb = sb("mTb", [128, 8, 128], BF16)
        x1 = sb("x1", [128, D])
        junk = sb("junk", [128, D], BF16)
        ss = sb("ss", [128, 1])
        rs = sb("rs", [128, 1])
        hf = sb("hf", [128, D])
        hfb = sb("hfb", [128, D], BF16)
        hfT = sb("hfT", [128, 8, 128], BF16)
        qkT = sb("qkT", [128, 16, 128], BF16)
        scw = sb("scw", [128, 16, 128])
        vals = sb("vals", [128, 16, 16])
        idx = sb("idx", [128, 16, 16], U32)
        idxf = sb("idxf", [128, 16, 16])
        cand = sb("cand", [128, 8, 256])
        wk2 = sb("wk2", [128, 2048])
        scw2 = wk2[:].rearrange("p (a b) -> p a b", b=128)
        cand2 = wk2[:].rearrange("p (a b) -> p a b", b=256)
        tops = sb("tops", [128, 8, 16])
        pos = sb("pos", [128, 8, 16], U32)
        au = sb("au", [128, 8, 16], U32)
        bu = sb("bu", [128, 8, 16], U32)
        af = sb("af", [128, 8, 16])
        bf = sb("bf", [128, 8, 16])
        eq = scw[:].rearrange("p a b -> p (a b)").rearrange("p (h k c) -> p h k c", h=8, k=16)
        sel1 = sb("sel1", [128, 8, 16])
        sel2 = sb("sel2", [128, 8, 16])
        ef = sb("ef", [128, 128])
        eidx = sb("eidx", [128, 128], U32)
        gat = sb("gat", [128, 8, 16])
        gsum = sb("gsum", [128, 8])
        sdot = sb("sdot", [128, 128])
        actv = sb("actv", [128, 128])
        NB = 4
        ug = [sb("ug%d" % i, [128, D], BF16) for i in range(NB)]
        dg = [sb("dg%d" % i, [128, 128], BF16) for i in range(NB)]
        vg = ug
        acc = sb("acc", [128, D])
        sg = acc

        gfunc = gelu_func if gelu_func is not None else AF.Gelu_apprx_tanh

        for t in range(NT):
            xb = xt[t % 2]
            kx = 'xt%d' % (t % 2)
            r0 = t * 128
            S.dma('sp', lambda: nc.sync.dma_start(out=xb[:], in_=x[r0:r0 + 128, :]), writes=[kx])
            S.dma('sp', lambda: nc.sync.dma_start(
                out=mT[:], in_=mixT[:, r0:r0 + 128].rearrange("(kc k) n -> k kc n", k=128)), writes=['mT'])
            S.op('act', lambda: A.copy(out=mTb[:], in_=mT[:]), reads=['mT'], writes=['mTb'])
            for nb in range(MIXN // 512):
                for kc in range(8):
                    S.op('pe', lambda: PE.matmul(psA[:, nb * 512:(nb + 1) * 512], lhsT=mTb[:, kc, :],
                                                 rhs=wmix[:, kc, nb * 512:(nb + 1) * 512], start=(kc == 0), stop=(kc == 7)),
                         reads=['mTb', 'wmix'], writes=['psA'], pe_chain=not (nb == 0 and kc == 0))
            if mode == 'glu':
                S.op('act', lambda: A.activation(out=sg[:], in_=psA[:, 1024:2048], func=AF.Sigmoid), reads=['psA'], writes=['acc'])
                S.op('dve', lambda: V.tensor_tensor(out=sg[:], in0=sg[:], in1=g1, op=ALU.mult), reads=['acc', 'modrep'], writes=['acc'])
                S.op('dve', lambda: V.tensor_tensor(out=x1[:], in0=psA[:, 0:1024], in1=sg[:], op=ALU.mult),
                     reads=['psA', 'acc'], writes=['x1'])
            else:
                S.op('dve', lambda: V.tensor_tensor(out=x1[:], in0=psA[:, 0:1024], in1=g1, op=ALU.mult),
                     reads=['psA', 'modrep'], writes=['x1'])
            S.op('dve', lambda: V.tensor_tensor(out=x1[:], in0=x1[:], in1=xb[:], op=ALU.add), reads=['x1', kx], writes=['x1'])
            S.op('act', lambda: A.activation(out=junk[:], in_=x1[:], func=AF.Square, accum_out=ss[:]), reads=['x1'], writes=['junk', 'ss'])
            S.op('act', lambda: A.activation(out=rs[:], in_=ss[:], func=AF.Sqrt, bias=epsT[:], scale=1.0 / D),
                 reads=['ss', 'epsT'], writes=['rs'])
            S.op('dve', lambda: V.reciprocal(out=rs[:], in_=rs[:]), reads=['rs'], writes=['rs'])
            S.op('dve', lambda: V.scalar_tensor_tensor(out=hf[:], in0=x1[:], scalar=rs[:], in1=gk2[:], op0=ALU.mult, op1=ALU.mult),
                 reads=['x1', 'rs', 'gk2'], writes=['hf'])
            S.op('dve', lambda: V.tensor_tensor(out=hf[:], in0=hf[:], in1=sh2, op=ALU.add), reads=['hf', 'modrep'], writes=['hf'])
            S.op('act', lambda: A.copy(out=hfb[:], in_=hf[:]), reads=['hf'], writes=['hfb'])
            for kc in range(8):
                S.op('pe', lambda: PE.transpose(out=psT[:, kc * 128:(kc + 1) * 128], in_=hfb[:, kc * 128:(kc + 1) * 128], identity=identb[:]),
                     reads=['hfb', 'identb'], writes=['psT'], pe_chain=(kc > 0))
            S.op('dve', lambda: V.tensor_copy(out=hfT[:].rearrange("p a b -> p (a b)"), in_=psT[:]), reads=['psT'], writes=['hfT'])
            for half in range(2):
                for jj in range(8):
                    j = half * 8 + jj
                    for kc in range(8):
                        S.op('pe', lambda: PE.matmul(psB[:, jj * 128:(jj + 1) * 128], lhsT=wq[:, kc, j * 128:(j + 1) * 128],
                                                     rhs=hfT[:, kc, :], start=(kc == 0), stop=(kc == 7)),
                             reads=['wq', 'hfT'], writes=['psB'], pe_chain=not (jj == 0 and kc == 0))
                S.op('act', lambda: A.copy(out=qkT[:, half * 8:(half + 1) * 8, :].rearrange("p a b -> p (a b)"), in_=psB[:]),
                     reads=['psB'], writes=['qkT'])
            for j in range(16):
                S.op('pe', lambda: PE.matmul(psA[:, j * 128:(j + 1) * 128], lhsT=qkT[:, j, :], rhs=skT[:, j, :], start=True, stop=True),
                     reads=['qkT', 'skT'], writes=['psA'], pe_chain=(j > 0))
            S.op('act', lambda: A.copy(out=scw[:].rearrange("p a b -> p (a b)"), in_=psA[:]), reads=['psA'], writes=['scw'])
            for j in range(16):
                S.op('dve', lambda: V.max(out=vals[:, j, 0:8], in_=scw[:, j, :]), reads=['scw'], writes=['vals'])
                S.op('dve', lambda: V.max_index(out=idx[:, j, 0:8], in_max=vals[:, j, 0:8], in_values=scw[:, j, :]),
                     reads=['scw', 'vals'], writes=['idx'])
                S.op('dve', lambda: V.match_replace(out=scw2[:, j, :], in_to_replace=vals[:, j, 0:8], in_values=scw[:, j, :], imm_value=-1e30),
                     reads=['scw', 'vals'], writes=['wk2'])
                S.op('dve', lambda: V.max(out=vals[:, j, 8:16], in_=scw2[:, j, :]), reads=['wk2'], writes=['vals'])
                S.op('dve', lambda: V.max_index(out=idx[:, j, 8:16], in_max=vals[:, j, 8:16], in_values=scw2[:, j, :]),
                     reads=['wk2', 'vals'], writes=['idx'])
            vals4 = vals[:].rearrange("p (h c) k -> p h c k", c=2)
            S.op('dve', lambda: V.tensor_tensor(out=cand[:].rearrange("p h (a b) -> p h a b", b=16),
                                                in0=vals4[:, :, 0, :].unsqueeze(3).to_broadcast([128, 8, 16, 16]),
                                                in1=vals4[:, :, 1, :].unsqueeze(2).to_broadcast([128, 8, 16, 16]), op=ALU.add),
                 reads=['vals'], writes=['cand'])
            for h in range(8):
                S.op('dve', lambda: V.max(out=tops[:, h, 0:8], in_=cand[:, h, :]), reads=['cand'], writes=['tops'])
                S.op('dve', lambda: V.max_index(out=pos[:, h, 0:8], in_max=tops[:, h, 0:8], in_values=cand[:, h, :]),
                     reads=['cand', 'tops'], writes=['pos'])
                S.op('dve', lambda: V.match_replace(out=cand2[:, h, :], in_to_replace=tops[:, h, 0:8], in_values=cand[:, h, :], imm_value=-1e30),
                     reads=['cand', 'tops'], writes=['wk2'])
                S.op('dve', lambda: V.max(out=tops[:, h, 8:16], in_=cand2[:, h, :]), reads=['wk2'], writes=['tops'])
                S.op('dve', lambda: V.max_index(out=pos[:, h, 8:16], in_max=tops[:, h, 8:16], in_values=cand2[:, h, :]),
                     reads=['wk2', 'tops'], writes=['pos'])
            S.op('dve', lambda: V.tensor_single_scalar(out=au[:], in_=pos[:], scalar=4, op=ALU.logical_shift_right), reads=['pos'], writes=['au'])
            S.op('dve', lambda: V.tensor_single_scalar(out=bu[:], in_=pos[:], scalar=15, op=ALU.bitwise_and), reads=['pos'], writes=['bu'])
            S.op('dve', lambda: V.tensor_copy(out=af[:], in_=au[:]), reads=['au'], writes=['af'])
            S.op('dve', lambda: V.tensor_copy(out=bf[:], in_=bu[:]), reads=['bu'], writes=['bf'])
            S.op('dve', lambda: V.tensor_copy(out=idxf[:], in_=idx[:]), reads=['idx'], writes=['idxf'])
            idxf4 = idxf[:].rearrange("p (h c) k -> p h c k", c=2)
            for (sf, cc, sel) in ((af, 0, sel1), (bf, 1, sel2)):
                ksel = 'sel1' if cc == 0 else 'sel2'
                S.op('dve', lambda: V.tensor_tensor(out=eq[:], in0=sf[:].unsqueeze(3).to_broadcast([128, 8, 16, 16]),
                                                    in1=iota16[:].unsqueeze(1).unsqueeze(1).to_broadcast([128, 8, 16, 16]), op=ALU.is_equal), reads=['af', 'bf', 'iota4'], writes=['scw'])
                S.op('dve', lambda: V.tensor_tensor(out=eq[:], in0=eq[:], in1=idxf4[:, :, cc, :].unsqueeze(2).to_broadcast([128, 8, 16, 16]),
                                                    op=ALU.mult), reads=['scw', 'idxf'], writes=['scw'])
                S.op('dve', lambda: V.tensor_reduce(out=sel[:], in_=eq[:], axis=AX.X, op=ALU.add), reads=['scw'], writes=[ksel])
            S.op('dve', lambda: V.scalar_tensor_tensor(out=ef[:], in0=sel1[:].rearrange("p h k -> p (h k)"), scalar=128.0,
                                                       in1=sel2[:].rearrange("p h k -> p (h k)"), op0=ALU.mult, op1=ALU.add),
                 reads=['sel1', 'sel2'], writes=['ef'])
            S.op('dve', lambda: V.tensor_copy(out=eidx[:], in_=ef[:]), reads=['ef'], writes=['eidx'])
            S.op('dve', lambda: V.tensor_tensor(out=gat[:], in0=tops[:], in1=tops[:, :, 0:1].to_broadcast([128, 8, 16]), op=ALU.subtract),
                 reads=['tops'], writes=['gat'])
            S.op('act', lambda: A.activation(out=gat[:], in_=gat[:], func=AF.Exp), reads=['gat'], writes=['gat'])
            S.op('dve', lambda: V.tensor_reduce(out=gsum[:], in_=gat[:], axis=AX.X, op=ALU.add), reads=['gat'], writes=['gsum'])
            S.op('dve', lambda: V.reciprocal(out=gsum[:], in_=gsum[:]), reads=['gsum'], writes=['gsum'])
            S.op('dve', lambda: V.tensor_tensor(out=gat[:], in0=gat[:], in1=gsum[:].unsqueeze(2).to_broadcast([128, 8, 16]), op=ALU.mult),
                 reads=['gat', 'gsum'], writes=['gat'])
            if dbg == 'idx':
                S.op('dve', lambda: V.tensor_copy(out=ot[:], in_=hf[:]), reads=['hf'], writes=['ot'])
                S.op('dve', lambda: V.tensor_copy(out=ot[:, 0:128], in_=ef[:]), reads=['ef'], writes=['ot'])
                S.op('dve', lambda: V.tensor_copy(out=ot[:, 128:256], in_=gat[:].rearrange("p h k -> p (h k)")), reads=['gat'], writes=['ot'])
                S.op('dve', lambda: V.tensor_copy(out=ot[:, 256:384], in_=tops[:].rearrange("p h k -> p (h k)")), reads=['tops'], writes=['ot'])
                S.op('dve', lambda: V.tensor_copy(out=ot[:, 384:640], in_=idxf[:].rearrange("p h k -> p (h k)")), reads=['idxf'], writes=['ot'])
                S.op('dve', lambda: V.tensor_copy(out=ot[:, 640:896], in_=vals[:].rearrange("p h k -> p (h k)")), reads=['vals'], writes=['ot'])
                S.dma('sp', lambda: nc.sync.dma_start(out=out[r0:r0 + 128, :], in_=ot[:]), reads=['ot'], writes=['out'])
                continue
            for s in range(128):
                b = s % NB
                S.dma('pool', lambda: P.indirect_dma_start(out=ug[b][:], out_offset=None, in_=u_bf[:, :],
                                                            in_offset=IOA(ap=eidx[:, s:s + 1], axis=0)),
                      reads=['eidx', 'tbf'], writes=['ug%d' % b])
                S.op('dve', lambda: V.scalar_tensor_tensor(out=junk[:], in0=ug[b][:], scalar=1.0, in1=hf[:],
                                                           op0=ALU.mult, op1=ALU.mult, accum_out=sdot[:, s:s + 1]),
                     reads=['ug%d' % b, 'hf'], writes=['junk', 'sdot'])
            if dbg == 'u':
                S.op('dve', lambda: V.tensor_copy(out=ot[:], in_=hf[:]), reads=['hf'], writes=['ot'])
                S.op('dve', lambda: V.tensor_copy(out=ot[:, 0:128], in_=sdot[:]), reads=['sdot'], writes=['ot'])
                S.op('dve', lambda: V.tensor_copy(out=ot[:, 128:256], in_=ef[:]), reads=['ef'], writes=['ot'])
                S.dma('sp', lambda: nc.sync.dma_start(out=out[r0:r0 + 128, :], in_=ot[:]), reads=['ot'], writes=['out'])
                continue
            S.op('dve', lambda: V.tensor_tensor(out=actv[:], in0=sdot[:], in1=sdot[:], op=ALU.mult), reads=['sdot'], writes=['actv'])
            S.op('dve', lambda: V.tensor_scalar(out=actv[:], in0=actv[:], scalar1=0.044715, scalar2=1.0, op0=ALU.mult, op1=ALU.add),
                 reads=['actv'], writes=['actv'])
            S.op('dve', lambda: V.tensor_tensor(out=actv[:], in0=actv[:], in1=sdot[:], op=ALU.mult), reads=['actv', 'sdot'], writes=['actv'])
            S.op('act', lambda: A.activation(out=actv[:], in_=actv[:], func=AF.Sigmoid, scale=1.5957691216), reads=['actv'], writes=['actv'])
            S.op('dve', lambda: V.tensor_tensor(out=actv[:], in0=actv[:], in1=sdot[:], op=ALU.mult), reads=['actv', 'sdot'], writes=['actv'])
            S.op('dve', lambda: V.tensor_tensor(out=actv[:], in0=actv[:], in1=gat[:].rearrange("p h k -> p (h k)"), op=ALU.mult),
                 reads=['actv', 'gat'], writes=['actv'])
            for s in range(128):
                b = s % NB
                S.dma('pool', lambda: P.indirect_dma_start(out=vg[b][:], out_offset=None, in_=v_bf[:, :],
                                                            in_offset=IOA(ap=eidx[:, s:s + 1], axis=0)),
                      reads=['eidx', 'tbf'], writes=['ug%d' % b])
                S.op('act', lambda: A.activation(out=dg[b][:], in_=identf[:], func=AF.Copy, scale=actv[:, s:s + 1]),
                     reads=['identf', 'actv'], writes=['dg%d' % b])
                for hb in range(2):
                    S.op('pe', lambda: PE.matmul(psB[:, hb * 512:(hb + 1) * 512], lhsT=dg[b][:], rhs=vg[b][:, hb * 512:(hb + 1) * 512],
                                                 start=(s == 0), stop=(s == 127)), reads=['dg%d' % b, 'ug%d' % b], writes=['psB'],
                         pe_chain=not (s == 0 and hb == 0))
            S.op('dve', lambda: V.tensor_tensor(out=acc[:], in0=psB[:], in1=g2, op=ALU.mult), reads=['psB', 'modrep'], writes=['acc'])
            S.op('dve', lambda: V.tensor_tensor(out=ot[:], in0=acc[:], in1=x1[:], op=ALU.add), reads=['acc', 'x1'], writes=['ot'])
            if final:
                S.op('act', lambda: A.activation(out=junk[:], in_=ot[:], func=AF.Square, accum_out=ss[:]), reads=['ot'], writes=['junk', 'ss'])
                S.op('act', lambda: A.activation(out=rs[:], in_=ss[:], func=AF.Sqrt, bias=epsT[:], scale=1.0 / D),
                     reads=['ss', 'epsT'], writes=['rs'])
                S.op('dve', lambda: V.reciprocal(out=rs[:], in_=rs[:]), reads=['rs'], writes=['rs'])
                S.op('dve', lambda: V.scalar_tensor_tensor(out=ot[:], in0=ot[:], scalar=rs[:], in1=fing_rep[:], op0=ALU.mult, op1=ALU.mult),
                     reads=['ot', 'rs', 'fing_rep'], writes=['ot'])
            S.dma('sp', lambda: nc.sync.dma_start(out=out[r0:r0 + 128, :], in_=ot[:]), reads=['ot'], writes=['out'])
        S.finish(['out'])
        print("tok program: instr", S.n_instr, "waits", S.n_wait)
    return nc


def tok_consts():
    iota16 = np.broadcast_to(np.arange(16, dtype=np.float32)[None, :], (128, 16)).copy()
    return {"identf": np.eye(128, dtype=np.float32), "iota16": iota16}

import math

RMS_EPS = 1e-6
PI = math.pi


def build_s5(NCH):
    nc = bass.Bass("TRN2", target_bir_lowering=False)
    T = NCH * 128
    D = 1024
    din = lambda n, s, d=F32: nc.dram_tensor(n, s, d, kind="ExternalInput").ap()
    x = din("x", [T, D])
    c_l = din("c_l", [128, 8])
    adaw = din("adaw", [D, 2048])
    adab = din("adab", [1, 2048])
    n1g = din("n1g", [1, D])
    win_d = din("win", [D, 256])
    are_c_d = din("are_c", [128, 16]); aim_c_d = din("aim_c", [128, 16]); ldt_c_d = din("ldt_c", [128, 16])
    are_r_d = din("are_r", [128, 2048]); aim_r_d = din("aim_r", [128, 2048]); ldt_r_d = din("ldt_r", [128, 2048])
    X1p_d = din("X1p", [128, 2048]); X2p_d = din("X2p", [128, 2048])
    CcP_d = din("CcP", [128, 2048]); CcSP_d = din("CcSP", [128, 2048])
    dcol_d = din("dcol", [128, 2])
    identf_d = din("identf", [128, 128]); swapm_d = din("swapm", [128, 128])
    sgnc_d = din("sgn_c", [128, 1]); sgnr_d = din("sgn_r", [128, 128])
    mrow_d = din("mrow", [128, 128]); mask01_d = din("mask01", [128, 512])
    out = nc.dram_tensor("out", [256, T], F32, kind="ExternalOutput").ap()

    es = ExitStack()
    with es:
        es.enter_context(nc.allow_low_precision("bf16 matmul operands"))
        es.enter_context(nc.allow_non_contiguous_dma("small layout loads"))
        S = Sched(nc, es)
        sb = lambda n, s, d=F32: es.enter_context(nc.sbuf_tensor("s_" + n, s, d))
        ps = lambda n, s, d=F32: es.enter_context(nc.psum_tensor("p_" + n, s, d))
        V, A, P, PE = nc.vector, nc.scalar, nc.gpsimd, nc.tensor

        def ld(dst, src, key):
            S.dma('sp', lambda: nc.sync.dma_start(out=dst, in_=src), writes=[key])

        identf = sb("identf", [128, 128]); identb = sb("identb", [128, 128], BF16)
        epsT = sb("epsT", [128, 1])
        modrep = sb("modrep", [128, 2048])
        gk1 = sb("gk1", [128, D])
        win = sb("win", [128, 8, 256], BF16)
        Ainv_r = sb("Ainv_r", [128, 16, 128]); Ainv_i = sb("Ainv_i", [128, 16, 128])
        Apow_r = sb("Apow_r", [128, 16, 128]); Apow_i = sb("Apow_i", [128, 16, 128])
        Rot = sb("Rot", [128, 16, 128])
        Bpad = sb("Bpad", [128, 16, 128], BF16); BpadS = sb("BpadS", [128, 16, 128], BF16)
        Cc = sb("Cc", [128, 16, 128], BF16); CcS = sb("CcS", [128, 16, 128], BF16)
        dcol = sb("dcol", [128, 2])
        mask01 = sb("mask01", [128, 512])

        psT = ps("psT", [128, 1024], BF16)
        psU = ps("psU", [128, 512])
        psBU = [ps("psBU%d" % i, [128, 512]) for i in range(2)]
        psBS = [ps("psBS%d" % i, [128, 512]) for i in range(2)]
        psI = ps("psI", [128, 512])
        psY = ps("psY", [128, 512])

        ld(identf[:], identf_d[:, :], 'identf')
        ld(dcol[:], dcol_d[:, :], 'dcol')
        ld(mask01[:], mask01_d[:, :], 'mask01')
        S.op('dve', lambda: V.tensor_copy(out=identb[:], in_=identf[:]), reads=['identf'], writes=['identb'])
        S.op('dve', lambda: V.memset(epsT[:], RMS_EPS), writes=['epsT'])

        es2 = ExitStack()
        with es2:
            tb = lambda n, s, d=F32: es2.enter_context(nc.sbuf_tensor("t_" + n, s, d))
            csil = tb("csil", [128, 8]); csil_rep = tb("csil_rep", [128, 8, 128])
            stg = [tb("stg%d" % i, [128, 8, 256]) for i in range(2)]
            adab_rep = tb("adab_rep", [128, 256])
            n1g_rep = tb("n1g_rep", [128, D])
            ld(csil[:], c_l[:, :], 'csil')
            ld(n1g_rep[:], n1g[0:1, :].partition_broadcast(128), 'n1g_rep')
            S.op('act', lambda: A.activation(out=csil[:], in_=csil[:], func=AF.Silu), reads=['csil'], writes=['csil'])
            S.op('dve', lambda: V.tensor_copy(out=csil_rep[:], in_=csil[:].unsqueeze(2).to_broadcast([128, 8, 128])),
                 reads=['csil'], writes=['csil_rep'])
            for j in range(8):
                st = stg[j % 2]; k_st = 'stg%d' % (j % 2)
                ld(st[:], adaw[:, j * 256:(j + 1) * 256].rearrange("(kc k) n -> k kc n", k=128), k_st)
                ld(adab_rep[:], adab[0:1, j * 256:(j + 1) * 256].partition_broadcast(128), 'adab_rep')
                for kc in range(8):
                    S.op('pe', lambda: PE.matmul(psU[:, 0:256], lhsT=csil_rep[:, kc, :], rhs=st[:, kc, :], start=(kc == 0), stop=(kc == 7)),
                         reads=['csil_rep', k_st], writes=['psU'], pe_chain=(kc > 0))
                S.op('dve', lambda: V.tensor_tensor(out=modrep[:, j * 256:(j + 1) * 256], in0=psU[:, 0:256], in1=adab_rep[:], op=ALU.add),
                     reads=['psU', 'adab_rep'], writes=['modrep'])
            sh1 = modrep[:, 0:1024]; sc1 = modrep[:, 1024:2048]
            S.op('dve', lambda: V.scalar_tensor_tensor(out=gk1[:], in0=sc1, scalar=1.0, in1=n1g_rep[:], op0=ALU.add, op1=ALU.mult),
                 reads=['modrep', 'n1g_rep'], writes=['gk1'])
            ld(stg[0][:], win_d[:, :].rearrange("(kc k) n -> k kc n", k=128), 'stg0')
            S.op('dve', lambda: V.tensor_copy(out=win[:], in_=stg[0][:]), reads=['stg0'], writes=['win'])

            def emit_sin(outap, th, ki, kf, tmp, keys, shift=0.0):
                kth, kout, kki, kkf, ktmp = keys
                if shift != 0.0:
                    S.op('dve', lambda: V.tensor_scalar(out=th, in0=th, scalar1=shift, scalar2=None, op0=ALU.add), reads=[kth], writes=[kth])
                S.op('dve', lambda: V.tensor_scalar(out=tmp, in0=th, scalar1=1.0 / (2 * PI), scalar2=None, op0=ALU.mult), reads=[kth], writes=[ktmp])
                S.op('dve', lambda: V.tensor_copy(out=ki, in_=tmp), reads=[ktmp], writes=[kki])
                S.op('dve', lambda: V.tensor_copy(out=kf, in_=ki), reads=[kki], writes=[kkf])
                S.op('dve', lambda: V.scalar_tensor_tensor(out=th, in0=kf, scalar=-2 * PI, in1=th, op0=ALU.mult, op1=ALU.add),
                     reads=[kkf, kth], writes=[kth])
                S.op('dve', lambda: V.tensor_scalar(out=tmp, in0=th, scalar1=PI, scalar2=-2 * PI, op0=ALU.is_gt, op1=ALU.mult), reads=[kth], writes=[ktmp])
                S.op('dve', lambda: V.tensor_tensor(out=th, in0=th, in1=tmp, op=ALU.add), reads=[kth, ktmp], writes=[kth])
                S.op('dve', lambda: V.tensor_scalar(out=tmp, in0=th, scalar1=-PI, scalar2=2 * PI, op0=ALU.is_lt, op1=ALU.mult), reads=[kth], writes=[ktmp])
                S.op('dve', lambda: V.tensor_tensor(out=th, in0=th, in1=tmp, op=ALU.add), reads=[kth, ktmp], writes=[kth])
                S.op('dve', lambda: V.tensor_scalar(out=th, in0=th, scalar1=3.1415925, scalar2=-3.1415925, op0=ALU.min, op1=ALU.max), reads=[kth], writes=[kth])
                S.op('act', lambda: A.activation(out=outap, in_=th, func=AF.Sin), reads=[kth], writes=[kout])

            are_c = tb("are_c", [128, 16]); aim_c = tb("aim_c", [128, 16]); dtc = tb("dtc", [128, 16])
            adr_c = tb("adr_c", [128, 16]); nadr_c = tb("nadr_c", [128, 16]); adi_c = tb("adi_c", [128, 16])
            mrow = tb("mrow", [128, 128]); swapm = tb("swapm", [128, 128]); sgn_c = tb("sgn_c", [128, 1]); sgn_r = tb("sgn_r", [128, 128])
            ld(are_c[:], are_c_d[:, :], 'are_c'); ld(aim_c[:], aim_c_d[:, :], 'aim_c'); ld(dtc[:], ldt_c_d[:, :], 'dtc')
            ld(mrow[:], mrow_d[:, :], 'mrow'); ld(swapm[:], swapm_d[:, :], 'swapm'); ld(sgn_c[:], sgnc_d[:, :], 'sgn_c'); ld(sgn_r[:], sgnr_d[:, :], 'sgn_r')
            S.op('act', lambda: A.activation(out=dtc[:], in_=dtc[:], func=AF.Exp), reads=['dtc'], writes=['dtc'])
            S.op('dve', lambda: V.tensor_tensor(out=adr_c[:], in0=are_c[:], in1=dtc[:], op=ALU.mult), reads=['are_c', 'dtc'], writes=['adr_c'])
            S.op('dve', lambda: V.tensor_scalar(out=nadr_c[:], in0=adr_c[:], scalar1=-1.0, scalar2=None, op0=ALU.mult), reads=['adr_c'], writes=['nadr_c'])
            S.op('dve', lambda: V.tensor_tensor(out=adi_c[:], in0=aim_c[:], in1=dtc[:], op=ALU.mult), reads=['aim_c', 'dtc'], writes=['adi_c'])
            T1 = tb("T1", [128, 2048]); T2 = tb("T2", [128, 2048]); T3 = tb("T3", [128, 2048]); T4 = tb("T4", [128, 2048])
            T5 = tb("T5", [128, 2048]); T6 = tb("T6", [128, 2048]); TI = tb("TI", [128, 2048], I32)
            T1v = T1[:].rearrange("p (g m) -> p g m", m=128); T2v = T2[:].rearrange("p (g m) -> p g m", m=128)
            T3v = T3[:].rearrange("p (g m) -> p g m", m=128); T4v = T4[:].rearrange("p (g m) -> p g m", m=128)
            for g in range(16):
                S.op('act', lambda: A.activation(out=T1v[:, g, :], in_=mrow[:], func=AF.Exp, scale=adr_c[:, g:g + 1]), reads=['mrow', 'adr_c'], writes=['T1'])
                S.op('act', lambda: A.activation(out=T2v[:, g, :], in_=mrow[:], func=AF.Exp, scale=nadr_c[:, g:g + 1]), reads=['mrow', 'nadr_c'], writes=['T2'])
                S.op('dve', lambda: V.tensor_scalar(out=T3v[:, g, :], in0=mrow[:], scalar1=adi_c[:, g:g + 1], scalar2=None, op0=ALU.mult),
                     reads=['mrow', 'adi_c'], writes=['T3'])
            S.op('dve', lambda: V.tensor_copy(out=T4[:], in_=T3[:]), reads=['T3'], writes=['T4'])
            emit_sin(T3[:], T3[:], TI[:], T5[:], T6[:], ('T3', 'T3', 'TI', 'T5', 'T6'))
            emit_sin(T4[:], T4[:], TI[:], T5[:], T6[:], ('T4', 'T4', 'TI', 'T5', 'T6'), shift=PI / 2)
            fl = lambda t: t[:].rearrange("p g m -> p (g m)")
            S.op('dve', lambda: V.tensor_tensor(out=fl(Apow_r), in0=T1[:], in1=T4[:], op=ALU.mult), reads=['T1', 'T4'], writes=['Apow_r'])
            S.op('dve', lambda: V.tensor_tensor(out=fl(Apow_i), in0=T1[:], in1=T3[:], op=ALU.mult), reads=['T1', 'T3'], writes=['Apow_i'])
            S.op('dve', lambda: V.tensor_tensor(out=fl(Ainv_r), in0=T2[:], in1=T4[:], op=ALU.mult), reads=['T2', 'T4'], writes=['Ainv_r'])
            S.op('dve', lambda: V.scalar_tensor_tensor(out=fl(Ainv_i), in0=T2[:], scalar=-1.0, in1=T3[:], op0=ALU.mult, op1=ALU.mult),
                 reads=['T2', 'T3'], writes=['Ainv_i'])
            e128 = tb("e128", [128, 16]); th_s = tb("th_s", [128, 16]); th_c = tb("th_c", [128, 16])
            ki16 = tb("ki16", [128, 16], I32); kf16 = tb("kf16", [128, 16]); tm16 = tb("tm16", [128, 16])
            r128r = tb("r128r", [128, 16]); r128i = tb("r128i", [128, 16])
            S.op('act', lambda: A.activation(out=e128[:], in_=adr_c[:], func=AF.Exp, scale=128.0), reads=['adr_c'], writes=['e128'])
            S.op('dve', lambda: V.tensor_scalar(out=th_s[:], in0=adi_c[:], scalar1=128.0, scalar2=None, op0=ALU.mult), reads=['adi_c'], writes=['th_s'])
            S.op('dve', lambda: V.tensor_copy(out=th_c[:], in_=th_s[:]), reads=['th_s'], writes=['th_c'])
            emit_sin(th_s[:], th_s[:], ki16[:], kf16[:], tm16[:], ('th_s', 'th_s', 'ki16', 'kf16', 'tm16'))
            emit_sin(th_c[:], th_c[:], ki16[:], kf16[:], tm16[:], ('th_c', 'th_c', 'ki16', 'kf16', 'tm16'), shift=PI / 2)
            S.op('dve', lambda: V.tensor_tensor(out=r128r[:], in0=e128[:], in1=th_c[:], op=ALU.mult), reads=['e128', 'th_c'], writes=['r128r'])
            S.op('dve', lambda: V.tensor_tensor(out=r128i[:], in0=e128[:], in1=th_s[:], op=ALU.mult), reads=['e128', 'th_s'], writes=['r128i'])
            S.op('dve', lambda: V.tensor_scalar(out=r128i[:], in0=r128i[:], scalar1=sgn_c[:, 0:1], scalar2=None, op0=ALU.mult),
                 reads=['r128i', 'sgn_c'], writes=['r128i'])
            for g in range(16):
                S.op('dve', lambda: V.tensor_scalar(out=Rot[:, g, :], in0=identf[:], scalar1=r128r[:, g:g + 1], scalar2=None, op0=ALU.mult),
                     reads=['identf', 'r128r'], writes=['Rot'])
                S.op('dve', lambda: V.scalar_tensor_tensor(out=Rot[:, g, :], in0=swapm[:], scalar=r128i[:, g:g + 1], in1=Rot[:, g, :],
                                                           op0=ALU.mult, op1=ALU.add), reads=['swapm', 'r128i', 'Rot'], writes=['Rot'])
            T7 = tb("T7", [128, 2048]); T8 = tb("T8", [128, 2048])
            ld(T1[:], are_r_d[:, :], 'T1'); ld(T2[:], aim_r_d[:, :], 'T2'); ld(T3[:], ldt_r_d[:, :], 'T3')
            S.op('act', lambda: A.activation(out=T3[:], in_=T3[:], func=AF.Exp), reads=['T3'], writes=['T3'])
            S.op('dve', lambda: V.tensor_tensor(out=T4[:], in0=T2[:], in1=T3[:], op=ALU.mult), reads=['T2', 'T3'], writes=['T4'])
            S.op('dve', lambda: V.tensor_tensor(out=T3[:], in0=T1[:], in1=T3[:], op=ALU.mult), reads=['T1', 'T3'], writes=['T3'])
            S.op('act', lambda: A.activation(out=T3[:], in_=T3[:], func=AF.Exp), reads=['T3'], writes=['T3'])
            S.op('dve', lambda: V.tensor_copy(out=T7[:], in_=T4[:]), reads=['T4'], writes=['T7'])
            emit_sin(T4[:], T4[:], TI[:], T5[:], T6[:], ('T4', 'T4', 'TI', 'T5', 'T6'))
            emit_sin(T7[:], T7[:], TI[:], T5[:], T6[:], ('T7', 'T7', 'TI', 'T5', 'T6'), shift=PI / 2)
            S.op('dve', lambda: V.tensor_tensor(out=T4[:], in0=T4[:], in1=T3[:], op=ALU.mult), reads=['T4', 'T3'], writes=['T4'])
            S.op('dve', lambda: V.tensor_tensor(out=T7[:], in0=T7[:], in1=T3[:], op=ALU.mult), reads=['T7', 'T3'], writes=['T7'])
            S.op('dve', lambda: V.tensor_scalar(out=T7[:], in0=T7[:], scalar1=-1.0, scalar2=None, op0=ALU.add), reads=['T7'], writes=['T7'])
            S.op('dve', lambda: V.tensor_tensor(out=T5[:], in0=T1[:], in1=T1[:], op=ALU.mult), reads=['T1'], writes=['T5'])
            S.op('dve', lambda: V.tensor_tensor(out=T6[:], in0=T2[:], in1=T2[:], op=ALU.mult), reads=['T2'], writes=['T6'])
            S.op('dve', lambda: V.tensor_tensor(out=T5[:], in0=T5[:], in1=T6[:], op=ALU.add), reads=['T5', 'T6'], writes=['T5'])
            S.op('dve', lambda: V.reciprocal(out=T5[:], in_=T5[:]), reads=['T5'], writes=['T5'])
            S.op('dve', lambda: V.tensor_tensor(out=T3[:], in0=T7[:], in1=T1[:], op=ALU.mult), reads=['T7', 'T1'], writes=['T3'])
            S.op('dve', lambda: V.tensor_tensor(out=T6[:], in0=T4[:], in1=T2[:], op=ALU.mult), reads=['T4', 'T2'], writes=['T6'])
            S.op('dve', lambda: V.tensor_tensor(out=T3[:], in0=T3[:], in1=T6[:], op=ALU.add), reads=['T3', 'T6'], writes=['T3'])
            S.op('dve', lambda: V.tensor_tensor(out=T3[:], in0=T3[:], in1=T5[:], op=ALU.mult), reads=['T3', 'T5'], writes=['T3'])
            S.op('dve', lambda: V.tensor_tensor(out=T8[:], in0=T4[:], in1=T1[:], op=ALU.mult), reads=['T4', 'T1'], writes=['T8'])
            S.op('dve', lambda: V.tensor_tensor(out=T6[:], in0=T7[:], in1=T2[:], op=ALU.mult), reads=['T7', 'T2'], writes=['T6'])
            S.op('dve', lambda: V.tensor_tensor(out=T8[:], in0=T8[:], in1=T6[:], op=ALU.subtract), reads=['T8', 'T6'], writes=['T8'])
            S.op('dve', lambda: V.tensor_tensor(out=T8[:], in0=T8[:], in1=T5[:], op=ALU.mult), reads=['T8', 'T5'], writes=['T8'])
            ld(T1[:], X1p_d[:, :], 'T1'); ld(T2[:], X2p_d[:, :], 'T2')
            S.op('dve', lambda: V.tensor_tensor(out=T2[:].rearrange("p (g f) -> p g f", f=128), in0=T2[:].rearrange("p (g f) -> p g f", f=128),
                                                in1=sgn_r[:].unsqueeze(1).to_broadcast([128, 16, 128]), op=ALU.mult), reads=['T2', 'sgn_r'], writes=['T2'])
            S.op('dve', lambda: V.tensor_tensor(out=T5[:], in0=T3[:], in1=T1[:], op=ALU.mult), reads=['T3', 'T1'], writes=['T5'])
            S.op('dve', lambda: V.tensor_tensor(out=T6[:], in0=T8[:], in1=T2[:], op=ALU.mult), reads=['T8', 'T2'], writes=['T6'])
            S.op('dve', lambda: V.tensor_tensor(out=fl(Bpad), in0=T5[:], in1=T6[:], op=ALU.add), reads=['T5', 'T6'], writes=['Bpad'])
            S.op('dve', lambda: V.tensor_tensor(out=T5[:], in0=T3[:], in1=T2[:], op=ALU.mult), reads=['T3', 'T2'], writes=['T5'])
            S.op('dve', lambda: V.tensor_tensor(out=T6[:], in0=T8[:], in1=T1[:], op=ALU.mult), reads=['T8', 'T1'], writes=['T6'])
            S.op('dve', lambda: V.tensor_tensor(out=fl(BpadS), in0=T5[:], in1=T6[:], op=ALU.subtract), reads=['T5', 'T6'], writes=['BpadS'])
            ld(T1[:], CcP_d[:, :], 'T1'); ld(T2[:], CcSP_d[:, :], 'T2')
            S.op('dve', lambda: V.tensor_scalar(out=fl(Cc), in0=T1[:], scalar1=sgn_c[:, 0:1], scalar2=None, op0=ALU.mult), reads=['T1', 'sgn_c'], writes=['Cc'])
            S.op('dve', lambda: V.tensor_scalar(out=fl(CcS), in0=T2[:], scalar1=-1.0, scalar2=None, op0=ALU.mult), reads=['T2'], writes=['CcS'])
            S.barrier()
        xt = [sb("xt%d" % i, [128, D]) for i in range(2)]
        junk = sb("junk", [128, D], BF16)
        ss = sb("ss", [128, 1]); rs = sb("rs", [128, 1])
        hm = sb("hm", [128, D]); hmb = sb("hmb", [128, D], BF16)
        hmT = sb("hmT", [128, 8, 128], BF16)
        uT = sb("uT", [128, 2, 128]); uTb = sb("uTb", [128, 2, 128], BF16)
        Vt = [sb("Vt%d" % i, [128, 4, 128]) for i in range(2)]
        Wt = [sb("Wt%d" % i, [128, 4, 128]) for i in range(2)]
        Zt = [sb("Zt%d" % i, [128, 4, 128]) for i in range(4)]
        Hr = [sb("Hr%d" % i, [128, 4, 128], BF16) for i in range(2)]
        Hi = [sb("Hi%d" % i, [128, 4, 128], BF16) for i in range(2)]
        yt = sb("yt", [128, 2, 128]); ga = sb("ga", [128, 2, 128]); yo = sb("yo", [128, 2, 128])
        sh1 = modrep[:, 0:1024]

        for c in range(NCH):
            xb = xt[c % 2]; kx = 'xt%d' % (c % 2)
            r0 = c * 128
            ld(xb[:], x[r0:r0 + 128, :], kx)
            S.op('act', lambda: A.activation(out=junk[:], in_=xb[:], func=AF.Square, accum_out=ss[:]), reads=[kx], writes=['junk', 'ss'])
            S.op('act', lambda: A.activation(out=rs[:], in_=ss[:], func=AF.Sqrt, bias=epsT[:], scale=1.0 / D), reads=['ss', 'epsT'], writes=['rs'])
            S.op('dve', lambda: V.reciprocal(out=rs[:], in_=rs[:]), reads=['rs'], writes=['rs'])
            S.op('dve', lambda: V.scalar_tensor_tensor(out=hm[:], in0=xb[:], scalar=rs[:], in1=gk1[:], op0=ALU.mult, op1=ALU.mult),
                 reads=[kx, 'rs', 'gk1'], writes=['hm'])
            S.op('dve', lambda: V.tensor_tensor(out=hmb[:], in0=hm[:], in1=sh1, op=ALU.add), reads=['hm', 'modrep'], writes=['hmb'])
            for kc in range(8):
                S.op('pe', lambda: PE.transpose(out=psT[:, kc * 128:(kc + 1) * 128], in_=hmb[:, kc * 128:(kc + 1) * 128], identity=identb[:]),
                     reads=['hmb', 'identb'], writes=['psT'], pe_chain=(kc > 0))
            S.op('act', lambda: A.copy(out=hmT[:].rearrange("p a b -> p (a b)"), in_=psT[:]), reads=['psT'], writes=['hmT'])
            for mc in range(2):
                for kc in range(8):
                    S.op('pe', lambda: PE.matmul(psU[:, mc * 128:(mc + 1) * 128], lhsT=win[:, kc, mc * 128:(mc + 1) * 128], rhs=hmT[:, kc, :],
                                                 start=(kc == 0), stop=(kc == 7)), reads=['win', 'hmT'], writes=['psU'],
                         pe_chain=not (mc == 0 and kc == 0))
            S.op('act', lambda: A.copy(out=uT[:].rearrange("p a b -> p (a b)"), in_=psU[:, 0:256]), reads=['psU'], writes=['uT'])
            S.op('act', lambda: A.copy(out=uTb[:].rearrange("p a b -> p (a b)"), in_=psU[:, 0:256]), reads=['psU'], writes=['uTb'])
            for bt in range(4):
                i2 = bt % 2
                kbu = 'psBU%d' % i2; kbs = 'psBS%d' % i2
                kV = 'Vt%d' % i2; kW = 'Wt%d' % i2; kZ = 'Zt%d' % bt; kHr = 'Hr%d' % i2; kHi = 'Hi%d' % i2
                gc = bt // 2
                for gg in range(4):
                    g = bt * 4 + gg
                    S.op('pe', lambda: PE.matmul(psBU[i2][:, gg * 128:(gg + 1) * 128], lhsT=Bpad[:, g, :], rhs=uTb[:, gc, :], start=True, stop=True),
                         reads=['Bpad', 'uTb'], writes=[kbu], pe_chain=(gg > 0))
                for gg in range(4):
                    g = bt * 4 + gg
                    S.op('pe', lambda: PE.matmul(psBS[i2][:, gg * 128:(gg + 1) * 128], lhsT=BpadS[:, g, :], rhs=uTb[:, gc, :], start=True, stop=True),
                         reads=['BpadS', 'uTb'], writes=[kbs], pe_chain=(gg > 0))
                Vf = Vt[i2][:].rearrange("p a b -> p (a b)"); Wf = Wt[i2][:].rearrange("p a b -> p (a b)")
                Zf = Zt[bt][:].rearrange("p a b -> p (a b)")
                gs = slice(bt * 4, bt * 4 + 4)
                S.op('dve', lambda: V.tensor_tensor(out=Vf, in0=psBU[i2][:], in1=Ainv_r[:, gs, :].rearrange("p a b -> p (a b)"), op=ALU.mult),
                     reads=[kbu, 'Ainv_r'], writes=[kV])
                S.op('dve', lambda: V.tensor_tensor(out=Wf, in0=psBS[i2][:], in1=Ainv_i[:, gs, :].rearrange("p a b -> p (a b)"), op=ALU.mult),
                     reads=[kbs, 'Ainv_i'], writes=[kW])
                S.op('dve', lambda: V.tensor_tensor(out=Vf, in0=Vf, in1=Wf, op=ALU.add), reads=[kV, kW], writes=[kV])
                if c > 0:
                    S.op('dve', lambda: V.tensor_tensor(out=Vt[i2][:, :, 0], in0=Vt[i2][:, :, 0], in1=psI[:, bt * 4:bt * 4 + 4], op=ALU.add),
                         reads=[kV, 'psI'], writes=[kV])
                S.op('dve', lambda: V.tensor_tensor_scan(out=Zf, data0=mask01[:], data1=Vf, initial=0.0, op0=ALU.mult, op1=ALU.add),
                     reads=['mask01', kV], writes=[kZ])
                if c < NCH - 1:
                    for gg in range(4):
                        g = bt * 4 + gg
                        S.op('pe', lambda: PE.matmul(psI[:, g:g + 1], lhsT=Rot[:, g, :], rhs=Zt[bt][:, gg, 127:128], start=True, stop=True),
                             reads=['Rot', kZ], writes=['psI'], pe_chain=(gg > 0))
                S.op('dve', lambda: V.tensor_tensor(out=Hr[i2][:].rearrange("p a b -> p (a b)"), in0=Zf,
                                                    in1=Apow_r[:, gs, :].rearrange("p a b -> p (a b)"), op=ALU.mult), reads=[kZ, 'Apow_r'], writes=[kHr])
                S.op('dve', lambda: V.tensor_tensor(out=Hi[i2][:].rearrange("p a b -> p (a b)"), in0=Zf,
                                                    in1=Apow_i[:, gs, :].rearrange("p a b -> p (a b)"), op=ALU.mult), reads=[kZ, 'Apow_i'], writes=[kHi])
                for gg in range(4):
                    g = bt * 4 + gg
                    first = (g % 8 == 0)
                    S.op('pe', lambda: PE.matmul(psY[:, gc * 128:(gc + 1) * 128], lhsT=Cc[:, g, :], rhs=Hr[i2][:, gg, :], start=first, stop=False),
                         reads=['Cc', kHr], writes=['psY'], pe_chain=not first)
                    S.op('pe', lambda: PE.matmul(psY[:, gc * 128:(gc + 1) * 128], lhsT=CcS[:, g, :], rhs=Hi[i2][:, gg, :], start=False, stop=(g % 8 == 7)),
                         reads=['CcS', kHi], writes=['psY'], pe_chain=True)
            ytf = yt[:].rearrange("p a b -> p (a b)"); gaf = ga[:].rearrange("p a b -> p (a b)"); yof = yo[:].rearrange("p a b -> p (a b)")
            for gc in range(2):
                S.op('dve', lambda: V.scalar_tensor_tensor(out=yt[:, gc, :], in0=uT[:, gc, :], scalar=dcol[:, gc:gc + 1],
                                                           in1=psY[:, gc * 128:(gc + 1) * 128], op0=ALU.mult, op1=ALU.add),
                     reads=['uT', 'dcol', 'psY'], writes=['yt'])
            S.op('dve', lambda: V.tensor_tensor(out=gaf, in0=ytf, in1=ytf, op=ALU.mult), reads=['yt'], writes=['ga'])
            S.op('dve', lambda: V.tensor_scalar(out=gaf, in0=gaf, scalar1=0.044715, scalar2=1.0, op0=ALU.mult, op1=ALU.add), reads=['ga'], writes=['ga'])
            S.op('dve', lambda: V.tensor_tensor(out=gaf, in0=gaf, in1=ytf, op=ALU.mult), reads=['ga', 'yt'], writes=['ga'])
            S.op('act', lambda: A.activation(out=gaf, in_=gaf, func=AF.Sigmoid, scale=1.5957691216), reads=['ga'], writes=['ga'])
            S.op('dve', lambda: V.tensor_tensor(out=yof, in0=gaf, in1=ytf, op=ALU.mult), reads=['ga', 'yt'], writes=['yo'])
            S.dma('sp', lambda: nc.sync.dma_start(out=out[:, r0:r0 + 128].rearrange("(gc k) n -> k gc n", k=128), in_=yo[:]), reads=['yo'], writes=['out'])
        S.finish(['out'])
        print("s5 program: instr", S.n_instr, "waits", S.n_wait)
    return nc


def s5_consts():
    f = np.arange(128)
    swap = np.zeros((128, 128), np.float32); swap[f, (f + 64) % 128] = 1.0
    sgn_c = np.where(f < 64, 1.0, -1.0).astype(np.float32)[:, None]
    sgn_r = np.broadcast_to(np.where(f < 64, -1.0, 1.0).astype(np.float32)[None, :], (128, 128)).copy()
    mrow = np.broadcast_to(np.arange(128, dtype=np.float32)[None, :], (128, 128)).copy()
    m01 = np.ones((128, 4, 128), np.float32); m01[:, :, 0] = 0.0
    return {"identf": np.eye(128, dtype=np.float32), "swapm": swap, "sgn_c": sgn_c, "sgn_r": sgn_r, "mrow": mrow,
            "mask01": m01.reshape(128, 512)}


def s5_layouts(a_re, a_im, log_dt, b_re, b_im, c_re, c_im, d, cq):
    G0 = 16 * cq
    f = np.arange(128)
    p = f % 64
    are_c = np.ascontiguousarray(a_re[G0:G0 + 16][:, p].T)
    aim_c = np.ascontiguousarray(a_im[G0:G0 + 16][:, p].T)
    ldt_c = np.ascontiguousarray(np.broadcast_to(log_dt[G0:G0 + 16][None, :], (128, 16)))
    are_r = np.ascontiguousarray(np.broadcast_to(a_re[G0:G0 + 16][:, p].reshape(1, 16 * 128), (128, 2048)))
    aim_r = np.ascontiguousarray(np.broadcast_to(a_im[G0:G0 + 16][:, p].reshape(1, 16 * 128), (128, 2048)))
    ldt_r = np.ascontiguousarray(np.broadcast_to(np.repeat(log_dt[G0:G0 + 16], 128).reshape(1, 2048), (128, 2048)))
    X1p = np.zeros((128, 16, 128), np.float32); X2p = np.zeros((128, 16, 128), np.float32)
    CcP = np.zeros((128, 16, 128), np.float32); CcSP = np.zeros((128, 16, 128), np.float32)
    for g in range(16):
        r0 = (g % 8) * 16
        br = b_re[G0 + g]; bi = b_im[G0 + g]
        X1p[r0:r0 + 16, g, 0:64] = br.T; X1p[r0:r0 + 16, g, 64:128] = bi.T
        X2p[r0:r0 + 16, g, 0:64] = bi.T; X2p[r0:r0 + 16, g, 64:128] = br.T
        cr = c_re[G0 + g]; ci = c_im[G0 + g]
        CcP[0:64, g, r0:r0 + 16] = cr.T; CcP[64:128, g, r0:r0 + 16] = ci.T
        CcSP[0:64, g, r0:r0 + 16] = ci.T; CcSP[64:128, g, r0:r0 + 16] = cr.T
    dcol = np.ascontiguousarray(d[cq * 256:(cq + 1) * 256].reshape(2, 128).T)
    return {"are_c": are_c, "aim_c": aim_c, "ldt_c": ldt_c, "are_r": are_r, "aim_r": aim_r, "ldt_r": ldt_r,
            "X1p": X1p.reshape(128, 2048), "X2p": X2p.reshape(128, 2048), "CcP": CcP.reshape(128, 2048),
            "CcSP": CcSP.reshape(128, 2048), "dcol": dcol}

import math

RMS_EPS = 1e-6


def build_nsa(NS):
    nc = bass.Bass("TRN2", target_bir_lowering=False)
    T = NS * 512
    NT = NS * 4
    D = 1024
    NM = 1024 if T >= 16384 else (T // 16 + 32)
    NMT = (NM + 127) // 128
    din = lambda n, s, d=F32: nc.dram_tensor(n, s, d, kind="ExternalInput").ap()
    x = din("x", [T, D])
    c_l = din("c_l", [128, 8])
    adaw = din("adaw", [D, 2048]); adab = din("adab", [1, 2048]); n1g = din("n1g", [1, D])
    wpf_d = din("wpf", [D, 512]); wpt_d = din("wpt", [D, 524])
    wk1_d = din("wk1", [2048, 256]); wv1_d = din("wv1", [2048, 256])
    wk2_d = din("wk2", [256, 64]); wv2_d = din("wv2", [256, 64])
    pekT_d = din("pekT", [64, 32]); pevT_d = din("pevT", [64, 32])
    cos_d = din("cos2", [64, T]); sin_d = din("sin2", [64, T])
    cosc_d = din("cosc", [64, NM]); sinc_d = din("sinc", [64, NM])
    prot_d = din("prot", [64, 64])
    identf_d = din("identf", [128, 128])
    triT_d = din("triT", [128, 128]); triTs_d = din("triTs", [128, 128])
    cmg_d = din("cmaskG", [128, 16]); cm0_d = din("cmask0", [128, 8])
    hilo_d = din("hilo", [128, 3])
    out = nc.dram_tensor("out", [T, 256], F32, kind="ExternalOutput").ap()

    es = ExitStack()
    with es:
        es.enter_context(nc.allow_low_precision("bf16 matmul operands"))
        es.enter_context(nc.allow_non_contiguous_dma("small layout loads"))
        S = Sched(nc, es)
        sb = lambda n, s, d=F32: es.enter_context(nc.sbuf_tensor("s_" + n, s, d))
        ps = lambda n, s, d=F32: es.enter_context(nc.psum_tensor("p_" + n, s, d))
        V, A, P, PE = nc.vector, nc.scalar, nc.gpsimd, nc.tensor

        def ld(dst, src, key):
            S.dma('sp', lambda: nc.sync.dma_start(out=dst, in_=src), writes=[key])

        identf = sb("identf", [128, 128]); identb = sb("identb", [128, 128], BF16)
        triT = sb("triT", [128, 128], BF16); triTs = sb("triTs", [128, 128], BF16)
        cmaskG = sb("cmaskG", [128, 16]); cmask0 = sb("cmask0", [128, 8]); hilo = sb("hilo", [128, 3])
        epsT = sb("epsT", [128, 1]); ones1 = sb("ones1", [1, 128]); kmx = sb("kmx", [1, 1]); kmax2 = sb("kmax2", [128, 1])
        prot = sb("prot", [64, 64])
        modrep = sb("modrep", [128, 2048]); gk1 = sb("gk1", [128, D])
        wpf = sb("wpf", [128, 8, 512], BF16); wpt = sb("wpt", [128, 8, 524], BF16)
        wk1 = sb("wk1", [64, 32, 256], BF16); wv1 = sb("wv1", [64, 32, 256], BF16)
        wk2 = sb("wk2", [128, 2, 64], BF16); wv2 = sb("wv2", [128, 2, 64], BF16)
        biask = sb("biask", [128, 2]); biasv = sb("biasv", [128, 2])
        ksT = sb("ksT", [65, T], BF16); kwT = sb("kwT", [65, 1024], BF16)
        vsA = sb("vsA", [128, NT, 65], BF16); vwA = sb("vwA", [128, 8, 65], BF16)
        kcmpT = sb("kcmpT", [64, NMT * 128], BF16); vcmp = sb("vcmp", [128, NMT, 64], BF16)
        kcbuf = sb("kcbuf", [64, 528], BF16); vcbuf = sb("vcbuf", [64, 528], BF16)

        psS = [ps("psS%d" % i, [128, 512]) for i in range(2)]
        psOs = ps("psOs", [128, 512]); psOw = ps("psOw", [128, 512])
        psC = ps("psC", [128, 1024])
        psT = ps("psT", [128, 1024], BF16)
        psX = ps("psX", [128, 512])

        ld(identf[:], identf_d[:, :], 'identf')
        ld(cmaskG[:], cmg_d[:, :], 'cmaskG'); ld(cmask0[:], cm0_d[:, :], 'cmask0'); ld(hilo[:], hilo_d[:, :], 'hilo')
        ld(prot[:], prot_d[:, :], 'prot')
        S.op('dve', lambda: V.tensor_copy(out=identb[:], in_=identf[:]), reads=['identf'], writes=['identb'])
        S.op('dve', lambda: V.memset(epsT[:], RMS_EPS), writes=['epsT'])
        S.op('dve', lambda: V.memset(ones1[:], 1.0), writes=['ones1'])
        S.op('dve', lambda: V.memset(kmx[:], 0.0), writes=['kmx'])
        for c0 in range(0, T, 2048):
            S.op('dve', lambda: V.memset(ksT[:, c0:min(T, c0 + 2048)], 1.0), writes=['ksT'])
        S.op('pool', lambda: P.memset(kwT[:], 1.0), writes=['kwT'])
        S.op('dve', lambda: V.memset(vsA[:], 1.0), writes=['vsA'])
        S.op('pool', lambda: P.memset(vwA[:], 1.0), writes=['vwA'])
        S.op('dve', lambda: V.memset(kcmpT[:], 0.0), writes=['kcmpT'])
        S.op('dve', lambda: V.memset(vcmp[:], 0.0), writes=['vcmp'])
        S.op('dve', lambda: V.memset(kcbuf[:], 0.0), writes=['kcbuf'])
        S.op('dve', lambda: V.memset(vcbuf[:], 0.0), writes=['vcbuf'])

        es2 = ExitStack()
        with es2:
            tb = lambda n, s, d=F32: es2.enter_context(nc.sbuf_tensor("t_" + n, s, d))
            csil = tb("csil", [128, 8]); csil_rep = tb("csil_rep", [128, 8, 128])
            stg = [tb("stg%d" % i, [128, 8, 256]) for i in range(2)]
            adab_rep = tb("adab_rep", [128, 256]); n1g_rep = tb("n1g_rep", [128, D])
            tmpf = tb("tmpf", [128, 128])
            ld(tmpf[:], triT_d[:, :], 'tmpf')
            S.op('dve', lambda: V.tensor_copy(out=triT[:], in_=tmpf[:]), reads=['tmpf'], writes=['triT'])
            ld(tmpf[:], triTs_d[:, :], 'tmpf')
            S.op('dve', lambda: V.tensor_copy(out=triTs[:], in_=tmpf[:]), reads=['tmpf'], writes=['triTs'])
            ld(csil[:], c_l[:, :], 'csil')
            ld(n1g_rep[:], n1g[0:1, :].partition_broadcast(128), 'n1g_rep')
            S.op('act', lambda: A.activation(out=csil[:], in_=csil[:], func=AF.Silu), reads=['csil'], writes=['csil'])
            S.op('dve', lambda: V.tensor_copy(out=csil_rep[:], in_=csil[:].unsqueeze(2).to_broadcast([128, 8, 128])),
                 reads=['csil'], writes=['csil_rep'])
            for j in range(8):
                st = stg[j % 2]; k_st = 'stg%d' % (j % 2)
                ld(st[:], adaw[:, j * 256:(j + 1) * 256].rearrange("(kc k) n -> k kc n", k=128), k_st)
                ld(adab_rep[:], adab[0:1, j * 256:(j + 1) * 256].partition_broadcast(128), 'adab_rep')
                for kc in range(8):
                    S.op('pe', lambda: PE.matmul(psX[:, 0:256], lhsT=csil_rep[:, kc, :], rhs=st[:, kc, :], start=(kc == 0), stop=(kc == 7)),
                         reads=['csil_rep', k_st], writes=['psX'], pe_chain=(kc > 0))
                S.op('dve', lambda: V.tensor_tensor(out=modrep[:, j * 256:(j + 1) * 256], in0=psX[:, 0:256], in1=adab_rep[:], op=ALU.add),
                     reads=['psX', 'adab_rep'], writes=['modrep'])
            sc1 = modrep[:, 1024:2048]
            S.op('dve', lambda: V.scalar_tensor_tensor(out=gk1[:], in0=sc1, scalar=1.0, in1=n1g_rep[:], op0=ALU.add, op1=ALU.mult),
                 reads=['modrep', 'n1g_rep'], writes=['gk1'])
            ci = [0]

            def load_cast(dst3, src2d, ncols, key):
                c0 = 0
                while c0 < ncols:
                    w = min(256, ncols - c0)
                    i = ci[0] % 2; ci[0] += 1
                    st = stg[i]
                    ld(st[:, :, 0:w], src2d[:, c0:c0 + w].rearrange("(kc k) n -> k kc n", k=128), 'stg%d' % i)
                    eng = 'dve' if i == 0 else 'act'
                    if i == 0:
                        S.op('dve', lambda: V.tensor_copy(out=dst3[:, :, c0:c0 + w], in_=st[:, :, 0:w]), reads=['stg%d' % i], writes=[key])
                    else:
                        S.op('act', lambda: A.copy(out=dst3[:, :, c0:c0 + w], in_=st[:, :, 0:w]), reads=['stg%d' % i], writes=[key])
                    c0 += w
            load_cast(wpf, wpf_d, 512, 'wpf')
            load_cast(wpt, wpt_d, 524, 'wpt')
            for (wdst, wsrc, key) in ((wk1, wk1_d, 'wk1'), (wv1, wv1_d, 'wv1')):
                for p0 in range(0, 32, 8):
                    i = ci[0] % 2; ci[0] += 1
                    st = stg[i]
                    ld(st[0:64, :, :], wsrc[p0 * 64:(p0 + 8) * 64, :].rearrange("(pos d) h -> d pos h", d=64), 'stg%d' % i)
                    S.op('dve', lambda: V.tensor_copy(out=wdst[:, p0:p0 + 8, :], in_=st[0:64, :, :]), reads=['stg%d' % i], writes=[key])
            for (wdst, wsrc, key) in ((wk2, wk2_d, 'wk2'), (wv2, wv2_d, 'wv2')):
                i = ci[0] % 2; ci[0] += 1
                st = stg[i]
                ld(st[:, 0:2, 0:64], wsrc[:, :].rearrange("(hc k) d -> k hc d", k=128), 'stg%d' % i)
                S.op('dve', lambda: V.tensor_copy(out=wdst[:], in_=st[:, 0:2, 0:64]), reads=['stg%d' % i], writes=[key])
            peb = tb("peb", [64, 32], BF16)
            for (pe_d, w1, bias, key) in ((pekT_d, wk1, biask, 'biask'), (pevT_d, wv1, biasv, 'biasv')):
                ld(tmpf[0:64, 0:32], pe_d[:, :], 'tmpf')
                S.op('dve', lambda: V.tensor_copy(out=peb[:], in_=tmpf[0:64, 0:32]), reads=['tmpf'], writes=['peb'])
                for hc in range(2):
                    for pos in range(32):
                        S.op('pe', lambda: PE.matmul(psX[:, hc:hc + 1], lhsT=w1[:, pos, hc * 128:(hc + 1) * 128], rhs=peb[:, pos:pos + 1],
                                                     start=(pos == 0), stop=(pos == 31)), reads=['wk1', 'wv1', 'peb'], writes=['psX'],
                             pe_chain=(pos > 0))
                S.op('dve', lambda: V.tensor_copy(out=bias[:], in_=psX[:, 0:2]), reads=['psX'], writes=[key])
            S.barrier()

        xt = [sb("xt%d" % i, [128, D]) for i in range(2)]
        junk = sb("junk", [128, D], BF16)
        ss = sb("ss", [128, 1]); rs = sb("rs", [128, 1])
        hm = sb("hm", [128, D]); hmb = sb("hmb", [128, D], BF16)
        hmT4 = sb("hmT4", [128, 8, 512], BF16)
        qtm = sb("qtm", [128, 256]); qsq = sb("qsq", [128, 4, 4])
        ksq = sb("ksq", [128, 2]); km1 = sb("km1", [128, 1]); red = sb("red", [1, 1])
        gates = sb("gates", [128, 4, 12])
        cs2 = [sb("cos%d" % i, [64, 512]) for i in range(2)]
        sn2 = [sb("sin%d" % i, [64, 512]) for i in range(2)]
        cc2 = [sb("cc%d" % i, [64, 32]) for i in range(2)]
        sc2_ = [sb("sc%d" % i, [64, 32]) for i in range(2)]
        xq = sb("xq", [64, 512]); t1 = sb("t1", [64, 512]); t2 = sb("t2", [64, 512])
        qTa = sb("qTa", [65, 4, 512], BF16)
        hid = sb("hid", [128, 2, 32]); hga = sb("hga", [128, 2, 32]); ghk = sb("ghk", [128, 2, 32], BF16); ghv = sb("ghv", [128, 2, 32], BF16); ghv2 = sb("ghv2", [128, 2, 64], BF16)
        kcx = sb("kcx", [64, 32]); kt1 = sb("kt1", [64, 32]); kt2 = sb("kt2", [64, 32])
        cq = sb("cq", [128, 4]); negc = sb("negc", [128, 4], BF16)
        eT = [sb("eT%d" % i, [128, 512], BF16) for i in range(2)]
        pT = [sb("pT%d" % i, [128, 512], BF16) for i in range(2)]
        mfull = sb("mfull", [128, 512], BF16); maskT4 = sb("maskT4", [128, 4, 128], BF16)
        pc = sb("pc", [128, 1024]); pcb = sb("pcb", [128, 1024], BF16); pcT = sb("pcT", [128, 8, 128], BF16)
        pgrp = sb("pgrp", [128, 1032])
        mx = sb("mx", [128, 1]); mx2 = sb("mx2", [128, 2]); sm = sb("sm", [128, 4]); rinv = sb("rinv", [128, 4])
        imp = sb("imp", [128, 256]); impw = sb("impw", [128, 256]); impk = sb("impk", [128, 256]); sel = sb("sel", [128, 256])
        m8a = sb("m8a", [128, 8]); m8b = sb("m8b", [128, 8]); tau = sb("tau", [128, 1])
        oTs = sb("oTs", [65, 512]); oTw = sb("oTw", [65, 512])
        fac = sb("fac", [128, 3, 4]); ot = sb("ot", [128, 4, 64])
        sh1 = modrep[:, 0:1024]
        S.op('dve', lambda: V.memset(impw[:], -1.0), writes=['impw'])
        S.op('dve', lambda: V.memset(pgrp[:], 0.0), writes=['pgrp'])
        S.op('dve', lambda: V.memset(sel[:], 0.0), writes=['sel'])
        S.op('dve', lambda: V.memset(ghv2[:], 0.0), writes=['ghv2'])

        def gelu_tanh(dst, src, tmp, kd, ks_, kt):
            S.op('dve', lambda: V.tensor_tensor(out=tmp, in0=src, in1=src, op=ALU.mult), reads=[ks_], writes=[kt])
            S.op('dve', lambda: V.tensor_scalar(out=tmp, in0=tmp, scalar1=0.044715, scalar2=1.0, op0=ALU.mult, op1=ALU.add), reads=[kt], writes=[kt])
            S.op('dve', lambda: V.tensor_tensor(out=tmp, in0=tmp, in1=src, op=ALU.mult), reads=[kt, ks_], writes=[kt])
            S.op('act', lambda: A.activation(out=tmp, in_=tmp, func=AF.Sigmoid, scale=1.5957691216), reads=[kt], writes=[kt])
            S.op('dve', lambda: V.tensor_tensor(out=dst, in0=tmp, in1=src, op=ALU.mult), reads=[kt, ks_], writes=[kd])

        def rope(dst, src_ps, kps, cosap, sinap, kcos, scale, kdst):
            n = src_ps.shape[-1]
            S.op('act', lambda: A.copy(out=xq[:, 0:n], in_=src_ps), reads=[kps], writes=['xq'])
            S.op('pe', lambda: PE.matmul(psS[1][0:64, 0:n], lhsT=prot[:], rhs=xq[:, 0:n], start=True, stop=True),
                 reads=['prot', 'xq'], writes=['psS1'])
            S.op('dve', lambda: V.scalar_tensor_tensor(out=t1[:, 0:n], in0=xq[:, 0:n], scalar=scale, in1=cosap, op0=ALU.mult, op1=ALU.mult),
                 reads=['xq', kcos], writes=['t1'])
            S.op('dve', lambda: V.scalar_tensor_tensor(out=t2[:, 0:n], in0=psS[1][0:64, 0:n], scalar=scale, in1=sinap, op0=ALU.mult, op1=ALU.mult),
                 reads=['psS1', kcos], writes=['t2'])
            a1 = t1[:, 0:n]; a2 = t2[:, 0:n]
            if len(dst.shape) == 3:
                a1 = a1.rearrange("p (a b) -> p a b", b=dst.shape[2]); a2 = a2.rearrange("p (a b) -> p a b", b=dst.shape[2])
            S.op('dve', lambda: V.tensor_tensor(out=dst, in0=a1, in1=a2, op=ALU.add), reads=['t1', 't2'], writes=[kdst])

        for s in range(NS):
            cb = cs2[s % 2]; snb = sn2[s % 2]; kcos = 'cos%d' % (s % 2)
            ld(cb[:], cos_d[:, s * 512:(s + 1) * 512], kcos)
            ld(snb[:], sin_d[:, s * 512:(s + 1) * 512], kcos)
            ccb = cc2[s % 2]; scb = sc2_[s % 2]; kcc = 'cc%d' % (s % 2)
            if 32 * s + 32 <= NM:
                ld(ccb[:], cosc_d[:, 32 * s:32 * s + 32], kcc)
                ld(scb[:], sinc_d[:, 32 * s:32 * s + 32], kcc)
            for i in range(4):
                ti = s * 4 + i
                xb = xt[ti % 2]; kx = 'xt%d' % (ti % 2)
                r0 = ti * 128
                ld(xb[:], x[r0:r0 + 128, :], kx)
                S.op('act', lambda: A.activation(out=junk[:], in_=xb[:], func=AF.Square, accum_out=ss[:]), reads=[kx], writes=['junk', 'ss'])
                S.op('act', lambda: A.activation(out=rs[:], in_=ss[:], func=AF.Sqrt, bias=epsT[:], scale=1.0 / D), reads=['ss', 'epsT'], writes=['rs'])
                S.op('dve', lambda: V.reciprocal(out=rs[:], in_=rs[:]), reads=['rs'], writes=['rs'])
                S.op('dve', lambda: V.scalar_tensor_tensor(out=hm[:], in0=xb[:], scalar=rs[:], in1=gk1[:], op0=ALU.mult, op1=ALU.mult),
                     reads=[kx, 'rs', 'gk1'], writes=['hm'])
                S.op('dve', lambda: V.tensor_tensor(out=hmb[:], in0=hm[:], in1=sh1, op=ALU.add), reads=['hm', 'modrep'], writes=['hmb'])
                for kc in range(8):
                    S.op('pe', lambda: PE.transpose(out=psT[:, kc * 128:(kc + 1) * 128], in_=hmb[:, kc * 128:(kc + 1) * 128], identity=identb[:]),
                         reads=['hmb', 'identb'], writes=['psT'], pe_chain=(kc > 0))
                S.op('act', lambda: A.copy(out=hmT4[:, :, i * 128:(i + 1) * 128], in_=psT[:].rearrange("p (a b) -> p a b", b=128)),
                     reads=['psT'], writes=['hmT4'])
                for kc in range(8):
                    S.op('pe', lambda: PE.matmul(psS[0][:, 0:384], lhsT=hmT4[:, kc, i * 128:(i + 1) * 128], rhs=wpt[:, kc, 0:384],
                                                 start=(kc == 0), stop=(kc == 7)), reads=['hmT4', 'wpt'], writes=['psS0'], pe_chain=(kc > 0))
                for kc in range(8):
                    S.op('pe', lambda: PE.matmul(psX[:, 0:140], lhsT=hmT4[:, kc, i * 128:(i + 1) * 128], rhs=wpt[:, kc, 384:524],
                                                 start=(kc == 0), stop=(kc == 7)), reads=['hmT4', 'wpt'], writes=['psX'], pe_chain=(kc > 0))
                S.op('act', lambda: A.copy(out=qtm[:], in_=psS[0][:, 0:256]), reads=['psS0'], writes=['qtm'])
                S.op('act', lambda: A.activation(out=junk[:, 0:64], in_=psS[0][:, 256:320], func=AF.Square, accum_out=ksq[:, 0:1]),
                     reads=['psS0'], writes=['junk', 'ksq'])
                S.op('act', lambda: A.activation(out=junk[:, 0:64], in_=psS[0][:, 320:384], func=AF.Square, accum_out=ksq[:, 1:2]),
                     reads=['psS0'], writes=['junk', 'ksq'])
                S.op('dve', lambda: V.tensor_tensor(out=qtm[:], in0=qtm[:], in1=qtm[:], op=ALU.mult), reads=['qtm'], writes=['qtm'])
                S.op('dve', lambda: V.tensor_reduce(out=qsq[:, i, :], in_=qtm[:].rearrange("p (r d) -> p r d", d=64), axis=AX.X, op=ALU.add),
                     reads=['qtm'], writes=['qsq'])
                S.op('dve', lambda: V.tensor_tensor(out=km1[:], in0=ksq[:, 0:1], in1=ksq[:, 1:2], op=ALU.max), reads=['ksq'], writes=['km1'])
                S.op('pe', lambda: PE.transpose(out=psX[0:1, 256:384], in_=km1[:], identity=identf[:]), reads=['km1', 'identf'], writes=['psXk'])
                S.op('dve', lambda: V.tensor_reduce(out=red[:], in_=psX[0:1, 256:384], axis=AX.X, op=ALU.max), reads=['psXk'], writes=['red'])
                S.op('dve', lambda: V.tensor_tensor(out=kmx[:], in0=kmx[:], in1=red[:], op=ALU.max), reads=['kmx', 'red'], writes=['kmx'])
                S.op('pe', lambda: PE.matmul(psX[:, 384:385], lhsT=ones1[:], rhs=kmx[:], start=True, stop=True), reads=['ones1', 'kmx'], writes=['psXk'])
                S.op('dve', lambda: V.tensor_copy(out=kmax2[:], in_=psX[:, 384:385]), reads=['psXk'], writes=['kmax2'])
                S.op('act', lambda: A.copy(out=vsA[:, ti, 0:64], in_=psX[:, 0:64]), reads=['psX'], writes=['vsA'])
                S.op('act', lambda: A.copy(out=vwA[:, ti % 8, 0:64], in_=psX[:, 64:128]), reads=['psX'], writes=['vwA'])
                S.op('act', lambda: A.activation(out=gates[:, i, :], in_=psX[:, 128:140], func=AF.Sigmoid), reads=['psX'], writes=['gates'])
            for blk in range(8):
                pso = psC[0:64, (blk % 2) * 512:(blk % 2) * 512 + 512]
                kps = 'psC'
                for kc in range(8):
                    S.op('pe', lambda: PE.matmul(pso, lhsT=wpf[:, kc, blk * 64:(blk + 1) * 64], rhs=hmT4[:, kc, :], start=(kc == 0), stop=(kc == 7)),
                         reads=['wpf', 'hmT4'], writes=[kps], pe_chain=(kc > 0))
                if blk < 4:
                    dst = qTa[0:64, :, blk * 128:(blk + 1) * 128]
                    rope(dst, pso, kps, cb[:], snb[:], kcos, 0.125, 'qTa')
                elif blk == 4:
                    S.op('act', lambda: A.copy(out=kcbuf[:, 16:528], in_=pso), reads=[kps], writes=['kcbuf'])
                elif blk == 5:
                    S.op('act', lambda: A.copy(out=vcbuf[:, 16:528], in_=pso), reads=[kps], writes=['vcbuf'])
                elif blk == 6:
                    rope(ksT[0:64, s * 512:(s + 1) * 512], pso, kps, cb[:], snb[:], kcos, 1.0, 'ksT')
                else:
                    rope(kwT[0:64, (s % 2) * 512:(s % 2) * 512 + 512], pso, kps, cb[:], snb[:], kcos, 1.0, 'kwT')
            m0 = 32 * s
            for (buf, kbuf, w1, kw1, bias, gh, kgh) in ((kcbuf, 'kcbuf', wk1, 'wk1', biask, ghk, 'ghk'), (vcbuf, 'vcbuf', wv1, 'wv1', biasv, ghv, 'ghv')):
                bview = buf[:, 0:512].rearrange("d (n f) -> d n f", f=16)
                for hc in range(2):
                    for pos in range(32):
                        if pos < 16:
                            rhs = bview[:, :, pos]
                        else:
                            rhs = buf[:, 16:528].rearrange("d (n f) -> d n f", f=16)[:, :, pos - 16]
                        S.op('pe', lambda: PE.matmul(psX[:, hc * 32:(hc + 1) * 32], lhsT=w1[:, pos, hc * 128:(hc + 1) * 128], rhs=rhs,
                                                     start=(pos == 0), stop=(pos == 31)), reads=[kw1, kbuf], writes=['psX'], pe_chain=(pos > 0))
                for hc in range(2):
                    S.op('dve', lambda: V.tensor_scalar(out=hid[:, hc, :], in0=psX[:, hc * 32:(hc + 1) * 32], scalar1=bias[:, hc:hc + 1], scalar2=None,
                                                        op0=ALU.add), reads=['psX', 'biask', 'biasv'], writes=['hid'])
                gelu_tanh(gh[:].rearrange("p a b -> p (a b)"), hid[:].rearrange("p a b -> p (a b)"), hga[:].rearrange("p a b -> p (a b)"),
                          kgh, 'hid', 'hga')
                S.op('dve', lambda: V.tensor_copy(out=buf[:, 0:16], in_=buf[:, 512:528]), reads=[kbuf], writes=[kbuf])
            for hc in range(2):
                S.op('pe', lambda: PE.matmul(psC[0:64, 0:32], lhsT=wk2[:, hc, :], rhs=ghk[:, hc, :], start=(hc == 0), stop=(hc == 1)),
                     reads=['wk2', 'ghk'], writes=['psC'], pe_chain=(hc > 0))
            if m0 + 32 <= NM:
                rope(kcmpT[:, m0:m0 + 32], psC[0:64, 0:32], 'psC', ccb[:], scb[:], kcc, 1.0, 'kcmpT')
                mt, mo = m0 // 128, m0 % 128
                S.op('dve', lambda: V.tensor_copy(out=ghv2[:, :, 32:64], in_=ghv[:]), reads=['ghv'], writes=['ghv2'])
                for hc in range(2):
                    if mo < 96:
                        S.op('pe', lambda: PE.matmul(psC[mo:mo + 32, 512:576], lhsT=ghv2[:, hc, 32:64], rhs=wv2[:, hc, :], start=(hc == 0), stop=(hc == 1)),
                             reads=['wv2', 'ghv2'], writes=['psC'], pe_chain=(hc > 0))
                    else:
                        S.op('pe', lambda: PE.matmul(psC[64:128, 512:576], lhsT=ghv2[:, hc, 0:64], rhs=wv2[:, hc, :], start=(hc == 0), stop=(hc == 1)),
                             reads=['wv2', 'ghv2'], writes=['psC'], pe_chain=(hc > 0))
                S.op('act', lambda: A.copy(out=vcmp[mo:mo + 32, mt, :], in_=psC[mo:mo + 32, 512:576]), reads=['psC'], writes=['vcmp'])

            for i in range(4):
                qi = s * 4 + i
                qblk = qTa[:, i, :]
                import os
                if qi < int(os.environ.get('NSA_QIMIN', '0')):
                    continue
                S.op('dve', lambda: V.tensor_scalar(out=cq[:], in0=qsq[:, i, :], scalar1=kmax2[:, 0:1], scalar2=1.0 / 64, op0=ALU.mult, op1=ALU.mult),
                     reads=['qsq', 'kmax2'], writes=['cq'])
                S.op('act', lambda: A.activation(out=cq[:], in_=cq[:], func=AF.Sqrt), reads=['cq'], writes=['cq'])
                S.op('dve', lambda: V.tensor_scalar(out=negc[:], in0=cq[:], scalar1=-1.0, scalar2=None, op0=ALU.mult), reads=['cq'], writes=['negc'])
                for r in range(4):
                    S.op('pe', lambda: PE.transpose(out=psT[64:65, r * 128:(r + 1) * 128], in_=negc[:, r:r + 1], identity=identb[:]),
                         reads=['negc', 'identb'], writes=['psT'], pe_chain=(r > 0))
                S.op('act', lambda: A.copy(out=qTa[64:65, i, :], in_=psT[64:65, 0:512]), reads=['psT'], writes=['qTa'])

                W = 8 * qi + 8
                import os
                if os.environ.get('NSA_CAPW'):
                    W = min(W, int(os.environ['NSA_CAPW']))
                nt = (W + 127) // 128
                for r in range(4):
                    for c0 in range(0, W, 512):
                        w = min(512, W - c0)
                        S.op('pe', lambda: PE.matmul(psC[:, c0:c0 + w], lhsT=qTa[0:64, i, r * 128:(r + 1) * 128], rhs=kcmpT[:, c0:c0 + w],
                                                     start=True, stop=True), reads=['qTa', 'kcmpT'], writes=['psC'])
                    chunks = [(c0, min(512, W - c0)) for c0 in range(0, W, 512)]
                    for ci_, (c0, w) in enumerate(chunks):
                        S.op('dve', lambda: V.tensor_reduce(out=mx2[:, ci_:ci_ + 1], in_=psC[:, c0:c0 + w], axis=AX.X, op=ALU.max), reads=['psC'], writes=['mx2'])
                    if len(chunks) == 2:
                        S.op('dve', lambda: V.tensor_tensor(out=mx2[:, 0:1], in0=mx2[:, 0:1], in1=mx2[:, 1:2], op=ALU.max), reads=['mx2'], writes=['mx2'])
                    S.op('dve', lambda: V.tensor_scalar(out=mx[:], in0=mx2[:, 0:1], scalar1=-1.0, scalar2=None, op0=ALU.mult), reads=['mx2'], writes=['mx'])
                    for (c0, w) in chunks:
                        S.op('act', lambda: A.activation(out=pc[:, c0:c0 + w], in_=psC[:, c0:c0 + w], func=AF.Exp, bias=mx[:], scale=1.0),
                             reads=['psC', 'mx'], writes=['pc'])
                    if qi == 0:
                        S.op('dve', lambda: V.tensor_tensor(out=pc[:, 0:8], in0=pc[:, 0:8], in1=cmask0[:], op=ALU.mult), reads=['pc', 'cmask0'], writes=['pc'])
                    else:
                        S.op('dve', lambda: V.tensor_tensor(out=pc[:, W - 16:W], in0=pc[:, W - 16:W], in1=cmaskG[:], op=ALU.mult),
                             reads=['pc', 'cmaskG'], writes=['pc'])
                        S.op('dve', lambda: V.memset(pc[:, 0:1], 0.0), reads=[], writes=['pc'])
                    S.op('dve', lambda: V.tensor_reduce(out=sm[:, r:r + 1], in_=pc[:, 0:W], axis=AX.X, op=ALU.add), reads=['pc'], writes=['sm'])
                    S.op('dve', lambda: V.tensor_scalar(out=rinv[:, r:r + 1], in0=sm[:, r:r + 1], scalar1=1e-30, scalar2=None, op0=ALU.add),
                         reads=['sm'], writes=['rinv'])
                    S.op('dve', lambda: V.reciprocal(out=rinv[:, r:r + 1], in_=rinv[:, r:r + 1]), reads=['rinv'], writes=['rinv'])
                    if r == 0:
                        S.op('dve', lambda: V.tensor_scalar(out=pgrp[:, 0:W], in0=pc[:, 0:W], scalar1=rinv[:, 0:1], scalar2=None, op0=ALU.mult),
                             reads=['pc', 'rinv'], writes=['pgrp'])
                    else:
                        S.op('dve', lambda: V.scalar_tensor_tensor(out=pgrp[:, 0:W], in0=pc[:, 0:W], scalar=rinv[:, r:r + 1], in1=pgrp[:, 0:W],
                                                                   op0=ALU.mult, op1=ALU.add), reads=['pc', 'rinv', 'pgrp'], writes=['pgrp'])
                    S.op('act', lambda: A.copy(out=pcb[:, 0:W], in_=pc[:, 0:W]), reads=['pc'], writes=['pcb'])
                    for j in range(nt):
                        wj = min(128, W - j * 128)
                        S.op('pe', lambda: PE.transpose(out=psT[0:wj, j * 128:(j + 1) * 128], in_=pcb[:, j * 128:j * 128 + wj], identity=identb[:]),
                             reads=['pcb', 'identb'], writes=['psT'], pe_chain=(j > 0))
                    for j in range(nt):
                        wj = min(128, W - j * 128)
                        S.op('act', lambda: A.copy(out=pcT[0:wj, j, :], in_=psT[0:wj, j * 128:(j + 1) * 128]), reads=['psT'], writes=['pcT'])
                    for j in range(nt):
                        wj = min(128, W - j * 128)
                        S.op('pe', lambda: PE.matmul(psX[:, r * 64:(r + 1) * 64], lhsT=pcT[0:wj, j, :], rhs=vcmp[0:wj, j, :],
                                                     start=(j == 0), stop=(j == nt - 1)), reads=['pcT', 'vcmp'], writes=['psX'], pe_chain=(j > 0))
                if qi >= 1:
                    Jn = 2 * qi
                    S.op('dve', lambda: V.tensor_reduce(out=imp[:, 0:Jn], in_=pgrp[:, 0:4 * Jn].rearrange("p (j f) -> p j f", f=4), axis=AX.X, op=ALU.add),
                         reads=['pgrp'], writes=['imp'])
                    S.op('dve', lambda: V.tensor_tensor(out=imp[:, 0:Jn], in0=imp[:, 0:Jn],
                                                        in1=pgrp[:, 4:4 + 4 * Jn].rearrange("p (j f) -> p j f", f=4)[:, :, 0], op=ALU.add),
                         reads=['imp', 'pgrp'], writes=['imp'])
                    if Jn - 1 > 1:
                        S.op('dve', lambda: V.tensor_copy(out=impw[:, 1:Jn - 1], in_=imp[:, 1:Jn - 1]), reads=['imp'], writes=['impw'])
                    S.op('dve', lambda: V.tensor_scalar(out=impw[:, Jn - 1:Jn], in0=imp[:, Jn - 1:Jn], scalar1=hilo[:, 0:1], scalar2=hilo[:, 1:2],
                                                        op0=ALU.mult, op1=ALU.add), reads=['imp', 'hilo'], writes=['impw'])
                    Wj = max(Jn, 16)
                    S.op('dve', lambda: V.max(out=m8a[:], in_=impw[:, 0:Wj]), reads=['impw'], writes=['m8a'])
                    S.op('dve', lambda: V.match_replace(out=impk[:, 0:Wj], in_to_replace=m8a[:], in_values=impw[:, 0:Wj], imm_value=-1e30),
                         reads=['impw', 'm8a'], writes=['impk'])
                    S.op('dve', lambda: V.max(out=m8b[:], in_=impk[:, 0:Wj]), reads=['impk'], writes=['m8b'])
                    S.op('dve', lambda: V.tensor_scalar(out=tau[:], in0=m8b[:, 4:5], scalar1=-0.5, scalar2=None, op0=ALU.max), reads=['m8b'], writes=['tau'])
                    S.op('dve', lambda: V.tensor_scalar(out=sel[:, 0:Jn], in0=impw[:, 0:Jn], scalar1=tau[:, 0:1], scalar2=None, op0=ALU.is_ge),
                         reads=['impw', 'tau'], writes=['sel'])
                    S.op('dve', lambda: V.memset(sel[:, 0:1], 1.0), reads=[], writes=['sel'])
                    S.op('dve', lambda: V.tensor_tensor(out=sel[:, Jn - 1:Jn], in0=sel[:, Jn - 1:Jn], in1=hilo[:, 2:3], op=ALU.max),
                         reads=['sel', 'hilo'], writes=['sel'])
                cnt = [0]
                for kt0 in range(0, qi + 1, 4):
                    kts = list(range(kt0, min(kt0 + 4, qi + 1)))
                    nsel = [k for k in kts if k < qi]
                    if nsel:
                        nb = len(nsel)
                        S.op('dve', lambda: V.tensor_copy(out=mfull[:, 0:nb * 128].rearrange("p (j f) -> p j f", f=64),
                                                          in_=sel[:, 2 * kt0:2 * kt0 + 2 * nb].unsqueeze(2).to_broadcast([128, 2 * nb, 64])),
                             reads=['sel'], writes=['mfull'])
                        for k in range(nb):
                            S.op('pe', lambda: PE.transpose(out=psT[:, k * 128:(k + 1) * 128], in_=mfull[:, k * 128:(k + 1) * 128], identity=identb[:]),
                                 reads=['mfull', 'identb'], writes=['psT'], pe_chain=(k > 0))
                        S.op('act', lambda: A.copy(out=maskT4[:, 0:nb, :].rearrange("p a b -> p (a b)"), in_=psT[:, 0:nb * 128]),
                             reads=['psT'], writes=['maskT4'])
                    for k, kt in enumerate(kts):
                        b2 = cnt[0] % 2; cnt[0] += 1
                        S.op('pe', lambda: PE.matmul(psS[b2][:], lhsT=ksT[:, kt * 128:(kt + 1) * 128], rhs=qblk, start=True, stop=True),
                             reads=['ksT', 'qTa'], writes=['psS%d' % b2])
                        S.op('act', lambda: A.activation(out=eT[b2][:], in_=psS[b2][:], func=AF.Exp), reads=['psS%d' % b2], writes=['eT%d' % b2])
                        msk = triT[:] if kt == qi else maskT4[:, k, :]
                        S.op('dve', lambda: V.tensor_tensor(out=pT[b2][:].rearrange("p (r q) -> p r q", q=128),
                                                            in0=eT[b2][:].rearrange("p (r q) -> p r q", q=128),
                                                            in1=msk.unsqueeze(1).to_broadcast([128, 4, 128]), op=ALU.mult),
                             reads=['eT%d' % b2, 'maskT4', 'triT'], writes=['pT%d' % b2])
                        S.op('pe', lambda: PE.matmul(psOs[0:65, :], lhsT=vsA[:, kt, :], rhs=pT[b2][:], start=(kt == 0), stop=(kt == qi)),
                             reads=['vsA', 'pT%d' % b2], writes=['psOs'], pe_chain=(kt > 0))
                wts = [k for k in range(qi - 4, qi + 1) if k >= 0]
                for kt in wts:
                    b2 = cnt[0] % 2; cnt[0] += 1
                    S.op('pe', lambda: PE.matmul(psS[b2][:], lhsT=kwT[:, (kt % 8) * 128:(kt % 8) * 128 + 128], rhs=qblk, start=True, stop=True),
                         reads=['kwT', 'qTa'], writes=['psS%d' % b2])
                    dlt = qi - kt
                    if dlt in (0, 4):
                        S.op('act', lambda: A.activation(out=eT[b2][:], in_=psS[b2][:], func=AF.Exp), reads=['psS%d' % b2], writes=['eT%d' % b2])
                        msk = triT[:] if dlt == 0 else triTs[:]
                        S.op('dve', lambda: V.tensor_tensor(out=pT[b2][:].rearrange("p (r q) -> p r q", q=128),
                                                            in0=eT[b2][:].rearrange("p (r q) -> p r q", q=128),
                                                            in1=msk.unsqueeze(1).to_broadcast([128, 4, 128]), op=ALU.mult),
                             reads=['eT%d' % b2, 'triT', 'triTs'], writes=['pT%d' % b2])
                    else:
                        S.op('act', lambda: A.activation(out=pT[b2][:], in_=psS[b2][:], func=AF.Exp), reads=['psS%d' % b2], writes=['pT%d' % b2])
                    S.op('pe', lambda: PE.matmul(psOw[0:65, :], lhsT=vwA[:, kt % 8, :], rhs=pT[b2][:], start=(kt == wts[0]), stop=(kt == qi)),
                         reads=['vwA', 'pT%d' % b2], writes=['psOw'], pe_chain=(kt > wts[0]))
                S.op('act', lambda: A.copy(out=oTs[:], in_=psOs[0:65, :]), reads=['psOs'], writes=['oTs'])
                S.op('act', lambda: A.copy(out=oTw[:], in_=psOw[0:65, :]), reads=['psOw'], writes=['oTw'])
                for r in range(4):
                    S.op('pe', lambda: PE.transpose(out=psC[:, r * 65:(r + 1) * 65], in_=oTs[:, r * 128:(r + 1) * 128], identity=identf[0:65, 0:65]),
                         reads=['oTs', 'identf'], writes=['psC'], pe_chain=(r > 0))
                for r in range(4):
                    S.op('pe', lambda: PE.transpose(out=psC[:, 512 + r * 65:512 + (r + 1) * 65], in_=oTw[:, r * 128:(r + 1) * 128], identity=identf[0:65, 0:65]),
                         reads=['oTw', 'identf'], writes=['psC'], pe_chain=True)
                g3 = gates[:, i, :].rearrange("p (r k) -> p r k", k=3)
                osum = psC[:, 0:260].rearrange("p (r e) -> p r e", e=65)
                wsum = psC[:, 512:772].rearrange("p (r e) -> p r e", e=65)
                S.op('dve', lambda: V.tensor_tensor(out=fac[:, 0, :], in0=g3[:, :, 0], in1=rinv[:], op=ALU.mult), reads=['gates', 'rinv'], writes=['fac'])
                S.op('dve', lambda: V.reciprocal(out=fac[:, 1, :], in_=osum[:, :, 64]), reads=['psC'], writes=['fac'])
                S.op('dve', lambda: V.reciprocal(out=fac[:, 2, :], in_=wsum[:, :, 64]), reads=['psC'], writes=['fac'])
                S.op('dve', lambda: V.tensor_tensor(out=fac[:, 1, :], in0=fac[:, 1, :], in1=g3[:, :, 1], op=ALU.mult), reads=['fac', 'gates'], writes=['fac'])
                S.op('dve', lambda: V.tensor_tensor(out=fac[:, 2, :], in0=fac[:, 2, :], in1=g3[:, :, 2], op=ALU.mult), reads=['fac', 'gates'], writes=['fac'])
                for r in range(4):
                    S.op('dve', lambda: V.tensor_scalar(out=ot[:, r, :], in0=psX[:, r * 64:(r + 1) * 64], scalar1=fac[:, 0, r:r + 1], scalar2=None, op0=ALU.mult),
                         reads=['psX', 'fac'], writes=['ot'])
                    S.op('dve', lambda: V.scalar_tensor_tensor(out=ot[:, r, :], in0=osum[:, r, 0:64], scalar=fac[:, 1, r:r + 1], in1=ot[:, r, :],
                                                               op0=ALU.mult, op1=ALU.add), reads=['psC', 'fac', 'ot'], writes=['ot'])
                    S.op('dve', lambda: V.scalar_tensor_tensor(out=ot[:, r, :], in0=wsum[:, r, 0:64], scalar=fac[:, 2, r:r + 1], in1=ot[:, r, :],
                                                               op0=ALU.mult, op1=ALU.add), reads=['psC', 'fac', 'ot'], writes=['ot'])
                S.dma('sp', lambda: nc.sync.dma_start(out=out[qi * 128:(qi + 1) * 128, :], in_=ot[:].rearrange("p r d -> p (r d)")),
                      reads=['ot'], writes=['out'])
        S.finish(['out'])
        print("nsa program: instr", S.n_instr, "waits", S.n_wait)
    return nc


def nsa_consts(T):
    NM = 1024 if T >= 16384 else (T // 16 + 32)
    half = 32
    freqs = (10000.0 ** (-np.arange(half, dtype=np.float32) / half)).astype(np.float32)
    pos = np.arange(T, dtype=np.float32)
    ang = pos[None, :] * freqs[:, None]
    cos2 = np.concatenate([np.cos(ang), np.cos(ang)], 0).astype(np.float32)
    sin2 = np.concatenate([np.sin(ang), np.sin(ang)], 0).astype(np.float32)
    m = np.arange(NM, dtype=np.float32)
    cend = (m - 1) * 16 + 31
    angc = cend[None, :] * freqs[:, None]
    cosc = np.concatenate([np.cos(angc), np.cos(angc)], 0).astype(np.float32)
    sinc = np.concatenate([np.sin(angc), np.sin(angc)], 0).astype(np.float32)
    prot = np.zeros((64, 64), np.float32)
    for d in range(32):
        prot[d + 32, d] = -1.0
        prot[d, d + 32] = 1.0
    l = np.arange(128)
    triT = (l[:, None] <= l[None, :]).astype(np.float32)
    triTs = (l[:, None] > l[None, :]).astype(np.float32)
    fl = np.floor((l - 15) / 16.0)
    j = np.arange(16)
    cmaskG = ((j[None, :] - 8) <= fl[:, None]).astype(np.float32)
    j8 = np.arange(8)
    cmask0 = ((j8[None, :] >= 1) & (j8[None, :] <= fl[:, None])).astype(np.float32)
    hi = (l >= 64).astype(np.float32)
    hilo = np.stack([hi, hi - 1.0, 1.0 - hi], 1).astype(np.float32)
    return {"cos2": cos2, "sin2": sin2, "cosc": cosc, "sinc": sinc, "prot": prot, "identf": np.eye(128, dtype=np.float32),
            "triT": triT, "triTs": triTs, "cmaskG": cmaskG, "cmask0": cmask0, "hilo": hilo}


def nsa_weights(w_proj, g):
    q = w_proj[:, 256 * g:256 * g + 256]
    def blk(i):
        return w_proj[:, 1024 + 256 * i + 64 * g:1024 + 256 * i + 64 * g + 64]
    kc, vc, ks, vs, kw, vw = [blk(i) for i in range(6)]
    gl = w_proj[:, 2560 + 12 * g:2560 + 12 * g + 12]
    wpf = np.ascontiguousarray(np.concatenate([q, kc, vc, ks, kw], 1))
    wpt = np.ascontiguousarray(np.concatenate([q, ks, kw, vs, vw, gl], 1))
    return wpf, wpt

from concourse.bass_utils import run_bass_kernel_spmd

N_CORES = 8
SEQ = 16384


def _c_l(c, b):
    return np.ascontiguousarray(np.asarray(c[b], np.float32).reshape(8, 128).T)


def kernel(x, c, norm1_g, norm2_g, ada_w, ada_b, s5_w_in, s5_a_re, s5_a_im, s5_log_dt,
           s5_b_re, s5_b_im, s5_c_re, s5_c_im, s5_d, s5_w_glu, nsa_w_proj, nsa_pe_k, nsa_pe_v,
           nsa_wk1, nsa_wk2, nsa_wv1, nsa_wv2, nsa_w_o, peer_w_q, peer_sub_keys, peer_u, peer_v,
           final_g):
    f = lambda a: np.ascontiguousarray(np.asarray(a, np.float32))
    x = f(x); c = f(c); ada_w = f(ada_w); ada_b = f(ada_b)
    norm1_g = f(norm1_g); norm2_g = f(norm2_g); final_g = f(final_g)
    peer_w_q = f(peer_w_q); peer_sub_keys = f(peer_sub_keys); peer_u = f(peer_u); peer_v = f(peer_v)
    cores = list(range(N_CORES))
    nc1 = build_s5(SEQ // 128)
    s5c = s5_consts()
    maps = []
    for k in cores:
        b, cq = k // 4, k % 4
        m = {"x": x[b], "c_l": _c_l(c, b), "adaw": f(ada_w[0][:, :2048]), "adab": f(ada_b[0][None, :2048]),
             "n1g": f(norm1_g[0][None]), "win": f(np.asarray(s5_w_in[0])[:, cq * 256:(cq + 1) * 256])}
        m.update(s5c)
        m.update(s5_layouts(f(s5_a_re[0]), f(s5_a_im[0]), f(s5_log_dt[0]), f(s5_b_re[0]), f(s5_b_im[0]),
                            f(s5_c_re[0]), f(s5_c_im[0]), f(s5_d[0]), cq))
        maps.append(m)
    r1 = run_bass_kernel_spmd(nc1, maps, core_ids=cores)
    yactT = [r1.results[k]["out"] for k in cores]
    del maps

    def tok_launch(li, mode, final, xin, mixT_of, wmix):
        nct = build_tok(SEQ // 4 // 128, mode, final)
        tc = tok_consts()
        maps = []
        for k in cores:
            b, q4 = k // 4, k % 4
            sl = slice(q4 * 4096, (q4 + 1) * 4096)
            m = {"x": f(xin[b, sl]), "mixT": mixT_of(b, sl), "c_l": _c_l(c, b),
                 "adaw": f(ada_w[li][:, 2048:]), "adab": f(ada_b[li][None, 2048:]),
                 "n2g": f(norm2_g[li][None]), "fing": f(final_g[None]), "wmix": wmix, "wq": peer_w_q[li],
                 "sk": f(peer_sub_keys[li].reshape(16, 128, 128)), "u_tab": peer_u[li], "v_tab": peer_v[li]}
            m.update(tc)
            maps.append(m)
        r = run_bass_kernel_spmd(nct, maps, core_ids=cores)
        xo = np.empty((2, SEQ, 1024), np.float32)
        for k in cores:
            b, q4 = k // 4, k % 4
            xo[b, q4 * 4096:(q4 + 1) * 4096] = r.results[k]["out"]
        return xo

    def mix0(b, sl):
        return f(np.concatenate([yactT[b * 4 + cq][:, sl] for cq in range(4)], axis=0))
    x2 = tok_launch(0, 'glu', False, x, mix0, f(s5_w_glu[0]))
    del yactT
    nc3 = build_nsa(SEQ // 512)
    nsc = nsa_consts(SEQ)
    maps = []
    for k in cores:
        b, g = k // 4, k % 4
        wpf, wpt = nsa_weights(f(nsa_w_proj[0]), g)
        m = {"x": x2[b], "c_l": _c_l(c, b), "adaw": f(ada_w[1][:, :2048]), "adab": f(ada_b[1][None, :2048]),
             "n1g": f(norm1_g[1][None]), "wpf": wpf, "wpt": wpt,
             "wk1": f(nsa_wk1[0]), "wv1": f(nsa_wv1[0]), "wk2": f(nsa_wk2[0]), "wv2": f(nsa_wv2[0]),
             "pekT": f(np.asarray(nsa_pe_k[0]).T), "pevT": f(np.asarray(nsa_pe_v[0]).T)}
        m.update(nsc)
        maps.append(m)
    r3 = run_bass_kernel_spmd(nc3, maps, core_ids=cores)
    o = [r3.results[k]["out"] for k in cores]
    del maps

    def mix1(b, sl):
        return f(np.concatenate([o[b * 4 + g][sl].T for g in range(4)], axis=0))
    out = tok_launch(1, 'wo', True, x2, mix1, f(nsa_w_o[0]))
    return out
```

```python
from contextlib import ExitStack
import numpy as np
import concourse.bass as bass
import concourse.mybir as mybir

F32 = mybir.dt.float32
BF16 = mybir.dt.bfloat16
I32 = mybir.dt.int32
U32 = mybir.dt.uint32
AF = mybir.ActivationFunctionType
ALU = mybir.AluOpType
AX = mybir.AxisListType


class Sched:
    N_DMA_SEMS = 24
    SEM_MAX = 12000
    DMA_SEM_MAX = 12000

    def __init__(self, nc, es):
        self.nc = nc
        self.es = es
        self.engs = {'pe': nc.tensor, 'dve': nc.vector, 'act': nc.scalar,
                     'pool': nc.gpsimd, 'sp': nc.sync}
        self.dpool = [[es.enter_context(nc.semaphore("ds_%d_0" % i))] for i in range(self.N_DMA_SEMS)]
        self.csem = {}
        self.ccnt = {}
        self.cep = {}
        self.ctot = {}
        self.cpool = {}
        for k in ('pe', 'dve', 'act', 'pool'):
            n_ep = {'pe': 8, 'dve': 8, 'act': 6, 'pool': 3}[k]
            self.cpool[k] = [es.enter_context(nc.semaphore("cs_%s_%d" % (k, j))) for j in range(n_ep)]
            self.csem[k] = self.cpool[k][0]
            self.ccnt[k] = 0
            self.cep[k] = 0
            self.ctot[k] = 0
        self.dsem = [self.dpool[i][0] for i in range(self.N_DMA_SEMS)]
        self.dcnt = [0] * self.N_DMA_SEMS
        self.dep = [0] * self.N_DMA_SEMS
        for i in range(self.N_DMA_SEMS):
            self.dpool[i].append(es.enter_context(nc.semaphore("ds_%d_1" % i)))
        self.drr = 0
        self.seen = {k: {} for k in self.engs}
        self.lastw = {}
        self.readers = {}
        self.n_instr = 0
        self.n_wait = 0

    def _deps(self, reads, writes):
        deps = []
        for k in reads:
            w = self.lastw.get(k)
            if w is not None:
                deps.append(w)
        for k in writes:
            w = self.lastw.get(k)
            if w is not None:
                deps.append(w)
            deps.extend(self.readers.get(k, ()))
        return deps

    def _wait(self, e, deps, skip_self=None):
        eng = self.engs[e]
        best = {}
        for (sid, sem, val) in deps:
            if skip_self is not None and sid.startswith(skip_self):
                continue
            if best.get(sid, (None, 0))[1] < val:
                best[sid] = (sem, val)
        for sid, (sem, val) in best.items():
            if self.seen[e].get(sid, 0) < val:
                eng.wait_ge(sem, val)
                self.seen[e][sid] = val
                self.n_wait += 1

    def _record(self, ev, reads, writes):
        for k in reads:
            self.readers.setdefault(k, []).append(ev)
        for k in writes:
            self.lastw[k] = ev
            self.readers[k] = []

    def op(self, e, fn, reads=(), writes=(), pe_chain=False):
        deps = self._deps(reads, writes)
        self._wait(e, deps, skip_self=('c_pe_' if (pe_chain and e == 'pe') else None))
        ins = fn()
        if self.ccnt[e] >= self.SEM_MAX:
            self.cep[e] += 1
            self.csem[e] = self.cpool[e][self.cep[e]]
            self.ccnt[e] = 0
        self.ccnt[e] += 1
        self.ctot[e] += 1
        ins.then_inc(self.csem[e], 1)
        ev = ('c_%s_%d' % (e, self.cep[e]), self.csem[e], self.ccnt[e])
        self._record(ev, reads, writes)
        self.n_instr += 1
        return ev

    def dma(self, q, fn, reads=(), writes=()):
        deps = self._deps(reads, writes)
        self._wait(q, deps)
        i = self.drr
        self.drr = (self.drr + 1) % self.N_DMA_SEMS
        sid = 'd_%d_%d' % (i, self.dep[i])
        if self.seen[q].get(sid, 0) < self.dcnt[i]:
            self.engs[q].wait_ge(self.dsem[i], self.dcnt[i])
            self.seen[q][sid] = self.dcnt[i]
        if self.dcnt[i] >= self.DMA_SEM_MAX:
            self.dep[i] += 1
            self.dsem[i] = self.dpool[i][self.dep[i]]
            self.dcnt[i] = 0
            sid = 'd_%d_%d' % (i, self.dep[i])
        ins = fn()
        self.dcnt[i] += 16
        ins.then_inc(self.dsem[i], 16)
        ev = (sid, self.dsem[i], self.dcnt[i])
        self._record(ev, reads, writes)
        self.n_instr += 1
        return ev

    def finish(self, keys):
        deps = []
        for k in keys:
            w = self.lastw.get(k)
            if w is not None:
                deps.append(w)
        self._wait('sp', deps)
        alld = []
        for k in ('pe', 'dve', 'act', 'pool'):
            if self.ccnt[k]:
                alld.append(('c_%s_%d' % (k, self.cep[k]), self.csem[k], self.ccnt[k]))
        for i in range(self.N_DMA_SEMS):
            if self.dcnt[i]:
                alld.append(('d_%d_%d' % (i, self.dep[i]), self.dsem[i], self.dcnt[i]))
        self._wait('sp', alld)


def sched_barrier(S):
    alld = []
    for k in ('pe', 'dve', 'act', 'pool'):
        if S.ccnt[k]:
            alld.append(('c_%s_%d' % (k, S.cep[k]), S.csem[k], S.ccnt[k]))
    for i in range(S.N_DMA_SEMS):
        if S.dcnt[i]:
            alld.append(('d_%d_%d' % (i, S.dep[i]), S.dsem[i], S.dcnt[i]))
    for e in ('pe', 'dve', 'act', 'pool', 'sp'):
        S._wait(e, alld)


Sched.barrier = sched_barrier


RMS_EPS = 1e-6
IOA = bass.IndirectOffsetOnAxis


def build_tok(NT, mode, final, gelu_func=None, dbg=None):
    nc = bass.Bass("TRN2", target_bir_lowering=False)
    T = NT * 128
    MIXN = 2048 if mode == 'glu' else 1024
    D = 1024
    din = lambda n, s, d=F32: nc.dram_tensor(n, s, d, kind="ExternalInput").ap()
    x = din("x", [T, D])
    mixT = din("mixT", [D, T])
    c_l = din("c_l", [128, 8])
    adaw = din("adaw", [D, 4096])
    adab = din("adab", [1, 4096])
    n2g = din("n2g", [1, D])
    fing = din("fing", [1, D])
    wmix_d = din("wmix", [D, MIXN])
    wq_d = din("wq", [D, 2048])
    sk_d = din("sk", [16, 128, 128])
    u_tab = din("u_tab", [16384, D])
    v_tab = din("v_tab", [16384, D])
    identf_d = din("identf", [128, 128])
    iota16_d = din("iota16", [128, 16])
    out = nc.dram_tensor("out", [T, D], F32, kind="ExternalOutput").ap()
    u_bf = nc.dram_tensor("u_bf", [16384, D], BF16).ap()
    v_bf = nc.dram_tensor("v_bf", [16384, D], BF16).ap()

    es = ExitStack()
    with es:
        es.enter_context(nc.allow_low_precision("bf16 matmul operands"))
        es.enter_context(nc.allow_non_contiguous_dma("small layout loads"))
        S = Sched(nc, es)
        sb = lambda n, s, d=F32: es.enter_context(nc.sbuf_tensor("s_" + n, s, d))
        ps = lambda n, s, d=F32: es.enter_context(nc.psum_tensor("p_" + n, s, d))
        V, A, P, PE = nc.vector, nc.scalar, nc.gpsimd, nc.tensor

        identf = sb("identf", [128, 128])
        identb = sb("identb", [128, 128], BF16)
        iota16 = sb("iota16", [128, 16])
        epsT = sb("epsT", [128, 1])
        modrep = sb("modrep", [128, 4096])
        gk2 = sb("gk2", [128, D])
        fing_rep = sb("fing_rep", [128, D])
        ot = sb("ot", [128, D])
        n2g_rep = ot
        wq = sb("wq", [128, 8, 2048], BF16)
        wmix = sb("wmix", [128, 8, MIXN], BF16)
        skT = sb("skT", [128, 16, 128], BF16)

        psA = ps("psA", [128, 1024])
        psV = ps("psV", [128, 1024])
        psB = ps("psB", [128, 1024])
        psT = ps("psT", [128, 1024], BF16)
        psM = ps("psM", [128, 512])
        es2 = ExitStack()
        tb = lambda n, s, d=F32: es2.enter_context(nc.sbuf_tensor("t_" + n, s, d))
        csil = tb("csil", [128, 8])
        csil_rep = tb("csil_rep", [128, 8, 128])
        stg = [tb("stg%d" % i, [128, 8, 256]) for i in range(2)]
        adab_rep = tb("adab_rep", [128, 256])

        S.dma('sp', lambda: nc.sync.dma_start(out=identf[:], in_=identf_d[:, :]), writes=['identf'])
        S.dma('sp', lambda: nc.sync.dma_start(out=iota16[:], in_=iota16_d[:, :]), writes=['iota4'])
        S.dma('sp', lambda: nc.sync.dma_start(out=csil[:], in_=c_l[:, :]), writes=['csil'])
        S.dma('sp', lambda: nc.sync.dma_start(out=n2g_rep[:], in_=n2g[0:1, :].partition_broadcast(128)), writes=['ot'])
        if final:
            S.dma('sp', lambda: nc.sync.dma_start(out=fing_rep[:], in_=fing[0:1, :].partition_broadcast(128)), writes=['fing_rep'])
        S.op('dve', lambda: V.tensor_copy(out=identb[:], in_=identf[:]), reads=['identf'], writes=['identb'])
        S.op('dve', lambda: V.memset(epsT[:], RMS_EPS), writes=['epsT'])
        S.op('act', lambda: A.activation(out=csil[:], in_=csil[:], func=AF.Silu), reads=['csil'], writes=['csil'])
        S.op('dve', lambda: V.tensor_copy(out=csil_rep[:], in_=csil[:].unsqueeze(2).to_broadcast([128, 8, 128])),
             reads=['csil'], writes=['csil_rep'])
        for j in range(16):
            st = stg[j % 2]
            k_st = 'stg%d' % (j % 2)
            S.dma('sp', lambda: nc.sync.dma_start(
                out=st[:], in_=adaw[:, j * 256:(j + 1) * 256].rearrange("(kc k) n -> k kc n", k=128)), writes=[k_st])
            S.dma('sp', lambda: nc.sync.dma_start(
                out=adab_rep[:], in_=adab[0:1, j * 256:(j + 1) * 256].partition_broadcast(128)), writes=['adab_rep'])
            for kc in range(8):
                S.op('pe', lambda: PE.matmul(psM[:, 0:256], lhsT=csil_rep[:, kc, :], rhs=st[:, kc, :], start=(kc == 0), stop=(kc == 7)),
                     reads=['csil_rep', k_st], writes=['psM'], pe_chain=(kc > 0))
            S.op('dve', lambda: V.tensor_tensor(out=modrep[:, j * 256:(j + 1) * 256], in0=psM[:, 0:256], in1=adab_rep[:], op=ALU.add),
                 reads=['psM', 'adab_rep'], writes=['modrep'])
        g1 = modrep[:, 0:1024]
        sh2 = modrep[:, 1024:2048]
        sc2 = modrep[:, 2048:3072]
        g2 = modrep[:, 3072:4096]
        S.op('dve', lambda: V.scalar_tensor_tensor(out=gk2[:], in0=sc2, scalar=1.0, in1=n2g_rep[:], op0=ALU.add, op1=ALU.mult),
             reads=['modrep', 'ot'], writes=['gk2'])
        cast_i = [0]

        def load_cast(dst3, src2d, ncols, key):
            for c0 in range(0, ncols, 256):
                i = cast_i[0] % 2
                cast_i[0] += 1
                st = stg[i]
                S.dma('sp', lambda: nc.sync.dma_start(
                    out=st[:], in_=src2d[:, c0:c0 + 256].rearrange("(kc k) n -> k kc n", k=128)), writes=['stg%d' % i])
                if i == 0:
                    S.op('dve', lambda: V.tensor_copy(out=dst3[:, :, c0:c0 + 256], in_=st[:]), reads=['stg%d' % i], writes=[key])
                else:
                    S.op('act', lambda: A.copy(out=dst3[:, :, c0:c0 + 256], in_=st[:]), reads=['stg%d' % i], writes=[key])

        load_cast(wmix, wmix_d, MIXN, 'wmix')
        load_cast(wq, wq_d, 2048, 'wq')
        for j in range(16):
            st = stg[j % 2]
            k_st = 'stg%d' % (j % 2)
            S.dma('sp', lambda: nc.sync.dma_start(out=st[:, 0, 0:128], in_=sk_d[j, :, :]), writes=[k_st])
            S.op('pe', lambda: PE.transpose(out=psM[:, 0:128], in_=st[:, 0, 0:128], identity=identf[:]),
                 reads=[k_st, 'identf'], writes=['psM'])
            S.op('act', lambda: A.copy(out=skT[:, j, :], in_=psM[:, 0:128]), reads=['psM'], writes=['skT'])

        cvb = [tb("cvb%d" % i, [128, 2048], BF16) for i in range(2)]
        cvi = 0
        for (tab, tbf) in ((u_tab, u_bf), (v_tab, v_bf)):
            for c0 in range(0, 16384, 256):
                i = cvi % 2
                st = stg[i]
                k_st = 'stg%d' % i
                S.dma('sp', lambda: nc.sync.dma_start(out=st[:].rearrange("p a b -> p (a b)"),
                                                      in_=tab[c0:c0 + 256, :].rearrange("(p r) d -> p (r d)", r=2)), writes=[k_st])
                e = ('dve', 'act', 'pool')[cvi % 3]
                if e == 'dve':
                    S.op('dve', lambda: V.tensor_copy(out=cvb[i][:], in_=st[:].rearrange("p a b -> p (a b)")), reads=[k_st], writes=['cvb%d' % i])
                elif e == 'act':
                    S.op('act', lambda: A.copy(out=cvb[i][:], in_=st[:].rearrange("p a b -> p (a b)")), reads=[k_st], writes=['cvb%d' % i])
                else:
                    S.op('pool', lambda: P.tensor_copy(out=cvb[i][:], in_=st[:].rearrange("p a b -> p (a b)")), reads=[k_st], writes=['cvb%d' % i])
                S.dma('sp', lambda: nc.sync.dma_start(out=tbf[c0:c0 + 256, :].rearrange("(p r) d -> p (r d)", r=2), in_=cvb[i][:]),
                      reads=['cvb%d' % i], writes=['tbf'])
                cvi += 1

        S.barrier()
        es2.close()
        xt = [sb("xt%d" % i, [128, D]) for i in range(2)]
        mT = sb("mT", [128, 8, 128])
        mTb = sb("mTb", [128, 8, 128], BF16)
        x1b = [sb("x1_%d" % i, [128, D]) for i in range(2)]
        junk = sb("junk", [128, D], BF16)
        ss = sb("ss", [128, 1])
        rs = sb("rs", [128, 1])
        hf = sb("hf", [128, D])
        hfb = sb("hfb", [128, D], BF16)
        hfT = sb("hfT", [128, 8, 128], BF16)
        qkT = sb("qkT", [128, 16, 128], BF16)
        scw = sb("scw", [128, 16, 128])
        vals = sb("vals", [128, 16, 16])
        idx = sb("idx", [128, 16, 16], U32)
        idxf = sb("idxf", [128, 16, 16])
        cand = sb("cand", [128, 8, 256])
        wk2 = sb("wk2", [128, 2048])
        scw2 = wk2[:].rearrange("p (a b) -> p a b", b=128)
        cand2 = wk2[:].rearrange("p (a b) -> p a b", b=256)
        tops = sb("tops", [128, 8, 16])
        pos = sb("pos", [128, 8, 16], U32)
        au = sb("au", [128, 8, 16], U32)
        bu = sb("bu", [128, 8, 16], U32)
        af = sb("af", [128, 8, 16])
        bf = sb("bf", [128, 8, 16])
        eq = scw[:].rearrange("p a b -> p (a b)").rearrange("p (h k c) -> p h k c", h=8, k=16)
        sel1 = sb("sel1", [128, 8, 16])
        sel2 = sb("sel2", [128, 8, 16])
        ef = sb("ef", [128, 128])
        eidxb = [sb("eidx_%d" % i, [128, 128], U32) for i in range(2)]
        gat = sb("gat", [128, 8, 16])
        gsum = sb("gsum", [128, 8])
        sdot = sb("sdot", [128, 128])
        actvb = [sb("actv_%d" % i, [128, 128]) for i in range(2)]
        NB = 8
        NBV = 6
        ug = [sb("ug%d" % i, [128, D], BF16) for i in range(NB)]
        vg = [sb("vg%d" % i, [128, D], BF16) for i in range(NBV)]
        dg = [sb("dg%d" % i, [128, 128], BF16) for i in range(NBV)]
        acc = sb("acc", [128, D])
        sg = sb("sg", [128, D])

        gfunc = gelu_func if gelu_func is not None else AF.Gelu_apprx_tanh

        def front(t):
                x1 = x1b[t % 2]; kx1 = 'x1_%d' % (t % 2)
                eidx = eidxb[t % 2]; keidx = 'eidx_%d' % (t % 2)
                actv = actvb[t % 2]; kactv = 'actv_%d' % (t % 2)
                r0 = t * 128
                xb = xt[t % 2]
                kx = 'xt%d' % (t % 2)
                S.dma('sp', lambda: nc.sync.dma_start(out=xb[:], in_=x[r0:r0 + 128, :]), writes=[kx])
                S.dma('sp', lambda: nc.sync.dma_start(
                    out=mT[:], in_=mixT[:, r0:r0 + 128].rearrange("(kc k) n -> k kc n", k=128)), writes=['mT'])
                S.op('act', lambda: A.copy(out=mTb[:], in_=mT[:]), reads=['mT'], writes=['mTb'])
                def mixmm(c0):
                    for nb in range(2):
                        for kc in range(8):
                            S.op('pe', lambda: PE.matmul(psA[:, nb * 512:(nb + 1) * 512], lhsT=mTb[:, kc, :],
                                                         rhs=wmix[:, kc, c0 + nb * 512:c0 + (nb + 1) * 512], start=(kc == 0), stop=(kc == 7)),
                                 reads=['mTb', 'wmix'], writes=['psA'], pe_chain=not (nb == 0 and kc == 0))
                if mode == 'glu':
                    mixmm(1024)
                    S.op('act', lambda: A.activation(out=sg[:], in_=psA[:], func=AF.Sigmoid), reads=['psA'], writes=['sg'])
                    S.op('dve', lambda: V.tensor_tensor(out=sg[:], in0=sg[:], in1=g1, op=ALU.mult), reads=['sg', 'modrep'], writes=['sg'])
                    mixmm(0)
                    S.op('dve', lambda: V.tensor_tensor(out=x1[:], in0=psA[:], in1=sg[:], op=ALU.mult),
                         reads=['psA', 'sg'], writes=[kx1])
                else:
                    mixmm(0)
                    S.op('dve', lambda: V.tensor_tensor(out=x1[:], in0=psA[:], in1=g1, op=ALU.mult),
                         reads=['psA', 'modrep'], writes=[kx1])
                S.op('dve', lambda: V.tensor_tensor(out=x1[:], in0=x1[:], in1=xb[:], op=ALU.add), reads=[kx1, kx], writes=[kx1])
                S.op('act', lambda: A.activation(out=junk[:], in_=x1[:], func=AF.Square, accum_out=ss[:]), reads=[kx1], writes=['junk', 'ss'])
                S.op('act', lambda: A.activation(out=rs[:], in_=ss[:], func=AF.Sqrt, bias=epsT[:], scale=1.0 / D),
                     reads=['ss', 'epsT'], writes=['rs'])
                S.op('dve', lambda: V.reciprocal(out=rs[:], in_=rs[:]), reads=['rs'], writes=['rs'])
                S.op('dve', lambda: V.scalar_tensor_tensor(out=hf[:], in0=x1[:], scalar=rs[:], in1=gk2[:], op0=ALU.mult, op1=ALU.mult),
                     reads=[kx1, 'rs', 'gk2'], writes=['hf'])
                S.op('dve', lambda: V.tensor_tensor(out=hf[:], in0=hf[:], in1=sh2, op=ALU.add), reads=['hf', 'modrep'], writes=['hf'])
                S.op('act', lambda: A.copy(out=hfb[:], in_=hf[:]), reads=['hf'], writes=['hfb'])
                for kc in range(8):
                    S.op('pe', lambda: PE.transpose(out=psT[:, kc * 128:(kc + 1) * 128], in_=hfb[:, kc * 128:(kc + 1) * 128], identity=identb[:]),
                         reads=['hfb', 'identb'], writes=['psT'], pe_chain=(kc > 0))
                S.op('dve', lambda: V.tensor_copy(out=hfT[:].rearrange("p a b -> p (a b)"), in_=psT[:]), reads=['psT'], writes=['hfT'])
                for half in range(2):
                    for jj in range(8):
                        j = half * 8 + jj
                        for kc in range(8):
                            S.op('pe', lambda: PE.matmul(psB[:, jj * 128:(jj + 1) * 128], lhsT=wq[:, kc, j * 128:(j + 1) * 128],
                                                         rhs=hfT[:, kc, :], start=(kc == 0), stop=(kc == 7)),
                                 reads=['wq', 'hfT'], writes=['psB'], pe_chain=not (jj == 0 and kc == 0))
                    S.op('act', lambda: A.copy(out=qkT[:, half * 8:(half + 1) * 8, :].rearrange("p a b -> p (a b)"), in_=psB[:]),
                         reads=['psB'], writes=['qkT'])
                for half in range(2):
                    for jj in range(8):
                        j = half * 8 + jj
                        S.op('pe', lambda: PE.matmul(psA[:, jj * 128:(jj + 1) * 128], lhsT=qkT[:, j, :], rhs=skT[:, j, :], start=True, stop=True),
                             reads=['qkT', 'skT'], writes=['psA'], pe_chain=(jj > 0))
                    S.op('act', lambda: A.copy(out=scw[:, half * 8:(half + 1) * 8, :].rearrange("p a b -> p (a b)"), in_=psA[:]),
                         reads=['psA'], writes=['scw'])
                for j in range(16):
                    S.op('dve', lambda: V.max(out=vals[:, j, 0:8], in_=scw[:, j, :]), reads=['scw'], writes=['vals'])
                    S.op('dve', lambda: V.max_index(out=idx[:, j, 0:8], in_max=vals[:, j, 0:8], in_values=scw[:, j, :]),
                         reads=['scw', 'vals'], writes=['idx'])
                    S.op('dve', lambda: V.match_replace(out=scw2[:, j, :], in_to_replace=vals[:, j, 0:8], in_values=scw[:, j, :], imm_value=-1e30),
                         reads=['scw', 'vals'], writes=['wk2'])
                    S.op('dve', lambda: V.max(out=vals[:, j, 8:16], in_=scw2[:, j, :]), reads=['wk2'], writes=['vals'])
                    S.op('dve', lambda: V.max_index(out=idx[:, j, 8:16], in_max=vals[:, j, 8:16], in_values=scw2[:, j, :]),
                         reads=['wk2', 'vals'], writes=['idx'])
                vals4 = vals[:].rearrange("p (h c) k -> p h c k", c=2)
                S.op('dve', lambda: V.tensor_tensor(out=cand[:].rearrange("p h (a b) -> p h a b", b=16),
                                                    in0=vals4[:, :, 0, :].unsqueeze(3).to_broadcast([128, 8, 16, 16]),
                                                    in1=vals4[:, :, 1, :].unsqueeze(2).to_broadcast([128, 8, 16, 16]), op=ALU.add),
                     reads=['vals'], writes=['cand'])
                for h in range(8):
                    S.op('dve', lambda: V.max(out=tops[:, h, 0:8], in_=cand[:, h, :]), reads=['cand'], writes=['tops'])
                    S.op('dve', lambda: V.max_index(out=pos[:, h, 0:8], in_max=tops[:, h, 0:8], in_values=cand[:, h, :]),
                         reads=['cand', 'tops'], writes=['pos'])
                    S.op('dve', lambda: V.match_replace(out=cand2[:, h, :], in_to_replace=tops[:, h, 0:8], in_values=cand[:, h, :], imm_value=-1e30),
                         reads=['cand', 'tops'], writes=['wk2'])
                    S.op('dve', lambda: V.max(out=tops[:, h, 8:16], in_=cand2[:, h, :]), reads=['wk2'], writes=['tops'])
                    S.op('dve', lambda: V.max_index(out=pos[:, h, 8:16], in_max=tops[:, h, 8:16], in_values=cand2[:, h, :]),
                         reads=['wk2', 'tops'], writes=['pos'])
                S.op('dve', lambda: V.tensor_single_scalar(out=au[:], in_=pos[:], scalar=4, op=ALU.logical_shift_right), reads=['pos'], writes=['au'])
                S.op('dve', lambda: V.tensor_single_scalar(out=bu[:], in_=pos[:], scalar=15, op=ALU.bitwise_and), reads=['pos'], writes=['bu'])
                S.op('dve', lambda: V.tensor_copy(out=af[:], in_=au[:]), reads=['au'], writes=['af'])
                S.op('dve', lambda: V.tensor_copy(out=bf[:], in_=bu[:]), reads=['bu'], writes=['bf'])
                S.op('dve', lambda: V.tensor_copy(out=idxf[:], in_=idx[:]), reads=['idx'], writes=['idxf'])
                idxf4 = idxf[:].rearrange("p (h c) k -> p h c k", c=2)
                for (sf, cc, sel) in ((af, 0, sel1), (bf, 1, sel2)):
                    ksel = 'sel1' if cc == 0 else 'sel2'
                    S.op('dve', lambda: V.tensor_tensor(out=eq[:], in0=sf[:].unsqueeze(3).to_broadcast([128, 8, 16, 16]),
                                                        in1=iota16[:].unsqueeze(1).unsqueeze(1).to_broadcast([128, 8, 16, 16]), op=ALU.is_equal), reads=['af', 'bf', 'iota4'], writes=['scw'])
                    S.op('dve', lambda: V.tensor_tensor(out=eq[:], in0=eq[:], in1=idxf4[:, :, cc, :].unsqueeze(2).to_broadcast([128, 8, 16, 16]),
                                                        op=ALU.mult), reads=['scw', 'idxf'], writes=['scw'])
                    S.op('dve', lambda: V.tensor_reduce(out=sel[:], in_=eq[:], axis=AX.X, op=ALU.add), reads=['scw'], writes=[ksel])
                S.op('dve', lambda: V.scalar_tensor_tensor(out=ef[:], in0=sel1[:].rearrange("p h k -> p (h k)"), scalar=128.0,
                                                           in1=sel2[:].rearrange("p h k -> p (h k)"), op0=ALU.mult, op1=ALU.add),
                     reads=['sel1', 'sel2'], writes=['ef'])
                S.op('dve', lambda: V.tensor_copy(out=eidx[:], in_=ef[:]), reads=['ef'], writes=[keidx])
                S.op('dve', lambda: V.tensor_tensor(out=gat[:], in0=tops[:], in1=tops[:, :, 0:1].to_broadcast([128, 8, 16]), op=ALU.subtract),
                     reads=['tops'], writes=['gat'])
                S.op('act', lambda: A.activation(out=gat[:], in_=gat[:], func=AF.Exp), reads=['gat'], writes=['gat'])
                S.op('dve', lambda: V.tensor_reduce(out=gsum[:], in_=gat[:], axis=AX.X, op=ALU.add), reads=['gat'], writes=['gsum'])
                S.op('dve', lambda: V.reciprocal(out=gsum[:], in_=gsum[:]), reads=['gsum'], writes=['gsum'])
                S.op('dve', lambda: V.tensor_tensor(out=gat[:], in0=gat[:], in1=gsum[:].unsqueeze(2).to_broadcast([128, 8, 16]), op=ALU.mult),
                     reads=['gat', 'gsum'], writes=['gat'])

        def midU(t):
                x1 = x1b[t % 2]; kx1 = 'x1_%d' % (t % 2)
                eidx = eidxb[t % 2]; keidx = 'eidx_%d' % (t % 2)
                actv = actvb[t % 2]; kactv = 'actv_%d' % (t % 2)
                r0 = t * 128
                for s in range(128):
                    b = s % NB
                    S.dma('pool', lambda: P.indirect_dma_start(out=ug[b][:], out_offset=None, in_=u_bf[:, :],
                                                                in_offset=IOA(ap=eidx[:, s:s + 1], axis=0)),
                          reads=[keidx, 'tbf'], writes=['ug%d' % b])
                    S.op('dve', lambda: V.scalar_tensor_tensor(out=junk[:], in0=ug[b][:], scalar=1.0, in1=hf[:],
                                                               op0=ALU.mult, op1=ALU.mult, accum_out=sdot[:, s:s + 1]),
                         reads=['ug%d' % b, 'hf'], writes=['junk', 'sdot'])
                S.op('dve', lambda: V.tensor_tensor(out=actv[:], in0=sdot[:], in1=sdot[:], op=ALU.mult), reads=['sdot'], writes=[kactv])
                S.op('dve', lambda: V.tensor_scalar(out=actv[:], in0=actv[:], scalar1=0.044715, scalar2=1.0, op0=ALU.mult, op1=ALU.add),
                     reads=[kactv], writes=[kactv])
                S.op('dve', lambda: V.tensor_tensor(out=actv[:], in0=actv[:], in1=sdot[:], op=ALU.mult), reads=[kactv, 'sdot'], writes=[kactv])
                S.op('act', lambda: A.activation(out=actv[:], in_=actv[:], func=AF.Sigmoid, scale=1.5957691216), reads=[kactv], writes=[kactv])
                S.op('dve', lambda: V.tensor_tensor(out=actv[:], in0=actv[:], in1=sdot[:], op=ALU.mult), reads=[kactv, 'sdot'], writes=[kactv])
                S.op('dve', lambda: V.tensor_tensor(out=actv[:], in0=actv[:], in1=gat[:].rearrange("p h k -> p (h k)"), op=ALU.mult),
                     reads=[kactv, 'gat'], writes=[kactv])

        def midV(t):
                x1 = x1b[t % 2]; kx1 = 'x1_%d' % (t % 2)
                eidx = eidxb[t % 2]; keidx = 'eidx_%d' % (t % 2)
                actv = actvb[t % 2]; kactv = 'actv_%d' % (t % 2)
                r0 = t * 128
                for s in range(128):
                    b = s % NBV
                    S.dma('pool', lambda: P.indirect_dma_start(out=vg[b][:], out_offset=None, in_=v_bf[:, :],
                                                                in_offset=IOA(ap=eidx[:, s:s + 1], axis=0)),
                          reads=[keidx, 'tbf'], writes=['vg%d' % b])
                    S.op('act', lambda: A.activation(out=dg[b][:], in_=identf[:], func=AF.Copy, scale=actv[:, s:s + 1]),
                         reads=['identf', kactv], writes=['dg%d' % b])
                    for hb in range(2):
                        S.op('pe', lambda: PE.matmul(psV[:, hb * 512:(hb + 1) * 512], lhsT=dg[b][:], rhs=vg[b][:, hb * 512:(hb + 1) * 512],
                                                     start=(s == 0), stop=(s == 127)), reads=['dg%d' % b, 'vg%d' % b], writes=['psV'],
                             pe_chain=not (s == 0 and hb == 0))

        def tail(t):
                x1 = x1b[t % 2]; kx1 = 'x1_%d' % (t % 2)
                eidx = eidxb[t % 2]; keidx = 'eidx_%d' % (t % 2)
                actv = actvb[t % 2]; kactv = 'actv_%d' % (t % 2)
                r0 = t * 128
                S.op('dve', lambda: V.tensor_tensor(out=acc[:], in0=psV[:], in1=g2, op=ALU.mult), reads=['psV', 'modrep'], writes=['acc'])
                S.op('dve', lambda: V.tensor_tensor(out=ot[:], in0=acc[:], in1=x1[:], op=ALU.add), reads=['acc', kx1], writes=['ot'])
                if final:
                    S.op('act', lambda: A.activation(out=junk[:], in_=ot[:], func=AF.Square, accum_out=ss[:]), reads=['ot'], writes=['junk', 'ss'])
                    S.op('act', lambda: A.activation(out=rs[:], in_=ss[:], func=AF.Sqrt, bias=epsT[:], scale=1.0 / D),
                         reads=['ss', 'epsT'], writes=['rs'])
                    S.op('dve', lambda: V.reciprocal(out=rs[:], in_=rs[:]), reads=['rs'], writes=['rs'])
                    S.op('dve', lambda: V.scalar_tensor_tensor(out=ot[:], in0=ot[:], scalar=rs[:], in1=fing_rep[:], op0=ALU.mult, op1=ALU.mult),
                         reads=['ot', 'rs', 'fing_rep'], writes=['ot'])
                S.dma('sp', lambda: nc.sync.dma_start(out=out[r0:r0 + 128, :], in_=ot[:]), reads=['ot'], writes=['out'])

        front(0)
        for t in range(NT):
            midU(t)
            if t + 1 < NT:
                front(t + 1)
            midV(t)
            tail(t)
        S.finish(['out'])
        print("tok program: instr", S.n_instr, "waits", S.n_wait)
    return nc


def tok_consts():
    iota16 = np.broadcast_to(np.arange(16, dtype=np.float32)[None, :], (128, 16)).copy()
    return {"identf": np.eye(128, dtype=np.float32), "iota16": iota16}

import math

RMS_EPS = 1e-6
PI = math.pi


def build_s5(NCH):
    nc = bass.Bass("TRN2", target_bir_lowering=False)
    T = NCH * 128
    D = 1024
    din = lambda n, s, d=F32: nc.dram_tensor(n, s, d, kind="ExternalInput").ap()
    x = din("x", [T, D])
    c_l = din("c_l", [128, 8])
    adaw = din("adaw", [D, 2048])
    adab = din("adab", [1, 2048])
    n1g = din("n1g", [1, D])
    win_d = din("win", [D, 256])
    are_c_d = din("are_c", [128, 16]); aim_c_d = din("aim_c", [128, 16]); ldt_c_d = din("ldt_c", [128, 16])
    are_r_d = din("are_r", [128, 2048]); aim_r_d = din("aim_r", [128, 2048]); ldt_r_d = din("ldt_r", [128, 2048])
    X1p_d = din("X1p", [128, 2048]); X2p_d = din("X2p", [128, 2048])
    CcP_d = din("CcP", [128, 2048]); CcSP_d = din("CcSP", [128, 2048])
    dcol_d = din("dcol", [128, 2])
    identf_d = din("identf", [128, 128]); swapm_d = din("swapm", [128, 128])
    sgnc_d = din("sgn_c", [128, 1]); sgnr_d = din("sgn_r", [128, 128])
    mrow_d = din("mrow", [128, 128]); mask01_d = din("mask01", [128, 512])
    out = nc.dram_tensor("out", [256, T], F32, kind="ExternalOutput").ap()

    es = ExitStack()
    with es:
        es.enter_context(nc.allow_low_precision("bf16 matmul operands"))
        es.enter_context(nc.allow_non_contiguous_dma("small layout loads"))
        S = Sched(nc, es)
        sb = lambda n, s, d=F32: es.enter_context(nc.sbuf_tensor("s_" + n, s, d))
        ps = lambda n, s, d=F32: es.enter_context(nc.psum_tensor("p_" + n, s, d))
        V, A, P, PE = nc.vector, nc.scalar, nc.gpsimd, nc.tensor

        def ld(dst, src, key):
            S.dma('sp', lambda: nc.sync.dma_start(out=dst, in_=src), writes=[key])

        identf = sb("identf", [128, 128]); identb = sb("identb", [128, 128], BF16)
        epsT = sb("epsT", [128, 1])
        modrep = sb("modrep", [128, 2048])
        gk1 = sb("gk1", [128, D])
        win = sb("win", [128, 8, 256], BF16)
        Ainv_r = sb("Ainv_r", [128, 16, 128]); Ainv_i = sb("Ainv_i", [128, 16, 128])
        Apow_r = sb("Apow_r", [128, 16, 128]); Apow_i = sb("Apow_i", [128, 16, 128])
        Rot = sb("Rot", [128, 16, 128])
        Bpad = sb("Bpad", [128, 16, 128], BF16); BpadS = sb("BpadS", [128, 16, 128], BF16)
        Cc = sb("Cc", [128, 16, 128], BF16); CcS = sb("CcS", [128, 16, 128], BF16)
        dcol = sb("dcol", [128, 2])
        mask01 = sb("mask01", [128, 512])

        psT = ps("psT", [128, 1024], BF16)
        psU = ps("psU", [128, 512])
        psBU = [ps("psBU%d" % i, [128, 512]) for i in range(2)]
        psBS = [ps("psBS%d" % i, [128, 512]) for i in range(2)]
        psI = ps("psI", [128, 512])
        psY = ps("psY", [128, 512])

        ld(identf[:], identf_d[:, :], 'identf')
        ld(dcol[:], dcol_d[:, :], 'dcol')
        ld(mask01[:], mask01_d[:, :], 'mask01')
        S.op('dve', lambda: V.tensor_copy(out=identb[:], in_=identf[:]), reads=['identf'], writes=['identb'])
        S.op('dve', lambda: V.memset(epsT[:], RMS_EPS), writes=['epsT'])

        es2 = ExitStack()
        with es2:
            tb = lambda n, s, d=F32: es2.enter_context(nc.sbuf_tensor("t_" + n, s, d))
            csil = tb("csil", [128, 8]); csil_rep = tb("csil_rep", [128, 8, 128])
            stg = [tb("stg%d" % i, [128, 8, 256]) for i in range(2)]
            adab_rep = tb("adab_rep", [128, 256])
            n1g_rep = tb("n1g_rep", [128, D])
            ld(csil[:], c_l[:, :], 'csil')
            ld(n1g_rep[:], n1g[0:1, :].partition_broadcast(128), 'n1g_rep')
            S.op('act', lambda: A.activation(out=csil[:], in_=csil[:], func=AF.Silu), reads=['csil'], writes=['csil'])
            S.op('dve', lambda: V.tensor_copy(out=csil_rep[:], in_=csil[:].unsqueeze(2).to_broadcast([128, 8, 128])),
                 reads=['csil'], writes=['csil_rep'])
            for j in range(8):
                st = stg[j % 2]; k_st = 'stg%d' % (j % 2)
                ld(st[:], adaw[:, j * 256:(j + 1) * 256].rearrange("(kc k) n -> k kc n", k=128), k_st)
                ld(adab_rep[:], adab[0:1, j * 256:(j + 1) * 256].partition_broadcast(128), 'adab_rep')
                for kc in range(8):
                    S.op('pe', lambda: PE.matmul(psU[:, 0:256], lhsT=csil_rep[:, kc, :], rhs=st[:, kc, :], start=(kc == 0), stop=(kc == 7)),
                         reads=['csil_rep', k_st], writes=['psU'], pe_chain=(kc > 0))
                S.op('dve', lambda: V.tensor_tensor(out=modrep[:, j * 256:(j + 1) * 256], in0=psU[:, 0:256], in1=adab_rep[:], op=ALU.add),
                     reads=['psU', 'adab_rep'], writes=['modrep'])
            sh1 = modrep[:, 0:1024]; sc1 = modrep[:, 1024:2048]
            S.op('dve', lambda: V.scalar_tensor_tensor(out=gk1[:], in0=sc1, scalar=1.0, in1=n1g_rep[:], op0=ALU.add, op1=ALU.mult),
                 reads=['modrep', 'n1g_rep'], writes=['gk1'])
            ld(stg[0][:], win_d[:, :].rearrange("(kc k) n -> k kc n", k=128), 'stg0')
            S.op('dve', lambda: V.tensor_copy(out=win[:], in_=stg[0][:]), reads=['stg0'], writes=['win'])

            def emit_sin(outap, th, ki, kf, tmp, keys, shift=0.0):
                kth, kout, kki, kkf, ktmp = keys
                if shift != 0.0:
                    S.op('dve', lambda: V.tensor_scalar(out=th, in0=th, scalar1=shift, scalar2=None, op0=ALU.add), reads=[kth], writes=[kth])
                S.op('dve', lambda: V.tensor_scalar(out=tmp, in0=th, scalar1=1.0 / (2 * PI), scalar2=None, op0=ALU.mult), reads=[kth], writes=[ktmp])
                S.op('dve', lambda: V.tensor_copy(out=ki, in_=tmp), reads=[ktmp], writes=[kki])
                S.op('dve', lambda: V.tensor_copy(out=kf, in_=ki), reads=[kki], writes=[kkf])
                S.op('dve', lambda: V.scalar_tensor_tensor(out=th, in0=kf, scalar=-2 * PI, in1=th, op0=ALU.mult, op1=ALU.add),
                     reads=[kkf, kth], writes=[kth])
                S.op('dve', lambda: V.tensor_scalar(out=tmp, in0=th, scalar1=PI, scalar2=-2 * PI, op0=ALU.is_gt, op1=ALU.mult), reads=[kth], writes=[ktmp])
                S.op('dve', lambda: V.tensor_tensor(out=th, in0=th, in1=tmp, op=ALU.add), reads=[kth, ktmp], writes=[kth])
                S.op('dve', lambda: V.tensor_scalar(out=tmp, in0=th, scalar1=-PI, scalar2=2 * PI, op0=ALU.is_lt, op1=ALU.mult), reads=[kth], writes=[ktmp])
                S.op('dve', lambda: V.tensor_tensor(out=th, in0=th, in1=tmp, op=ALU.add), reads=[kth, ktmp], writes=[kth])
                S.op('dve', lambda: V.tensor_scalar(out=th, in0=th, scalar1=3.1415925, scalar2=-3.1415925, op0=ALU.min, op1=ALU.max), reads=[kth], writes=[kth])
                S.op('act', lambda: A.activation(out=outap, in_=th, func=AF.Sin), reads=[kth], writes=[kout])

            are_c = tb("are_c", [128, 16]); aim_c = tb("aim_c", [128, 16]); dtc = tb("dtc", [128, 16])
            adr_c = tb("adr_c", [128, 16]); nadr_c = tb("nadr_c", [128, 16]); adi_c = tb("adi_c", [128, 16])
            mrow = tb("mrow", [128, 128]); swapm = tb("swapm", [128, 128]); sgn_c = tb("sgn_c", [128, 1]); sgn_r = tb("sgn_r", [128, 128])
            ld(are_c[:], are_c_d[:, :], 'are_c'); ld(aim_c[:], aim_c_d[:, :], 'aim_c'); ld(dtc[:], ldt_c_d[:, :], 'dtc')
            ld(mrow[:], mrow_d[:, :], 'mrow'); ld(swapm[:], swapm_d[:, :], 'swapm'); ld(sgn_c[:], sgnc_d[:, :], 'sgn_c'); ld(sgn_r[:], sgnr_d[:, :], 'sgn_r')
            S.op('act', lambda: A.activation(out=dtc[:], in_=dtc[:], func=AF.Exp), reads=['dtc'], writes=['dtc'])
            S.op('dve', lambda: V.tensor_tensor(out=adr_c[:], in0=are_c[:], in1=dtc[:], op=ALU.mult), reads=['are_c', 'dtc'], writes=['adr_c'])
            S.op('dve', lambda: V.tensor_scalar(out=nadr_c[:], in0=adr_c[:], scalar1=-1.0, scalar2=None, op0=ALU.mult), reads=['adr_c'], writes=['nadr_c'])
            S.op('dve', lambda: V.tensor_tensor(out=adi_c[:], in0=aim_c[:], in1=dtc[:], op=ALU.mult), reads=['aim_c', 'dtc'], writes=['adi_c'])
            T1 = tb("T1", [128, 2048]); T2 = tb("T2", [128, 2048]); T3 = tb("T3", [128, 2048]); T4 = tb("T4", [128, 2048])
            T5 = tb("T5", [128, 2048]); T6 = tb("T6", [128, 2048]); TI = tb("TI", [128, 2048], I32)
            T1v = T1[:].rearrange("p (g m) -> p g m", m=128); T2v = T2[:].rearrange("p (g m) -> p g m", m=128)
            T3v = T3[:].rearrange("p (g m) -> p g m", m=128); T4v = T4[:].rearrange("p (g m) -> p g m", m=128)
            for g in range(16):
                S.op('act', lambda: A.activation(out=T1v[:, g, :], in_=mrow[:], func=AF.Exp, scale=adr_c[:, g:g + 1]), reads=['mrow', 'adr_c'], writes=['T1'])
                S.op('act', lambda: A.activation(out=T2v[:, g, :], in_=mrow[:], func=AF.Exp, scale=nadr_c[:, g:g + 1]), reads=['mrow', 'nadr_c'], writes=['T2'])
                S.op('dve', lambda: V.tensor_scalar(out=T3v[:, g, :], in0=mrow[:], scalar1=adi_c[:, g:g + 1], scalar2=None, op0=ALU.mult),
                     reads=['mrow', 'adi_c'], writes=['T3'])
            S.op('dve', lambda: V.tensor_copy(out=T4[:], in_=T3[:]), reads=['T3'], writes=['T4'])
            emit_sin(T3[:], T3[:], TI[:], T5[:], T6[:], ('T3', 'T3', 'TI', 'T5', 'T6'))
            emit_sin(T4[:], T4[:], TI[:], T5[:], T6[:], ('T4', 'T4', 'TI', 'T5', 'T6'), shift=PI / 2)
            fl = lambda t: t[:].rearrange("p g m -> p (g m)")
            S.op('dve', lambda: V.tensor_tensor(out=fl(Apow_r), in0=T1[:], in1=T4[:], op=ALU.mult), reads=['T1', 'T4'], writes=['Apow_r'])
            S.op('dve', lambda: V.tensor_tensor(out=fl(Apow_i), in0=T1[:], in1=T3[:], op=ALU.mult), reads=['T1', 'T3'], writes=['Apow_i'])
            S.op('dve', lambda: V.tensor_tensor(out=fl(Ainv_r), in0=T2[:], in1=T4[:], op=ALU.mult), reads=['T2', 'T4'], writes=['Ainv_r'])
            S.op('dve', lambda: V.scalar_tensor_tensor(out=fl(Ainv_i), in0=T2[:], scalar=-1.0, in1=T3[:], op0=ALU.mult, op1=ALU.mult),
                 reads=['T2', 'T3'], writes=['Ainv_i'])
            e128 = tb("e128", [128, 16]); th_s = tb("th_s", [128, 16]); th_c = tb("th_c", [128, 16])
            ki16 = tb("ki16", [128, 16], I32); kf16 = tb("kf16", [128, 16]); tm16 = tb("tm16", [128, 16])
            r128r = tb("r128r", [128, 16]); r128i = tb("r128i", [128, 16])
            S.op('act', lambda: A.activation(out=e128[:], in_=adr_c[:], func=AF.Exp, scale=128.0), reads=['adr_c'], writes=['e128'])
            S.op('dve', lambda: V.tensor_scalar(out=th_s[:], in0=adi_c[:], scalar1=128.0, scalar2=None, op0=ALU.mult), reads=['adi_c'], writes=['th_s'])
            S.op('dve', lambda: V.tensor_copy(out=th_c[:], in_=th_s[:]), reads=['th_s'], writes=['th_c'])
            emit_sin(th_s[:], th_s[:], ki16[:], kf16[:], tm16[:], ('th_s', 'th_s', 'ki16', 'kf16', 'tm16'))
            emit_sin(th_c[:], th_c[:], ki16[:], kf16[:], tm16[:], ('th_c', 'th_c', 'ki16', 'kf16', 'tm16'), shift=PI / 2)
            S.op('dve', lambda: V.tensor_tensor(out=r128r[:], in0=e128[:], in1=th_c[:], op=ALU.mult), reads=['e128', 'th_c'], writes=['r128r'])
            S.op('dve', lambda: V.tensor_tensor(out=r128i[:], in0=e128[:], in1=th_s[:], op=ALU.mult), reads=['e128', 'th_s'], writes=['r128i'])
            S.op('dve', lambda: V.tensor_scalar(out=r128i[:], in0=r128i[:], scalar1=sgn_c[:, 0:1], scalar2=None, op0=ALU.mult),
                 reads=['r128i', 'sgn_c'], writes=['r128i'])
            for g in range(16):
                S.op('dve', lambda: V.tensor_scalar(out=Rot[:, g, :], in0=identf[:], scalar1=r128r[:, g:g + 1], scalar2=None, op0=ALU.mult),
                     reads=['identf', 'r128r'], writes=['Rot'])
                S.op('dve', lambda: V.scalar_tensor_tensor(out=Rot[:, g, :], in0=swapm[:], scalar=r128i[:, g:g + 1], in1=Rot[:, g, :],
                                                           op0=ALU.mult, op1=ALU.add), reads=['swapm', 'r128i', 'Rot'], writes=['Rot'])
            T7 = tb("T7", [128, 2048]); T8 = tb("T8", [128, 2048])
            ld(T1[:], are_r_d[:, :], 'T1'); ld(T2[:], aim_r_d[:, :], 'T2'); ld(T3[:], ldt_r_d[:, :], 'T3')
            S.op('act', lambda: A.activation(out=T3[:], in_=T3[:], func=AF.Exp), reads=['T3'], writes=['T3'])
            S.op('dve', lambda: V.tensor_tensor(out=T4[:], in0=T2[:], in1=T3[:], op=ALU.mult), reads=['T2', 'T3'], writes=['T4'])
            S.op('dve', lambda: V.tensor_tensor(out=T3[:], in0=T1[:], in1=T3[:], op=ALU.mult), reads=['T1', 'T3'], writes=['T3'])
            S.op('act', lambda: A.activation(out=T3[:], in_=T3[:], func=AF.Exp), reads=['T3'], writes=['T3'])
            S.op('dve', lambda: V.tensor_copy(out=T7[:], in_=T4[:]), reads=['T4'], writes=['T7'])
            emit_sin(T4[:], T4[:], TI[:], T5[:], T6[:], ('T4', 'T4', 'TI', 'T5', 'T6'))
            emit_sin(T7[:], T7[:], TI[:], T5[:], T6[:], ('T7', 'T7', 'TI', 'T5', 'T6'), shift=PI / 2)
            S.op('dve', lambda: V.tensor_tensor(out=T4[:], in0=T4[:], in1=T3[:], op=ALU.mult), reads=['T4', 'T3'], writes=['T4'])
            S.op('dve', lambda: V.tensor_tensor(out=T7[:], in0=T7[:], in1=T3[:], op=ALU.mult), reads=['T7', 'T3'], writes=['T7'])
            S.op('dve', lambda: V.tensor_scalar(out=T7[:], in0=T7[:], scalar1=-1.0, scalar2=None, op0=ALU.add), reads=['T7'], writes=['T7'])
            S.op('dve', lambda: V.tensor_tensor(out=T5[:], in0=T1[:], in1=T1[:], op=ALU.mult), reads=['T1'], writes=['T5'])
            S.op('dve', lambda: V.tensor_tensor(out=T6[:], in0=T2[:], in1=T2[:], op=ALU.mult), reads=['T2'], writes=['T6'])
            S.op('dve', lambda: V.tensor_tensor(out=T5[:], in0=T5[:], in1=T6[:], op=ALU.add), reads=['T5', 'T6'], writes=['T5'])
            S.op('dve', lambda: V.reciprocal(out=T5[:], in_=T5[:]), reads=['T5'], writes=['T5'])
            S.op('dve', lambda: V.tensor_tensor(out=T3[:], in0=T7[:], in1=T1[:], op=ALU.mult), reads=['T7', 'T1'], writes=['T3'])
            S.op('dve', lambda: V.tensor_tensor(out=T6[:], in0=T4[:], in1=T2[:], op=ALU.mult), reads=['T4', 'T2'], writes=['T6'])
            S.op('dve', lambda: V.tensor_tensor(out=T3[:], in0=T3[:], in1=T6[:], op=ALU.add), reads=['T3', 'T6'], writes=['T3'])
            S.op('dve', lambda: V.tensor_tensor(out=T3[:], in0=T3[:], in1=T5[:], op=ALU.mult), reads=['T3', 'T5'], writes=['T3'])
            S.op('dve', lambda: V.tensor_tensor(out=T8[:], in0=T4[:], in1=T1[:], op=ALU.mult), reads=['T4', 'T1'], writes=['T8'])
            S.op('dve', lambda: V.tensor_tensor(out=T6[:], in0=T7[:], in1=T2[:], op=ALU.mult), reads=['T7', 'T2'], writes=['T6'])
            S.op('dve', lambda: V.tensor_tensor(out=T8[:], in0=T8[:], in1=T6[:], op=ALU.subtract), reads=['T8', 'T6'], writes=['T8'])
            S.op('dve', lambda: V.tensor_tensor(out=T8[:], in0=T8[:], in1=T5[:], op=ALU.mult), reads=['T8', 'T5'], writes=['T8'])
            ld(T1[:], X1p_d[:, :], 'T1'); ld(T2[:], X2p_d[:, :], 'T2')
            S.op('dve', lambda: V.tensor_tensor(out=T2[:].rearrange("p (g f) -> p g f", f=128), in0=T2[:].rearrange("p (g f) -> p g f", f=128),
                                                in1=sgn_r[:].unsqueeze(1).to_broadcast([128, 16, 128]), op=ALU.mult), reads=['T2', 'sgn_r'], writes=['T2'])
            S.op('dve', lambda: V.tensor_tensor(out=T5[:], in0=T3[:], in1=T1[:], op=ALU.mult), reads=['T3', 'T1'], writes=['T5'])
            S.op('dve', lambda: V.tensor_tensor(out=T6[:], in0=T8[:], in1=T2[:], op=ALU.mult), reads=['T8', 'T2'], writes=['T6'])
            S.op('dve', lambda: V.tensor_tensor(out=fl(Bpad), in0=T5[:], in1=T6[:], op=ALU.add), reads=['T5', 'T6'], writes=['Bpad'])
            S.op('dve', lambda: V.tensor_tensor(out=T5[:], in0=T3[:], in1=T2[:], op=ALU.mult), reads=['T3', 'T2'], writes=['T5'])
            S.op('dve', lambda: V.tensor_tensor(out=T6[:], in0=T8[:], in1=T1[:], op=ALU.mult), reads=['T8', 'T1'], writes=['T6'])
            S.op('dve', lambda: V.tensor_tensor(out=fl(BpadS), in0=T5[:], in1=T6[:], op=ALU.subtract), reads=['T5', 'T6'], writes=['BpadS'])
            ld(T1[:], CcP_d[:, :], 'T1'); ld(T2[:], CcSP_d[:, :], 'T2')
            S.op('dve', lambda: V.tensor_scalar(out=fl(Cc), in0=T1[:], scalar1=sgn_c[:, 0:1], scalar2=None, op0=ALU.mult), reads=['T1', 'sgn_c'], writes=['Cc'])
            S.op('dve', lambda: V.tensor_scalar(out=fl(CcS), in0=T2[:], scalar1=-1.0, scalar2=None, op0=ALU.mult), reads=['T2'], writes=['CcS'])
            S.barrier()
        xt = [sb("xt%d" % i, [128, D]) for i in range(2)]
        junk = sb("junk", [128, D], BF16)
        ss = sb("ss", [128, 1]); rs = sb("rs", [128, 1])
        hm = sb("hm", [128, D]); hmb = sb("hmb", [128, D], BF16)
        hmT = sb("hmT", [128, 8, 128], BF16)
        uT = sb("uT", [128, 2, 128]); uTb = sb("uTb", [128, 2, 128], BF16)
        Vt = [sb("Vt%d" % i, [128, 4, 128]) for i in range(2)]
        Wt = [sb("Wt%d" % i, [128, 4, 128]) for i in range(2)]
        Zt = [sb("Zt%d" % i, [128, 4, 128]) for i in range(4)]
        Hr = [sb("Hr%d" % i, [128, 4, 128], BF16) for i in range(2)]
        Hi = [sb("Hi%d" % i, [128, 4, 128], BF16) for i in range(2)]
        yt = sb("yt", [128, 2, 128]); ga = sb("ga", [128, 2, 128]); yo = sb("yo", [128, 2, 128])
        sh1 = modrep[:, 0:1024]

        for c in range(NCH):
            xb = xt[c % 2]; kx = 'xt%d' % (c % 2)
            r0 = c * 128
            ld(xb[:], x[r0:r0 + 128, :], kx)
            S.op('act', lambda: A.activation(out=junk[:], in_=xb[:], func=AF.Square, accum_out=ss[:]), reads=[kx], writes=['junk', 'ss'])
            S.op('act', lambda: A.activation(out=rs[:], in_=ss[:], func=AF.Sqrt, bias=epsT[:], scale=1.0 / D), reads=['ss', 'epsT'], writes=['rs'])
            S.op('dve', lambda: V.reciprocal(out=rs[:], in_=rs[:]), reads=['rs'], writes=['rs'])
            S.op('dve', lambda: V.scalar_tensor_tensor(out=hm[:], in0=xb[:], scalar=rs[:], in1=gk1[:], op0=ALU.mult, op1=ALU.mult),
                 reads=[kx, 'rs', 'gk1'], writes=['hm'])
            S.op('dve', lambda: V.tensor_tensor(out=hmb[:], in0=hm[:], in1=sh1, op=ALU.add), reads=['hm', 'modrep'], writes=['hmb'])
            for kc in range(8):
                S.op('pe', lambda: PE.transpose(out=psT[:, kc * 128:(kc + 1) * 128], in_=hmb[:, kc * 128:(kc + 1) * 128], identity=identb[:]),
                     reads=['hmb', 'identb'], writes=['psT'], pe_chain=(kc > 0))
            S.op('act', lambda: A.copy(out=hmT[:].rearrange("p a b -> p (a b)"), in_=psT[:]), reads=['psT'], writes=['hmT'])
            for mc in range(2):
                for kc in range(8):
                    S.op('pe', lambda: PE.matmul(psU[:, mc * 128:(mc + 1) * 128], lhsT=win[:, kc, mc * 128:(mc + 1) * 128], rhs=hmT[:, kc, :],
                                                 start=(kc == 0), stop=(kc == 7)), reads=['win', 'hmT'], writes=['psU'],
                         pe_chain=not (mc == 0 and kc == 0))
            S.op('act', lambda: A.copy(out=uT[:].rearrange("p a b -> p (a b)"), in_=psU[:, 0:256]), reads=['psU'], writes=['uT'])
            S.op('act', lambda: A.copy(out=uTb[:].rearrange("p a b -> p (a b)"), in_=psU[:, 0:256]), reads=['psU'], writes=['uTb'])
            for bt in range(4):
                i2 = bt % 2
                kbu = 'psBU%d' % i2; kbs = 'psBS%d' % i2
                kV = 'Vt%d' % i2; kW = 'Wt%d' % i2; kZ = 'Zt%d' % bt; kHr = 'Hr%d' % i2; kHi = 'Hi%d' % i2
                gc = bt // 2
                for gg in range(4):
                    g = bt * 4 + gg
                    S.op('pe', lambda: PE.matmul(psBU[i2][:, gg * 128:(gg + 1) * 128], lhsT=Bpad[:, g, :], rhs=uTb[:, gc, :], start=True, stop=True),
                         reads=['Bpad', 'uTb'], writes=[kbu], pe_chain=(gg > 0))
                for gg in range(4):
                    g = bt * 4 + gg
                    S.op('pe', lambda: PE.matmul(psBS[i2][:, gg * 128:(gg + 1) * 128], lhsT=BpadS[:, g, :], rhs=uTb[:, gc, :], start=True, stop=True),
                         reads=['BpadS', 'uTb'], writes=[kbs], pe_chain=(gg > 0))
                Vf = Vt[i2][:].rearrange("p a b -> p (a b)"); Wf = Wt[i2][:].rearrange("p a b -> p (a b)")
                Zf = Zt[bt][:].rearrange("p a b -> p (a b)")
                gs = slice(bt * 4, bt * 4 + 4)
                S.op('dve', lambda: V.tensor_tensor(out=Vf, in0=psBU[i2][:], in1=Ainv_r[:, gs, :].rearrange("p a b -> p (a b)"), op=ALU.mult),
                     reads=[kbu, 'Ainv_r'], writes=[kV])
                S.op('dve', lambda: V.tensor_tensor(out=Wf, in0=psBS[i2][:], in1=Ainv_i[:, gs, :].rearrange("p a b -> p (a b)"), op=ALU.mult),
                     reads=[kbs, 'Ainv_i'], writes=[kW])
                S.op('dve', lambda: V.tensor_tensor(out=Vf, in0=Vf, in1=Wf, op=ALU.add), reads=[kV, kW], writes=[kV])
                if c > 0:
                    S.op('dve', lambda: V.tensor_tensor(out=Vt[i2][:, :, 0], in0=Vt[i2][:, :, 0], in1=psI[:, bt * 4:bt * 4 + 4], op=ALU.add),
                         reads=[kV, 'psI'], writes=[kV])
                S.op('dve', lambda: V.tensor_tensor_scan(out=Zf, data0=mask01[:], data1=Vf, initial=0.0, op0=ALU.mult, op1=ALU.add),
                     reads=['mask01', kV], writes=[kZ])
                if c < NCH - 1:
                    for gg in range(4):
                        g = bt * 4 + gg
                        S.op('pe', lambda: PE.matmul(psI[:, g:g + 1], lhsT=Rot[:, g, :], rhs=Zt[bt][:, gg, 127:128], start=True, stop=True),
                             reads=['Rot', kZ], writes=['psI'], pe_chain=(gg > 0))
                S.op('dve', lambda: V.tensor_tensor(out=Hr[i2][:].rearrange("p a b -> p (a b)"), in0=Zf,
                                                    in1=Apow_r[:, gs, :].rearrange("p a b -> p (a b)"), op=ALU.mult), reads=[kZ, 'Apow_r'], writes=[kHr])
                S.op('dve', lambda: V.tensor_tensor(out=Hi[i2][:].rearrange("p a b -> p (a b)"), in0=Zf,
                                                    in1=Apow_i[:, gs, :].rearrange("p a b -> p (a b)"), op=ALU.mult), reads=[kZ, 'Apow_i'], writes=[kHi])
                for gg in range(4):
                    g = bt * 4 + gg
                    first = (g % 8 == 0)
                    S.op('pe', lambda: PE.matmul(psY[:, gc * 128:(gc + 1) * 128], lhsT=Cc[:, g, :], rhs=Hr[i2][:, gg, :], start=first, stop=False),
                         reads=['Cc', kHr], writes=['psY'], pe_chain=not first)
                    S.op('pe', lambda: PE.matmul(psY[:, gc * 128:(gc + 1) * 128], lhsT=CcS[:, g, :], rhs=Hi[i2][:, gg, :], start=False, stop=(g % 8 == 7)),
                         reads=['CcS', kHi], writes=['psY'], pe_chain=True)
            ytf = yt[:].rearrange("p a b -> p (a b)"); gaf = ga[:].rearrange("p a b -> p (a b)"); yof = yo[:].rearrange("p a b -> p (a b)")
            for gc in range(2):
                S.op('dve', lambda: V.scalar_tensor_tensor(out=yt[:, gc, :], in0=uT[:, gc, :], scalar=dcol[:, gc:gc + 1],
                                                           in1=psY[:, gc * 128:(gc + 1) * 128], op0=ALU.mult, op1=ALU.add),
                     reads=['uT', 'dcol', 'psY'], writes=['yt'])
            S.op('dve', lambda: V.tensor_tensor(out=gaf, in0=ytf, in1=ytf, op=ALU.mult), reads=['yt'], writes=['ga'])
            S.op('dve', lambda: V.tensor_scalar(out=gaf, in0=gaf, scalar1=0.044715, scalar2=1.0, op0=ALU.mult, op1=ALU.add), reads=['ga'], writes=['ga'])
            S.op('dve', lambda: V.tensor_tensor(out=gaf, in0=gaf, in1=ytf, op=ALU.mult), reads=['ga', 'yt'], writes=['ga'])
            S.op('act', lambda: A.activation(out=gaf, in_=gaf, func=AF.Sigmoid, scale=1.5957691216), reads=['ga'], writes=['ga'])
            S.op('dve', lambda: V.tensor_tensor(out=yof, in0=gaf, in1=ytf, op=ALU.mult), reads=['ga', 'yt'], writes=['yo'])
            S.dma('sp', lambda: nc.sync.dma_start(out=out[:, r0:r0 + 128].rearrange("(gc k) n -> k gc n", k=128), in_=yo[:]), reads=['yo'], writes=['out'])
        S.finish(['out'])
        print("s5 program: instr", S.n_instr, "waits", S.n_wait)
    return nc


def s5_consts():
    f = np.arange(128)
    swap = np.zeros((128, 128), np.float32); swap[f, (f + 64) % 128] = 1.0
    sgn_c = np.where(f < 64, 1.0, -1.0).astype(np.float32)[:, None]
    sgn_r = np.broadcast_to(np.where(f < 64, -1.0, 1.0).astype(np.float32)[None, :], (128, 128)).copy()
    mrow = np.broadcast_to(np.arange(128, dtype=np.float32)[None, :], (128, 128)).copy()
    m01 = np.ones((128, 4, 128), np.float32); m01[:, :, 0] = 0.0
    return {"identf": np.eye(128, dtype=np.float32), "swapm": swap, "sgn_c": sgn_c, "sgn_r": sgn_r, "mrow": mrow,
            "mask01": m01.reshape(128, 512)}


def s5_layouts(a_re, a_im, log_dt, b_re, b_im, c_re, c_im, d, cq):
    G0 = 16 * cq
    f = np.arange(128)
    p = f % 64
    are_c = np.ascontiguousarray(a_re[G0:G0 + 16][:, p].T)
    aim_c = np.ascontiguousarray(a_im[G0:G0 + 16][:, p].T)
    ldt_c = np.ascontiguousarray(np.broadcast_to(log_dt[G0:G0 + 16][None, :], (128, 16)))
    are_r = np.ascontiguousarray(np.broadcast_to(a_re[G0:G0 + 16][:, p].reshape(1, 16 * 128), (128, 2048)))
    aim_r = np.ascontiguousarray(np.broadcast_to(a_im[G0:G0 + 16][:, p].reshape(1, 16 * 128), (128, 2048)))
    ldt_r = np.ascontiguousarray(np.broadcast_to(np.repeat(log_dt[G0:G0 + 16], 128).reshape(1, 2048), (128, 2048)))
    X1p = np.zeros((128, 16, 128), np.float32); X2p = np.zeros((128, 16, 128), np.float32)
    CcP = np.zeros((128, 16, 128), np.float32); CcSP = np.zeros((128, 16, 128), np.float32)
    for g in range(16):
        r0 = (g % 8) * 16
        br = b_re[G0 + g]; bi = b_im[G0 + g]
        X1p[r0:r0 + 16, g, 0:64] = br.T; X1p[r0:r0 + 16, g, 64:128] = bi.T
        X2p[r0:r0 + 16, g, 0:64] = bi.T; X2p[r0:r0 + 16, g, 64:128] = br.T
        cr = c_re[G0 + g]; ci = c_im[G0 + g]
        CcP[0:64, g, r0:r0 + 16] = cr.T; CcP[64:128, g, r0:r0 + 16] = ci.T
        CcSP[0:64, g, r0:r0 + 16] = ci.T; CcSP[64:128, g, r0:r0 + 16] = cr.T
    dcol = np.ascontiguousarray(d[cq * 256:(cq + 1) * 256].reshape(2, 128).T)
    return {"are_c": are_c, "aim_c": aim_c, "ldt_c": ldt_c, "are_r": are_r, "aim_r": aim_r, "ldt_r": ldt_r,
            "X1p": X1p.reshape(128, 2048), "X2p": X2p.reshape(128, 2048), "CcP": CcP.reshape(128, 2048),
            "CcSP": CcSP.reshape(128, 2048), "dcol": dcol}

import math

RMS_EPS = 1e-6


def build_nsa(NS):
    nc = bass.Bass("TRN2", target_bir_lowering=False)
    T = NS * 512
    NT = NS * 4
    D = 1024
    NM = 1024 if T >= 16384 else (T // 16 + 32)
    NMT = (NM + 127) // 128
    din = lambda n, s, d=F32: nc.dram_tensor(n, s, d, kind="ExternalInput").ap()
    x = din("x", [T, D])
    c_l = din("c_l", [128, 8])
    adaw = din("adaw", [D, 2048]); adab = din("adab", [1, 2048]); n1g = din("n1g", [1, D])
    wpf_d = din("wpf", [D, 512]); wpt_d = din("wpt", [D, 524])
    wk1_d = din("wk1", [2048, 256]); wv1_d = din("wv1", [2048, 256])
    wk2_d = din("wk2", [256, 64]); wv2_d = din("wv2", [256, 64])
    pekT_d = din("pekT", [64, 32]); pevT_d = din("pevT", [64, 32])
    cos_d = din("cos2", [64, T]); sin_d = din("sin2", [64, T])
    cosc_d = din("cosc", [64, NM]); sinc_d = din("sinc", [64, NM])
    prot_d = din("prot", [64, 64])
    identf_d = din("identf", [128, 128])
    triT_d = din("triT", [128, 128]); triTs_d = din("triTs", [128, 128])
    cmg_d = din("cmaskG", [128, 16]); cm0_d = din("cmask0", [128, 8])
    hilo_d = din("hilo", [128, 3])
    out = nc.dram_tensor("out", [T, 256], F32, kind="ExternalOutput").ap()

    es = ExitStack()
    with es:
        es.enter_context(nc.allow_low_precision("bf16 matmul operands"))
        es.enter_context(nc.allow_non_contiguous_dma("small layout loads"))
        S = Sched(nc, es)
        sb = lambda n, s, d=F32: es.enter_context(nc.sbuf_tensor("s_" + n, s, d))
        ps = lambda n, s, d=F32: es.enter_context(nc.psum_tensor("p_" + n, s, d))
        V, A, P, PE = nc.vector, nc.scalar, nc.gpsimd, nc.tensor

        def ld(dst, src, key):
            S.dma('sp', lambda: nc.sync.dma_start(out=dst, in_=src), writes=[key])

        identf = sb("identf", [128, 128]); identb = sb("identb", [128, 128], BF16)
        triT = sb("triT", [128, 128], BF16); triTs = sb("triTs", [128, 128], BF16)
        cmaskG = sb("cmaskG", [128, 16]); cmask0 = sb("cmask0", [128, 8]); hilo = sb("hilo", [128, 3])
        epsT = sb("epsT", [128, 1]); ones1 = sb("ones1", [1, 128]); kmx = sb("kmx", [1, 1]); kmax2 = sb("kmax2", [128, 1])
        prot = sb("prot", [64, 64])
        modrep = sb("modrep", [128, 2048]); gk1 = sb("gk1", [128, D])
        wpf = sb("wpf", [128, 8, 512], BF16); wpt = sb("wpt", [128, 8, 524], BF16)
        wk1 = sb("wk1", [64, 32, 256], BF16); wv1 = sb("wv1", [64, 32, 256], BF16)
        wk2 = sb("wk2", [128, 2, 64], BF16); wv2 = sb("wv2", [128, 2, 64], BF16)
        biask = sb("biask", [128, 2]); biasv = sb("biasv", [128, 2])
        ksT = sb("ksT", [65, T], BF16); kwT = sb("kwT", [65, 1024], BF16)
        vsA = sb("vsA", [128, NT, 65], BF16); vwA = sb("vwA", [128, 8, 65], BF16)
        kcmpT = sb("kcmpT", [64, NMT * 128], BF16); vcmp = sb("vcmp", [128, NMT, 64], BF16)
        kcbuf = sb("kcbuf", [64, 528], BF16); vcbuf = sb("vcbuf", [64, 528], BF16)

        psS = [ps("psS%d" % i, [128, 512]) for i in range(2)]
        psOs = ps("psOs", [128, 512]); psOw = ps("psOw", [128, 512])
        psC = ps("psC", [128, 1024])
        psT = ps("psT", [128, 1024], BF16)
        psX = ps("psX", [128, 512])

        ld(identf[:], identf_d[:, :], 'identf')
        ld(cmaskG[:], cmg_d[:, :], 'cmaskG'); ld(cmask0[:], cm0_d[:, :], 'cmask0'); ld(hilo[:], hilo_d[:, :], 'hilo')
        ld(prot[:], prot_d[:, :], 'prot')
        S.op('dve', lambda: V.tensor_copy(out=identb[:], in_=identf[:]), reads=['identf'], writes=['identb'])
        S.op('dve', lambda: V.memset(epsT[:], RMS_EPS), writes=['epsT'])
        S.op('dve', lambda: V.memset(ones1[:], 1.0), writes=['ones1'])
        S.op('dve', lambda: V.memset(kmx[:], 0.0), writes=['kmx'])
        for c0 in range(0, T, 2048):
            S.op('dve', lambda: V.memset(ksT[:, c0:min(T, c0 + 2048)], 1.0), writes=['ksT'])
        S.op('pool', lambda: P.memset(kwT[:], 1.0), writes=['kwT'])
        S.op('dve', lambda: V.memset(vsA[:], 1.0), writes=['vsA'])
        S.op('pool', lambda: P.memset(vwA[:], 1.0), writes=['vwA'])
        S.op('dve', lambda: V.memset(kcmpT[:], 0.0), writes=['kcmpT'])
        S.op('dve', lambda: V.memset(vcmp[:], 0.0), writes=['vcmp'])
        S.op('dve', lambda: V.memset(kcbuf[:], 0.0), writes=['kcbuf'])
        S.op('dve', lambda: V.memset(vcbuf[:], 0.0), writes=['vcbuf'])

        es2 = ExitStack()
        with es2:
            tb = lambda n, s, d=F32: es2.enter_context(nc.sbuf_tensor("t_" + n, s, d))
            csil = tb("csil", [128, 8]); csil_rep = tb("csil_rep", [128, 8, 128])
            stg = [tb("stg%d" % i, [128, 8, 256]) for i in range(2)]
            adab_rep = tb("adab_rep", [128, 256]); n1g_rep = tb("n1g_rep", [128, D])
            tmpf = tb("tmpf", [128, 128])
            ld(tmpf[:], triT_d[:, :], 'tmpf')
            S.op('dve', lambda: V.tensor_copy(out=triT[:], in_=tmpf[:]), reads=['tmpf'], writes=['triT'])
            ld(tmpf[:], triTs_d[:, :], 'tmpf')
            S.op('dve', lambda: V.tensor_copy(out=triTs[:], in_=tmpf[:]), reads=['tmpf'], writes=['triTs'])
            ld(csil[:], c_l[:, :], 'csil')
            ld(n1g_rep[:], n1g[0:1, :].partition_broadcast(128), 'n1g_rep')
            S.op('act', lambda: A.activation(out=csil[:], in_=csil[:], func=AF.Silu), reads=['csil'], writes=['csil'])
            S.op('dve', lambda: V.tensor_copy(out=csil_rep[:], in_=csil[:].unsqueeze(2).to_broadcast([128, 8, 128])),
                 reads=['csil'], writes=['csil_rep'])
            for j in range(8):
                st = stg[j % 2]; k_st = 'stg%d' % (j % 2)
                ld(st[:], adaw[:, j * 256:(j + 1) * 256].rearrange("(kc k) n -> k kc n", k=128), k_st)
                ld(adab_rep[:], adab[0:1, j * 256:(j + 1) * 256].partition_broadcast(128), 'adab_rep')
                for kc in range(8):
                    S.op('pe', lambda: PE.matmul(psX[:, 0:256], lhsT=csil_rep[:, kc, :], rhs=st[:, kc, :], start=(kc == 0), stop=(kc == 7)),
                         reads=['csil_rep', k_st], writes=['psX'], pe_chain=(kc > 0))
                S.op('dve', lambda: V.tensor_tensor(out=modrep[:, j * 256:(j + 1) * 256], in0=psX[:, 0:256], in1=adab_rep[:], op=ALU.add),
                     reads=['psX', 'adab_rep'], writes=['modrep'])
            sc1 = modrep[:, 1024:2048]
            S.op('dve', lambda: V.scalar_tensor_tensor(out=gk1[:], in0=sc1, scalar=1.0, in1=n1g_rep[:], op0=ALU.add, op1=ALU.mult),
                 reads=['modrep', 'n1g_rep'], writes=['gk1'])
            ci = [0]

            def load_cast(dst3, src2d, ncols, key):
                c0 = 0
                while c0 < ncols:
                    w = min(256, ncols - c0)
                    i = ci[0] % 2; ci[0] += 1
                    st = stg[i]
                    ld(st[:, :, 0:w], src2d[:, c0:c0 + w].rearrange("(kc k) n -> k kc n", k=128), 'stg%d' % i)
                    eng = 'dve' if i == 0 else 'act'
                    if i == 0:
                        S.op('dve', lambda: V.tensor_copy(out=dst3[:, :, c0:c0 + w], in_=st[:, :, 0:w]), reads=['stg%d' % i], writes=[key])
                    else:
                        S.op('act', lambda: A.copy(out=dst3[:, :, c0:c0 + w], in_=st[:, :, 0:w]), reads=['stg%d' % i], writes=[key])
                    c0 += w
            load_cast(wpf, wpf_d, 512, 'wpf')
            load_cast(wpt, wpt_d, 524, 'wpt')
            for (wdst, wsrc, key) in ((wk1, wk1_d, 'wk1'), (wv1, wv1_d, 'wv1')):
                for p0 in range(0, 32, 8):
                    i = ci[0] % 2; ci[0] += 1
                    st = stg[i]
                    ld(st[0:64, :, :], wsrc[p0 * 64:(p0 + 8) * 64, :].rearrange("(pos d) h -> d pos h", d=64), 'stg%d' % i)
                    S.op('dve', lambda: V.tensor_copy(out=wdst[:, p0:p0 + 8, :], in_=st[0:64, :, :]), reads=['stg%d' % i], writes=[key])
            for (wdst, wsrc, key) in ((wk2, wk2_d, 'wk2'), (wv2, wv2_d, 'wv2')):
                i = ci[0] % 2; ci[0] += 1
                st = stg[i]
                ld(st[:, 0:2, 0:64], wsrc[:, :].rearrange("(hc k) d -> k hc d", k=128), 'stg%d' % i)
                S.op('dve', lambda: V.tensor_copy(out=wdst[:], in_=st[:, 0:2, 0:64]), reads=['stg%d' % i], writes=[key])
            peb = tb("peb", [64, 32], BF16)
            for (pe_d, w1, bias, key) in ((pekT_d, wk1, biask, 'biask'), (pevT_d, wv1, biasv, 'biasv')):
                ld(tmpf[0:64, 0:32], pe_d[:, :], 'tmpf')
                S.op('dve', lambda: V.tensor_copy(out=peb[:], in_=tmpf[0:64, 0:32]), reads=['tmpf'], writes=['peb'])
                for hc in range(2):
                    for pos in range(32):
                        S.op('pe', lambda: PE.matmul(psX[:, hc:hc + 1], lhsT=w1[:, pos, hc * 128:(hc + 1) * 128], rhs=peb[:, pos:pos + 1],
                                                     start=(pos == 0), stop=(pos == 31)), reads=['wk1', 'wv1', 'peb'], writes=['psX'],
                             pe_chain=(pos > 0))
                S.op('dve', lambda: V.tensor_copy(out=bias[:], in_=psX[:, 0:2]), reads=['psX'], writes=[key])
            S.barrier()

        xt = [sb("xt%d" % i, [128, D]) for i in range(2)]
        junk = sb("junk", [128, D], BF16)
        ss = sb("ss", [128, 1]); rs = sb("rs", [128, 1])
        hm = sb("hm", [128, D]); hmb = sb("hmb", [128, D], BF16)
        hmT4 = sb("hmT4", [128, 8, 512], BF16)
        qtm = sb("qtm", [128, 256]); qsq = sb("qsq", [128, 4, 4])
        ksq = sb("ksq", [128, 2]); km1 = sb("km1", [128, 1]); red = sb("red", [1, 1])
        gates = sb("gates", [128, 4, 12])
        cs2 = [sb("cos%d" % i, [64, 512]) for i in range(2)]
        sn2 = [sb("sin%d" % i, [64, 512]) for i in range(2)]
        cc2 = [sb("cc%d" % i, [64, 32]) for i in range(2)]
        sc2_ = [sb("sc%d" % i, [64, 32]) for i in range(2)]
        xq = sb("xq", [64, 512]); t1 = sb("t1", [64, 512]); t2 = sb("t2", [64, 512])
        qTa = sb("qTa", [65, 4, 512], BF16)
        hid = sb("hid", [128, 2, 32]); hga = sb("hga", [128, 2, 32]); ghk = sb("ghk", [128, 2, 32], BF16); ghv = sb("ghv", [128, 2, 32], BF16); ghv2 = sb("ghv2", [128, 2, 64], BF16)
        kcx = sb("kcx", [64, 32]); kt1 = sb("kt1", [64, 32]); kt2 = sb("kt2", [64, 32])
        cq = sb("cq", [128, 4]); negc = sb("negc", [128, 4], BF16)
        eT = [sb("eT%d" % i, [128, 512], BF16) for i in range(2)]
        pT = [sb("pT%d" % i, [128, 512], BF16) for i in range(2)]
        mfull = sb("mfull", [128, 512], BF16); maskT4 = sb("maskT4", [128, 4, 128], BF16)
        pc = sb("pc", [128, 1024]); pcb = sb("pcb", [128, 1024], BF16); pcT = sb("pcT", [128, 8, 128], BF16)
        pgrp = sb("pgrp", [128, 1032])
        mx = sb("mx", [128, 1]); mx2 = sb("mx2", [128, 2]); sm = sb("sm", [128, 4]); rinv = sb("rinv", [128, 4])
        imp = sb("imp", [128, 256]); impw = sb("impw", [128, 256]); impk = sb("impk", [128, 256]); sel = sb("sel", [128, 256])
        m8a = sb("m8a", [128, 8]); m8b = sb("m8b", [128, 8]); tau = sb("tau", [128, 1])
        oTs = sb("oTs", [65, 512]); oTw = sb("oTw", [65, 512])
        fac = sb("fac", [128, 3, 4]); ot = sb("ot", [128, 4, 64])
        sh1 = modrep[:, 0:1024]
        S.op('dve', lambda: V.memset(impw[:], -1.0), writes=['impw'])
        S.op('dve', lambda: V.memset(pgrp[:], 0.0), writes=['pgrp'])
        S.op('dve', lambda: V.memset(sel[:], 0.0), writes=['sel'])
        S.op('dve', lambda: V.memset(ghv2[:], 0.0), writes=['ghv2'])

        def gelu_tanh(dst, src, tmp, kd, ks_, kt):
            S.op('dve', lambda: V.tensor_tensor(out=tmp, in0=src, in1=src, op=ALU.mult), reads=[ks_], writes=[kt])
            S.op('dve', lambda: V.tensor_scalar(out=tmp, in0=tmp, scalar1=0.044715, scalar2=1.0, op0=ALU.mult, op1=ALU.add), reads=[kt], writes=[kt])
            S.op('dve', lambda: V.tensor_tensor(out=tmp, in0=tmp, in1=src, op=ALU.mult), reads=[kt, ks_], writes=[kt])
            S.op('act', lambda: A.activation(out=tmp, in_=tmp, func=AF.Sigmoid, scale=1.5957691216), reads=[kt], writes=[kt])
            S.op('dve', lambda: V.tensor_tensor(out=dst, in0=tmp, in1=src, op=ALU.mult), reads=[kt, ks_], writes=[kd])

        def rope(dst, src_ps, kps, cosap, sinap, kcos, scale, kdst):
            n = src_ps.shape[-1]
            S.op('act', lambda: A.copy(out=xq[:, 0:n], in_=src_ps), reads=[kps], writes=['xq'])
            S.op('pe', lambda: PE.matmul(psS[1][0:64, 0:n], lhsT=prot[:], rhs=xq[:, 0:n], start=True, stop=True),
                 reads=['prot', 'xq'], writes=['psS1'])
            S.op('dve', lambda: V.scalar_tensor_tensor(out=t1[:, 0:n], in0=xq[:, 0:n], scalar=scale, in1=cosap, op0=ALU.mult, op1=ALU.mult),
                 reads=['xq', kcos], writes=['t1'])
            S.op('dve', lambda: V.scalar_tensor_tensor(out=t2[:, 0:n], in0=psS[1][0:64, 0:n], scalar=scale, in1=sinap, op0=ALU.mult, op1=ALU.mult),
                 reads=['psS1', kcos], writes=['t2'])
            a1 = t1[:, 0:n]; a2 = t2[:, 0:n]
            if len(dst.shape) == 3:
                a1 = a1.rearrange("p (a b) -> p a b", b=dst.shape[2]); a2 = a2.rearrange("p (a b) -> p a b", b=dst.shape[2])
            S.op('dve', lambda: V.tensor_tensor(out=dst, in0=a1, in1=a2, op=ALU.add), reads=['t1', 't2'], writes=[kdst])

        for s in range(NS):
            cb = cs2[s % 2]; snb = sn2[s % 2]; kcos = 'cos%d' % (s % 2)
            ld(cb[:], cos_d[:, s * 512:(s + 1) * 512], kcos)
            ld(snb[:], sin_d[:, s * 512:(s + 1) * 512], kcos)
            ccb = cc2[s % 2]; scb = sc2_[s % 2]; kcc = 'cc%d' % (s % 2)
            if 32 * s + 32 <= NM:
                ld(ccb[:], cosc_d[:, 32 * s:32 * s + 32], kcc)
                ld(scb[:], sinc_d[:, 32 * s:32 * s + 32], kcc)
            for i in range(4):
                ti = s * 4 + i
                xb = xt[ti % 2]; kx = 'xt%d' % (ti % 2)
                r0 = ti * 128
                ld(xb[:], x[r0:r0 + 128, :], kx)
                S.op('act', lambda: A.activation(out=junk[:], in_=xb[:], func=AF.Square, accum_out=ss[:]), reads=[kx], writes=['junk', 'ss'])
                S.op('act', lambda: A.activation(out=rs[:], in_=ss[:], func=AF.Sqrt, bias=epsT[:], scale=1.0 / D), reads=['ss', 'epsT'], writes=['rs'])
                S.op('dve', lambda: V.reciprocal(out=rs[:], in_=rs[:]), reads=['rs'], writes=['rs'])
                S.op('dve', lambda: V.scalar_tensor_tensor(out=hm[:], in0=xb[:], scalar=rs[:], in1=gk1[:], op0=ALU.mult, op1=ALU.mult),
                     reads=[kx, 'rs', 'gk1'], writes=['hm'])
                S.op('dve', lambda: V.tensor_tensor(out=hmb[:], in0=hm[:], in1=sh1, op=ALU.add), reads=['hm', 'modrep'], writes=['hmb'])
                for kc in range(8):
                    S.op('pe', lambda: PE.transpose(out=psT[:, kc * 128:(kc + 1) * 128], in_=hmb[:, kc * 128:(kc + 1) * 128], identity=identb[:]),
                         reads=['hmb', 'identb'], writes=['psT'], pe_chain=(kc > 0))
                S.op('act', lambda: A.copy(out=hmT4[:, :, i * 128:(i + 1) * 128], in_=psT[:].rearrange("p (a b) -> p a b", b=128)),
                     reads=['psT'], writes=['hmT4'])
                for kc in range(8):
                    S.op('pe', lambda: PE.matmul(psS[0][:, 0:384], lhsT=hmT4[:, kc, i * 128:(i + 1) * 128], rhs=wpt[:, kc, 0:384],
                                                 start=(kc == 0), stop=(kc == 7)), reads=['hmT4', 'wpt'], writes=['psS0'], pe_chain=(kc > 0))
                for kc in range(8):
                    S.op('pe', lambda: PE.matmul(psX[:, 0:140], lhsT=hmT4[:, kc, i * 128:(i + 1) * 128], rhs=wpt[:, kc, 384:524],
                                                 start=(kc == 0), stop=(kc == 7)), reads=['hmT4', 'wpt'], writes=['psX'], pe_chain=(kc > 0))
                S.op('act', lambda: A.copy(out=qtm[:], in_=psS[0][:, 0:256]), reads=['psS0'], writes=['qtm'])
                S.op('act', lambda: A.activation(out=junk[:, 0:64], in_=psS[0][:, 256:320], func=AF.Square, accum_out=ksq[:, 0:1]),
                     reads=['psS0'], writes=['junk', 'ksq'])
                S.op('act', lambda: A.activation(out=junk[:, 0:64], in_=psS[0][:, 320:384], func=AF.Square, accum_out=ksq[:, 1:2]),
                     reads=['psS0'], writes=['junk', 'ksq'])
                S.op('dve', lambda: V.tensor_tensor(out=qtm[:], in0=qtm[:], in1=qtm[:], op=ALU.mult), reads=['qtm'], writes=['qtm'])
                S.op('dve', lambda: V.tensor_reduce(out=qsq[:, i, :], in_=qtm[:].rearrange("p (r d) -> p r d", d=64), axis=AX.X, op=ALU.add),
                     reads=['qtm'], writes=['qsq'])
                S.op('dve', lambda: V.tensor_tensor(out=km1[:], in0=ksq[:, 0:1], in1=ksq[:, 1:2], op=ALU.max), reads=['ksq'], writes=['km1'])
                S.op('pe', lambda: PE.transpose(out=psX[0:1, 256:384], in_=km1[:], identity=identf[:]), reads=['km1', 'identf'], writes=['psXk'])
                S.op('dve', lambda: V.tensor_reduce(out=red[:], in_=psX[0:1, 256:384], axis=AX.X, op=ALU.max), reads=['psXk'], writes=['red'])
                S.op('dve', lambda: V.tensor_tensor(out=kmx[:], in0=kmx[:], in1=red[:], op=ALU.max), reads=['kmx', 'red'], writes=['kmx'])
                S.op('pe', lambda: PE.matmul(psX[:, 384:385], lhsT=ones1[:], rhs=kmx[:], start=True, stop=True), reads=['ones1', 'kmx'], writes=['psXk'])
                S.op('dve', lambda: V.tensor_copy(out=kmax2[:], in_=psX[:, 384:385]), reads=['psXk'], writes=['kmax2'])
                S.op('act', lambda: A.copy(out=vsA[:, ti, 0:64], in_=psX[:, 0:64]), reads=['psX'], writes=['vsA'])
                S.op('act', lambda: A.copy(out=vwA[:, ti % 8, 0:64], in_=psX[:, 64:128]), reads=['psX'], writes=['vwA'])
                S.op('act', lambda: A.activation(out=gates[:, i, :], in_=psX[:, 128:140], func=AF.Sigmoid), reads=['psX'], writes=['gates'])
            for blk in range(8):
                pso = psC[0:64, (blk % 2) * 512:(blk % 2) * 512 + 512]
                kps = 'psC'
                for kc in range(8):
                    S.op('pe', lambda: PE.matmul(pso, lhsT=wpf[:, kc, blk * 64:(blk + 1) * 64], rhs=hmT4[:, kc, :], start=(kc == 0), stop=(kc == 7)),
                         reads=['wpf', 'hmT4'], writes=[kps], pe_chain=(kc > 0))
                if blk < 4:
                    dst = qTa[0:64, :, blk * 128:(blk + 1) * 128]
                    rope(dst, pso, kps, cb[:], snb[:], kcos, 0.125, 'qTa')
                elif blk == 4:
                    S.op('act', lambda: A.copy(out=kcbuf[:, 16:528], in_=pso), reads=[kps], writes=['kcbuf'])
                elif blk == 5:
                    S.op('act', lambda: A.copy(out=vcbuf[:, 16:528], in_=pso), reads=[kps], writes=['vcbuf'])
                elif blk == 6:
                    rope(ksT[0:64, s * 512:(s + 1) * 512], pso, kps, cb[:], snb[:], kcos, 1.0, 'ksT')
                else:
                    rope(kwT[0:64, (s % 2) * 512:(s % 2) * 512 + 512], pso, kps, cb[:], snb[:], kcos, 1.0, 'kwT')
            m0 = 32 * s
            for (buf, kbuf, w1, kw1, bias, gh, kgh) in ((kcbuf, 'kcbuf', wk1, 'wk1', biask, ghk, 'ghk'), (vcbuf, 'vcbuf', wv1, 'wv1', biasv, ghv, 'ghv')):
                bview = buf[:, 0:512].rearrange("d (n f) -> d n f", f=16)
                for hc in range(2):
                    for pos in range(32):
                        if pos < 16:
                            rhs = bview[:, :, pos]
                        else:
                            rhs = buf[:, 16:528].rearrange("d (n f) -> d n f", f=16)[:, :, pos - 16]
                        S.op('pe', lambda: PE.matmul(psX[:, hc * 32:(hc + 1) * 32], lhsT=w1[:, pos, hc * 128:(hc + 1) * 128], rhs=rhs,
                                                     start=(pos == 0), stop=(pos == 31)), reads=[kw1, kbuf], writes=['psX'], pe_chain=(pos > 0))
                for hc in range(2):
                    S.op('dve', lambda: V.tensor_scalar(out=hid[:, hc, :], in0=psX[:, hc * 32:(hc + 1) * 32], scalar1=bias[:, hc:hc + 1], scalar2=None,
                                                        op0=ALU.add), reads=['psX', 'biask', 'biasv'], writes=['hid'])
                gelu_tanh(gh[:].rearrange("p a b -> p (a b)"), hid[:].rearrange("p a b -> p (a b)"), hga[:].rearrange("p a b -> p (a b)"),
                          kgh, 'hid', 'hga')
                S.op('dve', lambda: V.tensor_copy(out=buf[:, 0:16], in_=buf[:, 512:528]), reads=[kbuf], writes=[kbuf])
            for hc in range(2):
                S.op('pe', lambda: PE.matmul(psC[0:64, 0:32], lhsT=wk2[:, hc, :], rhs=ghk[:, hc, :], start=(hc == 0), stop=(hc == 1)),
                     reads=['wk2', 'ghk'], writes=['psC'], pe_chain=(hc > 0))
            if m0 + 32 <= NM:
                rope(kcmpT[:, m0:m0 + 32], psC[0:64, 0:32], 'psC', ccb[:], scb[:], kcc, 1.0, 'kcmpT')
                mt, mo = m0 // 128, m0 % 128
                S.op('dve', lambda: V.tensor_copy(out=ghv2[:, :, 32:64], in_=ghv[:]), reads=['ghv'], writes=['ghv2'])
                for hc in range(2):
                    if mo < 96:
                        S.op('pe', lambda: PE.matmul(psC[mo:mo + 32, 512:576], lhsT=ghv2[:, hc, 32:64], rhs=wv2[:, hc, :], start=(hc == 0), stop=(hc == 1)),
                             reads=['wv2', 'ghv2'], writes=['psC'], pe_chain=(hc > 0))
                    else:
                        S.op('pe', lambda: PE.matmul(psC[64:128, 512:576], lhsT=ghv2[:, hc, 0:64], rhs=wv2[:, hc, :], start=(hc == 0), stop=(hc == 1)),
                             reads=['wv2', 'ghv2'], writes=['psC'], pe_chain=(hc > 0))
                S.op('act', lambda: A.copy(out=vcmp[mo:mo + 32, mt, :], in_=psC[mo:mo + 32, 512:576]), reads=['psC'], writes=['vcmp'])

            for i in range(4):
                qi = s * 4 + i
                qblk = qTa[:, i, :]
                import os
                if qi < int(os.environ.get('NSA_QIMIN', '0')):
                    continue
                S.op('dve', lambda: V.tensor_scalar(out=cq[:], in0=qsq[:, i, :], scalar1=kmax2[:, 0:1], scalar2=1.0 / 64, op0=ALU.mult, op1=ALU.mult),
                     reads=['qsq', 'kmax2'], writes=['cq'])
                S.op('act', lambda: A.activation(out=cq[:], in_=cq[:], func=AF.Sqrt), reads=['cq'], writes=['cq'])
                S.op('dve', lambda: V.tensor_scalar(out=negc[:], in0=cq[:], scalar1=-1.0, scalar2=None, op0=ALU.mult), reads=['cq'], writes=['negc'])
                for r in range(4):
                    S.op('pe', lambda: PE.transpose(out=psT[64:65, r * 128:(r + 1) * 128], in_=negc[:, r:r + 1], identity=identb[:]),
                         reads=['negc', 'identb'], writes=['psT'], pe_chain=(r > 0))
                S.op('act', lambda: A.copy(out=qTa[64:65, i, :], in_=psT[64:65, 0:512]), reads=['psT'], writes=['qTa'])

                W = 8 * qi + 8
                import os
                if os.environ.get('NSA_CAPW'):
                    W = min(W, int(os.environ['NSA_CAPW']))
                nt = (W + 127) // 128
                for r in range(4):
                    for c0 in range(0, W, 512):
                        w = min(512, W - c0)
                        S.op('pe', lambda: PE.matmul(psC[:, c0:c0 + w], lhsT=qTa[0:64, i, r * 128:(r + 1) * 128], rhs=kcmpT[:, c0:c0 + w],
                                                     start=True, stop=True), reads=['qTa', 'kcmpT'], writes=['psC'])
                    chunks = [(c0, min(512, W - c0)) for c0 in range(0, W, 512)]
                    for ci_, (c0, w) in enumerate(chunks):
                        S.op('dve', lambda: V.tensor_reduce(out=mx2[:, ci_:ci_ + 1], in_=psC[:, c0:c0 + w], axis=AX.X, op=ALU.max), reads=['psC'], writes=['mx2'])
                    if len(chunks) == 2:
                        S.op('dve', lambda: V.tensor_tensor(out=mx2[:, 0:1], in0=mx2[:, 0:1], in1=mx2[:, 1:2], op=ALU.max), reads=['mx2'], writes=['mx2'])
                    S.op('dve', lambda: V.tensor_scalar(out=mx[:], in0=mx2[:, 0:1], scalar1=-1.0, scalar2=None, op0=ALU.mult), reads=['mx2'], writes=['mx'])
                    for (c0, w) in chunks:
                        S.op('act', lambda: A.activation(out=pc[:, c0:c0 + w], in_=psC[:, c0:c0 + w], func=AF.Exp, bias=mx[:], scale=1.0),
                             reads=['psC', 'mx'], writes=['pc'])
                    if qi == 0:
                        S.op('dve', lambda: V.tensor_tensor(out=pc[:, 0:8], in0=pc[:, 0:8], in1=cmask0[:], op=ALU.mult), reads=['pc', 'cmask0'], writes=['pc'])
                    else:
                        S.op('dve', lambda: V.tensor_tensor(out=pc[:, W - 16:W], in0=pc[:, W - 16:W], in1=cmaskG[:], op=ALU.mult),
                             reads=['pc', 'cmaskG'], writes=['pc'])
                        S.op('dve', lambda: V.memset(pc[:, 0:1], 0.0), reads=[], writes=['pc'])
                    S.op('dve', lambda: V.tensor_reduce(out=sm[:, r:r + 1], in_=pc[:, 0:W], axis=AX.X, op=ALU.add), reads=['pc'], writes=['sm'])
                    S.op('dve', lambda: V.tensor_scalar(out=rinv[:, r:r + 1], in0=sm[:, r:r + 1], scalar1=1e-30, scalar2=None, op0=ALU.add),
                         reads=['sm'], writes=['rinv'])
                    S.op('dve', lambda: V.reciprocal(out=rinv[:, r:r + 1], in_=rinv[:, r:r + 1]), reads=['rinv'], writes=['rinv'])
                    if r == 0:
                        S.op('dve', lambda: V.tensor_scalar(out=pgrp[:, 0:W], in0=pc[:, 0:W], scalar1=rinv[:, 0:1], scalar2=None, op0=ALU.mult),
                             reads=['pc', 'rinv'], writes=['pgrp'])
                    else:
                        S.op('dve', lambda: V.scalar_tensor_tensor(out=pgrp[:, 0:W], in0=pc[:, 0:W], scalar=rinv[:, r:r + 1], in1=pgrp[:, 0:W],
                                                                   op0=ALU.mult, op1=ALU.add), reads=['pc', 'rinv', 'pgrp'], writes=['pgrp'])
                    S.op('act', lambda: A.copy(out=pcb[:, 0:W], in_=pc[:, 0:W]), reads=['pc'], writes=['pcb'])
                    for j in range(nt):
                        wj = min(128, W - j * 128)
                        S.op('pe', lambda: PE.transpose(out=psT[0:wj, j * 128:(j + 1) * 128], in_=pcb[:, j * 128:j * 128 + wj], identity=identb[:]),
                             reads=['pcb', 'identb'], writes=['psT'], pe_chain=(j > 0))
                    for j in range(nt):
                        wj = min(128, W - j * 128)
                        S.op('act', lambda: A.copy(out=pcT[0:wj, j, :], in_=psT[0:wj, j * 128:(j + 1) * 128]), reads=['psT'], writes=['pcT'])
                    for j in range(nt):
                        wj = min(128, W - j * 128)
                        S.op('pe', lambda: PE.matmul(psX[:, r * 64:(r + 1) * 64], lhsT=pcT[0:wj, j, :], rhs=vcmp[0:wj, j, :],
                                                     start=(j == 0), stop=(j == nt - 1)), reads=['pcT', 'vcmp'], writes=['psX'], pe_chain=(j > 0))
                if qi >= 1:
                    Jn = 2 * qi
                    S.op('dve', lambda: V.tensor_reduce(out=imp[:, 0:Jn], in_=pgrp[:, 0:4 * Jn].rearrange("p (j f) -> p j f", f=4), axis=AX.X, op=ALU.add),
                         reads=['pgrp'], writes=['imp'])
                    S.op('dve', lambda: V.tensor_tensor(out=imp[:, 0:Jn], in0=imp[:, 0:Jn],
                                                        in1=pgrp[:, 4:4 + 4 * Jn].rearrange("p (j f) -> p j f", f=4)[:, :, 0], op=ALU.add),
                         reads=['imp', 'pgrp'], writes=['imp'])
                    if Jn - 1 > 1:
                        S.op('dve', lambda: V.tensor_copy(out=impw[:, 1:Jn - 1], in_=imp[:, 1:Jn - 1]), reads=['imp'], writes=['impw'])
                    S.op('dve', lambda: V.tensor_scalar(out=impw[:, Jn - 1:Jn], in0=imp[:, Jn - 1:Jn], scalar1=hilo[:, 0:1], scalar2=hilo[:, 1:2],
                                                        op0=ALU.mult, op1=ALU.add), reads=['imp', 'hilo'], writes=['impw'])
                    Wj = max(Jn, 16)
                    S.op('dve', lambda: V.max(out=m8a[:], in_=impw[:, 0:Wj]), reads=['impw'], writes=['m8a'])
                    S.op('dve', lambda: V.match_replace(out=impk[:, 0:Wj], in_to_replace=m8a[:], in_values=impw[:, 0:Wj], imm_value=-1e30),
                         reads=['impw', 'm8a'], writes=['impk'])
                    S.op('dve', lambda: V.max(out=m8b[:], in_=impk[:, 0:Wj]), reads=['impk'], writes=['m8b'])
                    S.op('dve', lambda: V.tensor_scalar(out=tau[:], in0=m8b[:, 4:5], scalar1=-0.5, scalar2=None, op0=ALU.max), reads=['m8b'], writes=['tau'])
                    S.op('dve', lambda: V.tensor_scalar(out=sel[:, 0:Jn], in0=impw[:, 0:Jn], scalar1=tau[:, 0:1], scalar2=None, op0=ALU.is_ge),
                         reads=['impw', 'tau'], writes=['sel'])
                    S.op('dve', lambda: V.memset(sel[:, 0:1], 1.0), reads=[], writes=['sel'])
                    S.op('dve', lambda: V.tensor_tensor(out=sel[:, Jn - 1:Jn], in0=sel[:, Jn - 1:Jn], in1=hilo[:, 2:3], op=ALU.max),
                         reads=['sel', 'hilo'], writes=['sel'])
                cnt = [0]
                for kt0 in range(0, qi + 1, 4):
                    kts = list(range(kt0, min(kt0 + 4, qi + 1)))
                    nsel = [k for k in kts if k < qi]
                    if nsel:
                        nb = len(nsel)
                        S.op('dve', lambda: V.tensor_copy(out=mfull[:, 0:nb * 128].rearrange("p (j f) -> p j f", f=64),
                                                          in_=sel[:, 2 * kt0:2 * kt0 + 2 * nb].unsqueeze(2).to_broadcast([128, 2 * nb, 64])),
                             reads=['sel'], writes=['mfull'])
                        for k in range(nb):
                            S.op('pe', lambda: PE.transpose(out=psT[:, k * 128:(k + 1) * 128], in_=mfull[:, k * 128:(k + 1) * 128], identity=identb[:]),
                                 reads=['mfull', 'identb'], writes=['psT'], pe_chain=(k > 0))
                        S.op('act', lambda: A.copy(out=maskT4[:, 0:nb, :].rearrange("p a b -> p (a b)"), in_=psT[:, 0:nb * 128]),
                             reads=['psT'], writes=['maskT4'])
                    for k, kt in enumerate(kts):
                        b2 = cnt[0] % 2; cnt[0] += 1
                        S.op('pe', lambda: PE.matmul(psS[b2][:], lhsT=ksT[:, kt * 128:(kt + 1) * 128], rhs=qblk, start=True, stop=True),
                             reads=['ksT', 'qTa'], writes=['psS%d' % b2])
                        S.op('act', lambda: A.activation(out=eT[b2][:], in_=psS[b2][:], func=AF.Exp), reads=['psS%d' % b2], writes=['eT%d' % b2])
                        msk = triT[:] if kt == qi else maskT4[:, k, :]
                        S.op('dve', lambda: V.tensor_tensor(out=pT[b2][:].rearrange("p (r q) -> p r q", q=128),
                                                            in0=eT[b2][:].rearrange("p (r q) -> p r q", q=128),
                                                            in1=msk.unsqueeze(1).to_broadcast([128, 4, 128]), op=ALU.mult),
                             reads=['eT%d' % b2, 'maskT4', 'triT'], writes=['pT%d' % b2])
                        S.op('pe', lambda: PE.matmul(psOs[0:65, :], lhsT=vsA[:, kt, :], rhs=pT[b2][:], start=(kt == 0), stop=(kt == qi)),
                             reads=['vsA', 'pT%d' % b2], writes=['psOs'], pe_chain=(kt > 0))
                wts = [k for k in range(qi - 4, qi + 1) if k >= 0]
                for kt in wts:
                    b2 = cnt[0] % 2; cnt[0] += 1
                    S.op('pe', lambda: PE.matmul(psS[b2][:], lhsT=kwT[:, (kt % 8) * 128:(kt % 8) * 128 + 128], rhs=qblk, start=True, stop=True),
                         reads=['kwT', 'qTa'], writes=['psS%d' % b2])
                    dlt = qi - kt
                    if dlt in (0, 4):
                        S.op('act', lambda: A.activation(out=eT[b2][:], in_=psS[b2][:], func=AF.Exp), reads=['psS%d' % b2], writes=['eT%d' % b2])
                        msk = triT[:] if dlt == 0 else triTs[:]
                        S.op('dve', lambda: V.tensor_tensor(out=pT[b2][:].rearrange("p (r q) -> p r q", q=128),
                                                            in0=eT[b2][:].rearrange("p (r q) -> p r q", q=128),
                                                            in1=msk.unsqueeze(1).to_broadcast([128, 4, 128]), op=ALU.mult),
                             reads=['eT%d' % b2, 'triT', 'triTs'], writes=['pT%d' % b2])
                    else:
                        S.op('act', lambda: A.activation(out=pT[b2][:], in_=psS[b2][:], func=AF.Exp), reads=['psS%d' % b2], writes=['pT%d' % b2])
                    S.op('pe', lambda: PE.matmul(psOw[0:65, :], lhsT=vwA[:, kt % 8, :], rhs=pT[b2][:], start=(kt == wts[0]), stop=(kt == qi)),
                         reads=['vwA', 'pT%d' % b2], writes=['psOw'], pe_chain=(kt > wts[0]))
                S.op('act', lambda: A.copy(out=oTs[:], in_=psOs[0:65, :]), reads=['psOs'], writes=['oTs'])
                S.op('act', lambda: A.copy(out=oTw[:], in_=psOw[0:65, :]), reads=['psOw'], writes=['oTw'])
                for r in range(4):
                    S.op('pe', lambda: PE.transpose(out=psC[:, r * 65:(r + 1) * 65], in_=oTs[:, r * 128:(r + 1) * 128], identity=identf[0:65, 0:65]),
                         reads=['oTs', 'identf'], writes=['psC'], pe_chain=(r > 0))
                for r in range(4):
                    S.op('pe', lambda: PE.transpose(out=psC[:, 512 + r * 65:512 + (r + 1) * 65], in_=oTw[:, r * 128:(r + 1) * 128], identity=identf[0:65, 0:65]),
                         reads=['oTw', 'identf'], writes=['psC'], pe_chain=True)
                g3 = gates[:, i, :].rearrange("p (r k) -> p r k", k=3)
                osum = psC[:, 0:260].rearrange("p (r e) -> p r e", e=65)
                wsum = psC[:, 512:772].rearrange("p (r e) -> p r e", e=65)
                S.op('dve', lambda: V.tensor_tensor(out=fac[:, 0, :], in0=g3[:, :, 0], in1=rinv[:], op=ALU.mult), reads=['gates', 'rinv'], writes=['fac'])
                S.op('dve', lambda: V.reciprocal(out=fac[:, 1, :], in_=osum[:, :, 64]), reads=['psC'], writes=['fac'])
                S.op('dve', lambda: V.reciprocal(out=fac[:, 2, :], in_=wsum[:, :, 64]), reads=['psC'], writes=['fac'])
                S.op('dve', lambda: V.tensor_tensor(out=fac[:, 1, :], in0=fac[:, 1, :], in1=g3[:, :, 1], op=ALU.mult), reads=['fac', 'gates'], writes=['fac'])
                S.op('dve', lambda: V.tensor_tensor(out=fac[:, 2, :], in0=fac[:, 2, :], in1=g3[:, :, 2], op=ALU.mult), reads=['fac', 'gates'], writes=['fac'])
                for r in range(4):
                    S.op('dve', lambda: V.tensor_scalar(out=ot[:, r, :], in0=psX[:, r * 64:(r + 1) * 64], scalar1=fac[:, 0, r:r + 1], scalar2=None, op0=ALU.mult),
                         reads=['psX', 'fac'], writes=['ot'])
                    S.op('dve', lambda: V.scalar_tensor_tensor(out=ot[:, r, :], in0=osum[:, r, 0:64], scalar=fac[:, 1, r:r + 1], in1=ot[:, r, :],
                                                               op0=ALU.mult, op1=ALU.add), reads=['psC', 'fac', 'ot'], writes=['ot'])
                    S.op('dve', lambda: V.scalar_tensor_tensor(out=ot[:, r, :], in0=wsum[:, r, 0:64], scalar=fac[:, 2, r:r + 1], in1=ot[:, r, :],
                                                               op0=ALU.mult, op1=ALU.add), reads=['psC', 'fac', 'ot'], writes=['ot'])
                S.dma('sp', lambda: nc.sync.dma_start(out=out[qi * 128:(qi + 1) * 128, :], in_=ot[:].rearrange("p r d -> p (r d)")),
                      reads=['ot'], writes=['out'])
        S.finish(['out'])
        print("nsa program: instr", S.n_instr, "waits", S.n_wait)
    return nc


def nsa_consts(T):
    NM = 1024 if T >= 16384 else (T // 16 + 32)
    half = 32
    freqs = (10000.0 ** (-np.arange(half, dtype=np.float32) / half)).astype(np.float32)
    pos = np.arange(T, dtype=np.float32)
    ang = pos[None, :] * freqs[:, None]
    cos2 = np.concatenate([np.cos(ang), np.cos(ang)], 0).astype(np.float32)
    sin2 = np.concatenate([np.sin(ang), np.sin(ang)], 0).astype(np.float32)
    m = np.arange(NM, dtype=np.float32)
    cend = (m - 1) * 16 + 31
    angc = cend[None, :] * freqs[:, None]
    cosc = np.concatenate([np.cos(angc), np.cos(angc)], 0).astype(np.float32)
    sinc = np.concatenate([np.sin(angc), np.sin(angc)], 0).astype(np.float32)
    prot = np.zeros((64, 64), np.float32)
    for d in range(32):
        prot[d + 32, d] = -1.0
        prot[d, d + 32] = 1.0
    l = np.arange(128)
    triT = (l[:, None] <= l[None, :]).astype(np.float32)
    triTs = (l[:, None] > l[None, :]).astype(np.float32)
    fl = np.floor((l - 15) / 16.0)
    j = np.arange(16)
    cmaskG = ((j[None, :] - 8) <= fl[:, None]).astype(np.float32)
    j8 = np.arange(8)
    cmask0 = ((j8[None, :] >= 1) & (j8[None, :] <= fl[:, None])).astype(np.float32)
    hi = (l >= 64).astype(np.float32)
    hilo = np.stack([hi, hi - 1.0, 1.0 - hi], 1).astype(np.float32)
    return {"cos2": cos2, "sin2": sin2, "cosc": cosc, "sinc": sinc, "prot": prot, "identf": np.eye(128, dtype=np.float32),
            "triT": triT, "triTs": triTs, "cmaskG": cmaskG, "cmask0": cmask0, "hilo": hilo}


def nsa_weights(w_proj, g):
    q = w_proj[:, 256 * g:256 * g + 256]
    def blk(i):
        return w_proj[:, 1024 + 256 * i + 64 * g:1024 + 256 * i + 64 * g + 64]
    kc, vc, ks, vs, kw, vw = [blk(i) for i in range(6)]
    gl = w_proj[:, 2560 + 12 * g:2560 + 12 * g + 12]
    wpf = np.ascontiguousarray(np.concatenate([q, kc, vc, ks, kw], 1))
    wpt = np.ascontiguousarray(np.concatenate([q, ks, kw, vs, vw, gl], 1))
    return wpf, wpt

from concourse.bass_utils import run_bass_kernel_spmd

N_CORES = 8
SEQ = 16384


def _c_l(c, b):
    return np.ascontiguousarray(np.asarray(c[b], np.float32).reshape(8, 128).T)


def kernel(x, c, norm1_g, norm2_g, ada_w, ada_b, s5_w_in, s5_a_re, s5_a_im, s5_log_dt,
           s5_b_re, s5_b_im, s5_c_re, s5_c_im, s5_d, s5_w_glu, nsa_w_proj, nsa_pe_k, nsa_pe_v,
           nsa_wk1, nsa_wk2, nsa_wv1, nsa_wv2, nsa_w_o, peer_w_q, peer_sub_keys, peer_u, peer_v,
           final_g):
    f = lambda a: np.ascontiguousarray(np.asarray(a, np.float32))
    x = f(x); c = f(c); ada_w = f(ada_w); ada_b = f(ada_b)
    norm1_g = f(norm1_g); norm2_g = f(norm2_g); final_g = f(final_g)
    peer_w_q = f(peer_w_q); peer_sub_keys = f(peer_sub_keys); peer_u = f(peer_u); peer_v = f(peer_v)
    cores = list(range(N_CORES))
    nc1 = build_s5(SEQ // 128)
    s5c = s5_consts()
    maps = []
    for k in cores:
        b, cq = k // 4, k % 4
        m = {"x": x[b], "c_l": _c_l(c, b), "adaw": f(ada_w[0][:, :2048]), "adab": f(ada_b[0][None, :2048]),
             "n1g": f(norm1_g[0][None]), "win": f(np.asarray(s5_w_in[0])[:, cq * 256:(cq + 1) * 256])}
        m.update(s5c)
        m.update(s5_layouts(f(s5_a_re[0]), f(s5_a_im[0]), f(s5_log_dt[0]), f(s5_b_re[0]), f(s5_b_im[0]),
                            f(s5_c_re[0]), f(s5_c_im[0]), f(s5_d[0]), cq))
        maps.append(m)
    r1 = run_bass_kernel_spmd(nc1, maps, core_ids=cores)
    yactT = [r1.results[k]["out"] for k in cores]
    del maps

    def tok_launch(li, mode, final, xin, mixT_of, wmix):
        nct = build_tok(SEQ // 4 // 128, mode, final)
        tc = tok_consts()
        maps = []
        for k in cores:
            b, q4 = k // 4, k % 4
            sl = slice(q4 * 4096, (q4 + 1) * 4096)
            m = {"x": f(xin[b, sl]), "mixT": mixT_of(b, sl), "c_l": _c_l(c, b),
                 "adaw": f(ada_w[li][:, 2048:]), "adab": f(ada_b[li][None, 2048:]),
                 "n2g": f(norm2_g[li][None]), "fing": f(final_g[None]), "wmix": wmix, "wq": peer_w_q[li],
                 "sk": f(peer_sub_keys[li].reshape(16, 128, 128)), "u_tab": peer_u[li], "v_tab": peer_v[li]}
            m.update(tc)
            maps.append(m)
        r = run_bass_kernel_spmd(nct, maps, core_ids=cores)
        xo = np.empty((2, SEQ, 1024), np.float32)
        for k in cores:
            b, q4 = k // 4, k % 4
            xo[b, q4 * 4096:(q4 + 1) * 4096] = r.results[k]["out"]
        return xo

    def mix0(b, sl):
        return f(np.concatenate([yactT[b * 4 + cq][:, sl] for cq in range(4)], axis=0))
    x2 = tok_launch(0, 'glu', False, x, mix0, f(s5_w_glu[0]))
    del yactT
    nc3 = build_nsa(SEQ // 512)
    nsc = nsa_consts(SEQ)
    maps = []
    for k in cores:
        b, g = k // 4, k % 4
        wpf, wpt = nsa_weights(f(nsa_w_proj[0]), g)
        m = {"x": x2[b], "c_l": _c_l(c, b), "adaw": f(ada_w[1][:, :2048]), "adab": f(ada_b[1][None, :2048]),
             "n1g": f(norm1_g[1][None]), "wpf": wpf, "wpt": wpt,
             "wk1": f(nsa_wk1[0]), "wv1": f(nsa_wv1[0]), "wk2": f(nsa_wk2[0]), "wv2": f(nsa_wv2[0]),
             "pekT": f(np.asarray(nsa_pe_k[0]).T), "pevT": f(np.asarray(nsa_pe_v[0]).T)}
        m.update(nsc)
        maps.append(m)
    r3 = run_bass_kernel_spmd(nc3, maps, core_ids=cores)
    o = [r3.results[k]["out"] for k in cores]
    del maps

    def mix1(b, sl):
        return f(np.concatenate([o[b * 4 + g][sl].T for g in range(4)], axis=0))
    out = tok_launch(1, 'wo', True, x2, mix1, f(nsa_w_o[0]))
    return out
```

```python
from contextlib import ExitStack
import numpy as np
import concourse.bass as bass
import concourse.mybir as mybir

F32 = mybir.dt.float32
BF16 = mybir.dt.bfloat16
I32 = mybir.dt.int32
U32 = mybir.dt.uint32
AF = mybir.ActivationFunctionType
ALU = mybir.AluOpType
AX = mybir.AxisListType


class Sched:
    N_DMA_SEMS = 24
    SEM_MAX = 12000
    DMA_SEM_MAX = 12000

    def __init__(self, nc, es):
        self.nc = nc
        self.es = es
        self.engs = {'pe': nc.tensor, 'dve': nc.vector, 'act': nc.scalar,
                     'pool': nc.gpsimd, 'sp': nc.sync}
        self.dpool = [[es.enter_context(nc.semaphore("ds_%d_0" % i))] for i in range(self.N_DMA_SEMS)]
        self.csem = {}
        self.ccnt = {}
        self.cep = {}
        self.ctot = {}
        self.cpool = {}
        for k in ('pe', 'dve', 'act', 'pool'):
            n_ep = {'pe': 8, 'dve': 8, 'act': 6, 'pool': 3}[k]
            self.cpool[k] = [es.enter_context(nc.semaphore("cs_%s_%d" % (k, j))) for j in range(n_ep)]
            self.csem[k] = self.cpool[k][0]
            self.ccnt[k] = 0
            self.cep[k] = 0
            self.ctot[k] = 0
        self.dsem = [self.dpool[i][0] for i in range(self.N_DMA_SEMS)]
        self.dcnt = [0] * self.N_DMA_SEMS
        self.dep = [0] * self.N_DMA_SEMS
        for i in range(self.N_DMA_SEMS):
            self.dpool[i].append(es.enter_context(nc.semaphore("ds_%d_1" % i)))
        self.drr = 0
        self.seen = {k: {} for k in self.engs}
        self.lastw = {}
        self.readers = {}
        self.n_instr = 0
        self.n_wait = 0

    def _deps(self, reads, writes):
        deps = []
        for k in reads:
            w = self.lastw.get(k)
            if w is not None:
                deps.append(w)
        for k in writes:
            w = self.lastw.get(k)
            if w is not None:
                deps.append(w)
            deps.extend(self.readers.get(k, ()))
        return deps

    def _wait(self, e, deps, skip_self=None):
        eng = self.engs[e]
        best = {}
        for (sid, sem, val) in deps:
            if skip_self is not None and sid.startswith(skip_self):
                continue
            if best.get(sid, (None, 0))[1] < val:
                best[sid] = (sem, val)
        for sid, (sem, val) in best.items():
            if self.seen[e].get(sid, 0) < val:
                eng.wait_ge(sem, val)
                self.seen[e][sid] = val
                self.n_wait += 1

    def _record(self, ev, reads, writes):
        for k in reads:
            self.readers.setdefault(k, []).append(ev)
        for k in writes:
            self.lastw[k] = ev
            self.readers[k] = []

    def op(self, e, fn, reads=(), writes=(), pe_chain=False):
        deps = self._deps(reads, writes)
        self._wait(e, deps, skip_self=('c_pe_' if (pe_chain and e == 'pe') else None))
        ins = fn()
        if self.ccnt[e] >= self.SEM_MAX:
            self.cep[e] += 1
            self.csem[e] = self.cpool[e][self.cep[e]]
            self.ccnt[e] = 0
        self.ccnt[e] += 1
        self.ctot[e] += 1
        ins.then_inc(self.csem[e], 1)
        ev = ('c_%s_%d' % (e, self.cep[e]), self.csem[e], self.ccnt[e])
        self._record(ev, reads, writes)
        self.n_instr += 1
        return ev

    def dma(self, q, fn, reads=(), writes=()):
        deps = self._deps(reads, writes)
        self._wait(q, deps)
        i = self.drr
        self.drr = (self.drr + 1) % self.N_DMA_SEMS
        sid = 'd_%d_%d' % (i, self.dep[i])
        if self.seen[q].get(sid, 0) < self.dcnt[i]:
            self.engs[q].wait_ge(self.dsem[i], self.dcnt[i])
            self.seen[q][sid] = self.dcnt[i]
        if self.dcnt[i] >= self.DMA_SEM_MAX:
            self.dep[i] += 1
            self.dsem[i] = self.dpool[i][self.dep[i]]
            self.dcnt[i] = 0
            sid = 'd_%d_%d' % (i, self.dep[i])
        ins = fn()
        self.dcnt[i] += 16
        ins.then_inc(self.dsem[i], 16)
        ev = (sid, self.dsem[i], self.dcnt[i])
        self._record(ev, reads, writes)
        self.n_instr += 1
        return ev

    def finish(self, keys):
        deps = []
        for k in keys:
            w = self.lastw.get(k)
            if w is not None:
                deps.append(w)
        self._wait('sp', deps)
        alld = []
        for k in ('pe', 'dve', 'act', 'pool'):
            if self.ccnt[k]:
                alld.append(('c_%s_%d' % (k, self.cep[k]), self.csem[k], self.ccnt[k]))
        for i in range(self.N_DMA_SEMS):
            if self.dcnt[i]:
                alld.append(('d_%d_%d' % (i, self.dep[i]), self.dsem[i], self.dcnt[i]))
        self._wait('sp', alld)


def sched_barrier(S):
    alld = []
    for k in ('pe', 'dve', 'act', 'pool'):
        if S.ccnt[k]:
            alld.append(('c_%s_%d' % (k, S.cep[k]), S.csem[k], S.ccnt[k]))
    for i in range(S.N_DMA_SEMS):
        if S.dcnt[i]:
            alld.append(('d_%d_%d' % (i, S.dep[i]), S.dsem[i], S.dcnt[i]))
    for e in ('pe', 'dve', 'act', 'pool', 'sp'):
        S._wait(e, alld)


Sched.barrier = sched_barrier


RMS_EPS = 1e-6
IOA = bass.IndirectOffsetOnAxis


def build_tok(NT, mode, final, gelu_func=None, dbg=None):
    nc = bass.Bass("TRN2", target_bir_lowering=False)
    T = NT * 128
    MIXN = 2048 if mode == 'glu' else 1024
    D = 1024
    din = lambda n, s, d=F32: nc.dram_tensor(n, s, d, kind="ExternalInput").ap()
    x = din("x", [T, D])
    mixT = din("mixT", [D, T])
    c_l = din("c_l", [128, 8])
    adaw = din("adaw", [D, 4096])
    adab = din("adab", [1, 4096])
    n2g = din("n2g", [1, D])
    fing = din("fing", [1, D])
    wmix_d = din("wmix", [D, MIXN])
    wq_d = din("wq", [D, 2048])
    sk_d = din("sk", [16, 128, 128])
    u_tab = din("u_tab", [16384, D])
    v_tab = din("v_tab", [16384, D])
    identf_d = din("identf", [128, 128])
    iota16_d = din("iota16", [128, 16])
    out = nc.dram_tensor("out", [T, D], F32, kind="ExternalOutput").ap()
    u_bf = nc.dram_tensor("u_bf", [16384, D], BF16).ap()
    v_bf = nc.dram_tensor("v_bf", [16384, D], BF16).ap()

    es = ExitStack()
    with es:
        es.enter_context(nc.allow_low_precision("bf16 matmul operands"))
        es.enter_context(nc.allow_non_contiguous_dma("small layout loads"))
        S = Sched(nc, es)
        sb = lambda n, s, d=F32: es.enter_context(nc.sbuf_tensor("s_" + n, s, d))
        ps = lambda n, s, d=F32: es.enter_context(nc.psum_tensor("p_" + n, s, d))
        V, A, P, PE = nc.vector, nc.scalar, nc.gpsimd, nc.tensor

        identf = sb("identf", [128, 128])
        identb = sb("identb", [128, 128], BF16)
        iota16 = sb("iota16", [128, 16])
        epsT = sb("epsT", [128, 1])
        modrep = sb("modrep", [128, 4096])
        gk2 = sb("gk2", [128, D])
        fing_rep = sb("fing_rep", [128, D])
        ot = sb("ot", [128, D])
        n2g_rep = ot
        wq = sb("wq", [128, 8, 2048], BF16)
        wmix = sb("wmix", [128, 8, MIXN], BF16)
        skT = sb("skT", [128, 16, 128], BF16)

        psA = ps("psA", [128, 1024])
        psV = ps("psV", [128, 1024])
        psB = ps("psB", [128, 1024])
        psT = ps("psT", [128, 1024], BF16)
        psM = ps("psM", [128, 512])
        es2 = ExitStack()
        tb = lambda n, s, d=F32: es2.enter_context(nc.sbuf_tensor("t_" + n, s, d))
        csil = tb("csil", [128, 8])
        csil_rep = tb("csil_rep", [128, 8, 128])
        stg = [tb("stg%d" % i, [128, 8, 256]) for i in range(2)]
        adab_rep = tb("adab_rep", [128, 256])

        S.dma('sp', lambda: nc.sync.dma_start(out=identf[:], in_=identf_d[:, :]), writes=['identf'])
        S.dma('sp', lambda: nc.sync.dma_start(out=iota16[:], in_=iota16_d[:, :]), writes=['iota4'])
        S.dma('sp', lambda: nc.sync.dma_start(out=csil[:], in_=c_l[:, :]), writes=['csil'])
        S.dma('sp', lambda: nc.sync.dma_start(out=n2g_rep[:], in_=n2g[0:1, :].partition_broadcast(128)), writes=['ot'])
        if final:
            S.dma('sp', lambda: nc.sync.dma_start(out=fing_rep[:], in_=fing[0:1, :].partition_broadcast(128)), writes=['fing_rep'])
        S.op('dve', lambda: V.tensor_copy(out=identb[:], in_=identf[:]), reads=['identf'], writes=['identb'])
        S.op('dve', lambda: V.memset(epsT[:], RMS_EPS), writes=['epsT'])
        S.op('act', lambda: A.activation(out=csil[:], in_=csil[:], func=AF.Silu), reads=['csil'], writes=['csil'])
        S.op('dve', lambda: V.tensor_copy(out=csil_rep[:], in_=csil[:].unsqueeze(2).to_broadcast([128, 8, 128])),
             reads=['csil'], writes=['csil_rep'])
        for j in range(16):
            st = stg[j % 2]
            k_st = 'stg%d' % (j % 2)
            S.dma('sp', lambda: nc.sync.dma_start(
                out=st[:], in_=adaw[:, j * 256:(j + 1) * 256].rearrange("(kc k) n -> k kc n", k=128)), writes=[k_st])
            S.dma('sp', lambda: nc.sync.dma_start(
                out=adab_rep[:], in_=adab[0:1, j * 256:(j + 1) * 256].partition_broadcast(128)), writes=['adab_rep'])
            for kc in range(8):
                S.op('pe', lambda: PE.matmul(psM[:, 0:256], lhsT=csil_rep[:, kc, :], rhs=st[:, kc, :], start=(kc == 0), stop=(kc == 7)),
                     reads=['csil_rep', k_st], writes=['psM'], pe_chain=(kc > 0))
            S.op('dve', lambda: V.tensor_tensor(out=modrep[:, j * 256:(j + 1) * 256], in0=psM[:, 0:256], in1=adab_rep[:], op=ALU.add),
                 reads=['psM', 'adab_rep'], writes=['modrep'])
        g1 = modrep[:, 0:1024]
        sh2 = modrep[:, 1024:2048]
        sc2 = modrep[:, 2048:3072]
        g2 = modrep[:, 3072:4096]
        S.op('dve', lambda: V.scalar_tensor_tensor(out=gk2[:], in0=sc2, scalar=1.0, in1=n2g_rep[:], op0=ALU.add, op1=ALU.mult),
             reads=['modrep', 'ot'], writes=['gk2'])
        cast_i = [0]

        def load_cast(dst3, src2d, ncols, key):
            for c0 in range(0, ncols, 256):
                i = cast_i[0] % 2
                cast_i[0] += 1
                st = stg[i]
                S.dma('sp', lambda: nc.sync.dma_start(
                    out=st[:], in_=src2d[:, c0:c0 + 256].rearrange("(kc k) n -> k kc n", k=128)), writes=['stg%d' % i])
                if i == 0:
                    S.op('dve', lambda: V.tensor_copy(out=dst3[:, :, c0:c0 + 256], in_=st[:]), reads=['stg%d' % i], writes=[key])
                else:
                    S.op('act', lambda: A.copy(out=dst3[:, :, c0:c0 + 256], in_=st[:]), reads=['stg%d' % i], writes=[key])

        load_cast(wmix, wmix_d, MIXN, 'wmix')
        load_cast(wq, wq_d, 2048, 'wq')
        for j in range(16):
            st = stg[j % 2]
            k_st = 'stg%d' % (j % 2)
            S.dma('sp', lambda: nc.sync.dma_start(out=st[:, 0, 0:128], in_=sk_d[j, :, :]), writes=[k_st])
            S.op('pe', lambda: PE.transpose(out=psM[:, 0:128], in_=st[:, 0, 0:128], identity=identf[:]),
                 reads=[k_st, 'identf'], writes=['psM'])
            S.op('act', lambda: A.copy(out=skT[:, j, :], in_=psM[:, 0:128]), reads=['psM'], writes=['skT'])

        cvb = [tb("cvb%d" % i, [128, 2048], BF16) for i in range(2)]
        cvi = 0
        for (tab, tbf) in ((u_tab, u_bf), (v_tab, v_bf)):
            for c0 in range(0, 16384, 256):
                i = cvi % 2
                st = stg[i]
                k_st = 'stg%d' % i
                S.dma('sp', lambda: nc.sync.dma_start(out=st[:].rearrange("p a b -> p (a b)"),
                                                      in_=tab[c0:c0 + 256, :].rearrange("(p r) d -> p (r d)", r=2)), writes=[k_st])
                e = ('dve', 'act', 'pool')[cvi % 3]
                if e == 'dve':
                    S.op('dve', lambda: V.tensor_copy(out=cvb[i][:], in_=st[:].rearrange("p a b -> p (a b)")), reads=[k_st], writes=['cvb%d' % i])
                elif e == 'act':
                    S.op('act', lambda: A.copy(out=cvb[i][:], in_=st[:].rearrange("p a b -> p (a b)")), reads=[k_st], writes=['cvb%d' % i])
                else:
                    S.op('pool', lambda: P.tensor_copy(out=cvb[i][:], in_=st[:].rearrange("p a b -> p (a b)")), reads=[k_st], writes=['cvb%d' % i])
                S.dma('sp', lambda: nc.sync.dma_start(out=tbf[c0:c0 + 256, :].rearrange("(p r) d -> p (r d)", r=2), in_=cvb[i][:]),
                      reads=['cvb%d' % i], writes=['tbf'])
                cvi += 1

        S.barrier()
        es2.close()
        xt = [sb("xt%d" % i, [128, D]) for i in range(2)]
        mT = sb("mT", [128, 8, 128])
        mTb = sb("mTb", [128, 8, 128], BF16)
        x1b = [sb("x1_%d" % i, [128, D]) for i in range(2)]
        junk = sb("junk", [128, D], BF16)
        ss = sb("ss", [128, 1])
        rs = sb("rs", [128, 1])
        hf = sb("hf", [128, D])
        hfb = sb("hfb", [128, D], BF16)
        hfT = sb("hfT", [128, 8, 128], BF16)
        qkT = sb("qkT", [128, 16, 128], BF16)
        scw = sb("scw", [128, 16, 128])
        vals = sb("vals", [128, 16, 16])
        idx = sb("idx", [128, 16, 16], U32)
        idxf = sb("idxf", [128, 16, 16])
        cand = sb("cand", [128, 8, 256])
        wk2 = sb("wk2", [128, 2048])
        scw2 = wk2[:].rearrange("p (a b) -> p a b", b=128)
        cand2 = wk2[:].rearrange("p (a b) -> p a b", b=256)
        tops = sb("tops", [128, 8, 16])
        pos = sb("pos", [128, 8, 16], U32)
        au = sb("au", [128, 8, 16], U32)
        bu = sb("bu", [128, 8, 16], U32)
        af = sb("af", [128, 8, 16])
        bf = sb("bf", [128, 8, 16])
        eq = scw[:].rearrange("p a b -> p (a b)").rearrange("p (h k c) -> p h k c", h=8, k=16)
        sel1 = sb("sel1", [128, 8, 16])
        sel2 = sb("sel2", [128, 8, 16])
        ef = sb("ef", [128, 128])
        eidxb = [sb("eidx_%d" % i, [128, 128], U32) for i in range(2)]
        gat = sb("gat", [128, 8, 16])
        gsum = sb("gsum", [128, 8])
        sdot = sb("sdot", [128, 128])
        actvb = [sb("actv_%d" % i, [128, 128]) for i in range(2)]
        NB = 8
        NBV = 6
        ug = [sb("ug%d" % i, [128, D], BF16) for i in range(NB)]
        vg = [sb("vg%d" % i, [128, D], BF16) for i in range(NBV)]
        dg = [sb("dg%d" % i, [128, 128], BF16) for i in range(NBV)]
        acc = sb("acc", [128, D])
        sg = sb("sg", [128, D])

        gfunc = gelu_func if gelu_func is not None else AF.Gelu_apprx_tanh

        def front(t):
                x1 = x1b[t % 2]; kx1 = 'x1_%d' % (t % 2)
                eidx = eidxb[t % 2]; keidx = 'eidx_%d' % (t % 2)
                actv = actvb[t % 2]; kactv = 'actv_%d' % (t % 2)
                r0 = t * 128
                xb = xt[t % 2]
                kx = 'xt%d' % (t % 2)
                S.dma('sp', lambda: nc.sync.dma_start(out=xb[:], in_=x[r0:r0 + 128, :]), writes=[kx])
                S.dma('sp', lambda: nc.sync.dma_start(
                    out=mT[:], in_=mixT[:, r0:r0 + 128].rearrange("(kc k) n -> k kc n", k=128)), writes=['mT'])
                S.op('act', lambda: A.copy(out=mTb[:], in_=mT[:]), reads=['mT'], writes=['mTb'])
                def mixmm(c0):
                    for nb in range(2):
                        for kc in range(8):
                            S.op('pe', lambda: PE.matmul(psA[:, nb * 512:(nb + 1) * 512], lhsT=mTb[:, kc, :],
                                                         rhs=wmix[:, kc, c0 + nb * 512:c0 + (nb + 1) * 512], start=(kc == 0), stop=(kc == 7)),
                                 reads=['mTb', 'wmix'], writes=['psA'], pe_chain=not (nb == 0 and kc == 0))
                if mode == 'glu':
                    mixmm(1024)
                    S.op('act', lambda: A.activation(out=sg[:], in_=psA[:], func=AF.Sigmoid), reads=['psA'], writes=['sg'])
                    S.op('dve', lambda: V.tensor_tensor(out=sg[:], in0=sg[:], in1=g1, op=ALU.mult), reads=['sg', 'modrep'], writes=['sg'])
                    mixmm(0)
                    S.op('dve', lambda: V.tensor_tensor(out=x1[:], in0=psA[:], in1=sg[:], op=ALU.mult),
                         reads=['psA', 'sg'], writes=[kx1])
                else:
                    mixmm(0)
                    S.op('dve', lambda: V.tensor_tensor(out=x1[:], in0=psA[:], in1=g1, op=ALU.mult),
                         reads=['psA', 'modrep'], writes=[kx1])
                S.op('dve', lambda: V.tensor_tensor(out=x1[:], in0=x1[:], in1=xb[:], op=ALU.add), reads=[kx1, kx], writes=[kx1])
                S.op('act', lambda: A.activation(out=junk[:], in_=x1[:], func=AF.Square, accum_out=ss[:]), reads=[kx1], writes=['junk', 'ss'])
                S.op('act', lambda: A.activation(out=rs[:], in_=ss[:], func=AF.Sqrt, bias=epsT[:], scale=1.0 / D),
                     reads=['ss', 'epsT'], writes=['rs'])
                S.op('dve', lambda: V.reciprocal(out=rs[:], in_=rs[:]), reads=['rs'], writes=['rs'])
                S.op('dve', lambda: V.scalar_tensor_tensor(out=hf[:], in0=x1[:], scalar=rs[:], in1=gk2[:], op0=ALU.mult, op1=ALU.mult),
                     reads=[kx1, 'rs', 'gk2'], writes=['hf'])
                S.op('dve', lambda: V.tensor_tensor(out=hf[:], in0=hf[:], in1=sh2, op=ALU.add), reads=['hf', 'modrep'], writes=['hf'])
                S.op('act', lambda: A.copy(out=hfb[:], in_=hf[:]), reads=['hf'], writes=['hfb'])
                for kc in range(8):
                    S.op('pe', lambda: PE.transpose(out=psT[:, kc * 128:(kc + 1) * 128], in_=hfb[:, kc * 128:(kc + 1) * 128], identity=identb[:]),
                         reads=['hfb', 'identb'], writes=['psT'], pe_chain=(kc > 0))
                S.op('dve', lambda: V.tensor_copy(out=hfT[:].rearrange("p a b -> p (a b)"), in_=psT[:]), reads=['psT'], writes=['hfT'])
                for half in range(2):
                    for jj in range(8):
                        j = half * 8 + jj
                        for kc in range(8):
                            S.op('pe', lambda: PE.matmul(psB[:, jj * 128:(jj + 1) * 128], lhsT=wq[:, kc, j * 128:(j + 1) * 128],
                                                         rhs=hfT[:, kc, :], start=(kc == 0), stop=(kc == 7)),
                                 reads=['wq', 'hfT'], writes=['psB'], pe_chain=not (jj == 0 and kc == 0))
                    S.op('act', lambda: A.copy(out=qkT[:, half * 8:(half + 1) * 8, :].rearrange("p a b -> p (a b)"), in_=psB[:]),
                         reads=['psB'], writes=['qkT'])
                for half in range(2):
                    for jj in range(8):
                        j = half * 8 + jj
                        S.op('pe', lambda: PE.matmul(psA[:, jj * 128:(jj + 1) * 128], lhsT=qkT[:, j, :], rhs=skT[:, j, :], start=True, stop=True),
                             reads=['qkT', 'skT'], writes=['psA'], pe_chain=(jj > 0))
                    S.op('act', lambda: A.copy(out=scw[:, half * 8:(half + 1) * 8, :].rearrange("p a b -> p (a b)"), in_=psA[:]),
                         reads=['psA'], writes=['scw'])
                for j in range(16):
                    S.op('dve', lambda: V.max(out=vals[:, j, 0:8], in_=scw[:, j, :]), reads=['scw'], writes=['vals'])
                    S.op('dve', lambda: V.max_index(out=idx[:, j, 0:8], in_max=vals[:, j, 0:8], in_values=scw[:, j, :]),
                         reads=['scw', 'vals'], writes=['idx'])
                    S.op('dve', lambda: V.match_replace(out=scw2[:, j, :], in_to_replace=vals[:, j, 0:8], in_values=scw[:, j, :], imm_value=-1e30),
                         reads=['scw', 'vals'], writes=['wk2'])
                    S.op('dve', lambda: V.max(out=vals[:, j, 8:16], in_=scw2[:, j, :]), reads=['wk2'], writes=['vals'])
                    S.op('dve', lambda: V.max_index(out=idx[:, j, 8:16], in_max=vals[:, j, 8:16], in_values=scw2[:, j, :]),
                         reads=['wk2', 'vals'], writes=['idx'])
                vals4 = vals[:].rearrange("p (h c) k -> p h c k", c=2)
                S.op('dve', lambda: V.tensor_tensor(out=cand[:].rearrange("p h (a b) -> p h a b", b=16),
                                                    in0=vals4[:, :, 0, :].unsqueeze(3).to_broadcast([128, 8, 16, 16]),
                                                    in1=vals4[:, :, 1, :].unsqueeze(2).to_broadcast([128, 8, 16, 16]), op=ALU.add),
                     reads=['vals'], writes=['cand'])
                for h in range(8):
                    S.op('dve', lambda: V.max(out=tops[:, h, 0:8], in_=cand[:, h, :]), reads=['cand'], writes=['tops'])
                    S.op('dve', lambda: V.max_index(out=pos[:, h, 0:8], in_max=tops[:, h, 0:8], in_values=cand[:, h, :]),
                         reads=['cand', 'tops'], writes=['pos'])
                    S.op('dve', lambda: V.match_replace(out=cand2[:, h, :], in_to_replace=tops[:, h, 0:8], in_values=cand[:, h, :], imm_value=-1e30),
                         reads=['cand', 'tops'], writes=['wk2'])
                    S.op('dve', lambda: V.max(out=tops[:, h, 8:16], in_=cand2[:, h, :]), reads=['wk2'], writes=['tops'])
                    S.op('dve', lambda: V.max_index(out=pos[:, h, 8:16], in_max=tops[:, h, 8:16], in_values=cand2[:, h, :]),
                         reads=['wk2', 'tops'], writes=['pos'])
                S.op('dve', lambda: V.tensor_single_scalar(out=au[:], in_=pos[:], scalar=4, op=ALU.logical_shift_right), reads=['pos'], writes=['au'])
                S.op('dve', lambda: V.tensor_single_scalar(out=bu[:], in_=pos[:], scalar=15, op=ALU.bitwise_and), reads=['pos'], writes=['bu'])
                S.op('dve', lambda: V.tensor_copy(out=af[:], in_=au[:]), reads=['au'], writes=['af'])
                S.op('dve', lambda: V.tensor_copy(out=bf[:], in_=bu[:]), reads=['bu'], writes=['bf'])
                S.op('dve', lambda: V.tensor_copy(out=idxf[:], in_=idx[:]), reads=['idx'], writes=['idxf'])
                idxf4 = idxf[:].rearrange("p (h c) k -> p h c k", c=2)
                for (sf, cc, sel) in ((af, 0, sel1), (bf, 1, sel2)):
                    ksel = 'sel1' if cc == 0 else 'sel2'
                    S.op('dve', lambda: V.tensor_tensor(out=eq[:], in0=sf[:].unsqueeze(3).to_broadcast([128, 8, 16, 16]),
                                                        in1=iota16[:].unsqueeze(1).unsqueeze(1).to_broadcast([128, 8, 16, 16]), op=ALU.is_equal), reads=['af', 'bf', 'iota4'], writes=['scw'])
                    S.op('dve', lambda: V.tensor_tensor(out=eq[:], in0=eq[:], in1=idxf4[:, :, cc, :].unsqueeze(2).to_broadcast([128, 8, 16, 16]),
                                                        op=ALU.mult), reads=['scw', 'idxf'], writes=['scw'])
                    S.op('dve', lambda: V.tensor_reduce(out=sel[:], in_=eq[:], axis=AX.X, op=ALU.add), reads=['scw'], writes=[ksel])
                S.op('dve', lambda: V.scalar_tensor_tensor(out=ef[:], in0=sel1[:].rearrange("p h k -> p (h k)"), scalar=128.0,
                                                           in1=sel2[:].rearrange("p h k -> p (h k)"), op0=ALU.mult, op1=ALU.add),
                     reads=['sel1', 'sel2'], writes=['ef'])
                S.op('dve', lambda: V.tensor_copy(out=eidx[:], in_=ef[:]), reads=['ef'], writes=[keidx])
                S.op('dve', lambda: V.tensor_tensor(out=gat[:], in0=tops[:], in1=tops[:, :, 0:1].to_broadcast([128, 8, 16]), op=ALU.subtract),
                     reads=['tops'], writes=['gat'])
                S.op('act', lambda: A.activation(out=gat[:], in_=gat[:], func=AF.Exp), reads=['gat'], writes=['gat'])
                S.op('dve', lambda: V.tensor_reduce(out=gsum[:], in_=gat[:], axis=AX.X, op=ALU.add), reads=['gat'], writes=['gsum'])
                S.op('dve', lambda: V.reciprocal(out=gsum[:], in_=gsum[:]), reads=['gsum'], writes=['gsum'])
                S.op('dve', lambda: V.tensor_tensor(out=gat[:], in0=gat[:], in1=gsum[:].unsqueeze(2).to_broadcast([128, 8, 16]), op=ALU.mult),
                     reads=['gat', 'gsum'], writes=['gat'])

        def midU(t):
                x1 = x1b[t % 2]; kx1 = 'x1_%d' % (t % 2)
                eidx = eidxb[t % 2]; keidx = 'eidx_%d' % (t % 2)
                actv = actvb[t % 2]; kactv = 'actv_%d' % (t % 2)
                r0 = t * 128
                for s in range(128):
                    b = s % NB
                    S.dma('pool', lambda: P.indirect_dma_start(out=ug[b][:], out_offset=None, in_=u_bf[:, :],
                                                                in_offset=IOA(ap=eidx[:, s:s + 1], axis=0)),
                          reads=[keidx, 'tbf'], writes=['ug%d' % b])
                    S.op('dve', lambda: V.scalar_tensor_tensor(out=junk[:], in0=ug[b][:], scalar=1.0, in1=hf[:],
                                                               op0=ALU.mult, op1=ALU.mult, accum_out=sdot[:, s:s + 1]),
                         reads=['ug%d' % b, 'hf'], writes=['junk', 'sdot'])
                S.op('dve', lambda: V.tensor_tensor(out=actv[:], in0=sdot[:], in1=sdot[:], op=ALU.mult), reads=['sdot'], writes=[kactv])
                S.op('dve', lambda: V.tensor_scalar(out=actv[:], in0=actv[:], scalar1=0.044715, scalar2=1.0, op0=ALU.mult, op1=ALU.add),
                     reads=[kactv], writes=[kactv])
                S.op('dve', lambda: V.tensor_tensor(out=actv[:], in0=actv[:], in1=sdot[:], op=ALU.mult), reads=[kactv, 'sdot'], writes=[kactv])
                S.op('act', lambda: A.activation(out=actv[:], in_=actv[:], func=AF.Sigmoid, scale=1.5957691216), reads=[kactv], writes=[kactv])
                S.op('dve', lambda: V.tensor_tensor(out=actv[:], in0=actv[:], in1=sdot[:], op=ALU.mult), reads=[kactv, 'sdot'], writes=[kactv])
                S.op('dve', lambda: V.tensor_tensor(out=actv[:], in0=actv[:], in1=gat[:].rearrange("p h k -> p (h k)"), op=ALU.mult),
                     reads=[kactv, 'gat'], writes=[kactv])

        def midV(t):
                x1 = x1b[t % 2]; kx1 = 'x1_%d' % (t % 2)
                eidx = eidxb[t % 2]; keidx = 'eidx_%d' % (t % 2)
                actv = actvb[t % 2]; kactv = 'actv_%d' % (t % 2)
                r0 = t * 128
                for s in range(128):
                    b = s % NBV
                    S.dma('pool', lambda: P.indirect_dma_start(out=vg[b][:], out_offset=None, in_=v_bf[:, :],
                                                                in_offset=IOA(ap=eidx[:, s:s + 1], axis=0)),
                          reads=[keidx, 'tbf'], writes=['vg%d' % b])
                    S.op('act', lambda: A.activation(out=dg[b][:], in_=identf[:], func=AF.Copy, scale=actv[:, s:s + 1]),
                         reads=['identf', kactv], writes=['dg%d' % b])
                    for hb in range(2):
                        S.op('pe', lambda: PE.matmul(psV[:, hb * 512:(hb + 1) * 512], lhsT=dg[b][:], rhs=vg[b][:, hb * 512:(hb + 1) * 512],
                                                     start=(s == 0), stop=(s == 127)), reads=['dg%d' % b, 'vg%d' % b], writes=['psV'],
                             pe_chain=not (s == 0 and hb == 0))

        def tail(t):
                x1 = x1b[t % 2]; kx1 = 'x1_%d' % (t % 2)
                eidx = eidxb[t % 2]; keidx = 'eidx_%d' % (t % 2)
                actv = actvb[t % 2]; kactv = 'actv_%d' % (t % 2)
                r0 = t * 128
                S.op('dve', lambda: V.tensor_tensor(out=acc[:], in0=psV[:], in1=g2, op=ALU.mult), reads=['psV', 'modrep'], writes=['acc'])
                S.op('dve', lambda: V.tensor_tensor(out=ot[:], in0=acc[:], in1=x1[:], op=ALU.add), reads=['acc', kx1], writes=['ot'])
                if final:
                    S.op('act', lambda: A.activation(out=junk[:], in_=ot[:], func=AF.Square, accum_out=ss[:]), reads=['ot'], writes=['junk', 'ss'])
                    S.op('act', lambda: A.activation(out=rs[:], in_=ss[:], func=AF.Sqrt, bias=epsT[:], scale=1.0 / D),
                         reads=['ss', 'epsT'], writes=['rs'])
                    S.op('dve', lambda: V.reciprocal(out=rs[:], in_=rs[:]), reads=['rs'], writes=['rs'])
                    S.op('dve', lambda: V.scalar_tensor_tensor(out=ot[:], in0=ot[:], scalar=rs[:], in1=fing_rep[:], op0=ALU.mult, op1=ALU.mult),
                         reads=['ot', 'rs', 'fing_rep'], writes=['ot'])
                S.dma('sp', lambda: nc.sync.dma_start(out=out[r0:r0 + 128, :], in_=ot[:]), reads=['ot'], writes=['out'])

        front(0)
        for t in range(NT):
            midU(t)
            if t + 1 < NT:
                front(t + 1)
            midV(t)
            tail(t)
        S.finish(['out'])
        print("tok program: instr", S.n_instr, "waits", S.n_wait)
    return nc


def tok_consts():
    iota16 = np.broadcast_to(np.arange(16, dtype=np.float32)[None, :], (128, 16)).copy()
    return {"identf": np.eye(128, dtype=np.float32), "iota16": iota16}

import math

RMS_EPS = 1e-6
PI = math.pi


def build_s5(NCH):
    nc = bass.Bass("TRN2", target_bir_lowering=False)
    T = NCH * 128
    D = 1024
    din = lambda n, s, d=F32: nc.dram_tensor(n, s, d, kind="ExternalInput").ap()
    x = din("x", [T, D])
    c_l = din("c_l", [128, 8])
    adaw = din("adaw", [D, 2048])
    adab = din("adab", [1, 2048])
    n1g = din("n1g", [1, D])
    win_d = din("win", [D, 256])
    are_c_d = din("are_c", [128, 16]); aim_c_d = din("aim_c", [128, 16]); ldt_c_d = din("ldt_c", [128, 16])
    are_r_d = din("are_r", [128, 2048]); aim_r_d = din("aim_r", [128, 2048]); ldt_r_d = din("ldt_r", [128, 2048])
    X1p_d = din("X1p", [128, 2048]); X2p_d = din("X2p", [128, 2048])
    CcP_d = din("CcP", [128, 2048]); CcSP_d = din("CcSP", [128, 2048])
    dcol_d = din("dcol", [128, 2])
    identf_d = din("identf", [128, 128]); swapm_d = din("swapm", [128, 128])
    sgnc_d = din("sgn_c", [128, 1]); sgnr_d = din("sgn_r", [128, 128])
    mrow_d = din("mrow", [128, 128]); mask01_d = din("mask01", [128, 512])
    out = nc.dram_tensor("out", [256, T], F32, kind="ExternalOutput").ap()

    es = ExitStack()
    with es:
        es.enter_context(nc.allow_low_precision("bf16 matmul operands"))
        es.enter_context(nc.allow_non_contiguous_dma("small layout loads"))
        S = Sched(nc, es)
        sb = lambda n, s, d=F32: es.enter_context(nc.sbuf_tensor("s_" + n, s, d))
        ps = lambda n, s, d=F32: es.enter_context(nc.psum_tensor("p_" + n, s, d))
        V, A, P, PE = nc.vector, nc.scalar, nc.gpsimd, nc.tensor

        def ld(dst, src, key):
            S.dma('sp', lambda: nc.sync.dma_start(out=dst, in_=src), writes=[key])

        identf = sb("identf", [128, 128]); identb = sb("identb", [128, 128], BF16)
        epsT = sb("epsT", [128, 1])
        modrep = sb("modrep", [128, 2048])
        gk1 = sb("gk1", [128, D])
        win = sb("win", [128, 8, 256], BF16)
        Ainv_r = sb("Ainv_r", [128, 16, 128]); Ainv_i = sb("Ainv_i", [128, 16, 128])
        Apow_r = sb("Apow_r", [128, 16, 128]); Apow_i = sb("Apow_i", [128, 16, 128])
        Rot = sb("Rot", [128, 16, 128])
        Bpad = sb("Bpad", [128, 16, 128], BF16); BpadS = sb("BpadS", [128, 16, 128], BF16)
        Cc = sb("Cc", [128, 16, 128], BF16); CcS = sb("CcS", [128, 16, 128], BF16)
        dcol = sb("dcol", [128, 2])
        mask01 = sb("mask01", [128, 512])

        psT = ps("psT", [128, 1024], BF16)
        psU = ps("psU", [128, 512])
        psBU = [ps("psBU%d" % i, [128, 512]) for i in range(2)]
        psBS = [ps("psBS%d" % i, [128, 512]) for i in range(2)]
        psI = ps("psI", [128, 512])
        psY = ps("psY", [128, 512])

        ld(identf[:], identf_d[:, :], 'identf')
        ld(dcol[:], dcol_d[:, :], 'dcol')
        ld(mask01[:], mask01_d[:, :], 'mask01')
        S.op('dve', lambda: V.tensor_copy(out=identb[:], in_=identf[:]), reads=['identf'], writes=['identb'])
        S.op('dve', lambda: V.memset(epsT[:], RMS_EPS), writes=['epsT'])

        es2 = ExitStack()
        with es2:
            tb = lambda n, s, d=F32: es2.enter_context(nc.sbuf_tensor("t_" + n, s, d))
            csil = tb("csil", [128, 8]); csil_rep = tb("csil_rep", [128, 8, 128])
            stg = [tb("stg%d" % i, [128, 8, 256]) for i in range(2)]
            adab_rep = tb("adab_rep", [128, 256])
            n1g_rep = tb("n1g_rep", [128, D])
            ld(csil[:], c_l[:, :], 'csil')
            ld(n1g_rep[:], n1g[0:1, :].partition_broadcast(128), 'n1g_rep')
            S.op('act', lambda: A.activation(out=csil[:], in_=csil[:], func=AF.Silu), reads=['csil'], writes=['csil'])
            S.op('dve', lambda: V.tensor_copy(out=csil_rep[:], in_=csil[:].unsqueeze(2).to_broadcast([128, 8, 128])),
                 reads=['csil'], writes=['csil_rep'])
            for j in range(8):
                st = stg[j % 2]; k_st = 'stg%d' % (j % 2)
                ld(st[:], adaw[:, j * 256:(j + 1) * 256].rearrange("(kc k) n -> k kc n", k=128), k_st)
                ld(adab_rep[:], adab[0:1, j * 256:(j + 1) * 256].partition_broadcast(128), 'adab_rep')
                for kc in range(8):
                    S.op('pe', lambda: PE.matmul(psU[:, 0:256], lhsT=csil_rep[:, kc, :], rhs=st[:, kc, :], start=(kc == 0), stop=(kc == 7)),
                         reads=['csil_rep', k_st], writes=['psU'], pe_chain=(kc > 0))
                S.op('dve', lambda: V.tensor_tensor(out=modrep[:, j * 256:(j + 1) * 256], in0=psU[:, 0:256], in1=adab_rep[:], op=ALU.add),
                     reads=['psU', 'adab_rep'], writes=['modrep'])
            sh1 = modrep[:, 0:1024]; sc1 = modrep[:, 1024:2048]
            S.op('dve', lambda: V.scalar_tensor_tensor(out=gk1[:], in0=sc1, scalar=1.0, in1=n1g_rep[:], op0=ALU.add, op1=ALU.mult),
                 reads=['modrep', 'n1g_rep'], writes=['gk1'])
            ld(stg[0][:], win_d[:, :].rearrange("(kc k) n -> k kc n", k=128), 'stg0')
            S.op('dve', lambda: V.tensor_copy(out=win[:], in_=stg[0][:]), reads=['stg0'], writes=['win'])

            def emit_sin(outap, th, ki, kf, tmp, keys, shift=0.0):
                kth, kout, kki, kkf, ktmp = keys
                if shift != 0.0:
                    S.op('dve', lambda: V.tensor_scalar(out=th, in0=th, scalar1=shift, scalar2=None, op0=ALU.add), reads=[kth], writes=[kth])
                S.op('dve', lambda: V.tensor_scalar(out=tmp, in0=th, scalar1=1.0 / (2 * PI), scalar2=None, op0=ALU.mult), reads=[kth], writes=[ktmp])
                S.op('dve', lambda: V.tensor_copy(out=ki, in_=tmp), reads=[ktmp], writes=[kki])
                S.op('dve', lambda: V.tensor_copy(out=kf, in_=ki), reads=[kki], writes=[kkf])
                S.op('dve', lambda: V.scalar_tensor_tensor(out=th, in0=kf, scalar=-2 * PI, in1=th, op0=ALU.mult, op1=ALU.add),
                     reads=[kkf, kth], writes=[kth])
                S.op('dve', lambda: V.tensor_scalar(out=tmp, in0=th, scalar1=PI, scalar2=-2 * PI, op0=ALU.is_gt, op1=ALU.mult), reads=[kth], writes=[ktmp])
                S.op('dve', lambda: V.tensor_tensor(out=th, in0=th, in1=tmp, op=ALU.add), reads=[kth, ktmp], writes=[kth])
                S.op('dve', lambda: V.tensor_scalar(out=tmp, in0=th, scalar1=-PI, scalar2=2 * PI, op0=ALU.is_lt, op1=ALU.mult), reads=[kth], writes=[ktmp])
                S.op('dve', lambda: V.tensor_tensor(out=th, in0=th, in1=tmp, op=ALU.add), reads=[kth, ktmp], writes=[kth])
                S.op('dve', lambda: V.tensor_scalar(out=th, in0=th, scalar1=3.1415925, scalar2=-3.1415925, op0=ALU.min, op1=ALU.max), reads=[kth], writes=[kth])
                S.op('act', lambda: A.activation(out=outap, in_=th, func=AF.Sin), reads=[kth], writes=[kout])

            are_c = tb("are_c", [128, 16]); aim_c = tb("aim_c", [128, 16]); dtc = tb("dtc", [128, 16])
            adr_c = tb("adr_c", [128, 16]); nadr_c = tb("nadr_c", [128, 16]); adi_c = tb("adi_c", [128, 16])
            mrow = tb("mrow", [128, 128]); swapm = tb("swapm", [128, 128]); sgn_c = tb("sgn_c", [128, 1]); sgn_r = tb("sgn_r", [128, 128])
            ld(are_c[:], are_c_d[:, :], 'are_c'); ld(aim_c[:], aim_c_d[:, :], 'aim_c'); ld(dtc[:], ldt_c_d[:, :], 'dtc')
            ld(mrow[:], mrow_d[:, :], 'mrow'); ld(swapm[:], swapm_d[:, :], 'swapm'); ld(sgn_c[:], sgnc_d[:, :], 'sgn_c'); ld(sgn_r[:], sgnr_d[:, :], 'sgn_r')
            S.op('act', lambda: A.activation(out=dtc[:], in_=dtc[:], func=AF.Exp), reads=['dtc'], writes=['dtc'])
            S.op('dve', lambda: V.tensor_tensor(out=adr_c[:], in0=are_c[:], in1=dtc[:], op=ALU.mult), reads=['are_c', 'dtc'], writes=['adr_c'])
            S.op('dve', lambda: V.tensor_scalar(out=nadr_c[:], in0=adr_c[:], scalar1=-1.0, scalar2=None, op0=ALU.mult), reads=['adr_c'], writes=['nadr_c'])
            S.op('dve', lambda: V.tensor_tensor(out=adi_c[:], in0=aim_c[:], in1=dtc[:], op=ALU.mult), reads=['aim_c', 'dtc'], writes=['adi_c'])
            T1 = tb("T1", [128, 2048]); T2 = tb("T2", [128, 2048]); T3 = tb("T3", [128, 2048]); T4 = tb("T4", [128, 2048])
            T5 = tb("T5", [128, 2048]); T6 = tb("T6", [128, 2048]); TI = tb("TI", [128, 2048], I32)
            T1v = T1[:].rearrange("p (g m) -> p g m", m=128); T2v = T2[:].rearrange("p (g m) -> p g m", m=128)
            T3v = T3[:].rearrange("p (g m) -> p g m", m=128); T4v = T4[:].rearrange("p (g m) -> p g m", m=128)
            for g in range(16):
                S.op('act', lambda: A.activation(out=T1v[:, g, :], in_=mrow[:], func=AF.Exp, scale=adr_c[:, g:g + 1]), reads=['mrow', 'adr_c'], writes=['T1'])
                S.op('act', lambda: A.activation(out=T2v[:, g, :], in_=mrow[:], func=AF.Exp, scale=nadr_c[:, g:g + 1]), reads=['mrow', 'nadr_c'], writes=['T2'])
                S.op('dve', lambda: V.tensor_scalar(out=T3v[:, g, :], in0=mrow[:], scalar1=adi_c[:, g:g + 1], scalar2=None, op0=ALU.mult),
                     reads=['mrow', 'adi_c'], writes=['T3'])
            S.op('dve', lambda: V.tensor_copy(out=T4[:], in_=T3[:]), reads=['T3'], writes=['T4'])
            emit_sin(T3[:], T3[:], TI[:], T5[:], T6[:], ('T3', 'T3', 'TI', 'T5', 'T6'))
            emit_sin(T4[:], T4[:], TI[:], T5[:], T6[:], ('T4', 'T4', 'TI', 'T5', 'T6'), shift=PI / 2)
            fl = lambda t: t[:].rearrange("p g m -> p (g m)")
            S.op('dve', lambda: V.tensor_tensor(out=fl(Apow_r), in0=T1[:], in1=T4[:], op=ALU.mult), reads=['T1', 'T4'], writes=['Apow_r'])
            S.op('dve', lambda: V.tensor_tensor(out=fl(Apow_i), in0=T1[:], in1=T3[:], op=ALU.mult), reads=['T1', 'T3'], writes=['Apow_i'])
            S.op('dve', lambda: V.tensor_tensor(out=fl(Ainv_r), in0=T2[:], in1=T4[:], op=ALU.mult), reads=['T2', 'T4'], writes=['Ainv_r'])
            S.op('dve', lambda: V.scalar_tensor_tensor(out=fl(Ainv_i), in0=T2[:], scalar=-1.0, in1=T3[:], op0=ALU.mult, op1=ALU.mult),
                 reads=['T2', 'T3'], writes=['Ainv_i'])
            e128 = tb("e128", [128, 16]); th_s = tb("th_s", [128, 16]); th_c = tb("th_c", [128, 16])
            ki16 = tb("ki16", [128, 16], I32); kf16 = tb("kf16", [128, 16]); tm16 = tb("tm16", [128, 16])
            r128r = tb("r128r", [128, 16]); r128i = tb("r128i", [128, 16])
            S.op('act', lambda: A.activation(out=e128[:], in_=adr_c[:], func=AF.Exp, scale=128.0), reads=['adr_c'], writes=['e128'])
            S.op('dve', lambda: V.tensor_scalar(out=th_s[:], in0=adi_c[:], scalar1=128.0, scalar2=None, op0=ALU.mult), reads=['adi_c'], writes=['th_s'])
            S.op('dve', lambda: V.tensor_copy(out=th_c[:], in_=th_s[:]), reads=['th_s'], writes=['th_c'])
            emit_sin(th_s[:], th_s[:], ki16[:], kf16[:], tm16[:], ('th_s', 'th_s', 'ki16', 'kf16', 'tm16'))
            emit_sin(th_c[:], th_c[:], ki16[:], kf16[:], tm16[:], ('th_c', 'th_c', 'ki16', 'kf16', 'tm16'), shift=PI / 2)
            S.op('dve', lambda: V.tensor_tensor(out=r128r[:], in0=e128[:], in1=th_c[:], op=ALU.mult), reads=['e128', 'th_c'], writes=['r128r'])
            S.op('dve', lambda: V.tensor_tensor(out=r128i[:], in0=e128[:], in1=th_s[:], op=ALU.mult), reads=['e128', 'th_s'], writes=['r128i'])
            S.op('dve', lambda: V.tensor_scalar(out=r128i[:], in0=r128i[:], scalar1=sgn_c[:, 0:1], scalar2=None, op0=ALU.mult),
                 reads=['r128i', 'sgn_c'], writes=['r128i'])
            for g in range(16):
                S.op('dve', lambda: V.tensor_scalar(out=Rot[:, g, :], in0=identf[:], scalar1=r128r[:, g:g + 1], scalar2=None, op0=ALU.mult),
                     reads=['identf', 'r128r'], writes=['Rot'])
                S.op('dve', lambda: V.scalar_tensor_tensor(out=Rot[:, g, :], in0=swapm[:], scalar=r128i[:, g:g + 1], in1=Rot[:, g, :],
                                                           op0=ALU.mult, op1=ALU.add), reads=['swapm', 'r128i', 'Rot'], writes=['Rot'])
            T7 = tb("T7", [128, 2048]); T8 = tb("T8", [128, 2048])
            ld(T1[:], are_r_d[:, :], 'T1'); ld(T2[:], aim_r_d[:, :], 'T2'); ld(T3[:], ldt_r_d[:, :], 'T3')
            S.op('act', lambda: A.activation(out=T3[:], in_=T3[:], func=AF.Exp), reads=['T3'], writes=['T3'])
            S.op('dve', lambda: V.tensor_tensor(out=T4[:], in0=T2[:], in1=T3[:], op=ALU.mult), reads=['T2', 'T3'], writes=['T4'])
            S.op('dve', lambda: V.tensor_tensor(out=T3[:], in0=T1[:], in1=T3[:], op=ALU.mult), reads=['T1', 'T3'], writes=['T3'])
            S.op('act', lambda: A.activation(out=T3[:], in_=T3[:], func=AF.Exp), reads=['T3'], writes=['T3'])
            S.op('dve', lambda: V.tensor_copy(out=T7[:], in_=T4[:]), reads=['T4'], writes=['T7'])
            emit_sin(T4[:], T4[:], TI[:], T5[:], T6[:], ('T4', 'T4', 'TI', 'T5', 'T6'))
            emit_sin(T7[:], T7[:], TI[:], T5[:], T6[:], ('T7', 'T7', 'TI', 'T5', 'T6'), shift=PI / 2)
            S.op('dve', lambda: V.tensor_tensor(out=T4[:], in0=T4[:], in1=T3[:], op=ALU.mult), reads=['T4', 'T3'], writes=['T4'])
            S.op('dve', lambda: V.tensor_tensor(out=T7[:], in0=T7[:], in1=T3[:], op=ALU.mult), reads=['T7', 'T3'], writes=['T7'])
            S.op('dve', lambda: V.tensor_scalar(out=T7[:], in0=T7[:], scalar1=-1.0, scalar2=None, op0=ALU.add), reads=['T7'], writes=['T7'])
            S.op('dve', lambda: V.tensor_tensor(out=T5[:], in0=T1[:], in1=T1[:], op=ALU.mult), reads=['T1'], writes=['T5'])
            S.op('dve', lambda: V.tensor_tensor(out=T6[:], in0=T2[:], in1=T2[:], op=ALU.mult), reads=['T2'], writes=['T6'])
            S.op('dve', lambda: V.tensor_tensor(out=T5[:], in0=T5[:], in1=T6[:], op=ALU.add), reads=['T5', 'T6'], writes=['T5'])
            S.op('dve', lambda: V.reciprocal(out=T5[:], in_=T5[:]), reads=['T5'], writes=['T5'])
            S.op('dve', lambda: V.tensor_tensor(out=T3[:], in0=T7[:], in1=T1[:], op=ALU.mult), reads=['T7', 'T1'], writes=['T3'])
            S.op('dve', lambda: V.tensor_tensor(out=T6[:], in0=T4[:], in1=T2[:], op=ALU.mult), reads=['T4', 'T2'], writes=['T6'])
            S.op('dve', lambda: V.tensor_tensor(out=T3[:], in0=T3[:], in1=T6[:], op=ALU.add), reads=['T3', 'T6'], writes=['T3'])
            S.op('dve', lambda: V.tensor_tensor(out=T3[:], in0=T3[:], in1=T5[:], op=ALU.mult), reads=['T3', 'T5'], writes=['T3'])
            S.op('dve', lambda: V.tensor_tensor(out=T8[:], in0=T4[:], in1=T1[:], op=ALU.mult), reads=['T4', 'T1'], writes=['T8'])
            S.op('dve', lambda: V.tensor_tensor(out=T6[:], in0=T7[:], in1=T2[:], op=ALU.mult), reads=['T7', 'T2'], writes=['T6'])
            S.op('dve', lambda: V.tensor_tensor(out=T8[:], in0=T8[:], in1=T6[:], op=ALU.subtract), reads=['T8', 'T6'], writes=['T8'])
            S.op('dve', lambda: V.tensor_tensor(out=T8[:], in0=T8[:], in1=T5[:], op=ALU.mult), reads=['T8', 'T5'], writes=['T8'])
            ld(T1[:], X1p_d[:, :], 'T1'); ld(T2[:], X2p_d[:, :], 'T2')
            S.op('dve', lambda: V.tensor_tensor(out=T2[:].rearrange("p (g f) -> p g f", f=128), in0=T2[:].rearrange("p (g f) -> p g f", f=128),
                                                in1=sgn_r[:].unsqueeze(1).to_broadcast([128, 16, 128]), op=ALU.mult), reads=['T2', 'sgn_r'], writes=['T2'])
            S.op('dve', lambda: V.tensor_tensor(out=T5[:], in0=T3[:], in1=T1[:], op=ALU.mult), reads=['T3', 'T1'], writes=['T5'])
            S.op('dve', lambda: V.tensor_tensor(out=T6[:], in0=T8[:], in1=T2[:], op=ALU.mult), reads=['T8', 'T2'], writes=['T6'])
            S.op('dve', lambda: V.tensor_tensor(out=fl(Bpad), in0=T5[:], in1=T6[:], op=ALU.add), reads=['T5', 'T6'], writes=['Bpad'])
            S.op('dve', lambda: V.tensor_tensor(out=T5[:], in0=T3[:], in1=T2[:], op=ALU.mult), reads=['T3', 'T2'], writes=['T5'])
            S.op('dve', lambda: V.tensor_tensor(out=T6[:], in0=T8[:], in1=T1[:], op=ALU.mult), reads=['T8', 'T1'], writes=['T6'])
            S.op('dve', lambda: V.tensor_tensor(out=fl(BpadS), in0=T5[:], in1=T6[:], op=ALU.subtract), reads=['T5', 'T6'], writes=['BpadS'])
            ld(T1[:], CcP_d[:, :], 'T1'); ld(T2[:], CcSP_d[:, :], 'T2')
            S.op('dve', lambda: V.tensor_scalar(out=fl(Cc), in0=T1[:], scalar1=sgn_c[:, 0:1], scalar2=None, op0=ALU.mult), reads=['T1', 'sgn_c'], writes=['Cc'])
            S.op('dve', lambda: V.tensor_scalar(out=fl(CcS), in0=T2[:], scalar1=-1.0, scalar2=None, op0=ALU.mult), reads=['T2'], writes=['CcS'])
            S.barrier()
        xt = [sb("xt%d" % i, [128, D]) for i in range(2)]
        junk = sb("junk", [128, D], BF16)
        ss = sb("ss", [128, 1]); rs = sb("rs", [128, 1])
        hm = sb("hm", [128, D]); hmb = sb("hmb", [128, D], BF16)
        hmT = sb("hmT", [128, 8, 128], BF16)
        uT = sb("uT", [128, 2, 128]); uTb = sb("uTb", [128, 2, 128], BF16)
        Vt = [sb("Vt%d" % i, [128, 4, 128]) for i in range(2)]
        Wt = [sb("Wt%d" % i, [128, 4, 128]) for i in range(2)]
        Zt = [sb("Zt%d" % i, [128, 4, 128]) for i in range(4)]
        Hr = [sb("Hr%d" % i, [128, 4, 128], BF16) for i in range(2)]
        Hi = [sb("Hi%d" % i, [128, 4, 128], BF16) for i in range(2)]
        yt = sb("yt", [128, 2, 128]); ga = sb("ga", [128, 2, 128]); yo = sb("yo", [128, 2, 128])
        sh1 = modrep[:, 0:1024]

        for c in range(NCH):
            xb = xt[c % 2]; kx = 'xt%d' % (c % 2)
            r0 = c * 128
            ld(xb[:], x[r0:r0 + 128, :], kx)
            S.op('act', lambda: A.activation(out=junk[:], in_=xb[:], func=AF.Square, accum_out=ss[:]), reads=[kx], writes=['junk', 'ss'])
            S.op('act', lambda: A.activation(out=rs[:], in_=ss[:], func=AF.Sqrt, bias=epsT[:], scale=1.0 / D), reads=['ss', 'epsT'], writes=['rs'])
            S.op('dve', lambda: V.reciprocal(out=rs[:], in_=rs[:]), reads=['rs'], writes=['rs'])
            S.op('dve', lambda: V.scalar_tensor_tensor(out=hm[:], in0=xb[:], scalar=rs[:], in1=gk1[:], op0=ALU.mult, op1=ALU.mult),
                 reads=[kx, 'rs', 'gk1'], writes=['hm'])
            S.op('dve', lambda: V.tensor_tensor(out=hmb[:], in0=hm[:], in1=sh1, op=ALU.add), reads=['hm', 'modrep'], writes=['hmb'])
            for kc in range(8):
                S.op('pe', lambda: PE.transpose(out=psT[:, kc * 128:(kc + 1) * 128], in_=hmb[:, kc * 128:(kc + 1) * 128], identity=identb[:]),
                     reads=['hmb', 'identb'], writes=['psT'], pe_chain=(kc > 0))
            S.op('act', lambda: A.copy(out=hmT[:].rearrange("p a b -> p (a b)"), in_=psT[:]), reads=['psT'], writes=['hmT'])
            for mc in range(2):
                for kc in range(8):
                    S.op('pe', lambda: PE.matmul(psU[:, mc * 128:(mc + 1) * 128], lhsT=win[:, kc, mc * 128:(mc + 1) * 128], rhs=hmT[:, kc, :],
                                                 start=(kc == 0), stop=(kc == 7)), reads=['win', 'hmT'], writes=['psU'],
                         pe_chain=not (mc == 0 and kc == 0))
            S.op('act', lambda: A.copy(out=uT[:].rearrange("p a b -> p (a b)"), in_=psU[:, 0:256]), reads=['psU'], writes=['uT'])
            S.op('act', lambda: A.copy(out=uTb[:].rearrange("p a b -> p (a b)"), in_=psU[:, 0:256]), reads=['psU'], writes=['uTb'])
            for bt in range(4):
                i2 = bt % 2
                kbu = 'psBU%d' % i2; kbs = 'psBS%d' % i2
                kV = 'Vt%d' % i2; kW = 'Wt%d' % i2; kZ = 'Zt%d' % bt; kHr = 'Hr%d' % i2; kHi = 'Hi%d' % i2
                gc = bt // 2
                for gg in range(4):
                    g = bt * 4 + gg
                    S.op('pe', lambda: PE.matmul(psBU[i2][:, gg * 128:(gg + 1) * 128], lhsT=Bpad[:, g, :], rhs=uTb[:, gc, :], start=True, stop=True),
                         reads=['Bpad', 'uTb'], writes=[kbu], pe_chain=(gg > 0))
                for gg in range(4):
                    g = bt * 4 + gg
                    S.op('pe', lambda: PE.matmul(psBS[i2][:, gg * 128:(gg + 1) * 128], lhsT=BpadS[:, g, :], rhs=uTb[:, gc, :], start=True, stop=True),
                         reads=['BpadS', 'uTb'], writes=[kbs], pe_chain=(gg > 0))
                Vf = Vt[i2][:].rearrange("p a b -> p (a b)"); Wf = Wt[i2][:].rearrange("p a b -> p (a b)")
                Zf = Zt[bt][:].rearrange("p a b -> p (a b)")
                gs = slice(bt * 4, bt * 4 + 4)
                S.op('dve', lambda: V.tensor_tensor(out=Vf, in0=psBU[i2][:], in1=Ainv_r[:, gs, :].rearrange("p a b -> p (a b)"), op=ALU.mult),
                     reads=[kbu, 'Ainv_r'], writes=[kV])
                S.op('dve', lambda: V.tensor_tensor(out=Wf, in0=psBS[i2][:], in1=Ainv_i[:, gs, :].rearrange("p a b -> p (a b)"), op=ALU.mult),
                     reads=[kbs, 'Ainv_i'], writes=[kW])
                S.op('dve', lambda: V.tensor_tensor(out=Vf, in0=Vf, in1=Wf, op=ALU.add), reads=[kV, kW], writes=[kV])
                if c > 0:
                    S.op('dve', lambda: V.tensor_tensor(out=Vt[i2][:, :, 0], in0=Vt[i2][:, :, 0], in1=psI[:, bt * 4:bt * 4 + 4], op=ALU.add),
                         reads=[kV, 'psI'], writes=[kV])
                S.op('dve', lambda: V.tensor_tensor_scan(out=Zf, data0=mask01[:], data1=Vf, initial=0.0, op0=ALU.mult, op1=ALU.add),
                     reads=['mask01', kV], writes=[kZ])
                if c < NCH - 1:
                    for gg in range(4):
                        g = bt * 4 + gg
                        S.op('pe', lambda: PE.matmul(psI[:, g:g + 1], lhsT=Rot[:, g, :], rhs=Zt[bt][:, gg, 127:128], start=True, stop=True),
                             reads=['Rot', kZ], writes=['psI'], pe_chain=(gg > 0))
                S.op('dve', lambda: V.tensor_tensor(out=Hr[i2][:].rearrange("p a b -> p (a b)"), in0=Zf,
                                                    in1=Apow_r[:, gs, :].rearrange("p a b -> p (a b)"), op=ALU.mult), reads=[kZ, 'Apow_r'], writes=[kHr])
                S.op('dve', lambda: V.tensor_tensor(out=Hi[i2][:].rearrange("p a b -> p (a b)"), in0=Zf,
                                                    in1=Apow_i[:, gs, :].rearrange("p a b -> p (a b)"), op=ALU.mult), reads=[kZ, 'Apow_i'], writes=[kHi])
                for gg in range(4):
                    g = bt * 4 + gg
                    first = (g % 8 == 0)
                    S.op('pe', lambda: PE.matmul(psY[:, gc * 128:(gc + 1) * 128], lhsT=Cc[:, g, :], rhs=Hr[i2][:, gg, :], start=first, stop=False),
                         reads=['Cc', kHr], writes=['psY'], pe_chain=not first)
                    S.op('pe', lambda: PE.matmul(psY[:, gc * 128:(gc + 1) * 128], lhsT=CcS[:, g, :], rhs=Hi[i2][:, gg, :], start=False, stop=(g % 8 == 7)),
                         reads=['CcS', kHi], writes=['psY'], pe_chain=True)
            ytf = yt[:].rearrange("p a b -> p (a b)"); gaf = ga[:].rearrange("p a b -> p (a b)"); yof = yo[:].rearrange("p a b -> p (a b)")
            for gc in range(2):
                S.op('dve', lambda: V.scalar_tensor_tensor(out=yt[:, gc, :], in0=uT[:, gc, :], scalar=dcol[:, gc:gc + 1],
                                                           in1=psY[:, gc * 128:(gc + 1) * 128], op0=ALU.mult, op1=ALU.add),
                     reads=['uT', 'dcol', 'psY'], writes=['yt'])
            S.op('dve', lambda: V.tensor_tensor(out=gaf, in0=ytf, in1=ytf, op=ALU.mult), reads=['yt'], writes=['ga'])
            S.op('dve', lambda: V.tensor_scalar(out=gaf, in0=gaf, scalar1=0.044715, scalar2=1.0, op0=ALU.mult, op1=ALU.add), reads=['ga'], writes=['ga'])
            S.op('dve', lambda: V.tensor_tensor(out=gaf, in0=gaf, in1=ytf, op=ALU.mult), reads=['ga', 'yt'], writes=['ga'])
            S.op('act', lambda: A.activation(out=gaf, in_=gaf, func=AF.Sigmoid, scale=1.5957691216), reads=['ga'], writes=['ga'])
            S.op('dve', lambda: V.tensor_tensor(out=yof, in0=gaf, in1=ytf, op=ALU.mult), reads=['ga', 'yt'], writes=['yo'])
            S.dma('sp', lambda: nc.sync.dma_start(out=out[:, r0:r0 + 128].rearrange("(gc k) n -> k gc n", k=128), in_=yo[:]), reads=['yo'], writes=['out'])
        S.finish(['out'])
        print("s5 program: instr", S.n_instr, "waits", S.n_wait)
    return nc


def s5_consts():
    f = np.arange(128)
    swap = np.zeros((128, 128), np.float32); swap[f, (f + 64) % 128] = 1.0
    sgn_c = np.where(f < 64, 1.0, -1.0).astype(np.float32)[:, None]
    sgn_r = np.broadcast_to(np.where(f < 64, -1.0, 1.0).astype(np.float32)[None, :], (128, 128)).copy()
    mrow = np.broadcast_to(np.arange(128, dtype=np.float32)[None, :], (128, 128)).copy()
    m01 = np.ones((128, 4, 128), np.float32); m01[:, :, 0] = 0.0
    return {"identf": np.eye(128, dtype=np.float32), "swapm": swap, "sgn_c": sgn_c, "sgn_r": sgn_r, "mrow": mrow,
            "mask01": m01.reshape(128, 512)}


def s5_layouts(a_re, a_im, log_dt, b_re, b_im, c_re, c_im, d, cq):
    G0 = 16 * cq
    f = np.arange(128)
    p = f % 64
    are_c = np.ascontiguousarray(a_re[G0:G0 + 16][:, p].T)
    aim_c = np.ascontiguousarray(a_im[G0:G0 + 16][:, p].T)
    ldt_c = np.ascontiguousarray(np.broadcast_to(log_dt[G0:G0 + 16][None, :], (128, 16)))
    are_r = np.ascontiguousarray(np.broadcast_to(a_re[G0:G0 + 16][:, p].reshape(1, 16 * 128), (128, 2048)))
    aim_r = np.ascontiguousarray(np.broadcast_to(a_im[G0:G0 + 16][:, p].reshape(1, 16 * 128), (128, 2048)))
    ldt_r = np.ascontiguousarray(np.broadcast_to(np.repeat(log_dt[G0:G0 + 16], 128).reshape(1, 2048), (128, 2048)))
    X1p = np.zeros((128, 16, 128), np.float32); X2p = np.zeros((128, 16, 128), np.float32)
    CcP = np.zeros((128, 16, 128), np.float32); CcSP = np.zeros((128, 16, 128), np.float32)
    for g in range(16):
        r0 = (g % 8) * 16
        br = b_re[G0 + g]; bi = b_im[G0 + g]
        X1p[r0:r0 + 16, g, 0:64] = br.T; X1p[r0:r0 + 16, g, 64:128] = bi.T
        X2p[r0:r0 + 16, g, 0:64] = bi.T; X2p[r0:r0 + 16, g, 64:128] = br.T
        cr = c_re[G0 + g]; ci = c_im[G0 + g]
        CcP[0:64, g, r0:r0 + 16] = cr.T; CcP[64:128, g, r0:r0 + 16] = ci.T
        CcSP[0:64, g, r0:r0 + 16] = ci.T; CcSP[64:128, g, r0:r0 + 16] = cr.T
    dcol = np.ascontiguousarray(d[cq * 256:(cq + 1) * 256].reshape(2, 128).T)
    return {"are_c": are_c, "aim_c": aim_c, "ldt_c": ldt_c, "are_r": are_r, "aim_r": aim_r, "ldt_r": ldt_r,
            "X1p": X1p.reshape(128, 2048), "X2p": X2p.reshape(128, 2048), "CcP": CcP.reshape(128, 2048),
            "CcSP": CcSP.reshape(128, 2048), "dcol": dcol}

import math

RMS_EPS = 1e-6


def build_nsa(NS, KXK='psXk'):
    nc = bass.Bass("TRN2", target_bir_lowering=False)
    T = NS * 512
    NT = NS * 4
    D = 1024
    NM = 1024 if T >= 16384 else (T // 16 + 32)
    NMT = (NM + 127) // 128
    din = lambda n, s, d=F32: nc.dram_tensor(n, s, d, kind="ExternalInput").ap()
    x = din("x", [T, D])
    c_l = din("c_l", [128, 8])
    adaw = din("adaw", [D, 2048]); adab = din("adab", [1, 2048]); n1g = din("n1g", [1, D])
    wpf_d = din("wpf", [D, 512]); wpt_d = din("wpt", [D, 524])
    wk1_d = din("wk1", [2048, 256]); wv1_d = din("wv1", [2048, 256])
    wk2_d = din("wk2", [256, 64]); wv2_d = din("wv2", [256, 64])
    pekT_d = din("pekT", [64, 32]); pevT_d = din("pevT", [64, 32])
    cos_d = din("cos2", [64, T]); sin_d = din("sin2", [64, T])
    cosc_d = din("cosc", [64, NM]); sinc_d = din("sinc", [64, NM])
    prot_d = din("prot", [64, 64])
    identf_d = din("identf", [128, 128])
    triT_d = din("triT", [128, 128]); triTs_d = din("triTs", [128, 128])
    cmg_d = din("cmaskG", [128, 16]); cm0_d = din("cmask0", [128, 8])
    hilo_d = din("hilo", [128, 3])
    out = nc.dram_tensor("out", [T, 256], F32, kind="ExternalOutput").ap()

    es = ExitStack()
    with es:
        es.enter_context(nc.allow_low_precision("bf16 matmul operands"))
        es.enter_context(nc.allow_non_contiguous_dma("small layout loads"))
        S = Sched(nc, es)
        sb = lambda n, s, d=F32: es.enter_context(nc.sbuf_tensor("s_" + n, s, d))
        ps = lambda n, s, d=F32: es.enter_context(nc.psum_tensor("p_" + n, s, d))
        V, A, P, PE = nc.vector, nc.scalar, nc.gpsimd, nc.tensor

        def ld(dst, src, key):
            S.dma('sp', lambda: nc.sync.dma_start(out=dst, in_=src), writes=[key])

        identf = sb("identf", [128, 128]); identb = sb("identb", [128, 128], BF16)
        triT = sb("triT", [128, 128], BF16); triTs = sb("triTs", [128, 128], BF16)
        cmaskG = sb("cmaskG", [128, 16]); cmask0 = sb("cmask0", [128, 8]); hilo = sb("hilo", [128, 3])
        epsT = sb("epsT", [128, 1]); ones1 = sb("ones1", [1, 128]); kmx = sb("kmx", [1, 1]); kmax2 = sb("kmax2", [128, 1])
        prot = sb("prot", [64, 64])
        modrep = sb("modrep", [128, 2048]); gk1 = sb("gk1", [128, D])
        wpf = sb("wpf", [128, 8, 512], BF16); wpt = sb("wpt", [128, 8, 524], BF16)
        wk1 = sb("wk1", [64, 32, 256], BF16); wv1 = sb("wv1", [64, 32, 256], BF16)
        wk2 = sb("wk2", [128, 2, 64], BF16); wv2 = sb("wv2", [128, 2, 64], BF16)
        biask = sb("biask", [128, 2]); biasv = sb("biasv", [128, 2])
        ksT = sb("ksT", [65, T], BF16); kwT = sb("kwT", [65, 1024], BF16)
        vsA = sb("vsA", [128, NT, 65], BF16); vwA = sb("vwA", [128, 8, 65], BF16)
        kcmpT = sb("kcmpT", [64, NMT * 128], BF16); vcmp = sb("vcmp", [128, NMT, 64], BF16)
        kcbuf = sb("kcbuf", [64, 528], BF16); vcbuf = sb("vcbuf", [64, 528], BF16)

        psS = [ps("psS%d" % i, [128, 512]) for i in range(2)]
        psOs = ps("psOs", [128, 512]); psOw = ps("psOw", [128, 512])
        psC = ps("psC", [128, 1024])
        psT = ps("psT", [128, 1024], BF16)
        psX = ps("psX", [128, 512])

        ld(identf[:], identf_d[:, :], 'identf')
        ld(cmaskG[:], cmg_d[:, :], 'cmaskG'); ld(cmask0[:], cm0_d[:, :], 'cmask0'); ld(hilo[:], hilo_d[:, :], 'hilo')
        ld(prot[:], prot_d[:, :], 'prot')
        S.op('dve', lambda: V.tensor_copy(out=identb[:], in_=identf[:]), reads=['identf'], writes=['identb'])
        S.op('dve', lambda: V.memset(epsT[:], RMS_EPS), writes=['epsT'])
        S.op('dve', lambda: V.memset(ones1[:], 1.0), writes=['ones1'])
        S.op('dve', lambda: V.memset(kmx[:], 0.0), writes=['kmx'])
        for c0 in range(0, T, 2048):
            S.op('dve', lambda: V.memset(ksT[:, c0:min(T, c0 + 2048)], 1.0), writes=['ksT'])
        S.op('pool', lambda: P.memset(kwT[:], 1.0), writes=['kwT'])
        S.op('dve', lambda: V.memset(vsA[:], 1.0), writes=['vsA'])
        S.op('pool', lambda: P.memset(vwA[:], 1.0), writes=['vwA'])
        S.op('dve', lambda: V.memset(kcmpT[:], 0.0), writes=['kcmpT'])
        S.op('dve', lambda: V.memset(vcmp[:], 0.0), writes=['vcmp'])
        S.op('dve', lambda: V.memset(kcbuf[:], 0.0), writes=['kcbuf'])
        S.op('dve', lambda: V.memset(vcbuf[:], 0.0), writes=['vcbuf'])

        es2 = ExitStack()
        with es2:
            tb = lambda n, s, d=F32: es2.enter_context(nc.sbuf_tensor("t_" + n, s, d))
            csil = tb("csil", [128, 8]); csil_rep = tb("csil_rep", [128, 8, 128])
            stg = [tb("stg%d" % i, [128, 8, 256]) for i in range(2)]
            adab_rep = tb("adab_rep", [128, 256]); n1g_rep = tb("n1g_rep", [128, D])
            tmpf = tb("tmpf", [128, 128])
            ld(tmpf[:], triT_d[:, :], 'tmpf')
            S.op('dve', lambda: V.tensor_copy(out=triT[:], in_=tmpf[:]), reads=['tmpf'], writes=['triT'])
            ld(tmpf[:], triTs_d[:, :], 'tmpf')
            S.op('dve', lambda: V.tensor_copy(out=triTs[:], in_=tmpf[:]), reads=['tmpf'], writes=['triTs'])
            ld(csil[:], c_l[:, :], 'csil')
            ld(n1g_rep[:], n1g[0:1, :].partition_broadcast(128), 'n1g_rep')
            S.op('act', lambda: A.activation(out=csil[:], in_=csil[:], func=AF.Silu), reads=['csil'], writes=['csil'])
            S.op('dve', lambda: V.tensor_copy(out=csil_rep[:], in_=csil[:].unsqueeze(2).to_broadcast([128, 8, 128])),
                 reads=['csil'], writes=['csil_rep'])
            for j in range(8):
                st = stg[j % 2]; k_st = 'stg%d' % (j % 2)
                ld(st[:], adaw[:, j * 256:(j + 1) * 256].rearrange("(kc k) n -> k kc n", k=128), k_st)
                ld(adab_rep[:], adab[0:1, j * 256:(j + 1) * 256].partition_broadcast(128), 'adab_rep')
                for kc in range(8):
                    S.op('pe', lambda: PE.matmul(psX[:, 0:256], lhsT=csil_rep[:, kc, :], rhs=st[:, kc, :], start=(kc == 0), stop=(kc == 7)),
                         reads=['csil_rep', k_st], writes=['psX'], pe_chain=(kc > 0))
                S.op('dve', lambda: V.tensor_tensor(out=modrep[:, j * 256:(j + 1) * 256], in0=psX[:, 0:256], in1=adab_rep[:], op=ALU.add),
                     reads=['psX', 'adab_rep'], writes=['modrep'])
            sc1 = modrep[:, 1024:2048]
            S.op('dve', lambda: V.scalar_tensor_tensor(out=gk1[:], in0=sc1, scalar=1.0, in1=n1g_rep[:], op0=ALU.add, op1=ALU.mult),
                 reads=['modrep', 'n1g_rep'], writes=['gk1'])
            ci = [0]

            def load_cast(dst3, src2d, ncols, key):
                c0 = 0
                while c0 < ncols:
                    w = min(256, ncols - c0)
                    i = ci[0] % 2; ci[0] += 1
                    st = stg[i]
                    ld(st[:, :, 0:w], src2d[:, c0:c0 + w].rearrange("(kc k) n -> k kc n", k=128), 'stg%d' % i)
                    eng = 'dve' if i == 0 else 'act'
                    if i == 0:
                        S.op('dve', lambda: V.tensor_copy(out=dst3[:, :, c0:c0 + w], in_=st[:, :, 0:w]), reads=['stg%d' % i], writes=[key])
                    else:
                        S.op('act', lambda: A.copy(out=dst3[:, :, c0:c0 + w], in_=st[:, :, 0:w]), reads=['stg%d' % i], writes=[key])
                    c0 += w
            load_cast(wpf, wpf_d, 512, 'wpf')
            load_cast(wpt, wpt_d, 524, 'wpt')
            for (wdst, wsrc, key) in ((wk1, wk1_d, 'wk1'), (wv1, wv1_d, 'wv1')):
                for p0 in range(0, 32, 8):
                    i = ci[0] % 2; ci[0] += 1
                    st = stg[i]
                    ld(st[0:64, :, :], wsrc[p0 * 64:(p0 + 8) * 64, :].rearrange("(pos d) h -> d pos h", d=64), 'stg%d' % i)
                    S.op('dve', lambda: V.tensor_copy(out=wdst[:, p0:p0 + 8, :], in_=st[0:64, :, :]), reads=['stg%d' % i], writes=[key])
            for (wdst, wsrc, key) in ((wk2, wk2_d, 'wk2'), (wv2, wv2_d, 'wv2')):
                i = ci[0] % 2; ci[0] += 1
                st = stg[i]
                ld(st[:, 0:2, 0:64], wsrc[:, :].rearrange("(hc k) d -> k hc d", k=128), 'stg%d' % i)
                S.op('dve', lambda: V.tensor_copy(out=wdst[:], in_=st[:, 0:2, 0:64]), reads=['stg%d' % i], writes=[key])
            peb = tb("peb", [64, 32], BF16)
            for (pe_d, w1, bias, key) in ((pekT_d, wk1, biask, 'biask'), (pevT_d, wv1, biasv, 'biasv')):
                ld(tmpf[0:64, 0:32], pe_d[:, :], 'tmpf')
                S.op('dve', lambda: V.tensor_copy(out=peb[:], in_=tmpf[0:64, 0:32]), reads=['tmpf'], writes=['peb'])
                for hc in range(2):
                    for pos in range(32):
                        S.op('pe', lambda: PE.matmul(psX[:, hc:hc + 1], lhsT=w1[:, pos, hc * 128:(hc + 1) * 128], rhs=peb[:, pos:pos + 1],
                                                     start=(pos == 0), stop=(pos == 31)), reads=['wk1', 'wv1', 'peb'], writes=['psX'],
                             pe_chain=(pos > 0))
                S.op('dve', lambda: V.tensor_copy(out=bias[:], in_=psX[:, 0:2]), reads=['psX'], writes=[key])
            S.barrier()

        xt = [sb("xt%d" % i, [128, D]) for i in range(2)]
        junk = sb("junk", [128, D], BF16)
        ss = sb("ss", [128, 1]); rs = sb("rs", [128, 1])
        hm = sb("hm", [128, D]); hmb = sb("hmb", [128, D], BF16)
        hmT4 = sb("hmT4", [128, 8, 512], BF16)
        qtm = sb("qtm", [128, 256]); qsq = sb("qsq", [128, 4, 4])
        ksq = sb("ksq", [128, 2]); km1 = sb("km1", [128, 1]); red = sb("red", [1, 1])
        gates = sb("gates", [128, 4, 12])
        cs2 = [sb("cos%d" % i, [64, 512]) for i in range(2)]
        sn2 = [sb("sin%d" % i, [64, 512]) for i in range(2)]
        cc2 = [sb("cc%d" % i, [64, 32]) for i in range(2)]
        sc2_ = [sb("sc%d" % i, [64, 32]) for i in range(2)]
        xq = sb("xq", [64, 512]); t1 = sb("t1", [64, 512]); t2 = sb("t2", [64, 512])
        qTa = sb("qTa", [65, 4, 512], BF16)
        hid = sb("hid", [128, 2, 32]); hga = sb("hga", [128, 2, 32]); ghk = sb("ghk", [128, 2, 32], BF16); ghv = sb("ghv", [128, 2, 32], BF16); ghv2 = sb("ghv2", [128, 2, 64], BF16)
        kcx = sb("kcx", [64, 32]); kt1 = sb("kt1", [64, 32]); kt2 = sb("kt2", [64, 32])
        cq = sb("cq", [128, 4]); negc = sb("negc", [128, 4], BF16)
        eT = [sb("eT%d" % i, [128, 512], BF16) for i in range(2)]
        pT = [sb("pT%d" % i, [128, 512], BF16) for i in range(2)]
        mfull = sb("mfull", [128, 512], BF16); maskT4 = [sb("maskT4_%d" % i, [128, 4, 128], BF16) for i in range(2)]
        NBK = 2
        LA = 1
        pc = sb("pc", [128, 1024]); pcb = sb("pcb", [128, 1024], BF16); pcT = sb("pcT", [128, 8, 128], BF16)
        pgrp = sb("pgrp", [128, 1032])
        mx = sb("mx", [128, 1]); mx2 = sb("mx2", [128, 2]); sm = sb("sm", [128, 4]); rinv = sb("rinv", [128, 4])
        imp = sb("imp", [128, 256]); impw = sb("impw", [128, 256]); impk = sb("impk", [128, 256]); sel = sb("sel", [128, 256])
        m8a = sb("m8a", [128, 8]); m8b = sb("m8b", [128, 8]); tau = sb("tau", [128, 1])
        oTs = sb("oTs", [65, 512]); oTw = sb("oTw", [65, 512])
        fac = sb("fac", [128, 3, 4]); ot = sb("ot", [128, 4, 64])
        sh1 = modrep[:, 0:1024]
        S.op('dve', lambda: V.memset(impw[:], -1.0), writes=['impw'])
        S.op('dve', lambda: V.memset(pgrp[:], 0.0), writes=['pgrp'])
        S.op('dve', lambda: V.memset(sel[:], 0.0), writes=['sel'])
        S.op('dve', lambda: V.memset(ghv2[:], 0.0), writes=['ghv2'])

        def gelu_tanh(dst, src, tmp, kd, ks_, kt):
            S.op('dve', lambda: V.tensor_tensor(out=tmp, in0=src, in1=src, op=ALU.mult), reads=[ks_], writes=[kt])
            S.op('dve', lambda: V.tensor_scalar(out=tmp, in0=tmp, scalar1=0.044715, scalar2=1.0, op0=ALU.mult, op1=ALU.add), reads=[kt], writes=[kt])
            S.op('dve', lambda: V.tensor_tensor(out=tmp, in0=tmp, in1=src, op=ALU.mult), reads=[kt, ks_], writes=[kt])
            S.op('act', lambda: A.activation(out=tmp, in_=tmp, func=AF.Sigmoid, scale=1.5957691216), reads=[kt], writes=[kt])
            S.op('dve', lambda: V.tensor_tensor(out=dst, in0=tmp, in1=src, op=ALU.mult), reads=[kt, ks_], writes=[kd])

        def rope(dst, src_ps, kps, cosap, sinap, kcos, scale, kdst):
            n = src_ps.shape[-1]
            S.op('act', lambda: A.copy(out=xq[:, 0:n], in_=src_ps), reads=[kps], writes=['xq'])
            S.op('pe', lambda: PE.matmul(psS[1][0:64, 0:n], lhsT=prot[:], rhs=xq[:, 0:n], start=True, stop=True),
                 reads=['prot', 'xq'], writes=['psS1'])
            S.op('dve', lambda: V.scalar_tensor_tensor(out=t1[:, 0:n], in0=xq[:, 0:n], scalar=scale, in1=cosap, op0=ALU.mult, op1=ALU.mult),
                 reads=['xq', kcos], writes=['t1'])
            S.op('dve', lambda: V.scalar_tensor_tensor(out=t2[:, 0:n], in0=psS[1][0:64, 0:n], scalar=scale, in1=sinap, op0=ALU.mult, op1=ALU.mult),
                 reads=['psS1', kcos], writes=['t2'])
            a1 = t1[:, 0:n]; a2 = t2[:, 0:n]
            if len(dst.shape) == 3:
                a1 = a1.rearrange("p (a b) -> p a b", b=dst.shape[2]); a2 = a2.rearrange("p (a b) -> p a b", b=dst.shape[2])
            S.op('dve', lambda: V.tensor_tensor(out=dst, in0=a1, in1=a2, op=ALU.add), reads=['t1', 't2'], writes=[kdst])

        for s in range(NS):
            cb = cs2[s % 2]; snb = sn2[s % 2]; kcos = 'cos%d' % (s % 2)
            ld(cb[:], cos_d[:, s * 512:(s + 1) * 512], kcos)
            ld(snb[:], sin_d[:, s * 512:(s + 1) * 512], kcos)
            ccb = cc2[s % 2]; scb = sc2_[s % 2]; kcc = 'cc%d' % (s % 2)
            if 32 * s + 32 <= NM:
                ld(ccb[:], cosc_d[:, 32 * s:32 * s + 32], kcc)
                ld(scb[:], sinc_d[:, 32 * s:32 * s + 32], kcc)
            for i in range(4):
                ti = s * 4 + i
                xb = xt[ti % 2]; kx = 'xt%d' % (ti % 2)
                r0 = ti * 128
                ld(xb[:], x[r0:r0 + 128, :], kx)
                S.op('act', lambda: A.activation(out=junk[:], in_=xb[:], func=AF.Square, accum_out=ss[:]), reads=[kx], writes=['junk', 'ss'])
                S.op('act', lambda: A.activation(out=rs[:], in_=ss[:], func=AF.Sqrt, bias=epsT[:], scale=1.0 / D), reads=['ss', 'epsT'], writes=['rs'])
                S.op('dve', lambda: V.reciprocal(out=rs[:], in_=rs[:]), reads=['rs'], writes=['rs'])
                S.op('dve', lambda: V.scalar_tensor_tensor(out=hm[:], in0=xb[:], scalar=rs[:], in1=gk1[:], op0=ALU.mult, op1=ALU.mult),
                     reads=[kx, 'rs', 'gk1'], writes=['hm'])
                S.op('dve', lambda: V.tensor_tensor(out=hmb[:], in0=hm[:], in1=sh1, op=ALU.add), reads=['hm', 'modrep'], writes=['hmb'])
                for kc in range(8):
                    S.op('pe', lambda: PE.transpose(out=psT[:, kc * 128:(kc + 1) * 128], in_=hmb[:, kc * 128:(kc + 1) * 128], identity=identb[:]),
                         reads=['hmb', 'identb'], writes=['psT'], pe_chain=(kc > 0))
                S.op('act', lambda: A.copy(out=hmT4[:, :, i * 128:(i + 1) * 128], in_=psT[:].rearrange("p (a b) -> p a b", b=128)),
                     reads=['psT'], writes=['hmT4'])
                for kc in range(8):
                    S.op('pe', lambda: PE.matmul(psS[0][:, 0:384], lhsT=hmT4[:, kc, i * 128:(i + 1) * 128], rhs=wpt[:, kc, 0:384],
                                                 start=(kc == 0), stop=(kc == 7)), reads=['hmT4', 'wpt'], writes=['psS0'], pe_chain=(kc > 0))
                for kc in range(8):
                    S.op('pe', lambda: PE.matmul(psX[:, 0:140], lhsT=hmT4[:, kc, i * 128:(i + 1) * 128], rhs=wpt[:, kc, 384:524],
                                                 start=(kc == 0), stop=(kc == 7)), reads=['hmT4', 'wpt'], writes=['psX'], pe_chain=(kc > 0))
                S.op('act', lambda: A.copy(out=qtm[:], in_=psS[0][:, 0:256]), reads=['psS0'], writes=['qtm'])
                S.op('act', lambda: A.activation(out=junk[:, 0:64], in_=psS[0][:, 256:320], func=AF.Square, accum_out=ksq[:, 0:1]),
                     reads=['psS0'], writes=['junk', 'ksq'])
                S.op('act', lambda: A.activation(out=junk[:, 0:64], in_=psS[0][:, 320:384], func=AF.Square, accum_out=ksq[:, 1:2]),
                     reads=['psS0'], writes=['junk', 'ksq'])
                S.op('dve', lambda: V.tensor_tensor(out=qtm[:], in0=qtm[:], in1=qtm[:], op=ALU.mult), reads=['qtm'], writes=['qtm'])
                S.op('dve', lambda: V.tensor_reduce(out=qsq[:, i, :], in_=qtm[:].rearrange("p (r d) -> p r d", d=64), axis=AX.X, op=ALU.add),
                     reads=['qtm'], writes=['qsq'])
                S.op('dve', lambda: V.tensor_tensor(out=km1[:], in0=ksq[:, 0:1], in1=ksq[:, 1:2], op=ALU.max), reads=['ksq'], writes=['km1'])
                S.op('pe', lambda: PE.transpose(out=psX[0:1, 256:384], in_=km1[:], identity=identf[:]), reads=['km1', 'identf'], writes=[KXK])
                S.op('dve', lambda: V.tensor_reduce(out=red[:], in_=psX[0:1, 256:384], axis=AX.X, op=ALU.max), reads=[KXK], writes=['red'])
                S.op('dve', lambda: V.tensor_tensor(out=kmx[:], in0=kmx[:], in1=red[:], op=ALU.max), reads=['kmx', 'red'], writes=['kmx'])
                S.op('pe', lambda: PE.matmul(psX[:, 384:385], lhsT=ones1[:], rhs=kmx[:], start=True, stop=True), reads=['ones1', 'kmx'], writes=[KXK])
                S.op('dve', lambda: V.tensor_copy(out=kmax2[:], in_=psX[:, 384:385]), reads=[KXK], writes=['kmax2'])
                S.op('act', lambda: A.copy(out=vsA[:, ti, 0:64], in_=psX[:, 0:64]), reads=['psX'], writes=['vsA'])
                S.op('act', lambda: A.copy(out=vwA[:, ti % 8, 0:64], in_=psX[:, 64:128]), reads=['psX'], writes=['vwA'])
                S.op('act', lambda: A.activation(out=gates[:, i, :], in_=psX[:, 128:140], func=AF.Sigmoid), reads=['psX'], writes=['gates'])
            for blk in range(8):
                pso = psC[0:64, (blk % 2) * 512:(blk % 2) * 512 + 512]
                kps = 'psC'
                for kc in range(8):
                    S.op('pe', lambda: PE.matmul(pso, lhsT=wpf[:, kc, blk * 64:(blk + 1) * 64], rhs=hmT4[:, kc, :], start=(kc == 0), stop=(kc == 7)),
                         reads=['wpf', 'hmT4'], writes=[kps], pe_chain=(kc > 0))
                if blk < 4:
                    dst = qTa[0:64, :, blk * 128:(blk + 1) * 128]
                    rope(dst, pso, kps, cb[:], snb[:], kcos, 0.125, 'qTa')
                elif blk == 4:
                    S.op('act', lambda: A.copy(out=kcbuf[:, 16:528], in_=pso), reads=[kps], writes=['kcbuf'])
                elif blk == 5:
                    S.op('act', lambda: A.copy(out=vcbuf[:, 16:528], in_=pso), reads=[kps], writes=['vcbuf'])
                elif blk == 6:
                    rope(ksT[0:64, s * 512:(s + 1) * 512], pso, kps, cb[:], snb[:], kcos, 1.0, 'ksT')
                else:
                    rope(kwT[0:64, (s % 2) * 512:(s % 2) * 512 + 512], pso, kps, cb[:], snb[:], kcos, 1.0, 'kwT')
            m0 = 32 * s
            for (buf, kbuf, w1, kw1, bias, gh, kgh) in ((kcbuf, 'kcbuf', wk1, 'wk1', biask, ghk, 'ghk'), (vcbuf, 'vcbuf', wv1, 'wv1', biasv, ghv, 'ghv')):
                bview = buf[:, 0:512].rearrange("d (n f) -> d n f", f=16)
                for hc in range(2):
                    for pos in range(32):
                        if pos < 16:
                            rhs = bview[:, :, pos]
                        else:
                            rhs = buf[:, 16:528].rearrange("d (n f) -> d n f", f=16)[:, :, pos - 16]
                        S.op('pe', lambda: PE.matmul(psX[:, hc * 32:(hc + 1) * 32], lhsT=w1[:, pos, hc * 128:(hc + 1) * 128], rhs=rhs,
                                                     start=(pos == 0), stop=(pos == 31)), reads=[kw1, kbuf], writes=['psX'], pe_chain=(pos > 0))
                for hc in range(2):
                    S.op('dve', lambda: V.tensor_scalar(out=hid[:, hc, :], in0=psX[:, hc * 32:(hc + 1) * 32], scalar1=bias[:, hc:hc + 1], scalar2=None,
                                                        op0=ALU.add), reads=['psX', 'biask', 'biasv'], writes=['hid'])
                gelu_tanh(gh[:].rearrange("p a b -> p (a b)"), hid[:].rearrange("p a b -> p (a b)"), hga[:].rearrange("p a b -> p (a b)"),
                          kgh, 'hid', 'hga')
                S.op('dve', lambda: V.tensor_copy(out=buf[:, 0:16], in_=buf[:, 512:528]), reads=[kbuf], writes=[kbuf])
            for hc in range(2):
                S.op('pe', lambda: PE.matmul(psC[0:64, 0:32], lhsT=wk2[:, hc, :], rhs=ghk[:, hc, :], start=(hc == 0), stop=(hc == 1)),
                     reads=['wk2', 'ghk'], writes=['psC'], pe_chain=(hc > 0))
            if m0 + 32 <= NM:
                rope(kcmpT[:, m0:m0 + 32], psC[0:64, 0:32], 'psC', ccb[:], scb[:], kcc, 1.0, 'kcmpT')
                mt, mo = m0 // 128, m0 % 128
                S.op('dve', lambda: V.tensor_copy(out=ghv2[:, :, 32:64], in_=ghv[:]), reads=['ghv'], writes=['ghv2'])
                for hc in range(2):
                    if mo < 96:
                        S.op('pe', lambda: PE.matmul(psC[mo:mo + 32, 512:576], lhsT=ghv2[:, hc, 32:64], rhs=wv2[:, hc, :], start=(hc == 0), stop=(hc == 1)),
                             reads=['wv2', 'ghv2'], writes=['psC'], pe_chain=(hc > 0))
                    else:
                        S.op('pe', lambda: PE.matmul(psC[64:128, 512:576], lhsT=ghv2[:, hc, 0:64], rhs=wv2[:, hc, :], start=(hc == 0), stop=(hc == 1)),
                             reads=['wv2', 'ghv2'], writes=['psC'], pe_chain=(hc > 0))
                S.op('act', lambda: A.copy(out=vcmp[mo:mo + 32, mt, :], in_=psC[mo:mo + 32, 512:576]), reads=['psC'], writes=['vcmp'])

            for i in range(4):
                qi = s * 4 + i
                qblk = qTa[:, i, :]
                import os
                if qi < int(os.environ.get('NSA_QIMIN', '0')):
                    continue
                S.op('dve', lambda: V.tensor_scalar(out=cq[:], in0=qsq[:, i, :], scalar1=kmax2[:, 0:1], scalar2=1.0 / 64, op0=ALU.mult, op1=ALU.mult),
                     reads=['qsq', 'kmax2'], writes=['cq'])
                S.op('act', lambda: A.activation(out=cq[:], in_=cq[:], func=AF.Sqrt), reads=['cq'], writes=['cq'])
                S.op('dve', lambda: V.tensor_scalar(out=negc[:], in0=cq[:], scalar1=-1.0, scalar2=None, op0=ALU.mult), reads=['cq'], writes=['negc'])
                for r in range(4):
                    S.op('pe', lambda: PE.transpose(out=psT[64:65, r * 128:(r + 1) * 128], in_=negc[:, r:r + 1], identity=identb[:]),
                         reads=['negc', 'identb'], writes=['psT'], pe_chain=(r > 0))
                S.op('act', lambda: A.copy(out=qTa[64:65, i, :], in_=psT[64:65, 0:512]), reads=['psT'], writes=['qTa'])

                W = 8 * qi + 8
                import os
                if os.environ.get('NSA_CAPW'):
                    W = min(W, int(os.environ['NSA_CAPW']))
                nt = (W + 127) // 128
                for r in range(4):
                    for c0 in range(0, W, 512):
                        w = min(512, W - c0)
                        S.op('pe', lambda: PE.matmul(psC[:, c0:c0 + w], lhsT=qTa[0:64, i, r * 128:(r + 1) * 128], rhs=kcmpT[:, c0:c0 + w],
                                                     start=True, stop=True), reads=['qTa', 'kcmpT'], writes=['psC'])
                    chunks = [(c0, min(512, W - c0)) for c0 in range(0, W, 512)]
                    for ci_, (c0, w) in enumerate(chunks):
                        S.op('dve', lambda: V.tensor_reduce(out=mx2[:, ci_:ci_ + 1], in_=psC[:, c0:c0 + w], axis=AX.X, op=ALU.max), reads=['psC'], writes=['mx2'])
                    if len(chunks) == 2:
                        S.op('dve', lambda: V.tensor_tensor(out=mx2[:, 0:1], in0=mx2[:, 0:1], in1=mx2[:, 1:2], op=ALU.max), reads=['mx2'], writes=['mx2'])
                    S.op('dve', lambda: V.tensor_scalar(out=mx[:], in0=mx2[:, 0:1], scalar1=-1.0, scalar2=None, op0=ALU.mult), reads=['mx2'], writes=['mx'])
                    for (c0, w) in chunks:
                        S.op('act', lambda: A.activation(out=pc[:, c0:c0 + w], in_=psC[:, c0:c0 + w], func=AF.Exp, bias=mx[:], scale=1.0),
                             reads=['psC', 'mx'], writes=['pc'])
                    if qi == 0:
                        S.op('dve', lambda: V.tensor_tensor(out=pc[:, 0:8], in0=pc[:, 0:8], in1=cmask0[:], op=ALU.mult), reads=['pc', 'cmask0'], writes=['pc'])
                    else:
                        S.op('dve', lambda: V.tensor_tensor(out=pc[:, W - 16:W], in0=pc[:, W - 16:W], in1=cmaskG[:], op=ALU.mult),
                             reads=['pc', 'cmaskG'], writes=['pc'])
                        S.op('dve', lambda: V.memset(pc[:, 0:1], 0.0), reads=[], writes=['pc'])
                    S.op('dve', lambda: V.tensor_reduce(out=sm[:, r:r + 1], in_=pc[:, 0:W], axis=AX.X, op=ALU.add), reads=['pc'], writes=['sm'])
                    S.op('dve', lambda: V.tensor_scalar(out=rinv[:, r:r + 1], in0=sm[:, r:r + 1], scalar1=1e-30, scalar2=None, op0=ALU.add),
                         reads=['sm'], writes=['rinv'])
                    S.op('dve', lambda: V.reciprocal(out=rinv[:, r:r + 1], in_=rinv[:, r:r + 1]), reads=['rinv'], writes=['rinv'])
                    if r == 0:
                        S.op('dve', lambda: V.tensor_scalar(out=pgrp[:, 0:W], in0=pc[:, 0:W], scalar1=rinv[:, 0:1], scalar2=None, op0=ALU.mult),
                             reads=['pc', 'rinv'], writes=['pgrp'])
                    else:
                        S.op('dve', lambda: V.scalar_tensor_tensor(out=pgrp[:, 0:W], in0=pc[:, 0:W], scalar=rinv[:, r:r + 1], in1=pgrp[:, 0:W],
                                                                   op0=ALU.mult, op1=ALU.add), reads=['pc', 'rinv', 'pgrp'], writes=['pgrp'])
                    S.op('act', lambda: A.copy(out=pcb[:, 0:W], in_=pc[:, 0:W]), reads=['pc'], writes=['pcb'])
                    for j in range(nt):
                        wj = min(128, W - j * 128)
                        S.op('pe', lambda: PE.transpose(out=psT[0:wj, j * 128:(j + 1) * 128], in_=pcb[:, j * 128:j * 128 + wj], identity=identb[:]),
                             reads=['pcb', 'identb'], writes=['psT'], pe_chain=(j > 0))
                    for j in range(nt):
                        wj = min(128, W - j * 128)
                        S.op('act', lambda: A.copy(out=pcT[0:wj, j, :], in_=psT[0:wj, j * 128:(j + 1) * 128]), reads=['psT'], writes=['pcT'])
                    for j in range(nt):
                        wj = min(128, W - j * 128)
                        S.op('pe', lambda: PE.matmul(psX[:, r * 64:(r + 1) * 64], lhsT=pcT[0:wj, j, :], rhs=vcmp[0:wj, j, :],
                                                     start=(j == 0), stop=(j == nt - 1)), reads=['pcT', 'vcmp'], writes=['psX'], pe_chain=(j > 0))
                if qi >= 1:
                    Jn = 2 * qi
                    S.op('dve', lambda: V.tensor_reduce(out=imp[:, 0:Jn], in_=pgrp[:, 0:4 * Jn].rearrange("p (j f) -> p j f", f=4), axis=AX.X, op=ALU.add),
                         reads=['pgrp'], writes=['imp'])
                    S.op('dve', lambda: V.tensor_tensor(out=imp[:, 0:Jn], in0=imp[:, 0:Jn],
                                                        in1=pgrp[:, 4:4 + 4 * Jn].rearrange("p (j f) -> p j f", f=4)[:, :, 0], op=ALU.add),
                         reads=['imp', 'pgrp'], writes=['imp'])
                    if Jn - 1 > 1:
                        S.op('dve', lambda: V.tensor_copy(out=impw[:, 1:Jn - 1], in_=imp[:, 1:Jn - 1]), reads=['imp'], writes=['impw'])
                    S.op('dve', lambda: V.tensor_scalar(out=impw[:, Jn - 1:Jn], in0=imp[:, Jn - 1:Jn], scalar1=hilo[:, 0:1], scalar2=hilo[:, 1:2],
                                                        op0=ALU.mult, op1=ALU.add), reads=['imp', 'hilo'], writes=['impw'])
                    Wj = max(Jn, 16)
                    S.op('dve', lambda: V.max(out=m8a[:], in_=impw[:, 0:Wj]), reads=['impw'], writes=['m8a'])
                    S.op('dve', lambda: V.match_replace(out=impk[:, 0:Wj], in_to_replace=m8a[:], in_values=impw[:, 0:Wj], imm_value=-1e30),
                         reads=['impw', 'm8a'], writes=['impk'])
                    S.op('dve', lambda: V.max(out=m8b[:], in_=impk[:, 0:Wj]), reads=['impk'], writes=['m8b'])
                    S.op('dve', lambda: V.tensor_scalar(out=tau[:], in0=m8b[:, 4:5], scalar1=-0.5, scalar2=None, op0=ALU.max), reads=['m8b'], writes=['tau'])
                    S.op('dve', lambda: V.tensor_scalar(out=sel[:, 0:Jn], in0=impw[:, 0:Jn], scalar1=tau[:, 0:1], scalar2=None, op0=ALU.is_ge),
                         reads=['impw', 'tau'], writes=['sel'])
                    S.op('dve', lambda: V.memset(sel[:, 0:1], 1.0), reads=[], writes=['sel'])
                    S.op('dve', lambda: V.tensor_tensor(out=sel[:, Jn - 1:Jn], in0=sel[:, Jn - 1:Jn], in1=hilo[:, 2:3], op=ALU.max),
                         reads=['sel', 'hilo'], writes=['sel'])
                items = []
                for kt in range(0, qi + 1):
                    items.append(('s', kt))
                wts = [k for k in range(qi - 4, qi + 1) if k >= 0]
                for kt in wts:
                    items.append(('w', kt))

                def emit_qk(idx):
                    kind, kt = items[idx]
                    b2 = idx % NBK
                    if kind == 's' and kt % 4 == 0 and kt < qi:
                        nb = min(4, qi - kt)
                        g2i = (kt // 4) % 2
                        S.op('dve', lambda: V.tensor_copy(out=mfull[:, 0:nb * 128].rearrange("p (j f) -> p j f", f=64),
                                                          in_=sel[:, 2 * kt:2 * kt + 2 * nb].unsqueeze(2).to_broadcast([128, 2 * nb, 64])),
                             reads=['sel'], writes=['mfull'])
                        for k in range(nb):
                            S.op('pe', lambda: PE.transpose(out=psT[:, k * 128:(k + 1) * 128], in_=mfull[:, k * 128:(k + 1) * 128], identity=identb[:]),
                                 reads=['mfull', 'identb'], writes=['psT'], pe_chain=(k > 0))
                        S.op('act', lambda: A.copy(out=maskT4[g2i][:, 0:nb, :].rearrange("p a b -> p (a b)"), in_=psT[:, 0:nb * 128]),
                             reads=['psT'], writes=['maskT4_%d' % g2i])
                    if kind == 's':
                        lhs = ksT[:, kt * 128:(kt + 1) * 128]; kk = 'ksT'
                    else:
                        lhs = kwT[:, (kt % 8) * 128:(kt % 8) * 128 + 128]; kk = 'kwT'
                    S.op('pe', lambda: PE.matmul(psS[b2][:], lhsT=lhs, rhs=qblk, start=True, stop=True),
                         reads=[kk, 'qTa'], writes=['psS%d' % b2])

                def emit_rest(idx):
                    kind, kt = items[idx]
                    b2 = idx % NBK
                    if kind == 's':
                        msk = triT[:] if kt == qi else maskT4[(kt // 4) % 2][:, kt % 4, :]
                    else:
                        dlt = qi - kt
                        msk = triT[:] if dlt == 0 else (triTs[:] if dlt == 4 else None)
                    if msk is not None:
                        S.op('act', lambda: A.activation(out=eT[b2][:], in_=psS[b2][:], func=AF.Exp), reads=['psS%d' % b2], writes=['eT%d' % b2])
                        S.op('dve', lambda: V.tensor_tensor(out=pT[b2][:].rearrange("p (r q) -> p r q", q=128),
                                                            in0=eT[b2][:].rearrange("p (r q) -> p r q", q=128),
                                                            in1=msk.unsqueeze(1).to_broadcast([128, 4, 128]), op=ALU.mult),
                             reads=['eT%d' % b2, 'maskT4_0', 'maskT4_1', 'triT', 'triTs'], writes=['pT%d' % b2])
                    else:
                        S.op('act', lambda: A.activation(out=pT[b2][:], in_=psS[b2][:], func=AF.Exp), reads=['psS%d' % b2], writes=['pT%d' % b2])
                    if kind == 's':
                        S.op('pe', lambda: PE.matmul(psOs[0:65, :], lhsT=vsA[:, kt, :], rhs=pT[b2][:], start=(kt == 0), stop=(kt == qi)),
                             reads=['vsA', 'pT%d' % b2], writes=['psOs'], pe_chain=(kt > 0))
                    else:
                        S.op('pe', lambda: PE.matmul(psOw[0:65, :], lhsT=vwA[:, kt % 8, :], rhs=pT[b2][:], start=(kt == wts[0]), stop=(kt == qi)),
                             reads=['vwA', 'pT%d' % b2], writes=['psOw'], pe_chain=(kt > wts[0]))

                for idx in range(len(items) + LA):
                    if idx < len(items):
                        emit_qk(idx)
                    if idx - LA >= 0:
                        emit_rest(idx - LA)
                S.op('act', lambda: A.copy(out=oTs[:], in_=psOs[0:65, :]), reads=['psOs'], writes=['oTs'])
                S.op('act', lambda: A.copy(out=oTw[:], in_=psOw[0:65, :]), reads=['psOw'], writes=['oTw'])
                for r in range(4):
                    S.op('pe', lambda: PE.transpose(out=psC[:, r * 65:(r + 1) * 65], in_=oTs[:, r * 128:(r + 1) * 128], identity=identf[0:65, 0:65]),
                         reads=['oTs', 'identf'], writes=['psC'], pe_chain=(r > 0))
                for r in range(4):
                    S.op('pe', lambda: PE.transpose(out=psC[:, 512 + r * 65:512 + (r + 1) * 65], in_=oTw[:, r * 128:(r + 1) * 128], identity=identf[0:65, 0:65]),
                         reads=['oTw', 'identf'], writes=['psC'], pe_chain=True)
                g3 = gates[:, i, :].rearrange("p (r k) -> p r k", k=3)
                osum = psC[:, 0:260].rearrange("p (r e) -> p r e", e=65)
                wsum = psC[:, 512:772].rearrange("p (r e) -> p r e", e=65)
                S.op('dve', lambda: V.tensor_tensor(out=fac[:, 0, :], in0=g3[:, :, 0], in1=rinv[:], op=ALU.mult), reads=['gates', 'rinv'], writes=['fac'])
                S.op('dve', lambda: V.reciprocal(out=fac[:, 1, :], in_=osum[:, :, 64]), reads=['psC'], writes=['fac'])
                S.op('dve', lambda: V.reciprocal(out=fac[:, 2, :], in_=wsum[:, :, 64]), reads=['psC'], writes=['fac'])
                S.op('dve', lambda: V.tensor_tensor(out=fac[:, 1, :], in0=fac[:, 1, :], in1=g3[:, :, 1], op=ALU.mult), reads=['fac', 'gates'], writes=['fac'])
                S.op('dve', lambda: V.tensor_tensor(out=fac[:, 2, :], in0=fac[:, 2, :], in1=g3[:, :, 2], op=ALU.mult), reads=['fac', 'gates'], writes=['fac'])
                for r in range(4):
                    S.op('dve', lambda: V.tensor_scalar(out=ot[:, r, :], in0=psX[:, r * 64:(r + 1) * 64], scalar1=fac[:, 0, r:r + 1], scalar2=None, op0=ALU.mult),
                         reads=['psX', 'fac'], writes=['ot'])
                    S.op('dve', lambda: V.scalar_tensor_tensor(out=ot[:, r, :], in0=osum[:, r, 0:64], scalar=fac[:, 1, r:r + 1], in1=ot[:, r, :],
                                                               op0=ALU.mult, op1=ALU.add), reads=['psC', 'fac', 'ot'], writes=['ot'])
                    S.op('dve', lambda: V.scalar_tensor_tensor(out=ot[:, r, :], in0=wsum[:, r, 0:64], scalar=fac[:, 2, r:r + 1], in1=ot[:, r, :],
                                                               op0=ALU.mult, op1=ALU.add), reads=['psC', 'fac', 'ot'], writes=['ot'])
                S.dma('sp', lambda: nc.sync.dma_start(out=out[qi * 128:(qi + 1) * 128, :], in_=ot[:].rearrange("p r d -> p (r d)")),
                      reads=['ot'], writes=['out'])
        S.finish(['out'])
        print("nsa program: instr", S.n_instr, "waits", S.n_wait)
    return nc


def nsa_consts(T):
    NM = 1024 if T >= 16384 else (T // 16 + 32)
    half = 32
    freqs = (10000.0 ** (-np.arange(half, dtype=np.float32) / half)).astype(np.float32)
    pos = np.arange(T, dtype=np.float32)
    ang = pos[None, :] * freqs[:, None]
    cos2 = np.concatenate([np.cos(ang), np.cos(ang)], 0).astype(np.float32)
    sin2 = np.concatenate([np.sin(ang), np.sin(ang)], 0).astype(np.float32)
    m = np.arange(NM, dtype=np.float32)
    cend = (m - 1) * 16 + 31
    angc = cend[None, :] * freqs[:, None]
    cosc = np.concatenate([np.cos(angc), np.cos(angc)], 0).astype(np.float32)
    sinc = np.concatenate([np.sin(angc), np.sin(angc)], 0).astype(np.float32)
    prot = np.zeros((64, 64), np.float32)
    for d in range(32):
        prot[d + 32, d] = -1.0
        prot[d, d + 32] = 1.0
    l = np.arange(128)
    triT = (l[:, None] <= l[None, :]).astype(np.float32)
    triTs = (l[:, None] > l[None, :]).astype(np.float32)
    fl = np.floor((l - 15) / 16.0)
    j = np.arange(16)
    cmaskG = ((j[None, :] - 8) <= fl[:, None]).astype(np.float32)
    j8 = np.arange(8)
    cmask0 = ((j8[None, :] >= 1) & (j8[None, :] <= fl[:, None])).astype(np.float32)
    hi = (l >= 64).astype(np.float32)
    hilo = np.stack([hi, hi - 1.0, 1.0 - hi], 1).astype(np.float32)
    return {"cos2": cos2, "sin2": sin2, "cosc": cosc, "sinc": sinc, "prot": prot, "identf": np.eye(128, dtype=np.float32),
            "triT": triT, "triTs": triTs, "cmaskG": cmaskG, "cmask0": cmask0, "hilo": hilo}


def nsa_weights(w_proj, g):
    q = w_proj[:, 256 * g:256 * g + 256]
    def blk(i):
        return w_proj[:, 1024 + 256 * i + 64 * g:1024 + 256 * i + 64 * g + 64]
    kc, vc, ks, vs, kw, vw = [blk(i) for i in range(6)]
    gl = w_proj[:, 2560 + 12 * g:2560 + 12 * g + 12]
    wpf = np.ascontiguousarray(np.concatenate([q, kc, vc, ks, kw], 1))
    wpt = np.ascontiguousarray(np.concatenate([q, ks, kw, vs, vw, gl], 1))
    return wpf, wpt

from concourse.bass_utils import run_bass_kernel_spmd

N_CORES = 8
SEQ = 16384


def _c_l(c, b):
    return np.ascontiguousarray(np.asarray(c[b], np.float32).reshape(8, 128).T)


def kernel(x, c, norm1_g, norm2_g, ada_w, ada_b, s5_w_in, s5_a_re, s5_a_im, s5_log_dt,
           s5_b_re, s5_b_im, s5_c_re, s5_c_im, s5_d, s5_w_glu, nsa_w_proj, nsa_pe_k, nsa_pe_v,
           nsa_wk1, nsa_wk2, nsa_wv1, nsa_wv2, nsa_w_o, peer_w_q, peer_sub_keys, peer_u, peer_v,
           final_g):
    f = lambda a: np.ascontiguousarray(np.asarray(a, np.float32))
    x = f(x); c = f(c); ada_w = f(ada_w); ada_b = f(ada_b)
    norm1_g = f(norm1_g); norm2_g = f(norm2_g); final_g = f(final_g)
    peer_w_q = f(peer_w_q); peer_sub_keys = f(peer_sub_keys); peer_u = f(peer_u); peer_v = f(peer_v)
    cores = list(range(N_CORES))
    nc1 = build_s5(SEQ // 128)
    s5c = s5_consts()
    maps = []
    for k in cores:
        b, cq = k // 4, k % 4
        m = {"x": x[b], "c_l": _c_l(c, b), "adaw": f(ada_w[0][:, :2048]), "adab": f(ada_b[0][None, :2048]),
             "n1g": f(norm1_g[0][None]), "win": f(np.asarray(s5_w_in[0])[:, cq * 256:(cq + 1) * 256])}
        m.update(s5c)
        m.update(s5_layouts(f(s5_a_re[0]), f(s5_a_im[0]), f(s5_log_dt[0]), f(s5_b_re[0]), f(s5_b_im[0]),
                            f(s5_c_re[0]), f(s5_c_im[0]), f(s5_d[0]), cq))
        maps.append(m)
    r1 = run_bass_kernel_spmd(nc1, maps, core_ids=cores)
    yactT = [r1.results[k]["out"] for k in cores]
    del maps

    def tok_launch(li, mode, final, xin, mixT_of, wmix):
        nct = build_tok(SEQ // 4 // 128, mode, final)
        tc = tok_consts()
        maps = []
        for k in cores:
            b, q4 = k // 4, k % 4
            sl = slice(q4 * 4096, (q4 + 1) * 4096)
            m = {"x": f(xin[b, sl]), "mixT": mixT_of(b, sl), "c_l": _c_l(c, b),
                 "adaw": f(ada_w[li][:, 2048:]), "adab": f(ada_b[li][None, 2048:]),
                 "n2g": f(norm2_g[li][None]), "fing": f(final_g[None]), "wmix": wmix, "wq": peer_w_q[li],
                 "sk": f(peer_sub_keys[li].reshape(16, 128, 128)), "u_tab": peer_u[li], "v_tab": peer_v[li]}
            m.update(tc)
            maps.append(m)
        r = run_bass_kernel_spmd(nct, maps, core_ids=cores)
        xo = np.empty((2, SEQ, 1024), np.float32)
        for k in cores:
            b, q4 = k // 4, k % 4
            xo[b, q4 * 4096:(q4 + 1) * 4096] = r.results[k]["out"]
        return xo

    def mix0(b, sl):
        return f(np.concatenate([yactT[b * 4 + cq][:, sl] for cq in range(4)], axis=0))
    x2 = tok_launch(0, 'glu', False, x, mix0, f(s5_w_glu[0]))
    del yactT
    nc3 = build_nsa(SEQ // 512)
    nsc = nsa_consts(SEQ)
    maps = []
    for k in cores:
        b, g = k // 4, k % 4
        wpf, wpt = nsa_weights(f(nsa_w_proj[0]), g)
        m = {"x": x2[b], "c_l": _c_l(c, b), "adaw": f(ada_w[1][:, :2048]), "adab": f(ada_b[1][None, :2048]),
             "n1g": f(norm1_g[1][None]), "wpf": wpf, "wpt": wpt,
             "wk1": f(nsa_wk1[0]), "wv1": f(nsa_wv1[0]), "wk2": f(nsa_wk2[0]), "wv2": f(nsa_wv2[0]),
             "pekT": f(np.asarray(nsa_pe_k[0]).T), "pevT": f(np.asarray(nsa_pe_v[0]).T)}
        m.update(nsc)
        maps.append(m)
    r3 = run_bass_kernel_spmd(nc3, maps, core_ids=cores)
    o = [r3.results[k]["out"] for k in cores]
    del maps

    def mix1(b, sl):
        return f(np.concatenate([o[b * 4 + g][sl].T for g in range(4)], axis=0))
    out = tok_launch(1, 'wo', True, x2, mix1, f(nsa_w_o[0]))
    return out
```

```python
from contextlib import ExitStack
import numpy as np
import concourse.bass as bass
import concourse.mybir as mybir

F32 = mybir.dt.float32
BF16 = mybir.dt.bfloat16
I32 = mybir.dt.int32
U32 = mybir.dt.uint32
AF = mybir.ActivationFunctionType
ALU = mybir.AluOpType
AX = mybir.AxisListType


class Sched:
    N_DMA_SEMS = 24
    SEM_MAX = 12000
    DMA_SEM_MAX = 12000

    def __init__(self, nc, es):
        self.nc = nc
        self.es = es
        self.engs = {'pe': nc.tensor, 'dve': nc.vector, 'act': nc.scalar,
                     'pool': nc.gpsimd, 'sp': nc.sync}
        self.dpool = [[es.enter_context(nc.semaphore("ds_%d_0" % i))] for i in range(self.N_DMA_SEMS)]
        self.csem = {}
        self.ccnt = {}
        self.cep = {}
        self.ctot = {}
        self.cpool = {}
        for k in ('pe', 'dve', 'act', 'pool'):
            n_ep = {'pe': 8, 'dve': 8, 'act': 6, 'pool': 3}[k]
            self.cpool[k] = [es.enter_context(nc.semaphore("cs_%s_%d" % (k, j))) for j in range(n_ep)]
            self.csem[k] = self.cpool[k][0]
            self.ccnt[k] = 0
            self.cep[k] = 0
            self.ctot[k] = 0
        self.dsem = [self.dpool[i][0] for i in range(self.N_DMA_SEMS)]
        self.dcnt = [0] * self.N_DMA_SEMS
        self.dep = [0] * self.N_DMA_SEMS
        for i in range(self.N_DMA_SEMS):
            self.dpool[i].append(es.enter_context(nc.semaphore("ds_%d_1" % i)))
        self.drr = 0
        self.seen = {k: {} for k in self.engs}
        self.lastw = {}
        self.readers = {}
        self.n_instr = 0
        self.n_wait = 0

    def _deps(self, reads, writes):
        deps = []
        for k in reads:
            w = self.lastw.get(k)
            if w is not None:
                deps.append(w)
        for k in writes:
            w = self.lastw.get(k)
            if w is not None:
                deps.append(w)
            deps.extend(self.readers.get(k, ()))
        return deps

    def _wait(self, e, deps, skip_self=None):
        eng = self.engs[e]
        best = {}
        for (sid, sem, val) in deps:
            if skip_self is not None and sid.startswith(skip_self):
                continue
            if best.get(sid, (None, 0))[1] < val:
                best[sid] = (sem, val)
        for sid, (sem, val) in best.items():
            if self.seen[e].get(sid, 0) < val:
                eng.wait_ge(sem, val)
                self.seen[e][sid] = val
                self.n_wait += 1

    def _record(self, ev, reads, writes):
        for k in reads:
            self.readers.setdefault(k, []).append(ev)
        for k in writes:
            self.lastw[k] = ev
            self.readers[k] = []

    def op(self, e, fn, reads=(), writes=(), pe_chain=False):
        deps = self._deps(reads, writes)
        self._wait(e, deps, skip_self=('c_pe_' if (pe_chain and e == 'pe') else None))
        ins = fn()
        if self.ccnt[e] >= self.SEM_MAX:
            self.cep[e] += 1
            self.csem[e] = self.cpool[e][self.cep[e]]
            self.ccnt[e] = 0
        self.ccnt[e] += 1
        self.ctot[e] += 1
        ins.then_inc(self.csem[e], 1)
        ev = ('c_%s_%d' % (e, self.cep[e]), self.csem[e], self.ccnt[e])
        self._record(ev, reads, writes)
        self.n_instr += 1
        return ev

    def dma(self, q, fn, reads=(), writes=()):
        deps = self._deps(reads, writes)
        self._wait(q, deps)
        i = self.drr
        self.drr = (self.drr + 1) % self.N_DMA_SEMS
        sid = 'd_%d_%d' % (i, self.dep[i])
        if self.seen[q].get(sid, 0) < self.dcnt[i]:
            self.engs[q].wait_ge(self.dsem[i], self.dcnt[i])
            self.seen[q][sid] = self.dcnt[i]
        if self.dcnt[i] >= self.DMA_SEM_MAX:
            self.dep[i] += 1
            self.dsem[i] = self.dpool[i][self.dep[i]]
            self.dcnt[i] = 0
            sid = 'd_%d_%d' % (i, self.dep[i])
        ins = fn()
        self.dcnt[i] += 16
        ins.then_inc(self.dsem[i], 16)
        ev = (sid, self.dsem[i], self.dcnt[i])
        self._record(ev, reads, writes)
        self.n_instr += 1
        return ev

    def finish(self, keys):
        deps = []
        for k in keys:
            w = self.lastw.get(k)
            if w is not None:
                deps.append(w)
        self._wait('sp', deps)
        alld = []
        for k in ('pe', 'dve', 'act', 'pool'):
            if self.ccnt[k]:
                alld.append(('c_%s_%d' % (k, self.cep[k]), self.csem[k], self.ccnt[k]))
        for i in range(self.N_DMA_SEMS):
            if self.dcnt[i]:
                alld.append(('d_%d_%d' % (i, self.dep[i]), self.dsem[i], self.dcnt[i]))
        self._wait('sp', alld)


def sched_barrier(S):
    alld = []
    for k in ('pe', 'dve', 'act', 'pool'):
        if S.ccnt[k]:
            alld.append(('c_%s_%d' % (k, S.cep[k]), S.csem[k], S.ccnt[k]))
    for i in range(S.N_DMA_SEMS):
        if S.dcnt[i]:
            alld.append(('d_%d_%d' % (i, S.dep[i]), S.dsem[i], S.dcnt[i]))
    for e in ('pe', 'dve', 'act', 'pool', 'sp'):
        S._wait(e, alld)


Sched.barrier = sched_barrier


RMS_EPS = 1e-6
IOA = bass.IndirectOffsetOnAxis


def build_tok(NT, mode, final, gelu_func=None, dbg=None):
    nc = bass.Bass("TRN2", target_bir_lowering=False)
    T = NT * 128
    MIXN = 2048 if mode == 'glu' else 1024
    D = 1024
    din = lambda n, s, d=F32: nc.dram_tensor(n, s, d, kind="ExternalInput").ap()
    x = din("x", [T, D])
    mixT = din("mixT", [D, T])
    c_l = din("c_l", [128, 8])
    adaw = din("adaw", [D, 4096])
    adab = din("adab", [1, 4096])
    n2g = din("n2g", [1, D])
    fing = din("fing", [1, D])
    wmix_d = din("wmix", [D, MIXN])
    wq_d = din("wq", [D, 2048])
    sk_d = din("sk", [16, 128, 128])
    u_tab = din("u_tab", [16384, D])
    v_tab = din("v_tab", [16384, D])
    identf_d = din("identf", [128, 128])
    iota16_d = din("iota16", [128, 16])
    out = nc.dram_tensor("out", [T, D], F32, kind="ExternalOutput").ap()
    u_bf = nc.dram_tensor("u_bf", [16384, D], BF16).ap()
    v_bf = nc.dram_tensor("v_bf", [16384, D], BF16).ap()

    es = ExitStack()
    with es:
        es.enter_context(nc.allow_low_precision("bf16 matmul operands"))
        es.enter_context(nc.allow_non_contiguous_dma("small layout loads"))
        S = Sched(nc, es)
        sb = lambda n, s, d=F32: es.enter_context(nc.sbuf_tensor("s_" + n, s, d))
        ps = lambda n, s, d=F32: es.enter_context(nc.psum_tensor("p_" + n, s, d))
        V, A, P, PE = nc.vector, nc.scalar, nc.gpsimd, nc.tensor

        identf = sb("identf", [128, 128])
        identb = sb("identb", [128, 128], BF16)
        iota16 = sb("iota16", [128, 16])
        epsT = sb("epsT", [128, 1])
        modrep = sb("modrep", [128, 4096])
        gk2 = sb("gk2", [128, D])
        fing_rep = sb("fing_rep", [128, D])
        ot = sb("ot", [128, D])
        n2g_rep = ot
        wq = sb("wq", [128, 8, 2048], BF16)
        wmix = sb("wmix", [128, 8, MIXN], BF16)
        skT = sb("skT", [128, 16, 128], BF16)

        psA = ps("psA", [128, 1024])
        psV = ps("psV", [128, 1024])
        psB = ps("psB", [128, 1024])
        psT = ps("psT", [128, 1024], BF16)
        psM = ps("psM", [128, 512])
        es2 = ExitStack()
        tb = lambda n, s, d=F32: es2.enter_context(nc.sbuf_tensor("t_" + n, s, d))
        csil = tb("csil", [128, 8])
        csil_rep = tb("csil_rep", [128, 8, 128])
        stg = [tb("stg%d" % i, [128, 8, 256]) for i in range(2)]
        adab_rep = tb("adab_rep", [128, 256])

        S.dma('sp', lambda: nc.sync.dma_start(out=identf[:], in_=identf_d[:, :]), writes=['identf'])
        S.dma('sp', lambda: nc.sync.dma_start(out=iota16[:], in_=iota16_d[:, :]), writes=['iota4'])
        S.dma('sp', lambda: nc.sync.dma_start(out=csil[:], in_=c_l[:, :]), writes=['csil'])
        S.dma('sp', lambda: nc.sync.dma_start(out=n2g_rep[:], in_=n2g[0:1, :].partition_broadcast(128)), writes=['ot'])
        if final:
            S.dma('sp', lambda: nc.sync.dma_start(out=fing_rep[:], in_=fing[0:1, :].partition_broadcast(128)), writes=['fing_rep'])
        S.op('dve', lambda: V.tensor_copy(out=identb[:], in_=identf[:]), reads=['identf'], writes=['identb'])
        S.op('dve', lambda: V.memset(epsT[:], RMS_EPS), writes=['epsT'])
        S.op('act', lambda: A.activation(out=csil[:], in_=csil[:], func=AF.Silu), reads=['csil'], writes=['csil'])
        S.op('dve', lambda: V.tensor_copy(out=csil_rep[:], in_=csil[:].unsqueeze(2).to_broadcast([128, 8, 128])),
             reads=['csil'], writes=['csil_rep'])
        for j in range(16):
            st = stg[j % 2]
            k_st = 'stg%d' % (j % 2)
            S.dma('sp', lambda: nc.sync.dma_start(
                out=st[:], in_=adaw[:, j * 256:(j + 1) * 256].rearrange("(kc k) n -> k kc n", k=128)), writes=[k_st])
            S.dma('sp', lambda: nc.sync.dma_start(
                out=adab_rep[:], in_=adab[0:1, j * 256:(j + 1) * 256].partition_broadcast(128)), writes=['adab_rep'])
            for kc in range(8):
                S.op('pe', lambda: PE.matmul(psM[:, 0:256], lhsT=csil_rep[:, kc, :], rhs=st[:, kc, :], start=(kc == 0), stop=(kc == 7)),
                     reads=['csil_rep', k_st], writes=['psM'], pe_chain=(kc > 0))
            S.op('dve', lambda: V.tensor_tensor(out=modrep[:, j * 256:(j + 1) * 256], in0=psM[:, 0:256], in1=adab_rep[:], op=ALU.add),
                 reads=['psM', 'adab_rep'], writes=['modrep'])
        g1 = modrep[:, 0:1024]
        sh2 = modrep[:, 1024:2048]
        sc2 = modrep[:, 2048:3072]
        g2 = modrep[:, 3072:4096]
        S.op('dve', lambda: V.scalar_tensor_tensor(out=gk2[:], in0=sc2, scalar=1.0, in1=n2g_rep[:], op0=ALU.add, op1=ALU.mult),
             reads=['modrep', 'ot'], writes=['gk2'])
        cast_i = [0]

        def load_cast(dst3, src2d, ncols, key):
            for c0 in range(0, ncols, 256):
                i = cast_i[0] % 2
                cast_i[0] += 1
                st = stg[i]
                S.dma('sp', lambda: nc.sync.dma_start(
                    out=st[:], in_=src2d[:, c0:c0 + 256].rearrange("(kc k) n -> k kc n", k=128)), writes=['stg%d' % i])
                if i == 0:
                    S.op('dve', lambda: V.tensor_copy(out=dst3[:, :, c0:c0 + 256], in_=st[:]), reads=['stg%d' % i], writes=[key])
                else:
                    S.op('act', lambda: A.copy(out=dst3[:, :, c0:c0 + 256], in_=st[:]), reads=['stg%d' % i], writes=[key])

        load_cast(wmix, wmix_d, MIXN, 'wmix')
        load_cast(wq, wq_d, 2048, 'wq')
        for j in range(16):
            st = stg[j % 2]
            k_st = 'stg%d' % (j % 2)
            S.dma('sp', lambda: nc.sync.dma_start(out=st[:, 0, 0:128], in_=sk_d[j, :, :]), writes=[k_st])
            S.op('pe', lambda: PE.transpose(out=psM[:, 0:128], in_=st[:, 0, 0:128], identity=identf[:]),
                 reads=[k_st, 'identf'], writes=['psM'])
            S.op('act', lambda: A.copy(out=skT[:, j, :], in_=psM[:, 0:128]), reads=['psM'], writes=['skT'])

        cvb = [tb("cvb%d" % i, [128, 2048], BF16) for i in range(2)]
        cvi = 0
        for (tab, tbf) in ((u_tab, u_bf), (v_tab, v_bf)):
            for c0 in range(0, 16384, 256):
                i = cvi % 2
                st = stg[i]
                k_st = 'stg%d' % i
                S.dma('sp', lambda: nc.sync.dma_start(out=st[:].rearrange("p a b -> p (a b)"),
                                                      in_=tab[c0:c0 + 256, :].rearrange("(p r) d -> p (r d)", r=2)), writes=[k_st])
                e = ('dve', 'act', 'pool')[cvi % 3]
                if e == 'dve':
                    S.op('dve', lambda: V.tensor_copy(out=cvb[i][:], in_=st[:].rearrange("p a b -> p (a b)")), reads=[k_st], writes=['cvb%d' % i])
                elif e == 'act':
                    S.op('act', lambda: A.copy(out=cvb[i][:], in_=st[:].rearrange("p a b -> p (a b)")), reads=[k_st], writes=['cvb%d' % i])
                else:
                    S.op('pool', lambda: P.tensor_copy(out=cvb[i][:], in_=st[:].rearrange("p a b -> p (a b)")), reads=[k_st], writes=['cvb%d' % i])
                S.dma('sp', lambda: nc.sync.dma_start(out=tbf[c0:c0 + 256, :].rearrange("(p r) d -> p (r d)", r=2), in_=cvb[i][:]),
                      reads=['cvb%d' % i], writes=['tbf'])
                cvi += 1

        S.barrier()
        es2.close()
        xt = [sb("xt%d" % i, [128, D]) for i in range(2)]
        mT = sb("mT", [128, 8, 128])
        mTb = sb("mTb", [128, 8, 128], BF16)
        x1b = [sb("x1_%d" % i, [128, D]) for i in range(2)]
        junk = sb("junk", [128, D], BF16)
        ss = sb("ss", [128, 1])
        rs = sb("rs", [128, 1])
        hf = sb("hf", [128, D])
        hfb = sb("hfb", [128, D], BF16)
        hfT = sb("hfT", [128, 8, 128], BF16)
        qkT = sb("qkT", [128, 16, 128], BF16)
        scw = sb("scw", [128, 16, 128])
        vals = sb("vals", [128, 16, 16])
        idx = sb("idx", [128, 16, 16], U32)
        idxf = sb("idxf", [128, 16, 16])
        cand = sb("cand", [128, 8, 256])
        wk2 = sb("wk2", [128, 2048])
        scw2 = wk2[:].rearrange("p (a b) -> p a b", b=128)
        cand2 = wk2[:].rearrange("p (a b) -> p a b", b=256)
        tops = sb("tops", [128, 8, 16])
        pos = sb("pos", [128, 8, 16], U32)
        au = sb("au", [128, 8, 16], U32)
        bu = sb("bu", [128, 8, 16], U32)
        af = sb("af", [128, 8, 16])
        bf = sb("bf", [128, 8, 16])
        eq = scw[:].rearrange("p a b -> p (a b)").rearrange("p (h k c) -> p h k c", h=8, k=16)
        sel1 = sb("sel1", [128, 8, 16])
        sel2 = sb("sel2", [128, 8, 16])
        ef = sb("ef", [128, 128])
        eidxb = [sb("eidx_%d" % i, [128, 128], U32) for i in range(2)]
        gat = sb("gat", [128, 8, 16])
        gsum = sb("gsum", [128, 8])
        sdot = sb("sdot", [128, 128])
        actvb = [sb("actv_%d" % i, [128, 128]) for i in range(2)]
        NB = 8
        NBV = 6
        ug = [sb("ug%d" % i, [128, D], BF16) for i in range(NB)]
        vg = [sb("vg%d" % i, [128, D], BF16) for i in range(NBV)]
        dg = [sb("dg%d" % i, [128, 128], BF16) for i in range(NBV)]
        acc = sb("acc", [128, D])
        sg = sb("sg", [128, D])

        gfunc = gelu_func if gelu_func is not None else AF.Gelu_apprx_tanh

        def front(t):
                x1 = x1b[t % 2]; kx1 = 'x1_%d' % (t % 2)
                eidx = eidxb[t % 2]; keidx = 'eidx_%d' % (t % 2)
                actv = actvb[t % 2]; kactv = 'actv_%d' % (t % 2)
                r0 = t * 128
                xb = xt[t % 2]
                kx = 'xt%d' % (t % 2)
                S.dma('sp', lambda: nc.sync.dma_start(out=xb[:], in_=x[r0:r0 + 128, :]), writes=[kx])
                S.dma('sp', lambda: nc.sync.dma_start(
                    out=mT[:], in_=mixT[:, r0:r0 + 128].rearrange("(kc k) n -> k kc n", k=128)), writes=['mT'])
                S.op('act', lambda: A.copy(out=mTb[:], in_=mT[:]), reads=['mT'], writes=['mTb'])
                def mixmm(c0):
                    for nb in range(2):
                        for kc in range(8):
                            S.op('pe', lambda: PE.matmul(psA[:, nb * 512:(nb + 1) * 512], lhsT=mTb[:, kc, :],
                                                         rhs=wmix[:, kc, c0 + nb * 512:c0 + (nb + 1) * 512], start=(kc == 0), stop=(kc == 7)),
                                 reads=['mTb', 'wmix'], writes=['psA'], pe_chain=not (nb == 0 and kc == 0))
                if mode == 'glu':
                    mixmm(1024)
                    S.op('act', lambda: A.activation(out=sg[:], in_=psA[:], func=AF.Sigmoid), reads=['psA'], writes=['sg'])
                    S.op('dve', lambda: V.tensor_tensor(out=sg[:], in0=sg[:], in1=g1, op=ALU.mult), reads=['sg', 'modrep'], writes=['sg'])
                    mixmm(0)
                    S.op('dve', lambda: V.tensor_tensor(out=x1[:], in0=psA[:], in1=sg[:], op=ALU.mult),
                         reads=['psA', 'sg'], writes=[kx1])
                else:
                    mixmm(0)
                    S.op('dve', lambda: V.tensor_tensor(out=x1[:], in0=psA[:], in1=g1, op=ALU.mult),
                         reads=['psA', 'modrep'], writes=[kx1])
                S.op('dve', lambda: V.tensor_tensor(out=x1[:], in0=x1[:], in1=xb[:], op=ALU.add), reads=[kx1, kx], writes=[kx1])
                yield
                S.op('act', lambda: A.activation(out=junk[:], in_=x1[:], func=AF.Square, accum_out=ss[:]), reads=[kx1], writes=['junk', 'ss'])
                S.op('act', lambda: A.activation(out=rs[:], in_=ss[:], func=AF.Sqrt, bias=epsT[:], scale=1.0 / D),
                     reads=['ss', 'epsT'], writes=['rs'])
                S.op('dve', lambda: V.reciprocal(out=rs[:], in_=rs[:]), reads=['rs'], writes=['rs'])
                S.op('dve', lambda: V.scalar_tensor_tensor(out=hf[:], in0=x1[:], scalar=rs[:], in1=gk2[:], op0=ALU.mult, op1=ALU.mult),
                     reads=[kx1, 'rs', 'gk2'], writes=['hf'])
                S.op('dve', lambda: V.tensor_tensor(out=hf[:], in0=hf[:], in1=sh2, op=ALU.add), reads=['hf', 'modrep'], writes=['hf'])
                yield
                S.op('act', lambda: A.copy(out=hfb[:], in_=hf[:]), reads=['hf'], writes=['hfb'])
                for kc in range(8):
                    S.op('pe', lambda: PE.transpose(out=psT[:, kc * 128:(kc + 1) * 128], in_=hfb[:, kc * 128:(kc + 1) * 128], identity=identb[:]),
                         reads=['hfb', 'identb'], writes=['psT'], pe_chain=(kc > 0))
                S.op('dve', lambda: V.tensor_copy(out=hfT[:].rearrange("p a b -> p (a b)"), in_=psT[:]), reads=['psT'], writes=['hfT'])
                yield
                for half in range(2):
                    for jj in range(8):
                        j = half * 8 + jj
                        for kc in range(8):
                            S.op('pe', lambda: PE.matmul(psB[:, jj * 128:(jj + 1) * 128], lhsT=wq[:, kc, j * 128:(j + 1) * 128],
                                                         rhs=hfT[:, kc, :], start=(kc == 0), stop=(kc == 7)),
                                 reads=['wq', 'hfT'], writes=['psB'], pe_chain=not (jj == 0 and kc == 0))
                    S.op('act', lambda: A.copy(out=qkT[:, half * 8:(half + 1) * 8, :].rearrange("p a b -> p (a b)"), in_=psB[:]),
                         reads=['psB'], writes=['qkT'])
                yield
                for half in range(2):
                    for jj in range(8):
                        j = half * 8 + jj
                        S.op('pe', lambda: PE.matmul(psA[:, jj * 128:(jj + 1) * 128], lhsT=qkT[:, j, :], rhs=skT[:, j, :], start=True, stop=True),
                             reads=['qkT', 'skT'], writes=['psA'], pe_chain=(jj > 0))
                    S.op('act', lambda: A.copy(out=scw[:, half * 8:(half + 1) * 8, :].rearrange("p a b -> p (a b)"), in_=psA[:]),
                         reads=['psA'], writes=['scw'])
                yield
                for j in range(16):
                    if j == 8:
                        yield
                    S.op('dve', lambda: V.max(out=vals[:, j, 0:8], in_=scw[:, j, :]), reads=['scw'], writes=['vals'])
                    S.op('dve', lambda: V.max_index(out=idx[:, j, 0:8], in_max=vals[:, j, 0:8], in_values=scw[:, j, :]),
                         reads=['scw', 'vals'], writes=['idx'])
                    S.op('dve', lambda: V.match_replace(out=scw2[:, j, :], in_to_replace=vals[:, j, 0:8], in_values=scw[:, j, :], imm_value=-1e30),
                         reads=['scw', 'vals'], writes=['wk2'])
                    S.op('dve', lambda: V.max(out=vals[:, j, 8:16], in_=scw2[:, j, :]), reads=['wk2'], writes=['vals'])
                    S.op('dve', lambda: V.max_index(out=idx[:, j, 8:16], in_max=vals[:, j, 8:16], in_values=scw2[:, j, :]),
                         reads=['wk2', 'vals'], writes=['idx'])
                yield
                vals4 = vals[:].rearrange("p (h c) k -> p h c k", c=2)
                S.op('dve', lambda: V.tensor_tensor(out=cand[:].rearrange("p h (a b) -> p h a b", b=16),
                                                    in0=vals4[:, :, 0, :].unsqueeze(3).to_broadcast([128, 8, 16, 16]),
                                                    in1=vals4[:, :, 1, :].unsqueeze(2).to_broadcast([128, 8, 16, 16]), op=ALU.add),
                     reads=['vals'], writes=['cand'])
                for h in range(8):
                    S.op('dve', lambda: V.max(out=tops[:, h, 0:8], in_=cand[:, h, :]), reads=['cand'], writes=['tops'])
                    S.op('dve', lambda: V.max_index(out=pos[:, h, 0:8], in_max=tops[:, h, 0:8], in_values=cand[:, h, :]),
                         reads=['cand', 'tops'], writes=['pos'])
                    S.op('dve', lambda: V.match_replace(out=cand2[:, h, :], in_to_replace=tops[:, h, 0:8], in_values=cand[:, h, :], imm_value=-1e30),
                         reads=['cand', 'tops'], writes=['wk2'])
                    S.op('dve', lambda: V.max(out=tops[:, h, 8:16], in_=cand2[:, h, :]), reads=['wk2'], writes=['tops'])
                    S.op('dve', lambda: V.max_index(out=pos[:, h, 8:16], in_max=tops[:, h, 8:16], in_values=cand2[:, h, :]),
                         reads=['wk2', 'tops'], writes=['pos'])
                yield
                S.op('dve', lambda: V.tensor_single_scalar(out=au[:], in_=pos[:], scalar=4, op=ALU.logical_shift_right), reads=['pos'], writes=['au'])
                S.op('dve', lambda: V.tensor_single_scalar(out=bu[:], in_=pos[:], scalar=15, op=ALU.bitwise_and), reads=['pos'], writes=['bu'])
                S.op('dve', lambda: V.tensor_copy(out=af[:], in_=au[:]), reads=['au'], writes=['af'])
                S.op('dve', lambda: V.tensor_copy(out=bf[:], in_=bu[:]), reads=['bu'], writes=['bf'])
                S.op('dve', lambda: V.tensor_copy(out=idxf[:], in_=idx[:]), reads=['idx'], writes=['idxf'])
                idxf4 = idxf[:].rearrange("p (h c) k -> p h c k", c=2)
                for (sf, cc, sel) in ((af, 0, sel1), (bf, 1, sel2)):
                    ksel = 'sel1' if cc == 0 else 'sel2'
                    S.op('dve', lambda: V.tensor_tensor(out=eq[:], in0=sf[:].unsqueeze(3).to_broadcast([128, 8, 16, 16]),
                                                        in1=iota16[:].unsqueeze(1).unsqueeze(1).to_broadcast([128, 8, 16, 16]), op=ALU.is_equal), reads=['af', 'bf', 'iota4'], writes=['scw'])
                    S.op('dve', lambda: V.tensor_tensor(out=eq[:], in0=eq[:], in1=idxf4[:, :, cc, :].unsqueeze(2).to_broadcast([128, 8, 16, 16]),
                                                        op=ALU.mult), reads=['scw', 'idxf'], writes=['scw'])
                    S.op('dve', lambda: V.tensor_reduce(out=sel[:], in_=eq[:], axis=AX.X, op=ALU.add), reads=['scw'], writes=[ksel])
                S.op('dve', lambda: V.scalar_tensor_tensor(out=ef[:], in0=sel1[:].rearrange("p h k -> p (h k)"), scalar=128.0,
                                                           in1=sel2[:].rearrange("p h k -> p (h k)"), op0=ALU.mult, op1=ALU.add),
                     reads=['sel1', 'sel2'], writes=['ef'])
                S.op('dve', lambda: V.tensor_copy(out=eidx[:], in_=ef[:]), reads=['ef'], writes=[keidx])
                yield
                S.op('dve', lambda: V.tensor_tensor(out=gat[:], in0=tops[:], in1=tops[:, :, 0:1].to_broadcast([128, 8, 16]), op=ALU.subtract),
                     reads=['tops'], writes=['gat'])
                S.op('act', lambda: A.activation(out=gat[:], in_=gat[:], func=AF.Exp), reads=['gat'], writes=['gat'])
                S.op('dve', lambda: V.tensor_reduce(out=gsum[:], in_=gat[:], axis=AX.X, op=ALU.add), reads=['gat'], writes=['gsum'])
                S.op('dve', lambda: V.reciprocal(out=gsum[:], in_=gsum[:]), reads=['gsum'], writes=['gsum'])
                S.op('dve', lambda: V.tensor_tensor(out=gat[:], in0=gat[:], in1=gsum[:].unsqueeze(2).to_broadcast([128, 8, 16]), op=ALU.mult),
                     reads=['gat', 'gsum'], writes=['gat'])

        def midU(t):
                x1 = x1b[t % 2]; kx1 = 'x1_%d' % (t % 2)
                eidx = eidxb[t % 2]; keidx = 'eidx_%d' % (t % 2)
                actv = actvb[t % 2]; kactv = 'actv_%d' % (t % 2)
                r0 = t * 128
                for s in range(128):
                    b = s % NB
                    S.dma('pool', lambda: P.indirect_dma_start(out=ug[b][:], out_offset=None, in_=u_bf[:, :],
                                                                in_offset=IOA(ap=eidx[:, s:s + 1], axis=0)),
                          reads=[keidx, 'tbf'], writes=['ug%d' % b])
                    S.op('dve', lambda: V.scalar_tensor_tensor(out=junk[:], in0=ug[b][:], scalar=1.0, in1=hf[:],
                                                               op0=ALU.mult, op1=ALU.mult, accum_out=sdot[:, s:s + 1]),
                         reads=['ug%d' % b, 'hf'], writes=['junk', 'sdot'])
                S.op('dve', lambda: V.tensor_tensor(out=actv[:], in0=sdot[:], in1=sdot[:], op=ALU.mult), reads=['sdot'], writes=[kactv])
                S.op('dve', lambda: V.tensor_scalar(out=actv[:], in0=actv[:], scalar1=0.044715, scalar2=1.0, op0=ALU.mult, op1=ALU.add),
                     reads=[kactv], writes=[kactv])
                S.op('dve', lambda: V.tensor_tensor(out=actv[:], in0=actv[:], in1=sdot[:], op=ALU.mult), reads=[kactv, 'sdot'], writes=[kactv])
                S.op('act', lambda: A.activation(out=actv[:], in_=actv[:], func=AF.Sigmoid, scale=1.5957691216), reads=[kactv], writes=[kactv])
                S.op('dve', lambda: V.tensor_tensor(out=actv[:], in0=actv[:], in1=sdot[:], op=ALU.mult), reads=[kactv, 'sdot'], writes=[kactv])
                S.op('dve', lambda: V.tensor_tensor(out=actv[:], in0=actv[:], in1=gat[:].rearrange("p h k -> p (h k)"), op=ALU.mult),
                     reads=[kactv, 'gat'], writes=[kactv])

        def midV(t, gen=None):
                x1 = x1b[t % 2]; kx1 = 'x1_%d' % (t % 2)
                eidx = eidxb[t % 2]; keidx = 'eidx_%d' % (t % 2)
                actv = actvb[t % 2]; kactv = 'actv_%d' % (t % 2)
                r0 = t * 128
                for s in range(128):
                    b = s % NBV
                    if gen is not None and s % 12 == 6:
                        next(gen, None)
                    S.dma('pool', lambda: P.indirect_dma_start(out=vg[b][:], out_offset=None, in_=v_bf[:, :],
                                                                in_offset=IOA(ap=eidx[:, s:s + 1], axis=0)),
                          reads=[keidx, 'tbf'], writes=['vg%d' % b])
                    S.op('act', lambda: A.activation(out=dg[b][:], in_=identf[:], func=AF.Copy, scale=actv[:, s:s + 1]),
                         reads=['identf', kactv], writes=['dg%d' % b])
                    for hb in range(2):
                        S.op('pe', lambda: PE.matmul(psV[:, hb * 512:(hb + 1) * 512], lhsT=dg[b][:], rhs=vg[b][:, hb * 512:(hb + 1) * 512],
                                                     start=(s == 0), stop=(s == 127)), reads=['dg%d' % b, 'vg%d' % b], writes=['psV'],
                             pe_chain=not (s == 0 and hb == 0))

        def tail(t):
                x1 = x1b[t % 2]; kx1 = 'x1_%d' % (t % 2)
                eidx = eidxb[t % 2]; keidx = 'eidx_%d' % (t % 2)
                actv = actvb[t % 2]; kactv = 'actv_%d' % (t % 2)
                r0 = t * 128
                S.op('dve', lambda: V.tensor_tensor(out=acc[:], in0=psV[:], in1=g2, op=ALU.mult), reads=['psV', 'modrep'], writes=['acc'])
                S.op('dve', lambda: V.tensor_tensor(out=ot[:], in0=acc[:], in1=x1[:], op=ALU.add), reads=['acc', kx1], writes=['ot'])
                if final:
                    S.op('act', lambda: A.activation(out=junk[:], in_=ot[:], func=AF.Square, accum_out=ss[:]), reads=['ot'], writes=['junk', 'ss'])
                    S.op('act', lambda: A.activation(out=rs[:], in_=ss[:], func=AF.Sqrt, bias=epsT[:], scale=1.0 / D),
                         reads=['ss', 'epsT'], writes=['rs'])
                    S.op('dve', lambda: V.reciprocal(out=rs[:], in_=rs[:]), reads=['rs'], writes=['rs'])
                    S.op('dve', lambda: V.scalar_tensor_tensor(out=ot[:], in0=ot[:], scalar=rs[:], in1=fing_rep[:], op0=ALU.mult, op1=ALU.mult),
                         reads=['ot', 'rs', 'fing_rep'], writes=['ot'])
                S.dma('sp', lambda: nc.sync.dma_start(out=out[r0:r0 + 128, :], in_=ot[:]), reads=['ot'], writes=['out'])

        for _ in front(0):
            pass
        for t in range(NT):
            midU(t)
            gen = front(t + 1) if t + 1 < NT else None
            midV(t, gen)
            if gen is not None:
                for _ in gen:
                    pass
            tail(t)
        S.finish(['out'])
        print("tok program: instr", S.n_instr, "waits", S.n_wait)
    return nc


def tok_consts():
    iota16 = np.broadcast_to(np.arange(16, dtype=np.float32)[None, :], (128, 16)).copy()
    return {"identf": np.eye(128, dtype=np.float32), "iota16": iota16}

import math

RMS_EPS = 1e-6
PI = math.pi


def build_s5(NCH):
    nc = bass.Bass("TRN2", target_bir_lowering=False)
    T = NCH * 128
    D = 1024
    din = lambda n, s, d=F32: nc.dram_tensor(n, s, d, kind="ExternalInput").ap()
    x = din("x", [T, D])
    c_l = din("c_l", [128, 8])
    adaw = din("adaw", [D, 2048])
    adab = din("adab", [1, 2048])
    n1g = din("n1g", [1, D])
    win_d = din("win", [D, 256])
    are_c_d = din("are_c", [128, 16]); aim_c_d = din("aim_c", [128, 16]); ldt_c_d = din("ldt_c", [128, 16])
    are_r_d = din("are_r", [128, 2048]); aim_r_d = din("aim_r", [128, 2048]); ldt_r_d = din("ldt_r", [128, 2048])
    X1p_d = din("X1p", [128, 2048]); X2p_d = din("X2p", [128, 2048])
    CcP_d = din("CcP", [128, 2048]); CcSP_d = din("CcSP", [128, 2048])
    dcol_d = din("dcol", [128, 2])
    identf_d = din("identf", [128, 128]); swapm_d = din("swapm", [128, 128])
    sgnc_d = din("sgn_c", [128, 1]); sgnr_d = din("sgn_r", [128, 128])
    mrow_d = din("mrow", [128, 128]); mask01_d = din("mask01", [128, 512])
    out = nc.dram_tensor("out", [256, T], F32, kind="ExternalOutput").ap()

    es = ExitStack()
    with es:
        es.enter_context(nc.allow_low_precision("bf16 matmul operands"))
        es.enter_context(nc.allow_non_contiguous_dma("small layout loads"))
        S = Sched(nc, es)
        sb = lambda n, s, d=F32: es.enter_context(nc.sbuf_tensor("s_" + n, s, d))
        ps = lambda n, s, d=F32: es.enter_context(nc.psum_tensor("p_" + n, s, d))
        V, A, P, PE = nc.vector, nc.scalar, nc.gpsimd, nc.tensor

        def ld(dst, src, key):
            S.dma('sp', lambda: nc.sync.dma_start(out=dst, in_=src), writes=[key])

        identf = sb("identf", [128, 128]); identb = sb("identb", [128, 128], BF16)
        epsT = sb("epsT", [128, 1])
        modrep = sb("modrep", [128, 2048])
        gk1 = sb("gk1", [128, D])
        win = sb("win", [128, 8, 256], BF16)
        Ainv_r = sb("Ainv_r", [128, 16, 128]); Ainv_i = sb("Ainv_i", [128, 16, 128])
        Apow_r = sb("Apow_r", [128, 16, 128]); Apow_i = sb("Apow_i", [128, 16, 128])
        Rot = sb("Rot", [128, 16, 128])
        Bpad = sb("Bpad", [128, 16, 128], BF16); BpadS = sb("BpadS", [128, 16, 128], BF16)
        Cc = sb("Cc", [128, 16, 128], BF16); CcS = sb("CcS", [128, 16, 128], BF16)
        dcol = sb("dcol", [128, 2])
        mask01 = sb("mask01", [128, 512])

        psT = ps("psT", [128, 1024], BF16)
        psU = ps("psU", [128, 512])
        psBU = [ps("psBU%d" % i, [128, 512]) for i in range(2)]
        psBS = [ps("psBS%d" % i, [128, 512]) for i in range(2)]
        psI = ps("psI", [128, 512])
        psY = ps("psY", [128, 512])

        ld(identf[:], identf_d[:, :], 'identf')
        ld(dcol[:], dcol_d[:, :], 'dcol')
        ld(mask01[:], mask01_d[:, :], 'mask01')
        S.op('dve', lambda: V.tensor_copy(out=identb[:], in_=identf[:]), reads=['identf'], writes=['identb'])
        S.op('dve', lambda: V.memset(epsT[:], RMS_EPS), writes=['epsT'])

        es2 = ExitStack()
        with es2:
            tb = lambda n, s, d=F32: es2.enter_context(nc.sbuf_tensor("t_" + n, s, d))
            csil = tb("csil", [128, 8]); csil_rep = tb("csil_rep", [128, 8, 128])
            stg = [tb("stg%d" % i, [128, 8, 256]) for i in range(2)]
            adab_rep = tb("adab_rep", [128, 256])
            n1g_rep = tb("n1g_rep", [128, D])
            ld(csil[:], c_l[:, :], 'csil')
            ld(n1g_rep[:], n1g[0:1, :].partition_broadcast(128), 'n1g_rep')
            S.op('act', lambda: A.activation(out=csil[:], in_=csil[:], func=AF.Silu), reads=['csil'], writes=['csil'])
            S.op('dve', lambda: V.tensor_copy(out=csil_rep[:], in_=csil[:].unsqueeze(2).to_broadcast([128, 8, 128])),
                 reads=['csil'], writes=['csil_rep'])
            for j in range(8):
                st = stg[j % 2]; k_st = 'stg%d' % (j % 2)
                ld(st[:], adaw[:, j * 256:(j + 1) * 256].rearrange("(kc k) n -> k kc n", k=128), k_st)
                ld(adab_rep[:], adab[0:1, j * 256:(j + 1) * 256].partition_broadcast(128), 'adab_rep')
                for kc in range(8):
                    S.op('pe', lambda: PE.matmul(psU[:, 0:256], lhsT=csil_rep[:, kc, :], rhs=st[:, kc, :], start=(kc == 0), stop=(kc == 7)),
                         reads=['csil_rep', k_st], writes=['psU'], pe_chain=(kc > 0))
                S.op('dve', lambda: V.tensor_tensor(out=modrep[:, j * 256:(j + 1) * 256], in0=psU[:, 0:256], in1=adab_rep[:], op=ALU.add),
                     reads=['psU', 'adab_rep'], writes=['modrep'])
            sh1 = modrep[:, 0:1024]; sc1 = modrep[:, 1024:2048]
            S.op('dve', lambda: V.scalar_tensor_tensor(out=gk1[:], in0=sc1, scalar=1.0, in1=n1g_rep[:], op0=ALU.add, op1=ALU.mult),
                 reads=['modrep', 'n1g_rep'], writes=['gk1'])
            ld(stg[0][:], win_d[:, :].rearrange("(kc k) n -> k kc n", k=128), 'stg0')
            S.op('dve', lambda: V.tensor_copy(out=win[:], in_=stg[0][:]), reads=['stg0'], writes=['win'])

            def emit_sin(outap, th, ki, kf, tmp, keys, shift=0.0):
                kth, kout, kki, kkf, ktmp = keys
                if shift != 0.0:
                    S.op('dve', lambda: V.tensor_scalar(out=th, in0=th, scalar1=shift, scalar2=None, op0=ALU.add), reads=[kth], writes=[kth])
                S.op('dve', lambda: V.tensor_scalar(out=tmp, in0=th, scalar1=1.0 / (2 * PI), scalar2=None, op0=ALU.mult), reads=[kth], writes=[ktmp])
                S.op('dve', lambda: V.tensor_copy(out=ki, in_=tmp), reads=[ktmp], writes=[kki])
                S.op('dve', lambda: V.tensor_copy(out=kf, in_=ki), reads=[kki], writes=[kkf])
                S.op('dve', lambda: V.scalar_tensor_tensor(out=th, in0=kf, scalar=-2 * PI, in1=th, op0=ALU.mult, op1=ALU.add),
                     reads=[kkf, kth], writes=[kth])
                S.op('dve', lambda: V.tensor_scalar(out=tmp, in0=th, scalar1=PI, scalar2=-2 * PI, op0=ALU.is_gt, op1=ALU.mult), reads=[kth], writes=[ktmp])
                S.op('dve', lambda: V.tensor_tensor(out=th, in0=th, in1=tmp, op=ALU.add), reads=[kth, ktmp], writes=[kth])
                S.op('dve', lambda: V.tensor_scalar(out=tmp, in0=th, scalar1=-PI, scalar2=2 * PI, op0=ALU.is_lt, op1=ALU.mult), reads=[kth], writes=[ktmp])
                S.op('dve', lambda: V.tensor_tensor(out=th, in0=th, in1=tmp, op=ALU.add), reads=[kth, ktmp], writes=[kth])
                S.op('dve', lambda: V.tensor_scalar(out=th, in0=th, scalar1=3.1415925, scalar2=-3.1415925, op0=ALU.min, op1=ALU.max), reads=[kth], writes=[kth])
                S.op('act', lambda: A.activation(out=outap, in_=th, func=AF.Sin), reads=[kth], writes=[kout])

            are_c = tb("are_c", [128, 16]); aim_c = tb("aim_c", [128, 16]); dtc = tb("dtc", [128, 16])
            adr_c = tb("adr_c", [128, 16]); nadr_c = tb("nadr_c", [128, 16]); adi_c = tb("adi_c", [128, 16])
            mrow = tb("mrow", [128, 128]); swapm = tb("swapm", [128, 128]); sgn_c = tb("sgn_c", [128, 1]); sgn_r = tb("sgn_r", [128, 128])
            ld(are_c[:], are_c_d[:, :], 'are_c'); ld(aim_c[:], aim_c_d[:, :], 'aim_c'); ld(dtc[:], ldt_c_d[:, :], 'dtc')
            ld(mrow[:], mrow_d[:, :], 'mrow'); ld(swapm[:], swapm_d[:, :], 'swapm'); ld(sgn_c[:], sgnc_d[:, :], 'sgn_c'); ld(sgn_r[:], sgnr_d[:, :], 'sgn_r')
            S.op('act', lambda: A.activation(out=dtc[:], in_=dtc[:], func=AF.Exp), reads=['dtc'], writes=['dtc'])
            S.op('dve', lambda: V.tensor_tensor(out=adr_c[:], in0=are_c[:], in1=dtc[:], op=ALU.mult), reads=['are_c', 'dtc'], writes=['adr_c'])
            S.op('dve', lambda: V.tensor_scalar(out=nadr_c[:], in0=adr_c[:], scalar1=-1.0, scalar2=None, op0=ALU.mult), reads=['adr_c'], writes=['nadr_c'])
            S.op('dve', lambda: V.tensor_tensor(out=adi_c[:], in0=aim_c[:], in1=dtc[:], op=ALU.mult), reads=['aim_c', 'dtc'], writes=['adi_c'])
            T1 = tb("T1", [128, 2048]); T2 = tb("T2", [128, 2048]); T3 = tb("T3", [128, 2048]); T4 = tb("T4", [128, 2048])
            T5 = tb("T5", [128, 2048]); T6 = tb("T6", [128, 2048]); TI = tb("TI", [128, 2048], I32)
            T1v = T1[:].rearrange("p (g m) -> p g m", m=128); T2v = T2[:].rearrange("p (g m) -> p g m", m=128)
            T3v = T3[:].rearrange("p (g m) -> p g m", m=128); T4v = T4[:].rearrange("p (g m) -> p g m", m=128)
            for g in range(16):
                S.op('act', lambda: A.activation(out=T1v[:, g, :], in_=mrow[:], func=AF.Exp, scale=adr_c[:, g:g + 1]), reads=['mrow', 'adr_c'], writes=['T1'])
                S.op('act', lambda: A.activation(out=T2v[:, g, :], in_=mrow[:], func=AF.Exp, scale=nadr_c[:, g:g + 1]), reads=['mrow', 'nadr_c'], writes=['T2'])
                S.op('dve', lambda: V.tensor_scalar(out=T3v[:, g, :], in0=mrow[:], scalar1=adi_c[:, g:g + 1], scalar2=None, op0=ALU.mult),
                     reads=['mrow', 'adi_c'], writes=['T3'])
            S.op('dve', lambda: V.tensor_copy(out=T4[:], in_=T3[:]), reads=['T3'], writes=['T4'])
            emit_sin(T3[:], T3[:], TI[:], T5[:], T6[:], ('T3', 'T3', 'TI', 'T5', 'T6'))
            emit_sin(T4[:], T4[:], TI[:], T5[:], T6[:], ('T4', 'T4', 'TI', 'T5', 'T6'), shift=PI / 2)
            fl = lambda t: t[:].rearrange("p g m -> p (g m)")
            S.op('dve', lambda: V.tensor_tensor(out=fl(Apow_r), in0=T1[:], in1=T4[:], op=ALU.mult), reads=['T1', 'T4'], writes=['Apow_r'])
            S.op('dve', lambda: V.tensor_tensor(out=fl(Apow_i), in0=T1[:], in1=T3[:], op=ALU.mult), reads=['T1', 'T3'], writes=['Apow_i'])
            S.op('dve', lambda: V.tensor_tensor(out=fl(Ainv_r), in0=T2[:], in1=T4[:], op=ALU.mult), reads=['T2', 'T4'], writes=['Ainv_r'])
            S.op('dve', lambda: V.scalar_tensor_tensor(out=fl(Ainv_i), in0=T2[:], scalar=-1.0, in1=T3[:], op0=ALU.mult, op1=ALU.mult),
                 reads=['T2', 'T3'], writes=['Ainv_i'])
            e128 = tb("e128", [128, 16]); th_s = tb("th_s", [128, 16]); th_c = tb("th_c", [128, 16])
            ki16 = tb("ki16", [128, 16], I32); kf16 = tb("kf16", [128, 16]); tm16 = tb("tm16", [128, 16])
            r128r = tb("r128r", [128, 16]); r128i = tb("r128i", [128, 16])
            S.op('act', lambda: A.activation(out=e128[:], in_=adr_c[:], func=AF.Exp, scale=128.0), reads=['adr_c'], writes=['e128'])
            S.op('dve', lambda: V.tensor_scalar(out=th_s[:], in0=adi_c[:], scalar1=128.0, scalar2=None, op0=ALU.mult), reads=['adi_c'], writes=['th_s'])
            S.op('dve', lambda: V.tensor_copy(out=th_c[:], in_=th_s[:]), reads=['th_s'], writes=['th_c'])
            emit_sin(th_s[:], th_s[:], ki16[:], kf16[:], tm16[:], ('th_s', 'th_s', 'ki16', 'kf16', 'tm16'))
            emit_sin(th_c[:], th_c[:], ki16[:], kf16[:], tm16[:], ('th_c', 'th_c', 'ki16', 'kf16', 'tm16'), shift=PI / 2)
            S.op('dve', lambda: V.tensor_tensor(out=r128r[:], in0=e128[:], in1=th_c[:], op=ALU.mult), reads=['e128', 'th_c'], writes=['r128r'])
            S.op('dve', lambda: V.tensor_tensor(out=r128i[:], in0=e128[:], in1=th_s[:], op=ALU.mult), reads=['e128', 'th_s'], writes=['r128i'])
            S.op('dve', lambda: V.tensor_scalar(out=r128i[:], in0=r128i[:], scalar1=sgn_c[:, 0:1], scalar2=None, op0=ALU.mult),
                 reads=['r128i', 'sgn_c'], writes=['r128i'])
            for g in range(16):
                S.op('dve', lambda: V.tensor_scalar(out=Rot[:, g, :], in0=identf[:], scalar1=r128r[:, g:g + 1], scalar2=None, op0=ALU.mult),
                     reads=['identf', 'r128r'], writes=['Rot'])
                S.op('dve', lambda: V.scalar_tensor_tensor(out=Rot[:, g, :], in0=swapm[:], scalar=r128i[:, g:g + 1], in1=Rot[:, g, :],
                                                           op0=ALU.mult, op1=ALU.add), reads=['swapm', 'r128i', 'Rot'], writes=['Rot'])
            T7 = tb("T7", [128, 2048]); T8 = tb("T8", [128, 2048])
            ld(T1[:], are_r_d[:, :], 'T1'); ld(T2[:], aim_r_d[:, :], 'T2'); ld(T3[:], ldt_r_d[:, :], 'T3')
            S.op('act', lambda: A.activation(out=T3[:], in_=T3[:], func=AF.Exp), reads=['T3'], writes=['T3'])
            S.op('dve', lambda: V.tensor_tensor(out=T4[:], in0=T2[:], in1=T3[:], op=ALU.mult), reads=['T2', 'T3'], writes=['T4'])
            S.op('dve', lambda: V.tensor_tensor(out=T3[:], in0=T1[:], in1=T3[:], op=ALU.mult), reads=['T1', 'T3'], writes=['T3'])
            S.op('act', lambda: A.activation(out=T3[:], in_=T3[:], func=AF.Exp), reads=['T3'], writes=['T3'])
            S.op('dve', lambda: V.tensor_copy(out=T7[:], in_=T4[:]), reads=['T4'], writes=['T7'])
            emit_sin(T4[:], T4[:], TI[:], T5[:], T6[:], ('T4', 'T4', 'TI', 'T5', 'T6'))
            emit_sin(T7[:], T7[:], TI[:], T5[:], T6[:], ('T7', 'T7', 'TI', 'T5', 'T6'), shift=PI / 2)
            S.op('dve', lambda: V.tensor_tensor(out=T4[:], in0=T4[:], in1=T3[:], op=ALU.mult), reads=['T4', 'T3'], writes=['T4'])
            S.op('dve', lambda: V.tensor_tensor(out=T7[:], in0=T7[:], in1=T3[:], op=ALU.mult), reads=['T7', 'T3'], writes=['T7'])
            S.op('dve', lambda: V.tensor_scalar(out=T7[:], in0=T7[:], scalar1=-1.0, scalar2=None, op0=ALU.add), reads=['T7'], writes=['T7'])
            S.op('dve', lambda: V.tensor_tensor(out=T5[:], in0=T1[:], in1=T1[:], op=ALU.mult), reads=['T1'], writes=['T5'])
            S.op('dve', lambda: V.tensor_tensor(out=T6[:], in0=T2[:], in1=T2[:], op=ALU.mult), reads=['T2'], writes=['T6'])
            S.op('dve', lambda: V.tensor_tensor(out=T5[:], in0=T5[:], in1=T6[:], op=ALU.add), reads=['T5', 'T6'], writes=['T5'])
            S.op('dve', lambda: V.reciprocal(out=T5[:], in_=T5[:]), reads=['T5'], writes=['T5'])
            S.op('dve', lambda: V.tensor_tensor(out=T3[:], in0=T7[:], in1=T1[:], op=ALU.mult), reads=['T7', 'T1'], writes=['T3'])
            S.op('dve', lambda: V.tensor_tensor(out=T6[:], in0=T4[:], in1=T2[:], op=ALU.mult), reads=['T4', 'T2'], writes=['T6'])
            S.op('dve', lambda: V.tensor_tensor(out=T3[:], in0=T3[:], in1=T6[:], op=ALU.add), reads=['T3', 'T6'], writes=['T3'])
            S.op('dve', lambda: V.tensor_tensor(out=T3[:], in0=T3[:], in1=T5[:], op=ALU.mult), reads=['T3', 'T5'], writes=['T3'])
            S.op('dve', lambda: V.tensor_tensor(out=T8[:], in0=T4[:], in1=T1[:], op=ALU.mult), reads=['T4', 'T1'], writes=['T8'])
            S.op('dve', lambda: V.tensor_tensor(out=T6[:], in0=T7[:], in1=T2[:], op=ALU.mult), reads=['T7', 'T2'], writes=['T6'])
            S.op('dve', lambda: V.tensor_tensor(out=T8[:], in0=T8[:], in1=T6[:], op=ALU.subtract), reads=['T8', 'T6'], writes=['T8'])
            S.op('dve', lambda: V.tensor_tensor(out=T8[:], in0=T8[:], in1=T5[:], op=ALU.mult), reads=['T8', 'T5'], writes=['T8'])
            ld(T1[:], X1p_d[:, :], 'T1'); ld(T2[:], X2p_d[:, :], 'T2')
            S.op('dve', lambda: V.tensor_tensor(out=T2[:].rearrange("p (g f) -> p g f", f=128), in0=T2[:].rearrange("p (g f) -> p g f", f=128),
                                                in1=sgn_r[:].unsqueeze(1).to_broadcast([128, 16, 128]), op=ALU.mult), reads=['T2', 'sgn_r'], writes=['T2'])
            S.op('dve', lambda: V.tensor_tensor(out=T5[:], in0=T3[:], in1=T1[:], op=ALU.mult), reads=['T3', 'T1'], writes=['T5'])
            S.op('dve', lambda: V.tensor_tensor(out=T6[:], in0=T8[:], in1=T2[:], op=ALU.mult), reads=['T8', 'T2'], writes=['T6'])
            S.op('dve', lambda: V.tensor_tensor(out=fl(Bpad), in0=T5[:], in1=T6[:], op=ALU.add), reads=['T5', 'T6'], writes=['Bpad'])
            S.op('dve', lambda: V.tensor_tensor(out=T5[:], in0=T3[:], in1=T2[:], op=ALU.mult), reads=['T3', 'T2'], writes=['T5'])
            S.op('dve', lambda: V.tensor_tensor(out=T6[:], in0=T8[:], in1=T1[:], op=ALU.mult), reads=['T8', 'T1'], writes=['T6'])
            S.op('dve', lambda: V.tensor_tensor(out=fl(BpadS), in0=T5[:], in1=T6[:], op=ALU.subtract), reads=['T5', 'T6'], writes=['BpadS'])
            ld(T1[:], CcP_d[:, :], 'T1'); ld(T2[:], CcSP_d[:, :], 'T2')
            S.op('dve', lambda: V.tensor_scalar(out=fl(Cc), in0=T1[:], scalar1=sgn_c[:, 0:1], scalar2=None, op0=ALU.mult), reads=['T1', 'sgn_c'], writes=['Cc'])
            S.op('dve', lambda: V.tensor_scalar(out=fl(CcS), in0=T2[:], scalar1=-1.0, scalar2=None, op0=ALU.mult), reads=['T2'], writes=['CcS'])
            S.barrier()
        xt = [sb("xt%d" % i, [128, D]) for i in range(2)]
        junk = sb("junk", [128, D], BF16)
        ss = sb("ss", [128, 1]); rs = sb("rs", [128, 1])
        hm = sb("hm", [128, D]); hmb = sb("hmb", [128, D], BF16)
        hmT = sb("hmT", [128, 8, 128], BF16)
        uT = sb("uT", [128, 2, 128]); uTb = sb("uTb", [128, 2, 128], BF16)
        Vt = [sb("Vt%d" % i, [128, 4, 128]) for i in range(2)]
        Wt = [sb("Wt%d" % i, [128, 4, 128]) for i in range(2)]
        Zt = [sb("Zt%d" % i, [128, 4, 128]) for i in range(4)]
        Hr = [sb("Hr%d" % i, [128, 4, 128], BF16) for i in range(2)]
        Hi = [sb("Hi%d" % i, [128, 4, 128], BF16) for i in range(2)]
        yt = sb("yt", [128, 2, 128]); ga = sb("ga", [128, 2, 128]); yo = sb("yo", [128, 2, 128])
        sh1 = modrep[:, 0:1024]

        for c in range(NCH):
            xb = xt[c % 2]; kx = 'xt%d' % (c % 2)
            r0 = c * 128
            ld(xb[:], x[r0:r0 + 128, :], kx)
            S.op('act', lambda: A.activation(out=junk[:], in_=xb[:], func=AF.Square, accum_out=ss[:]), reads=[kx], writes=['junk', 'ss'])
            S.op('act', lambda: A.activation(out=rs[:], in_=ss[:], func=AF.Sqrt, bias=epsT[:], scale=1.0 / D), reads=['ss', 'epsT'], writes=['rs'])
            S.op('dve', lambda: V.reciprocal(out=rs[:], in_=rs[:]), reads=['rs'], writes=['rs'])
            S.op('dve', lambda: V.scalar_tensor_tensor(out=hm[:], in0=xb[:], scalar=rs[:], in1=gk1[:], op0=ALU.mult, op1=ALU.mult),
                 reads=[kx, 'rs', 'gk1'], writes=['hm'])
            S.op('dve', lambda: V.tensor_tensor(out=hmb[:], in0=hm[:], in1=sh1, op=ALU.add), reads=['hm', 'modrep'], writes=['hmb'])
            for kc in range(8):
                S.op('pe', lambda: PE.transpose(out=psT[:, kc * 128:(kc + 1) * 128], in_=hmb[:, kc * 128:(kc + 1) * 128], identity=identb[:]),
                     reads=['hmb', 'identb'], writes=['psT'], pe_chain=(kc > 0))
            S.op('act', lambda: A.copy(out=hmT[:].rearrange("p a b -> p (a b)"), in_=psT[:]), reads=['psT'], writes=['hmT'])
            for mc in range(2):
                for kc in range(8):
                    S.op('pe', lambda: PE.matmul(psU[:, mc * 128:(mc + 1) * 128], lhsT=win[:, kc, mc * 128:(mc + 1) * 128], rhs=hmT[:, kc, :],
                                                 start=(kc == 0), stop=(kc == 7)), reads=['win', 'hmT'], writes=['psU'],
                         pe_chain=not (mc == 0 and kc == 0))
            S.op('act', lambda: A.copy(out=uT[:].rearrange("p a b -> p (a b)"), in_=psU[:, 0:256]), reads=['psU'], writes=['uT'])
            S.op('act', lambda: A.copy(out=uTb[:].rearrange("p a b -> p (a b)"), in_=psU[:, 0:256]), reads=['psU'], writes=['uTb'])
            def s5_bu(bt):
                i2 = bt % 2
                kbu = 'psBU%d' % i2; kbs = 'psBS%d' % i2
                kV = 'Vt%d' % i2; kW = 'Wt%d' % i2; kZ = 'Zt%d' % bt; kHr = 'Hr%d' % i2; kHi = 'Hi%d' % i2
                gc = bt // 2
                for gg in range(4):
                    g = bt * 4 + gg
                    S.op('pe', lambda: PE.matmul(psBU[i2][:, gg * 128:(gg + 1) * 128], lhsT=Bpad[:, g, :], rhs=uTb[:, gc, :], start=True, stop=True),
                         reads=['Bpad', 'uTb'], writes=[kbu], pe_chain=(gg > 0))
                for gg in range(4):
                    g = bt * 4 + gg
                    S.op('pe', lambda: PE.matmul(psBS[i2][:, gg * 128:(gg + 1) * 128], lhsT=BpadS[:, g, :], rhs=uTb[:, gc, :], start=True, stop=True),
                         reads=['BpadS', 'uTb'], writes=[kbs], pe_chain=(gg > 0))

            def s5_dve(bt):
                i2 = bt % 2
                kbu = 'psBU%d' % i2; kbs = 'psBS%d' % i2
                kV = 'Vt%d' % i2; kW = 'Wt%d' % i2; kZ = 'Zt%d' % bt; kHr = 'Hr%d' % i2; kHi = 'Hi%d' % i2
                gc = bt // 2
                Vf = Vt[i2][:].rearrange("p a b -> p (a b)"); Wf = Wt[i2][:].rearrange("p a b -> p (a b)")
                Zf = Zt[bt][:].rearrange("p a b -> p (a b)")
                gs = slice(bt * 4, bt * 4 + 4)
                S.op('dve', lambda: V.tensor_tensor(out=Vf, in0=psBU[i2][:], in1=Ainv_r[:, gs, :].rearrange("p a b -> p (a b)"), op=ALU.mult),
                     reads=[kbu, 'Ainv_r'], writes=[kV])
                S.op('dve', lambda: V.tensor_tensor(out=Wf, in0=psBS[i2][:], in1=Ainv_i[:, gs, :].rearrange("p a b -> p (a b)"), op=ALU.mult),
                     reads=[kbs, 'Ainv_i'], writes=[kW])
                S.op('dve', lambda: V.tensor_tensor(out=Vf, in0=Vf, in1=Wf, op=ALU.add), reads=[kV, kW], writes=[kV])
                if c > 0:
                    S.op('dve', lambda: V.tensor_tensor(out=Vt[i2][:, :, 0], in0=Vt[i2][:, :, 0], in1=psI[:, bt * 4:bt * 4 + 4], op=ALU.add),
                         reads=[kV, 'psI'], writes=[kV])
                S.op('dve', lambda: V.tensor_tensor_scan(out=Zf, data0=mask01[:], data1=Vf, initial=0.0, op0=ALU.mult, op1=ALU.add),
                     reads=['mask01', kV], writes=[kZ])
                if c < NCH - 1:
                    for gg in range(4):
                        g = bt * 4 + gg
                        S.op('pe', lambda: PE.matmul(psI[:, g:g + 1], lhsT=Rot[:, g, :], rhs=Zt[bt][:, gg, 127:128], start=True, stop=True),
                             reads=['Rot', kZ], writes=['psI'], pe_chain=(gg > 0))
                S.op('dve', lambda: V.tensor_tensor(out=Hr[i2][:].rearrange("p a b -> p (a b)"), in0=Zf,
                                                    in1=Apow_r[:, gs, :].rearrange("p a b -> p (a b)"), op=ALU.mult), reads=[kZ, 'Apow_r'], writes=[kHr])
                S.op('dve', lambda: V.tensor_tensor(out=Hi[i2][:].rearrange("p a b -> p (a b)"), in0=Zf,
                                                    in1=Apow_i[:, gs, :].rearrange("p a b -> p (a b)"), op=ALU.mult), reads=[kZ, 'Apow_i'], writes=[kHi])

            def s5_cm(bt):
                i2 = bt % 2
                kbu = 'psBU%d' % i2; kbs = 'psBS%d' % i2
                kV = 'Vt%d' % i2; kW = 'Wt%d' % i2; kZ = 'Zt%d' % bt; kHr = 'Hr%d' % i2; kHi = 'Hi%d' % i2
                gc = bt // 2
                for gg in range(4):
                    g = bt * 4 + gg
                    first = (g % 8 == 0)
                    S.op('pe', lambda: PE.matmul(psY[:, gc * 128:(gc + 1) * 128], lhsT=Cc[:, g, :], rhs=Hr[i2][:, gg, :], start=first, stop=False),
                         reads=['Cc', kHr], writes=['psY'], pe_chain=not first)
                    S.op('pe', lambda: PE.matmul(psY[:, gc * 128:(gc + 1) * 128], lhsT=CcS[:, g, :], rhs=Hi[i2][:, gg, :], start=False, stop=(g % 8 == 7)),
                         reads=['CcS', kHi], writes=['psY'], pe_chain=True)

            s5_bu(0)
            for bt in range(4):
                if bt + 1 < 4:
                    s5_bu(bt + 1)
                s5_dve(bt)
                s5_cm(bt)
            ytf = yt[:].rearrange("p a b -> p (a b)"); gaf = ga[:].rearrange("p a b -> p (a b)"); yof = yo[:].rearrange("p a b -> p (a b)")
            for gc in range(2):
                S.op('dve', lambda: V.scalar_tensor_tensor(out=yt[:, gc, :], in0=uT[:, gc, :], scalar=dcol[:, gc:gc + 1],
                                                           in1=psY[:, gc * 128:(gc + 1) * 128], op0=ALU.mult, op1=ALU.add),
                     reads=['uT', 'dcol', 'psY'], writes=['yt'])
            S.op('dve', lambda: V.tensor_tensor(out=gaf, in0=ytf, in1=ytf, op=ALU.mult), reads=['yt'], writes=['ga'])
            S.op('dve', lambda: V.tensor_scalar(out=gaf, in0=gaf, scalar1=0.044715, scalar2=1.0, op0=ALU.mult, op1=ALU.add), reads=['ga'], writes=['ga'])
            S.op('dve', lambda: V.tensor_tensor(out=gaf, in0=gaf, in1=ytf, op=ALU.mult), reads=['ga', 'yt'], writes=['ga'])
            S.op('act', lambda: A.activation(out=gaf, in_=gaf, func=AF.Sigmoid, scale=1.5957691216), reads=['ga'], writes=['ga'])
            S.op('dve', lambda: V.tensor_tensor(out=yof, in0=gaf, in1=ytf, op=ALU.mult), reads=['ga', 'yt'], writes=['yo'])
            S.dma('sp', lambda: nc.sync.dma_start(out=out[:, r0:r0 + 128].rearrange("(gc k) n -> k gc n", k=128), in_=yo[:]), reads=['yo'], writes=['out'])
        S.finish(['out'])
        print("s5 program: instr", S.n_instr, "waits", S.n_wait)
    return nc


def s5_consts():
    f = np.arange(128)
    swap = np.zeros((128, 128), np.float32); swap[f, (f + 64) % 128] = 1.0
    sgn_c = np.where(f < 64, 1.0, -1.0).astype(np.float32)[:, None]
    sgn_r = np.broadcast_to(np.where(f < 64, -1.0, 1.0).astype(np.float32)[None, :], (128, 128)).copy()
    mrow = np.broadcast_to(np.arange(128, dtype=np.float32)[None, :], (128, 128)).copy()
    m01 = np.ones((128, 4, 128), np.float32); m01[:, :, 0] = 0.0
    return {"identf": np.eye(128, dtype=np.float32), "swapm": swap, "sgn_c": sgn_c, "sgn_r": sgn_r, "mrow": mrow,
            "mask01": m01.reshape(128, 512)}


def s5_layouts(a_re, a_im, log_dt, b_re, b_im, c_re, c_im, d, cq):
    G0 = 16 * cq
    f = np.arange(128)
    p = f % 64
    are_c = np.ascontiguousarray(a_re[G0:G0 + 16][:, p].T)
    aim_c = np.ascontiguousarray(a_im[G0:G0 + 16][:, p].T)
    ldt_c = np.ascontiguousarray(np.broadcast_to(log_dt[G0:G0 + 16][None, :], (128, 16)))
    are_r = np.ascontiguousarray(np.broadcast_to(a_re[G0:G0 + 16][:, p].reshape(1, 16 * 128), (128, 2048)))
    aim_r = np.ascontiguousarray(np.broadcast_to(a_im[G0:G0 + 16][:, p].reshape(1, 16 * 128), (128, 2048)))
    ldt_r = np.ascontiguousarray(np.broadcast_to(np.repeat(log_dt[G0:G0 + 16], 128).reshape(1, 2048), (128, 2048)))
    X1p = np.zeros((128, 16, 128), np.float32); X2p = np.zeros((128, 16, 128), np.float32)
    CcP = np.zeros((128, 16, 128), np.float32); CcSP = np.zeros((128, 16, 128), np.float32)
    for g in range(16):
        r0 = (g % 8) * 16
        br = b_re[G0 + g]; bi = b_im[G0 + g]
        X1p[r0:r0 + 16, g, 0:64] = br.T; X1p[r0:r0 + 16, g, 64:128] = bi.T
        X2p[r0:r0 + 16, g, 0:64] = bi.T; X2p[r0:r0 + 16, g, 64:128] = br.T
        cr = c_re[G0 + g]; ci = c_im[G0 + g]
        CcP[0:64, g, r0:r0 + 16] = cr.T; CcP[64:128, g, r0:r0 + 16] = ci.T
        CcSP[0:64, g, r0:r0 + 16] = ci.T; CcSP[64:128, g, r0:r0 + 16] = cr.T
    dcol = np.ascontiguousarray(d[cq * 256:(cq + 1) * 256].reshape(2, 128).T)
    return {"are_c": are_c, "aim_c": aim_c, "ldt_c": ldt_c, "are_r": are_r, "aim_r": aim_r, "ldt_r": ldt_r,
            "X1p": X1p.reshape(128, 2048), "X2p": X2p.reshape(128, 2048), "CcP": CcP.reshape(128, 2048),
            "CcSP": CcSP.reshape(128, 2048), "dcol": dcol}

import math

RMS_EPS = 1e-6


def build_nsa(NS, KXK='psXk'):
    nc = bass.Bass("TRN2", target_bir_lowering=False)
    T = NS * 512
    NT = NS * 4
    D = 1024
    NM = 1024 if T >= 16384 else (T // 16 + 32)
    NMT = (NM + 127) // 128
    din = lambda n, s, d=F32: nc.dram_tensor(n, s, d, kind="ExternalInput").ap()
    x = din("x", [T, D])
    c_l = din("c_l", [128, 8])
    adaw = din("adaw", [D, 2048]); adab = din("adab", [1, 2048]); n1g = din("n1g", [1, D])
    wpf_d = din("wpf", [D, 512]); wpt_d = din("wpt", [D, 524])
    wk1_d = din("wk1", [2048, 256]); wv1_d = din("wv1", [2048, 256])
    wk2_d = din("wk2", [256, 64]); wv2_d = din("wv2", [256, 64])
    pekT_d = din("pekT", [64, 32]); pevT_d = din("pevT", [64, 32])
    cos_d = din("cos2", [64, T]); sin_d = din("sin2", [64, T])
    cosc_d = din("cosc", [64, NM]); sinc_d = din("sinc", [64, NM])
    prot_d = din("prot", [64, 64])
    identf_d = din("identf", [128, 128])
    triT_d = din("triT", [128, 128]); triTs_d = din("triTs", [128, 128])
    cmg_d = din("cmaskG", [128, 16]); cm0_d = din("cmask0", [128, 8])
    hilo_d = din("hilo", [128, 3])
    out = nc.dram_tensor("out", [T, 256], F32, kind="ExternalOutput").ap()

    es = ExitStack()
    with es:
        es.enter_context(nc.allow_low_precision("bf16 matmul operands"))
        es.enter_context(nc.allow_non_contiguous_dma("small layout loads"))
        S = Sched(nc, es)
        sb = lambda n, s, d=F32: es.enter_context(nc.sbuf_tensor("s_" + n, s, d))
        ps = lambda n, s, d=F32: es.enter_context(nc.psum_tensor("p_" + n, s, d))
        V, A, P, PE = nc.vector, nc.scalar, nc.gpsimd, nc.tensor

        def ld(dst, src, key):
            S.dma('sp', lambda: nc.sync.dma_start(out=dst, in_=src), writes=[key])

        identf = sb("identf", [128, 128]); identb = sb("identb", [128, 128], BF16)
        triT = sb("triT", [128, 128], BF16); triTs = sb("triTs", [128, 128], BF16)
        cmaskG = sb("cmaskG", [128, 16]); cmask0 = sb("cmask0", [128, 8]); hilo = sb("hilo", [128, 3])
        epsT = sb("epsT", [128, 1]); ones1 = sb("ones1", [1, 128]); kmx = sb("kmx", [1, 1]); kmax2 = sb("kmax2", [128, 1])
        prot = sb("prot", [64, 64])
        modrep = sb("modrep", [128, 2048]); gk1 = sb("gk1", [128, D])
        wpf = sb("wpf", [128, 8, 512], BF16); wpt = sb("wpt", [128, 8, 524], BF16)
        wk1 = sb("wk1", [64, 32, 256], BF16); wv1 = sb("wv1", [64, 32, 256], BF16)
        wk2 = sb("wk2", [128, 2, 64], BF16); wv2 = sb("wv2", [128, 2, 64], BF16)
        biask = sb("biask", [128, 2]); biasv = sb("biasv", [128, 2])
        ksT = sb("ksT", [65, T], BF16); kwT = sb("kwT", [65, 1024], BF16)
        vsA = sb("vsA", [128, NT, 65], BF16); vwA = sb("vwA", [128, 8, 65], BF16)
        kcmpT = sb("kcmpT", [64, NMT * 128], BF16); vcmp = sb("vcmp", [128, NMT, 64], BF16)
        kcbuf = sb("kcbuf", [64, 528], BF16); vcbuf = sb("vcbuf", [64, 528], BF16)

        psS = [ps("psS%d" % i, [128, 512]) for i in range(2)]
        psOs = ps("psOs", [128, 512]); psOw = ps("psOw", [128, 512])
        psC = ps("psC", [128, 1024])
        psT = ps("psT", [128, 1024], BF16)
        psX = ps("psX", [128, 512])

        ld(identf[:], identf_d[:, :], 'identf')
        ld(cmaskG[:], cmg_d[:, :], 'cmaskG'); ld(cmask0[:], cm0_d[:, :], 'cmask0'); ld(hilo[:], hilo_d[:, :], 'hilo')
        ld(prot[:], prot_d[:, :], 'prot')
        S.op('dve', lambda: V.tensor_copy(out=identb[:], in_=identf[:]), reads=['identf'], writes=['identb'])
        S.op('dve', lambda: V.memset(epsT[:], RMS_EPS), writes=['epsT'])
        S.op('dve', lambda: V.memset(ones1[:], 1.0), writes=['ones1'])
        S.op('dve', lambda: V.memset(kmx[:], 0.0), writes=['kmx'])
        for c0 in range(0, T, 2048):
            S.op('dve', lambda: V.memset(ksT[:, c0:min(T, c0 + 2048)], 1.0), writes=['ksT'])
        S.op('pool', lambda: P.memset(kwT[:], 1.0), writes=['kwT'])
        S.op('dve', lambda: V.memset(vsA[:], 1.0), writes=['vsA'])
        S.op('pool', lambda: P.memset(vwA[:], 1.0), writes=['vwA'])
        S.op('dve', lambda: V.memset(kcmpT[:], 0.0), writes=['kcmpT'])
        S.op('dve', lambda: V.memset(vcmp[:], 0.0), writes=['vcmp'])
        S.op('dve', lambda: V.memset(kcbuf[:], 0.0), writes=['kcbuf'])
        S.op('dve', lambda: V.memset(vcbuf[:], 0.0), writes=['vcbuf'])

        es2 = ExitStack()
        with es2:
            tb = lambda n, s, d=F32: es2.enter_context(nc.sbuf_tensor("t_" + n, s, d))
            csil = tb("csil", [128, 8]); csil_rep = tb("csil_rep", [128, 8, 128])
            stg = [tb("stg%d" % i, [128, 8, 256]) for i in range(2)]
            adab_rep = tb("adab_rep", [128, 256]); n1g_rep = tb("n1g_rep", [128, D])
            tmpf = tb("tmpf", [128, 128])
            ld(tmpf[:], triT_d[:, :], 'tmpf')
            S.op('dve', lambda: V.tensor_copy(out=triT[:], in_=tmpf[:]), reads=['tmpf'], writes=['triT'])
            ld(tmpf[:], triTs_d[:, :], 'tmpf')
            S.op('dve', lambda: V.tensor_copy(out=triTs[:], in_=tmpf[:]), reads=['tmpf'], writes=['triTs'])
            ld(csil[:], c_l[:, :], 'csil')
            ld(n1g_rep[:], n1g[0:1, :].partition_broadcast(128), 'n1g_rep')
            S.op('act', lambda: A.activation(out=csil[:], in_=csil[:], func=AF.Silu), reads=['csil'], writes=['csil'])
            S.op('dve', lambda: V.tensor_copy(out=csil_rep[:], in_=csil[:].unsqueeze(2).to_broadcast([128, 8, 128])),
                 reads=['csil'], writes=['csil_rep'])
            for j in range(8):
                st = stg[j % 2]; k_st = 'stg%d' % (j % 2)
                ld(st[:], adaw[:, j * 256:(j + 1) * 256].rearrange("(kc k) n -> k kc n", k=128), k_st)
                ld(adab_rep[:], adab[0:1, j * 256:(j + 1) * 256].partition_broadcast(128), 'adab_rep')
                for kc in range(8):
                    S.op('pe', lambda: PE.matmul(psX[:, 0:256], lhsT=csil_rep[:, kc, :], rhs=st[:, kc, :], start=(kc == 0), stop=(kc == 7)),
                         reads=['csil_rep', k_st], writes=['psX'], pe_chain=(kc > 0))
                S.op('dve', lambda: V.tensor_tensor(out=modrep[:, j * 256:(j + 1) * 256], in0=psX[:, 0:256], in1=adab_rep[:], op=ALU.add),
                     reads=['psX', 'adab_rep'], writes=['modrep'])
            sc1 = modrep[:, 1024:2048]
            S.op('dve', lambda: V.scalar_tensor_tensor(out=gk1[:], in0=sc1, scalar=1.0, in1=n1g_rep[:], op0=ALU.add, op1=ALU.mult),
                 reads=['modrep', 'n1g_rep'], writes=['gk1'])
            ci = [0]

            def load_cast(dst3, src2d, ncols, key):
                c0 = 0
                while c0 < ncols:
                    w = min(256, ncols - c0)
                    i = ci[0] % 2; ci[0] += 1
                    st = stg[i]
                    ld(st[:, :, 0:w], src2d[:, c0:c0 + w].rearrange("(kc k) n -> k kc n", k=128), 'stg%d' % i)
                    eng = 'dve' if i == 0 else 'act'
                    if i == 0:
                        S.op('dve', lambda: V.tensor_copy(out=dst3[:, :, c0:c0 + w], in_=st[:, :, 0:w]), reads=['stg%d' % i], writes=[key])
                    else:
                        S.op('act', lambda: A.copy(out=dst3[:, :, c0:c0 + w], in_=st[:, :, 0:w]), reads=['stg%d' % i], writes=[key])
                    c0 += w
            load_cast(wpf, wpf_d, 512, 'wpf')
            load_cast(wpt, wpt_d, 524, 'wpt')
            for (wdst, wsrc, key) in ((wk1, wk1_d, 'wk1'), (wv1, wv1_d, 'wv1')):
                for p0 in range(0, 32, 8):
                    i = ci[0] % 2; ci[0] += 1
                    st = stg[i]
                    ld(st[0:64, :, :], wsrc[p0 * 64:(p0 + 8) * 64, :].rearrange("(pos d) h -> d pos h", d=64), 'stg%d' % i)
                    S.op('dve', lambda: V.tensor_copy(out=wdst[:, p0:p0 + 8, :], in_=st[0:64, :, :]), reads=['stg%d' % i], writes=[key])
            for (wdst, wsrc, key) in ((wk2, wk2_d, 'wk2'), (wv2, wv2_d, 'wv2')):
                i = ci[0] % 2; ci[0] += 1
                st = stg[i]
                ld(st[:, 0:2, 0:64], wsrc[:, :].rearrange("(hc k) d -> k hc d", k=128), 'stg%d' % i)
                S.op('dve', lambda: V.tensor_copy(out=wdst[:], in_=st[:, 0:2, 0:64]), reads=['stg%d' % i], writes=[key])
            peb = tb("peb", [64, 32], BF16)
            for (pe_d, w1, bias, key) in ((pekT_d, wk1, biask, 'biask'), (pevT_d, wv1, biasv, 'biasv')):
                ld(tmpf[0:64, 0:32], pe_d[:, :], 'tmpf')
                S.op('dve', lambda: V.tensor_copy(out=peb[:], in_=tmpf[0:64, 0:32]), reads=['tmpf'], writes=['peb'])
                for hc in range(2):
                    for pos in range(32):
                        S.op('pe', lambda: PE.matmul(psX[:, hc:hc + 1], lhsT=w1[:, pos, hc * 128:(hc + 1) * 128], rhs=peb[:, pos:pos + 1],
                                                     start=(pos == 0), stop=(pos == 31)), reads=['wk1', 'wv1', 'peb'], writes=['psX'],
                             pe_chain=(pos > 0))
                S.op('dve', lambda: V.tensor_copy(out=bias[:], in_=psX[:, 0:2]), reads=['psX'], writes=[key])
            S.barrier()

        xt = [sb("xt%d" % i, [128, D]) for i in range(2)]
        junk = sb("junk", [128, D], BF16)
        ss = sb("ss", [128, 1]); rs = sb("rs", [128, 1])
        hm = sb("hm", [128, D]); hmb = sb("hmb", [128, D], BF16)
        hmT4 = sb("hmT4", [128, 8, 512], BF16)
        qtm = sb("qtm", [128, 256]); qsq = sb("qsq", [128, 4, 4])
        ksq = sb("ksq", [128, 2]); km1 = sb("km1", [128, 1]); red = sb("red", [1, 1])
        gates = sb("gates", [128, 4, 12])
        cs2 = [sb("cos%d" % i, [64, 512]) for i in range(2)]
        sn2 = [sb("sin%d" % i, [64, 512]) for i in range(2)]
        cc2 = [sb("cc%d" % i, [64, 32]) for i in range(2)]
        sc2_ = [sb("sc%d" % i, [64, 32]) for i in range(2)]
        xq = sb("xq", [64, 512]); t1 = sb("t1", [64, 512]); t2 = sb("t2", [64, 512])
        qTa = sb("qTa", [65, 4, 512], BF16)
        hid = sb("hid", [128, 2, 32]); hga = sb("hga", [128, 2, 32]); ghk = sb("ghk", [128, 2, 32], BF16); ghv = sb("ghv", [128, 2, 32], BF16); ghv2 = sb("ghv2", [128, 2, 64], BF16)
        kcx = sb("kcx", [64, 32]); kt1 = sb("kt1", [64, 32]); kt2 = sb("kt2", [64, 32])
        cq = sb("cq", [128, 4]); negc = sb("negc", [128, 4], BF16)
        eT = [sb("eT%d" % i, [128, 512], BF16) for i in range(4)]
        pT = [sb("pT%d" % i, [128, 512], BF16) for i in range(4)]
        mfull = sb("mfull", [128, 512], BF16); maskT4 = [sb("maskT4_%d" % i, [128, 4, 128], BF16) for i in range(2)]
        NBK = 4
        LA = 3
        SBK = [(psS[0][:], 'psS0'), (psS[1][:], 'psS1'), (psC[:, 0:512], 'psCa'), (psC[:, 512:1024], 'psCb')]
        pc = sb("pc", [128, 1024]); pcb = sb("pcb", [128, 1024], BF16); pcT = sb("pcT", [128, 8, 128], BF16)
        pgrp = sb("pgrp", [128, 1032])
        mx = sb("mx", [128, 1]); mx2 = sb("mx2", [128, 2]); sm = sb("sm", [128, 4]); rinv = sb("rinv", [128, 4])
        imp = sb("imp", [128, 256]); impw = sb("impw", [128, 256]); impk = sb("impk", [128, 256]); sel = sb("sel", [128, 256])
        m8a = sb("m8a", [128, 8]); m8b = sb("m8b", [128, 8]); tau = sb("tau", [128, 1])
        oTs = sb("oTs", [65, 512]); oTw = sb("oTw", [65, 512])
        fac = sb("fac", [128, 3, 4]); ot = sb("ot", [128, 4, 64])
        sh1 = modrep[:, 0:1024]
        S.op('dve', lambda: V.memset(impw[:], -1.0), writes=['impw'])
        S.op('dve', lambda: V.memset(pgrp[:], 0.0), writes=['pgrp'])
        S.op('dve', lambda: V.memset(sel[:], 0.0), writes=['sel'])
        S.op('dve', lambda: V.memset(ghv2[:], 0.0), writes=['ghv2'])

        def gelu_tanh(dst, src, tmp, kd, ks_, kt):
            S.op('dve', lambda: V.tensor_tensor(out=tmp, in0=src, in1=src, op=ALU.mult), reads=[ks_], writes=[kt])
            S.op('dve', lambda: V.tensor_scalar(out=tmp, in0=tmp, scalar1=0.044715, scalar2=1.0, op0=ALU.mult, op1=ALU.add), reads=[kt], writes=[kt])
            S.op('dve', lambda: V.tensor_tensor(out=tmp, in0=tmp, in1=src, op=ALU.mult), reads=[kt, ks_], writes=[kt])
            S.op('act', lambda: A.activation(out=tmp, in_=tmp, func=AF.Sigmoid, scale=1.5957691216), reads=[kt], writes=[kt])
            S.op('dve', lambda: V.tensor_tensor(out=dst, in0=tmp, in1=src, op=ALU.mult), reads=[kt, ks_], writes=[kd])

        def rope(dst, src_ps, kps, cosap, sinap, kcos, scale, kdst):
            n = src_ps.shape[-1]
            S.op('act', lambda: A.copy(out=xq[:, 0:n], in_=src_ps), reads=['psCa', 'psCb'], writes=['xq'])
            S.op('pe', lambda: PE.matmul(psS[1][0:64, 0:n], lhsT=prot[:], rhs=xq[:, 0:n], start=True, stop=True),
                 reads=['prot', 'xq'], writes=['psS1'])
            S.op('dve', lambda: V.scalar_tensor_tensor(out=t1[:, 0:n], in0=xq[:, 0:n], scalar=scale, in1=cosap, op0=ALU.mult, op1=ALU.mult),
                 reads=['xq', kcos], writes=['t1'])
            S.op('dve', lambda: V.scalar_tensor_tensor(out=t2[:, 0:n], in0=psS[1][0:64, 0:n], scalar=scale, in1=sinap, op0=ALU.mult, op1=ALU.mult),
                 reads=['psS1', kcos], writes=['t2'])
            a1 = t1[:, 0:n]; a2 = t2[:, 0:n]
            if len(dst.shape) == 3:
                a1 = a1.rearrange("p (a b) -> p a b", b=dst.shape[2]); a2 = a2.rearrange("p (a b) -> p a b", b=dst.shape[2])
            S.op('dve', lambda: V.tensor_tensor(out=dst, in0=a1, in1=a2, op=ALU.add), reads=['t1', 't2'], writes=[kdst])

        for s in range(NS):
            cb = cs2[s % 2]; snb = sn2[s % 2]; kcos = 'cos%d' % (s % 2)
            ld(cb[:], cos_d[:, s * 512:(s + 1) * 512], kcos)
            ld(snb[:], sin_d[:, s * 512:(s + 1) * 512], kcos)
            ccb = cc2[s % 2]; scb = sc2_[s % 2]; kcc = 'cc%d' % (s % 2)
            if 32 * s + 32 <= NM:
                ld(ccb[:], cosc_d[:, 32 * s:32 * s + 32], kcc)
                ld(scb[:], sinc_d[:, 32 * s:32 * s + 32], kcc)
            for i in range(4):
                ti = s * 4 + i
                xb = xt[ti % 2]; kx = 'xt%d' % (ti % 2)
                r0 = ti * 128
                ld(xb[:], x[r0:r0 + 128, :], kx)
                S.op('act', lambda: A.activation(out=junk[:], in_=xb[:], func=AF.Square, accum_out=ss[:]), reads=[kx], writes=['junk', 'ss'])
                S.op('act', lambda: A.activation(out=rs[:], in_=ss[:], func=AF.Sqrt, bias=epsT[:], scale=1.0 / D), reads=['ss', 'epsT'], writes=['rs'])
                S.op('dve', lambda: V.reciprocal(out=rs[:], in_=rs[:]), reads=['rs'], writes=['rs'])
                S.op('dve', lambda: V.scalar_tensor_tensor(out=hm[:], in0=xb[:], scalar=rs[:], in1=gk1[:], op0=ALU.mult, op1=ALU.mult),
                     reads=[kx, 'rs', 'gk1'], writes=['hm'])
                S.op('dve', lambda: V.tensor_tensor(out=hmb[:], in0=hm[:], in1=sh1, op=ALU.add), reads=['hm', 'modrep'], writes=['hmb'])
                for kc in range(8):
                    S.op('pe', lambda: PE.transpose(out=psT[:, kc * 128:(kc + 1) * 128], in_=hmb[:, kc * 128:(kc + 1) * 128], identity=identb[:]),
                         reads=['hmb', 'identb'], writes=['psT'], pe_chain=(kc > 0))
                S.op('act', lambda: A.copy(out=hmT4[:, :, i * 128:(i + 1) * 128], in_=psT[:].rearrange("p (a b) -> p a b", b=128)),
                     reads=['psT'], writes=['hmT4'])
                for kc in range(8):
                    S.op('pe', lambda: PE.matmul(psS[0][:, 0:384], lhsT=hmT4[:, kc, i * 128:(i + 1) * 128], rhs=wpt[:, kc, 0:384],
                                                 start=(kc == 0), stop=(kc == 7)), reads=['hmT4', 'wpt'], writes=['psS0'], pe_chain=(kc > 0))
                for kc in range(8):
                    S.op('pe', lambda: PE.matmul(psX[:, 0:140], lhsT=hmT4[:, kc, i * 128:(i + 1) * 128], rhs=wpt[:, kc, 384:524],
                                                 start=(kc == 0), stop=(kc == 7)), reads=['hmT4', 'wpt'], writes=['psX'], pe_chain=(kc > 0))
                S.op('act', lambda: A.copy(out=qtm[:], in_=psS[0][:, 0:256]), reads=['psS0'], writes=['qtm'])
                S.op('act', lambda: A.activation(out=junk[:, 0:64], in_=psS[0][:, 256:320], func=AF.Square, accum_out=ksq[:, 0:1]),
                     reads=['psS0'], writes=['junk', 'ksq'])
                S.op('act', lambda: A.activation(out=junk[:, 0:64], in_=psS[0][:, 320:384], func=AF.Square, accum_out=ksq[:, 1:2]),
                     reads=['psS0'], writes=['junk', 'ksq'])
                S.op('dve', lambda: V.tensor_tensor(out=qtm[:], in0=qtm[:], in1=qtm[:], op=ALU.mult), reads=['qtm'], writes=['qtm'])
                S.op('dve', lambda: V.tensor_reduce(out=qsq[:, i, :], in_=qtm[:].rearrange("p (r d) -> p r d", d=64), axis=AX.X, op=ALU.add),
                     reads=['qtm'], writes=['qsq'])
                S.op('dve', lambda: V.tensor_tensor(out=km1[:], in0=ksq[:, 0:1], in1=ksq[:, 1:2], op=ALU.max), reads=['ksq'], writes=['km1'])
                S.op('pe', lambda: PE.transpose(out=psX[0:1, 256:384], in_=km1[:], identity=identf[:]), reads=['km1', 'identf'], writes=[KXK])
                S.op('dve', lambda: V.tensor_reduce(out=red[:], in_=psX[0:1, 256:384], axis=AX.X, op=ALU.max), reads=[KXK], writes=['red'])
                S.op('dve', lambda: V.tensor_tensor(out=kmx[:], in0=kmx[:], in1=red[:], op=ALU.max), reads=['kmx', 'red'], writes=['kmx'])
                S.op('pe', lambda: PE.matmul(psX[:, 384:385], lhsT=ones1[:], rhs=kmx[:], start=True, stop=True), reads=['ones1', 'kmx'], writes=[KXK])
                S.op('dve', lambda: V.tensor_copy(out=kmax2[:], in_=psX[:, 384:385]), reads=[KXK], writes=['kmax2'])
                S.op('act', lambda: A.copy(out=vsA[:, ti, 0:64], in_=psX[:, 0:64]), reads=['psX'], writes=['vsA'])
                S.op('act', lambda: A.copy(out=vwA[:, ti % 8, 0:64], in_=psX[:, 64:128]), reads=['psX'], writes=['vwA'])
                S.op('act', lambda: A.activation(out=gates[:, i, :], in_=psX[:, 128:140], func=AF.Sigmoid), reads=['psX'], writes=['gates'])
            for blk in range(8):
                pso = psC[0:64, (blk % 2) * 512:(blk % 2) * 512 + 512]
                kps = 'psC'
                for kc in range(8):
                    S.op('pe', lambda: PE.matmul(pso, lhsT=wpf[:, kc, blk * 64:(blk + 1) * 64], rhs=hmT4[:, kc, :], start=(kc == 0), stop=(kc == 7)),
                         reads=['wpf', 'hmT4'], writes=['psCa', 'psCb'], pe_chain=(kc > 0))
                if blk < 4:
                    dst = qTa[0:64, :, blk * 128:(blk + 1) * 128]
                    rope(dst, pso, kps, cb[:], snb[:], kcos, 0.125, 'qTa')
                elif blk == 4:
                    S.op('act', lambda: A.copy(out=kcbuf[:, 16:528], in_=pso), reads=['psCa', 'psCb'], writes=['kcbuf'])
                elif blk == 5:
                    S.op('act', lambda: A.copy(out=vcbuf[:, 16:528], in_=pso), reads=['psCa', 'psCb'], writes=['vcbuf'])
                elif blk == 6:
                    rope(ksT[0:64, s * 512:(s + 1) * 512], pso, kps, cb[:], snb[:], kcos, 1.0, 'ksT')
                else:
                    rope(kwT[0:64, (s % 2) * 512:(s % 2) * 512 + 512], pso, kps, cb[:], snb[:], kcos, 1.0, 'kwT')
            m0 = 32 * s
            for (buf, kbuf, w1, kw1, bias, gh, kgh) in ((kcbuf, 'kcbuf', wk1, 'wk1', biask, ghk, 'ghk'), (vcbuf, 'vcbuf', wv1, 'wv1', biasv, ghv, 'ghv')):
                bview = buf[:, 0:512].rearrange("d (n f) -> d n f", f=16)
                for hc in range(2):
                    for pos in range(32):
                        if pos < 16:
                            rhs = bview[:, :, pos]
                        else:
                            rhs = buf[:, 16:528].rearrange("d (n f) -> d n f", f=16)[:, :, pos - 16]
                        S.op('pe', lambda: PE.matmul(psX[:, hc * 32:(hc + 1) * 32], lhsT=w1[:, pos, hc * 128:(hc + 1) * 128], rhs=rhs,
                                                     start=(pos == 0), stop=(pos == 31)), reads=[kw1, kbuf], writes=['psX'], pe_chain=(pos > 0))
                for hc in range(2):
                    S.op('dve', lambda: V.tensor_scalar(out=hid[:, hc, :], in0=psX[:, hc * 32:(hc + 1) * 32], scalar1=bias[:, hc:hc + 1], scalar2=None,
                                                        op0=ALU.add), reads=['psX', 'biask', 'biasv'], writes=['hid'])
                gelu_tanh(gh[:].rearrange("p a b -> p (a b)"), hid[:].rearrange("p a b -> p (a b)"), hga[:].rearrange("p a b -> p (a b)"),
                          kgh, 'hid', 'hga')
                S.op('dve', lambda: V.tensor_copy(out=buf[:, 0:16], in_=buf[:, 512:528]), reads=[kbuf], writes=[kbuf])
            for hc in range(2):
                S.op('pe', lambda: PE.matmul(psC[0:64, 0:32], lhsT=wk2[:, hc, :], rhs=ghk[:, hc, :], start=(hc == 0), stop=(hc == 1)),
                     reads=['wk2', 'ghk'], writes=['psCa', 'psCb'], pe_chain=(hc > 0))
            if m0 + 32 <= NM:
                rope(kcmpT[:, m0:m0 + 32], psC[0:64, 0:32], 'psC', ccb[:], scb[:], kcc, 1.0, 'kcmpT')
                mt, mo = m0 // 128, m0 % 128
                S.op('dve', lambda: V.tensor_copy(out=ghv2[:, :, 32:64], in_=ghv[:]), reads=['ghv'], writes=['ghv2'])
                for hc in range(2):
                    if mo < 96:
                        S.op('pe', lambda: PE.matmul(psC[mo:mo + 32, 512:576], lhsT=ghv2[:, hc, 32:64], rhs=wv2[:, hc, :], start=(hc == 0), stop=(hc == 1)),
                             reads=['wv2', 'ghv2'], writes=['psCa', 'psCb'], pe_chain=(hc > 0))
                    else:
                        S.op('pe', lambda: PE.matmul(psC[64:128, 512:576], lhsT=ghv2[:, hc, 0:64], rhs=wv2[:, hc, :], start=(hc == 0), stop=(hc == 1)),
                             reads=['wv2', 'ghv2'], writes=['psCa', 'psCb'], pe_chain=(hc > 0))
                S.op('act', lambda: A.copy(out=vcmp[mo:mo + 32, mt, :], in_=psC[mo:mo + 32, 512:576]), reads=['psCa', 'psCb'], writes=['vcmp'])

            for i in range(4):
                qi = s * 4 + i
                qblk = qTa[:, i, :]
                import os
                if qi < int(os.environ.get('NSA_QIMIN', '0')):
                    continue
                S.op('dve', lambda: V.tensor_scalar(out=cq[:], in0=qsq[:, i, :], scalar1=kmax2[:, 0:1], scalar2=1.0 / 64, op0=ALU.mult, op1=ALU.mult),
                     reads=['qsq', 'kmax2'], writes=['cq'])
                S.op('act', lambda: A.activation(out=cq[:], in_=cq[:], func=AF.Sqrt), reads=['cq'], writes=['cq'])
                S.op('dve', lambda: V.tensor_scalar(out=negc[:], in0=cq[:], scalar1=-1.0, scalar2=None, op0=ALU.mult), reads=['cq'], writes=['negc'])
                for r in range(4):
                    S.op('pe', lambda: PE.transpose(out=psT[64:65, r * 128:(r + 1) * 128], in_=negc[:, r:r + 1], identity=identb[:]),
                         reads=['negc', 'identb'], writes=['psT'], pe_chain=(r > 0))
                S.op('act', lambda: A.copy(out=qTa[64:65, i, :], in_=psT[64:65, 0:512]), reads=['psT'], writes=['qTa'])

                W = 8 * qi + 8
                import os
                if os.environ.get('NSA_CAPW'):
                    W = min(W, int(os.environ['NSA_CAPW']))
                nt = (W + 127) // 128
                for r in range(4):
                    for c0 in range(0, W, 512):
                        w = min(512, W - c0)
                        S.op('pe', lambda: PE.matmul(psC[:, c0:c0 + w], lhsT=qTa[0:64, i, r * 128:(r + 1) * 128], rhs=kcmpT[:, c0:c0 + w],
                                                     start=True, stop=True), reads=['qTa', 'kcmpT'], writes=['psCa', 'psCb'])
                    chunks = [(c0, min(512, W - c0)) for c0 in range(0, W, 512)]
                    for ci_, (c0, w) in enumerate(chunks):
                        S.op('dve', lambda: V.tensor_reduce(out=mx2[:, ci_:ci_ + 1], in_=psC[:, c0:c0 + w], axis=AX.X, op=ALU.max), reads=['psCa', 'psCb'], writes=['mx2'])
                    if len(chunks) == 2:
                        S.op('dve', lambda: V.tensor_tensor(out=mx2[:, 0:1], in0=mx2[:, 0:1], in1=mx2[:, 1:2], op=ALU.max), reads=['mx2'], writes=['mx2'])
                    S.op('dve', lambda: V.tensor_scalar(out=mx[:], in0=mx2[:, 0:1], scalar1=-1.0, scalar2=None, op0=ALU.mult), reads=['mx2'], writes=['mx'])
                    for (c0, w) in chunks:
                        S.op('act', lambda: A.activation(out=pc[:, c0:c0 + w], in_=psC[:, c0:c0 + w], func=AF.Exp, bias=mx[:], scale=1.0),
                             reads=['psCa', 'psCb', 'mx'], writes=['pc'])
                    if qi == 0:
                        S.op('dve', lambda: V.tensor_tensor(out=pc[:, 0:8], in0=pc[:, 0:8], in1=cmask0[:], op=ALU.mult), reads=['pc', 'cmask0'], writes=['pc'])
                    else:
                        S.op('dve', lambda: V.tensor_tensor(out=pc[:, W - 16:W], in0=pc[:, W - 16:W], in1=cmaskG[:], op=ALU.mult),
                             reads=['pc', 'cmaskG'], writes=['pc'])
                        S.op('dve', lambda: V.memset(pc[:, 0:1], 0.0), reads=[], writes=['pc'])
                    S.op('dve', lambda: V.tensor_reduce(out=sm[:, r:r + 1], in_=pc[:, 0:W], axis=AX.X, op=ALU.add), reads=['pc'], writes=['sm'])
                    S.op('dve', lambda: V.tensor_scalar(out=rinv[:, r:r + 1], in0=sm[:, r:r + 1], scalar1=1e-30, scalar2=None, op0=ALU.add),
                         reads=['sm'], writes=['rinv'])
                    S.op('dve', lambda: V.reciprocal(out=rinv[:, r:r + 1], in_=rinv[:, r:r + 1]), reads=['rinv'], writes=['rinv'])
                    if r == 0:
                        S.op('dve', lambda: V.tensor_scalar(out=pgrp[:, 0:W], in0=pc[:, 0:W], scalar1=rinv[:, 0:1], scalar2=None, op0=ALU.mult),
                             reads=['pc', 'rinv'], writes=['pgrp'])
                    else:
                        S.op('dve', lambda: V.scalar_tensor_tensor(out=pgrp[:, 0:W], in0=pc[:, 0:W], scalar=rinv[:, r:r + 1], in1=pgrp[:, 0:W],
                                                                   op0=ALU.mult, op1=ALU.add), reads=['pc', 'rinv', 'pgrp'], writes=['pgrp'])
                    S.op('act', lambda: A.copy(out=pcb[:, 0:W], in_=pc[:, 0:W]), reads=['pc'], writes=['pcb'])
                    for j in range(nt):
                        wj = min(128, W - j * 128)
                        S.op('pe', lambda: PE.transpose(out=psT[0:wj, j * 128:(j + 1) * 128], in_=pcb[:, j * 128:j * 128 + wj], identity=identb[:]),
                             reads=['pcb', 'identb'], writes=['psT'], pe_chain=(j > 0))
                    for j in range(nt):
                        wj = min(128, W - j * 128)
                        S.op('act', lambda: A.copy(out=pcT[0:wj, j, :], in_=psT[0:wj, j * 128:(j + 1) * 128]), reads=['psT'], writes=['pcT'])
                    for j in range(nt):
                        wj = min(128, W - j * 128)
                        S.op('pe', lambda: PE.matmul(psX[:, r * 64:(r + 1) * 64], lhsT=pcT[0:wj, j, :], rhs=vcmp[0:wj, j, :],
                                                     start=(j == 0), stop=(j == nt - 1)), reads=['pcT', 'vcmp'], writes=['psX'], pe_chain=(j > 0))
                if qi >= 1:
                    Jn = 2 * qi
                    S.op('dve', lambda: V.tensor_reduce(out=imp[:, 0:Jn], in_=pgrp[:, 0:4 * Jn].rearrange("p (j f) -> p j f", f=4), axis=AX.X, op=ALU.add),
                         reads=['pgrp'], writes=['imp'])
                    S.op('dve', lambda: V.tensor_tensor(out=imp[:, 0:Jn], in0=imp[:, 0:Jn],
                                                        in1=pgrp[:, 4:4 + 4 * Jn].rearrange("p (j f) -> p j f", f=4)[:, :, 0], op=ALU.add),
                         reads=['imp', 'pgrp'], writes=['imp'])
                    if Jn - 1 > 1:
                        S.op('dve', lambda: V.tensor_copy(out=impw[:, 1:Jn - 1], in_=imp[:, 1:Jn - 1]), reads=['imp'], writes=['impw'])
                    S.op('dve', lambda: V.tensor_scalar(out=impw[:, Jn - 1:Jn], in0=imp[:, Jn - 1:Jn], scalar1=hilo[:, 0:1], scalar2=hilo[:, 1:2],
                                                        op0=ALU.mult, op1=ALU.add), reads=['imp', 'hilo'], writes=['impw'])
                    Wj = max(Jn, 16)
                    S.op('dve', lambda: V.max(out=m8a[:], in_=impw[:, 0:Wj]), reads=['impw'], writes=['m8a'])
                    S.op('dve', lambda: V.match_replace(out=impk[:, 0:Wj], in_to_replace=m8a[:], in_values=impw[:, 0:Wj], imm_value=-1e30),
                         reads=['impw', 'm8a'], writes=['impk'])
                    S.op('dve', lambda: V.max(out=m8b[:], in_=impk[:, 0:Wj]), reads=['impk'], writes=['m8b'])
                    S.op('dve', lambda: V.tensor_scalar(out=tau[:], in0=m8b[:, 4:5], scalar1=-0.5, scalar2=None, op0=ALU.max), reads=['m8b'], writes=['tau'])
                    S.op('dve', lambda: V.tensor_scalar(out=sel[:, 0:Jn], in0=impw[:, 0:Jn], scalar1=tau[:, 0:1], scalar2=None, op0=ALU.is_ge),
                         reads=['impw', 'tau'], writes=['sel'])
                    S.op('dve', lambda: V.memset(sel[:, 0:1], 1.0), reads=[], writes=['sel'])
                    S.op('dve', lambda: V.tensor_tensor(out=sel[:, Jn - 1:Jn], in0=sel[:, Jn - 1:Jn], in1=hilo[:, 2:3], op=ALU.max),
                         reads=['sel', 'hilo'], writes=['sel'])
                items = []
                for kt in range(0, qi + 1):
                    items.append(('s', kt))
                wts = [k for k in range(qi - 4, qi + 1) if k >= 0]
                for kt in wts:
                    items.append(('w', kt))

                def emit_qk(idx):
                    kind, kt = items[idx]
                    b2 = idx % NBK
                    if kind == 's' and kt % 4 == 0 and kt < qi:
                        nb = min(4, qi - kt)
                        g2i = (kt // 4) % 2
                        S.op('dve', lambda: V.tensor_copy(out=mfull[:, 0:nb * 128].rearrange("p (j f) -> p j f", f=64),
                                                          in_=sel[:, 2 * kt:2 * kt + 2 * nb].unsqueeze(2).to_broadcast([128, 2 * nb, 64])),
                             reads=['sel'], writes=['mfull'])
                        for k in range(nb):
                            S.op('pe', lambda: PE.transpose(out=psT[:, k * 128:(k + 1) * 128], in_=mfull[:, k * 128:(k + 1) * 128], identity=identb[:]),
                                 reads=['mfull', 'identb'], writes=['psT'], pe_chain=(k > 0))
                        S.op('act', lambda: A.copy(out=maskT4[g2i][:, 0:nb, :].rearrange("p a b -> p (a b)"), in_=psT[:, 0:nb * 128]),
                             reads=['psT'], writes=['maskT4_%d' % g2i])
                    if kind == 's':
                        lhs = ksT[:, kt * 128:(kt + 1) * 128]; kk = 'ksT'
                    else:
                        lhs = kwT[:, (kt % 8) * 128:(kt % 8) * 128 + 128]; kk = 'kwT'
                    bank, kbank = SBK[b2]
                    S.op('pe', lambda: PE.matmul(bank, lhsT=lhs, rhs=qblk, start=True, stop=True),
                         reads=[kk, 'qTa'], writes=[kbank])

                def emit_rest(idx):
                    kind, kt = items[idx]
                    b2 = idx % NBK
                    bank, kbank = SBK[b2]
                    if kind == 's':
                        msk = triT[:] if kt == qi else maskT4[(kt // 4) % 2][:, kt % 4, :]
                    else:
                        dlt = qi - kt
                        msk = triT[:] if dlt == 0 else (triTs[:] if dlt == 4 else None)
                    if msk is not None:
                        S.op('act', lambda: A.activation(out=eT[b2][:], in_=bank, func=AF.Exp), reads=[kbank], writes=['eT%d' % b2])
                        S.op('dve', lambda: V.tensor_tensor(out=pT[b2][:].rearrange("p (r q) -> p r q", q=128),
                                                            in0=eT[b2][:].rearrange("p (r q) -> p r q", q=128),
                                                            in1=msk.unsqueeze(1).to_broadcast([128, 4, 128]), op=ALU.mult),
                             reads=['eT%d' % b2, 'maskT4_0', 'maskT4_1', 'triT', 'triTs'], writes=['pT%d' % b2])
                    else:
                        S.op('act', lambda: A.activation(out=pT[b2][:], in_=bank, func=AF.Exp), reads=[kbank], writes=['pT%d' % b2])
                    if kind == 's':
                        S.op('pe', lambda: PE.matmul(psOs[0:65, :], lhsT=vsA[:, kt, :], rhs=pT[b2][:], start=(kt == 0), stop=(kt == qi)),
                             reads=['vsA', 'pT%d' % b2], writes=['psOs'], pe_chain=(kt > 0))
                    else:
                        S.op('pe', lambda: PE.matmul(psOw[0:65, :], lhsT=vwA[:, kt % 8, :], rhs=pT[b2][:], start=(kt == wts[0]), stop=(kt == qi)),
                             reads=['vwA', 'pT%d' % b2], writes=['psOw'], pe_chain=(kt > wts[0]))

                for idx in range(len(items) + LA):
                    if idx < len(items):
                        emit_qk(idx)
                    if idx - LA >= 0:
                        emit_rest(idx - LA)
                S.op('act', lambda: A.copy(out=oTs[:], in_=psOs[0:65, :]), reads=['psOs'], writes=['oTs'])
                S.op('act', lambda: A.copy(out=oTw[:], in_=psOw[0:65, :]), reads=['psOw'], writes=['oTw'])
                for r in range(4):
                    S.op('pe', lambda: PE.transpose(out=psC[:, r * 65:(r + 1) * 65], in_=oTs[:, r * 128:(r + 1) * 128], identity=identf[0:65, 0:65]),
                         reads=['oTs', 'identf'], writes=['psCa', 'psCb'], pe_chain=(r > 0))
                for r in range(4):
                    S.op('pe', lambda: PE.transpose(out=psC[:, 512 + r * 65:512 + (r + 1) * 65], in_=oTw[:, r * 128:(r + 1) * 128], identity=identf[0:65, 0:65]),
                         reads=['oTw', 'identf'], writes=['psCa', 'psCb'], pe_chain=True)
                g3 = gates[:, i, :].rearrange("p (r k) -> p r k", k=3)
                osum = psC[:, 0:260].rearrange("p (r e) -> p r e", e=65)
                wsum = psC[:, 512:772].rearrange("p (r e) -> p r e", e=65)
                S.op('dve', lambda: V.tensor_tensor(out=fac[:, 0, :], in0=g3[:, :, 0], in1=rinv[:], op=ALU.mult), reads=['gates', 'rinv'], writes=['fac'])
                S.op('dve', lambda: V.reciprocal(out=fac[:, 1, :], in_=osum[:, :, 64]), reads=['psCa', 'psCb'], writes=['fac'])
                S.op('dve', lambda: V.reciprocal(out=fac[:, 2, :], in_=wsum[:, :, 64]), reads=['psCa', 'psCb'], writes=['fac'])
                S.op('dve', lambda: V.tensor_tensor(out=fac[:, 1, :], in0=fac[:, 1, :], in1=g3[:, :, 1], op=ALU.mult), reads=['fac', 'gates'], writes=['fac'])
                S.op('dve', lambda: V.tensor_tensor(out=fac[:, 2, :], in0=fac[:, 2, :], in1=g3[:, :, 2], op=ALU.mult), reads=['fac', 'gates'], writes=['fac'])
                for r in range(4):
                    S.op('dve', lambda: V.tensor_scalar(out=ot[:, r, :], in0=psX[:, r * 64:(r + 1) * 64], scalar1=fac[:, 0, r:r + 1], scalar2=None, op0=ALU.mult),
                         reads=['psX', 'fac'], writes=['ot'])
                    S.op('dve', lambda: V.scalar_tensor_tensor(out=ot[:, r, :], in0=osum[:, r, 0:64], scalar=fac[:, 1, r:r + 1], in1=ot[:, r, :],
                                                               op0=ALU.mult, op1=ALU.add), reads=['psCa', 'psCb', 'fac', 'ot'], writes=['ot'])
                    S.op('dve', lambda: V.scalar_tensor_tensor(out=ot[:, r, :], in0=wsum[:, r, 0:64], scalar=fac[:, 2, r:r + 1], in1=ot[:, r, :],
                                                               op0=ALU.mult, op1=ALU.add), reads=['psCa', 'psCb', 'fac', 'ot'], writes=['ot'])
                S.dma('sp', lambda: nc.sync.dma_start(out=out[qi * 128:(qi + 1) * 128, :], in_=ot[:].rearrange("p r d -> p (r d)")),
                      reads=['ot'], writes=['out'])
        S.finish(['out'])
        print("nsa program: instr", S.n_instr, "waits", S.n_wait)
    return nc


def nsa_consts(T):
    NM = 1024 if T >= 16384 else (T // 16 + 32)
    half = 32
    freqs = (10000.0 ** (-np.arange(half, dtype=np.float32) / half)).astype(np.float32)
    pos = np.arange(T, dtype=np.float32)
    ang = pos[None, :] * freqs[:, None]
    cos2 = np.concatenate([np.cos(ang), np.cos(ang)], 0).astype(np.float32)
    sin2 = np.concatenate([np.sin(ang), np.sin(ang)], 0).astype(np.float32)
    m = np.arange(NM, dtype=np.float32)
    cend = (m - 1) * 16 + 31
    angc = cend[None, :] * freqs[:, None]
    cosc = np.concatenate([np.cos(angc), np.cos(angc)], 0).astype(np.float32)
    sinc = np.concatenate([np.sin(angc), np.sin(angc)], 0).astype(np.float32)
    prot = np.zeros((64, 64), np.float32)
    for d in range(32):
        prot[d + 32, d] = -1.0
        prot[d, d + 32] = 1.0
    l = np.arange(128)
    triT = (l[:, None] <= l[None, :]).astype(np.float32)
    triTs = (l[:, None] > l[None, :]).astype(np.float32)
    fl = np.floor((l - 15) / 16.0)
    j = np.arange(16)
    cmaskG = ((j[None, :] - 8) <= fl[:, None]).astype(np.float32)
    j8 = np.arange(8)
    cmask0 = ((j8[None, :] >= 1) & (j8[None, :] <= fl[:, None])).astype(np.float32)
    hi = (l >= 64).astype(np.float32)
    hilo = np.stack([hi, hi - 1.0, 1.0 - hi], 1).astype(np.float32)
    return {"cos2": cos2, "sin2": sin2, "cosc": cosc, "sinc": sinc, "prot": prot, "identf": np.eye(128, dtype=np.float32),
            "triT": triT, "triTs": triTs, "cmaskG": cmaskG, "cmask0": cmask0, "hilo": hilo}


def nsa_weights(w_proj, g):
    q = w_proj[:, 256 * g:256 * g + 256]
    def blk(i):
        return w_proj[:, 1024 + 256 * i + 64 * g:1024 + 256 * i + 64 * g + 64]
    kc, vc, ks, vs, kw, vw = [blk(i) for i in range(6)]
    gl = w_proj[:, 2560 + 12 * g:2560 + 12 * g + 12]
    wpf = np.ascontiguousarray(np.concatenate([q, kc, vc, ks, kw], 1))
    wpt = np.ascontiguousarray(np.concatenate([q, ks, kw, vs, vw, gl], 1))
    return wpf, wpt

from concourse.bass_utils import run_bass_kernel_spmd

N_CORES = 8
SEQ = 16384


def _c_l(c, b):
    return np.ascontiguousarray(np.asarray(c[b], np.float32).reshape(8, 128).T)


def kernel(x, c, norm1_g, norm2_g, ada_w, ada_b, s5_w_in, s5_a_re, s5_a_im, s5_log_dt,
           s5_b_re, s5_b_im, s5_c_re, s5_c_im, s5_d, s5_w_glu, nsa_w_proj, nsa_pe_k, nsa_pe_v,
           nsa_wk1, nsa_wk2, nsa_wv1, nsa_wv2, nsa_w_o, peer_w_q, peer_sub_keys, peer_u, peer_v,
           final_g):
    f = lambda a: np.ascontiguousarray(np.asarray(a, np.float32))
    x = f(x); c = f(c); ada_w = f(ada_w); ada_b = f(ada_b)
    norm1_g = f(norm1_g); norm2_g = f(norm2_g); final_g = f(final_g)
    peer_w_q = f(peer_w_q); peer_sub_keys = f(peer_sub_keys); peer_u = f(peer_u); peer_v = f(peer_v)
    cores = list(range(N_CORES))
    nc1 = build_s5(SEQ // 128)
    s5c = s5_consts()
    maps = []
    for k in cores:
        b, cq = k // 4, k % 4
        m = {"x": x[b], "c_l": _c_l(c, b), "adaw": f(ada_w[0][:, :2048]), "adab": f(ada_b[0][None, :2048]),
             "n1g": f(norm1_g[0][None]), "win": f(np.asarray(s5_w_in[0])[:, cq * 256:(cq + 1) * 256])}
        m.update(s5c)
        m.update(s5_layouts(f(s5_a_re[0]), f(s5_a_im[0]), f(s5_log_dt[0]), f(s5_b_re[0]), f(s5_b_im[0]),
                            f(s5_c_re[0]), f(s5_c_im[0]), f(s5_d[0]), cq))
        maps.append(m)
    r1 = run_bass_kernel_spmd(nc1, maps, core_ids=cores)
    yactT = [r1.results[k]["out"] for k in cores]
    del maps

    def tok_launch(li, mode, final, xin, mixT_of, wmix):
        nct = build_tok(SEQ // 4 // 128, mode, final)
        tc = tok_consts()
        maps = []
        for k in cores:
            b, q4 = k // 4, k % 4
            sl = slice(q4 * 4096, (q4 + 1) * 4096)
            m = {"x": f(xin[b, sl]), "mixT": mixT_of(b, sl), "c_l": _c_l(c, b),
                 "adaw": f(ada_w[li][:, 2048:]), "adab": f(ada_b[li][None, 2048:]),
                 "n2g": f(norm2_g[li][None]), "fing": f(final_g[None]), "wmix": wmix, "wq": peer_w_q[li],
                 "sk": f(peer_sub_keys[li].reshape(16, 128, 128)), "u_tab": peer_u[li], "v_tab": peer_v[li]}
            m.update(tc)
            maps.append(m)
        r = run_bass_kernel_spmd(nct, maps, core_ids=cores)
        xo = np.empty((2, SEQ, 1024), np.float32)
        for k in cores:
            b, q4 = k // 4, k % 4
            xo[b, q4 * 4096:(q4 + 1) * 4096] = r.results[k]["out"]
        return xo

    def mix0(b, sl):
        return f(np.concatenate([yactT[b * 4 + cq][:, sl] for cq in range(4)], axis=0))
    x2 = tok_launch(0, 'glu', False, x, mix0, f(s5_w_glu[0]))
    del yactT
    nc3 = build_nsa(SEQ // 512)
    nsc = nsa_consts(SEQ)
    maps = []
    for k in cores:
        b, g = k // 4, k % 4
        wpf, wpt = nsa_weights(f(nsa_w_proj[0]), g)
        m = {"x": x2[b], "c_l": _c_l(c, b), "adaw": f(ada_w[1][:, :2048]), "adab": f(ada_b[1][None, :2048]),
             "n1g": f(norm1_g[1][None]), "wpf": wpf, "wpt": wpt,
             "wk1": f(nsa_wk1[0]), "wv1": f(nsa_wv1[0]), "wk2": f(nsa_wk2[0]), "wv2": f(nsa_wv2[0]),
             "pekT": f(np.asarray(nsa_pe_k[0]).T), "pevT": f(np.asarray(nsa_pe_v[0]).T)}
        m.update(nsc)
        maps.append(m)
    r3 = run_bass_kernel_spmd(nc3, maps, core_ids=cores)
    o = [r3.results[k]["out"] for k in cores]
    del maps

    def mix1(b, sl):
        return f(np.concatenate([o[b * 4 + g][sl].T for g in range(4)], axis=0))
    out = tok_launch(1, 'wo', True, x2, mix1, f(nsa_w_o[0]))
    return out
```
